# Optimizing a Trainium2 kernel written in Bass

```python
import math
import jax
import jax.numpy as jnp
from jax import lax
import numpy as np

D_MODEL = 1024
BATCH = 8
SEQ = 4096
DEPTH = 2

CTX_LEN = 256
GRID_W = 64
NORM_EPS = 1e-6

HY_CH = 256
HY_ORDER = 2
HY_DIRS = 2
HY_POS_EMB = 33
HY_FILT_HID = 64
HY_FILT_OUT = HY_ORDER * HY_DIRS * HY_CH
HY_FAST_DECAY = 0.3
HY_SLOW_DECAY = 1.5
HY_DECAY_TARGET = 1e-2

ML_HEADS = 4
ML_HD = 64
ML_W = ML_HEADS * ML_HD
ML_CHUNK = 64

DA_HEADS = 4
DA_HD = 64
DA_QK = DA_HEADS * 2 * DA_HD
DA_V = DA_HEADS * 2 * DA_HD
ROPE_THETA = 10000.0
Q_BLOCK = 128

HY_OFF = 0
ML_OFF = HY_OFF + 3 * HY_CH
DA_OFF = ML_OFF + 4 * ML_W + 2 * 2 * ML_HEADS
N_IN = DA_OFF + 2 * DA_QK + DA_V
MIX_W = HY_CH + ML_W + DA_V

MOE_GROUPS = 4
MOE_PER_GROUP = 8
MOE_EXPERTS = MOE_GROUPS * MOE_PER_GROUP
MOE_TOPK = 2
MOE_FF = 512
MOE_BLOCK = 128

kernel_name = 'hybrid_hyena_mlstm_diffattn_hmoe_dit'


def rmsnorm(x, w):
    xf = x.astype(jnp.float32)
    y = xf * lax.rsqrt(jnp.mean(xf * xf, axis=-1, keepdims=True) + NORM_EPS)
    return (y * w.astype(jnp.float32)).astype(x.dtype)


def short_conv(u, w, b):
    L = u.shape[1]
    up = jnp.pad(u, ((0, 0), (1, 1), (0, 0)))
    return up[:, :L] * w[0] + up[:, 1:L + 1] * w[1] + up[:, 2:] * w[2] + b


def hyena_filters(L, p):
    f32 = jnp.float32
    t = jnp.linspace(0.0, 1.0, L, dtype=f32)[:, None]
    bands = (HY_POS_EMB - 1) // 2
    w = (2.0 * math.pi / L) * jnp.arange(L, dtype=f32)[:, None]
    f = jnp.linspace(1e-4, bands - 1, bands, dtype=f32)[None, :]
    z = jnp.concatenate([t, jnp.cos(f * w), -jnp.sin(f * w)], axis=-1)
    freq = p['hy_sin_freq']
    h = jnp.sin(freq * (z @ p['hy_filt_w1'] + p['hy_filt_b1']))
    h = jnp.sin(freq * (h @ p['hy_filt_w2'] + p['hy_filt_b2']))
    h = (h @ p['hy_filt_w3'] + p['hy_filt_b3']).astype(f32).reshape(L, HY_ORDER, HY_DIRS, HY_CH)
    deltas = jnp.abs(jnp.linspace(math.log(HY_DECAY_TARGET) / HY_SLOW_DECAY,
                                  math.log(HY_DECAY_TARGET) / HY_FAST_DECAY, HY_CH, dtype=f32))
    h = h * jnp.exp(-t[:, :, None, None] * deltas)
    k_fwd, k_bwd = h[:, :, 0], h[:, :, 1]
    k = jnp.concatenate([k_fwd, jnp.zeros_like(k_fwd[:1]), k_bwd[:0:-1]], axis=0)
    return k / jnp.sum(jnp.abs(k), axis=0, keepdims=True)


def fft_long_conv(u, k, d):
    L = u.shape[1]
    y = jnp.fft.irfft(jnp.fft.rfft(u, n=2 * L, axis=1) * jnp.fft.rfft(k, axis=0)[None], n=2 * L, axis=1)[:, :L]
    return y + u * d


def hyena_mix(u, p):
    L = u.shape[1]
    u = short_conv(u, p['hy_conv_w'], p['hy_conv_b']).astype(jnp.float32)
    v, x1, x2 = jnp.split(u, 3, axis=-1)
    k = hyena_filters(L, p)
    z = v
    for o, gate in enumerate((x1, x2)):
        z = gate * fft_long_conv(z, k[:, o], p['hy_bias_d'][o].astype(jnp.float32))
    return z


def to_chunks(a):
    B, L, H = a.shape[:3]
    a = a.reshape((B, L // ML_CHUNK, ML_CHUNK, H) + a.shape[3:])
    return jnp.moveaxis(a, 3, 1)


def from_chunks(a):
    a = jnp.moveaxis(a, 1, 3)
    return a.reshape((a.shape[0], a.shape[1] * a.shape[2]) + a.shape[3:])


def mlstm_states(k, v, ig, lf, state0):
    b = jnp.cumsum(lf, axis=-1)
    a = b[..., -1:] - b + ig
    m_loc = jnp.max(a, axis=-1)
    wv = jnp.exp(a - m_loc[..., None])[..., None] * v
    C_loc = jnp.einsum('bhnli,bhnlj->bhnij', wv, k)
    n_loc = jnp.einsum('bhnl,bhnlj->bhnj', jnp.exp(a - m_loc[..., None]), k)

    def step(carry, xs):
        C, n, m = carry
        bL, Cl, nl, ml = xs
        m_new = jnp.maximum(bL + m, ml)
        s_old = jnp.exp(bL + m - m_new)
        s_loc = jnp.exp(ml - m_new)
        new = (s_old[..., None, None] * C + s_loc[..., None, None] * Cl,
               s_old[..., None] * n + s_loc[..., None] * nl, m_new)
        return new, carry

    xs = tuple(jnp.moveaxis(t, 2, 0) for t in (b[..., -1], C_loc, n_loc, m_loc))
    final, entering = lax.scan(step, state0, xs)
    entering = tuple(jnp.moveaxis(t, 0, 2) for t in entering)
    return b, entering, final


def mlstm_out(q, k, v, ig, b, entering):
    C0, n0, m0 = entering
    Lc = q.shape[3]
    lower = jnp.tril(jnp.ones((Lc, Lc), bool))
    D = jnp.where(lower, b[..., :, None] - b[..., None, :] + ig[..., None, :], -jnp.inf)
    inter = b + m0[..., None]
    m = jnp.maximum(inter, jnp.max(D, axis=-1))
    P = jnp.exp(D - m[..., None]) * jnp.einsum('bhnti,bhnsi->bhnts', q, k)
    w_inter = jnp.exp(inter - m)
    num = jnp.einsum('bhnts,bhnsj->bhntj', P, v) + w_inter[..., None] * jnp.einsum('bhnij,bhntj->bhnti', C0, q)
    den = jnp.sum(P, axis=-1) + w_inter * jnp.einsum('bhnj,bhntj->bhnt', n0, q)
    return num / jnp.maximum(jnp.abs(den), jnp.exp(-m))[..., None]


def mlstm_direction(q, k, v, ig, fg, state0, reverse, need_out):
    if reverse:
        q, k, v, ig, fg = (a[:, ::-1] for a in (q, k, v, ig, fg))
    lf = to_chunks(jax.nn.log_sigmoid(fg))
    q, k, v, ig = (to_chunks(a) for a in (q, k, v, ig))
    b, entering, final = mlstm_states(k, v, ig, lf, state0)
    if not need_out:
        return None, final
    h = from_chunks(mlstm_out(q, k, v, ig, b, entering))
    return (h[:, ::-1] if reverse else h), final


def mlstm_prep(u, p):
    B, L, _ = u.shape
    f32 = jnp.float32
    qk = jax.nn.silu(short_conv(u[..., :2 * ML_W], p['ml_conv_w'], p['ml_conv_b'])).astype(f32)
    q = qk[..., :ML_W].reshape(B, L, ML_HEADS, ML_HD)
    k = qk[..., ML_W:].reshape(B, L, ML_HEADS, ML_HD) * (ML_HD ** -0.5)
    v = u[..., 2 * ML_W:3 * ML_W].astype(f32).reshape(B, L, ML_HEADS, ML_HD)
    o = u[..., 3 * ML_W:4 * ML_W]
    g = u[..., 4 * ML_W:].astype(f32).reshape(B, L, 2, 2, ML_HEADS)
    return q, k, v, o, g


def mlstm_finish(h, o, w):
    B, L = h.shape[:2]
    hn = rmsnorm(h, w.reshape(ML_HEADS, ML_HD)).reshape(B, L, ML_W)
    return jax.nn.sigmoid(o.astype(jnp.float32)) * hn


def mlstm_mix(u_lat, u_ctx, p, ctx_out):
    ql, kl, vl, ol, gl = mlstm_prep(u_lat, p)
    qc, kc, vc, oc, gc = mlstm_prep(u_ctx, p)
    B = u_lat.shape[0]
    f32 = jnp.float32
    state0 = (jnp.zeros((B, ML_HEADS, ML_HD, ML_HD), f32), jnp.zeros((B, ML_HEADS, ML_HD), f32),
              jnp.zeros((B, ML_HEADS), f32))
    h_lat, h_ctx = [], []
    for d in range(2):
        hc_d, ctx_final = mlstm_direction(qc, kc, vc, gc[..., d, 0, :], gc[..., d, 1, :], state0, d == 1, ctx_out)
        hl_d, _ = mlstm_direction(ql, kl, vl, gl[..., d, 0, :], gl[..., d, 1, :], ctx_final, d == 1, True)
        h_lat.append(hl_d)
        h_ctx.append(hc_d)
    y_lat = mlstm_finish(h_lat[0] + h_lat[1], ol, p['ml_norm_w'])
    y_ctx = mlstm_finish(h_ctx[0] + h_ctx[1], oc, p['ml_norm_w']) if ctx_out else None
    return y_lat, y_ctx


def rope_tables(row, col):
    half = DA_HD // 2
    inv = ROPE_THETA ** (-jnp.arange(0, half, 2, dtype=jnp.float32) / half)
    ang = jnp.stack([row, col], axis=-1)[:, :, None] * inv
    ang = jnp.stack([ang, ang], axis=-2).reshape(-1, DA_HD)
    return jnp.cos(ang), jnp.sin(ang)


def axial_rope(x, cos, sin):
    xs = x.reshape(x.shape[:-1] + (2, 2, DA_HD // 4))
    rot = jnp.stack([-xs[..., 1, :], xs[..., 0, :]], axis=-2).reshape(x.shape)
    cos = cos[None, :, None, None, :]
    sin = sin[None, :, None, None, :]
    return (x * cos + rot * sin).astype(x.dtype)


def diff_core(q, k, v, lam):
    s = jnp.einsum('bqhmd,bkhmd->bhmqk', q, k, preferred_element_type=jnp.float32) * (DA_HD ** -0.5)
    pr = jax.nn.softmax(s, axis=-1)
    a = pr[:, :, 0] - lam * pr[:, :, 1]
    return jnp.einsum('bhqk,bkhe->bqhe', a.astype(v.dtype), v)


def diff_attention(u_lat, u_ctx, p, layer_idx, cos, sin, ctx_out):
    lam_init = 0.8 - 0.6 * math.exp(-0.3 * layer_idx)
    lp = p['da_lambda'].astype(jnp.float32)
    lam = jnp.exp(jnp.sum(lp[0] * lp[1])) - jnp.exp(jnp.sum(lp[2] * lp[3])) + lam_init

    def split(u):
        B, L, _ = u.shape
        q = u[..., :DA_QK].reshape(B, L, DA_HEADS, 2, DA_HD)
        k = u[..., DA_QK:2 * DA_QK].reshape(B, L, DA_HEADS, 2, DA_HD)
        v = u[..., 2 * DA_QK:].reshape(B, L, DA_HEADS, 2 * DA_HD)
        return q, k, v

    ql, kl, vl = split(u_lat)
    qc, kc, vc = split(u_ctx)
    ql = axial_rope(ql, cos, sin)
    kl = axial_rope(kl, cos, sin)
    k_all = jnp.concatenate([kl, kc], axis=1)
    v_all = jnp.concatenate([vl, vc], axis=1)
    B, L = ql.shape[:2]
    nb = L // Q_BLOCK
    qb = jnp.moveaxis(ql.reshape((B, nb, Q_BLOCK) + ql.shape[2:]), 1, 0)
    ob = lax.map(lambda qi: diff_core(qi, k_all, v_all, lam), qb)
    o_lat = jnp.moveaxis(ob, 0, 1).reshape(B, L, DA_HEADS, 2 * DA_HD)

    def finish(o):
        return (rmsnorm(o, p['da_subln_w']) * (1.0 - lam_init)).reshape(o.shape[0], o.shape[1], DA_V)

    y_lat = finish(o_lat)
    y_ctx = finish(diff_core(qc, kc, vc, lam)) if ctx_out else None
    return y_lat, y_ctx


def hier_moe(h, p):
    N, D = h.shape
    f32 = jnp.float32
    g_logits = (h @ p['moe_wg'] + p['moe_bg']).astype(f32)
    g_idx = jnp.argmax(g_logits, axis=-1)
    g_w = jnp.take_along_axis(jax.nn.softmax(g_logits, axis=-1), g_idx[:, None], axis=-1)
    e_logits = (h @ p['moe_we'] + p['moe_be']).astype(f32).reshape(N, MOE_GROUPS, MOE_PER_GROUP)
    e_logits = jnp.take_along_axis(e_logits, g_idx[:, None, None], axis=1)[:, 0]
    top_v, top_i = lax.top_k(e_logits, MOE_TOPK)
    gate = jax.nn.softmax(top_v, axis=-1) * g_w
    expert = g_idx[:, None] * MOE_PER_GROUP + top_i
    A = N * MOE_TOPK
    flat_e = expert.reshape(-1).astype(jnp.int32)
    flat_t = jnp.repeat(jnp.arange(N, dtype=jnp.int32), MOE_TOPK)
    flat_g = gate.reshape(-1)
    order = jnp.argsort(flat_e)
    se, st, sg = flat_e[order], flat_t[order], flat_g[order]
    counts = jnp.bincount(flat_e, length=MOE_EXPERTS)
    starts = jnp.cumsum(counts) - counts
    pcounts = (counts + MOE_BLOCK - 1) // MOE_BLOCK * MOE_BLOCK
    pends = jnp.cumsum(pcounts)
    dest = (pends - pcounts)[se] + jnp.arange(A, dtype=jnp.int32) - starts[se]
    P = -(-A // MOE_BLOCK) * MOE_BLOCK + MOE_EXPERTS * MOE_BLOCK
    nblk = P // MOE_BLOCK
    slot_tok = jnp.full((P,), N, jnp.int32).at[dest].set(st)
    slot_g = jnp.zeros((P,), f32).at[dest].set(sg)
    blk_e = jnp.minimum(jnp.searchsorted(pends, jnp.arange(nblk) * MOE_BLOCK, side='right'), MOE_EXPERTS - 1)
    xb = jnp.concatenate([h, jnp.zeros((1, D), h.dtype)], axis=0)[slot_tok].reshape(nblk, MOE_BLOCK, D)

    def expert_block(args):
        xi, e = args
        return (jax.nn.silu(xi @ p['moe_w1'][e]) * (xi @ p['moe_w3'][e])) @ p['moe_w2'][e]

    yb = lax.map(expert_block, (xb, blk_e)).reshape(P, D)
    out = jax.ops.segment_sum(yb.astype(f32) * slot_g[:, None], slot_tok, num_segments=N + 1)
    return out[:N].astype(h.dtype)


def trunk_layer(x, xc, c_act, cc_act, p, cos, sin, layer_idx, last):
    B, L, D = x.shape
    mod = (c_act @ p['ada_w'] + p['ada_b'])[:, None, :]
    modc = cc_act @ p['ada_w'] + p['ada_b']
    sh1, sc1, g1, sh2, sc2, g2 = jnp.split(mod, 6, axis=-1)
    csh1, csc1, cg1, csh2, csc2, cg2 = jnp.split(modc, 6, axis=-1)

    h = rmsnorm(x, p['norm1_w']) * (1.0 + sc1) + sh1
    hc = rmsnorm(xc, p['norm1_w']) * (1.0 + csc1) + csh1
    u = h @ p['w_in'] + p['b_in']
    c0 = ML_OFF if last else 0
    uc = hc @ p['w_in'][:, c0:] + p['b_in'][c0:]

    y_hy = hyena_mix(u[..., HY_OFF:ML_OFF], p).astype(x.dtype)
    y_ml, yc_ml = mlstm_mix(u[..., ML_OFF:DA_OFF], uc[..., ML_OFF - c0:DA_OFF - c0], p, not last)
    y_da, yc_da = diff_attention(u[..., DA_OFF:], uc[..., DA_OFF - c0:], p, layer_idx, cos, sin, not last)
    y = jnp.concatenate([y_hy, y_ml.astype(x.dtype), y_da.astype(x.dtype)], axis=-1) @ p['w_out']
    x = x + g1 * y
    if not last:
        yc_hy = hyena_mix(uc[..., HY_OFF:ML_OFF], p).astype(xc.dtype)
        yc = jnp.concatenate([yc_hy, yc_ml.astype(xc.dtype), yc_da.astype(xc.dtype)], axis=-1) @ p['w_out']
        xc = xc + cg1 * yc

    h2 = rmsnorm(x, p['norm2_w']) * (1.0 + sc2) + sh2
    if last:
        f = hier_moe(h2.reshape(B * L, D), p)
        return x + g2 * f.reshape(B, L, D), xc
    hc2 = rmsnorm(xc, p['norm2_w']) * (1.0 + csc2) + csh2
    f = hier_moe(jnp.concatenate([h2.reshape(B * L, D), hc2.reshape(-1, D)], axis=0), p)
    x = x + g2 * f[:B * L].reshape(B, L, D)
    xc = xc + cg2 * f[B * L:].reshape(xc.shape)
    return x, xc


def setup_inputs(seed: int = 0) -> dict:
    key = jax.random.key(seed)
    keys = jax.random.split(key, 34)
    f32 = jnp.float32

    def nrm(i, shape, scale):
        return scale * jax.random.normal(keys[i], shape, f32)

    D = D_MODEL
    fg_idx = np.array([ML_OFF + 4 * ML_W + d * 2 * ML_HEADS + ML_HEADS + hh for d in range(2) for hh in range(ML_HEADS)])
    fg_bias = jnp.asarray(np.tile(np.linspace(3.0, 6.0, ML_HEADS, dtype=np.float32), 2))
    return {
        'x': nrm(0, (BATCH, SEQ, D), 1.0),
        'c': nrm(1, (BATCH, D), 1.0),
        'ctx': nrm(2, (BATCH, CTX_LEN, D), 1.0),
        'c_ctx': nrm(3, (D,), 1.0),
        'ada_w': nrm(4, (DEPTH, D, 6 * D), 0.5 * D ** -0.5),
        'ada_b': nrm(5, (DEPTH, 6 * D), 0.02),
        'norm1_w': 1.0 + nrm(6, (DEPTH, D), 0.02),
        'norm2_w': 1.0 + nrm(7, (DEPTH, D), 0.02),
        'w_in': nrm(8, (DEPTH, D, N_IN), D ** -0.5),
        'b_in': nrm(9, (DEPTH, N_IN), 0.02).at[:, fg_idx].add(fg_bias),
        'w_out': nrm(10, (DEPTH, MIX_W, D), MIX_W ** -0.5),
        'hy_conv_w': nrm(11, (DEPTH, 3, 3 * HY_CH), 3 ** -0.5),
        'hy_conv_b': nrm(12, (DEPTH, 3 * HY_CH), 0.02),
        'hy_filt_w1': nrm(13, (DEPTH, HY_POS_EMB, HY_FILT_HID), HY_POS_EMB ** -0.5),
        'hy_filt_b1': nrm(14, (DEPTH, HY_FILT_HID), 0.02),
        'hy_filt_w2': nrm(15, (DEPTH, HY_FILT_HID, HY_FILT_HID), HY_FILT_HID ** -0.5),
        'hy_filt_b2': nrm(16, (DEPTH, HY_FILT_HID), 0.02),
        'hy_filt_w3': nrm(17, (DEPTH, HY_FILT_HID, HY_FILT_OUT), HY_FILT_HID ** -0.5),
        'hy_filt_b3': nrm(18, (DEPTH, HY_FILT_OUT), 0.02),
        'hy_sin_freq': 1.0 + nrm(19, (DEPTH, HY_FILT_HID), 0.02),
        'hy_bias_d': nrm(20, (DEPTH, HY_ORDER, HY_CH), 1.0),
        'ml_conv_w': nrm(21, (DEPTH, 3, 2 * ML_W), 3 ** -0.5),
        'ml_conv_b': nrm(22, (DEPTH, 2 * ML_W), 0.02),
        'ml_norm_w': 1.0 + nrm(23, (DEPTH, ML_W), 0.02),
        'da_lambda': nrm(24, (DEPTH, 4, DA_HD), 0.1),
        'da_subln_w': 1.0 + nrm(25, (DEPTH, 2 * DA_HD), 0.02),
        'moe_wg': nrm(26, (DEPTH, D, MOE_GROUPS), D ** -0.5),
        'moe_bg': nrm(27, (DEPTH, MOE_GROUPS), 0.01),
        'moe_we': nrm(28, (DEPTH, D, MOE_EXPERTS), D ** -0.5),
        'moe_be': nrm(29, (DEPTH, MOE_EXPERTS), 0.01),
        'moe_w1': nrm(30, (DEPTH, MOE_EXPERTS, D, MOE_FF), D ** -0.5),
        'moe_w3': nrm(31, (DEPTH, MOE_EXPERTS, D, MOE_FF), D ** -0.5),
        'moe_w2': nrm(32, (DEPTH, MOE_EXPERTS, MOE_FF, D), MOE_FF ** -0.5),
        'final_norm_w': 1.0 + nrm(33, (D,), 0.02),
    }


def reference(x, c, ctx, c_ctx, ada_w, ada_b, norm1_w, norm2_w, w_in, b_in, w_out,
              hy_conv_w, hy_conv_b, hy_filt_w1, hy_filt_b1, hy_filt_w2, hy_filt_b2, hy_filt_w3, hy_filt_b3,
              hy_sin_freq, hy_bias_d, ml_conv_w, ml_conv_b, ml_norm_w, da_lambda, da_subln_w,
              moe_wg, moe_bg, moe_we, moe_be, moe_w1, moe_w3, moe_w2, final_norm_w):
    B, L, D = x.shape
    ROWS = L // GRID_W
    row = jnp.repeat(jnp.arange(ROWS, dtype=jnp.float32), GRID_W)
    col = jnp.tile(jnp.arange(GRID_W, dtype=jnp.float32), ROWS)
    cos, sin = rope_tables(row, col)
    c_act = jax.nn.silu(c)
    cc_act = jax.nn.silu(c_ctx)
    xc = ctx
    for l in range(DEPTH):
        p = {
            'ada_w': ada_w[l], 'ada_b': ada_b[l], 'norm1_w': norm1_w[l], 'norm2_w': norm2_w[l],
            'w_in': w_in[l], 'b_in': b_in[l], 'w_out': w_out[l],
            'hy_conv_w': hy_conv_w[l], 'hy_conv_b': hy_conv_b[l],
            'hy_filt_w1': hy_filt_w1[l], 'hy_filt_b1': hy_filt_b1[l],
            'hy_filt_w2': hy_filt_w2[l], 'hy_filt_b2': hy_filt_b2[l],
            'hy_filt_w3': hy_filt_w3[l], 'hy_filt_b3': hy_filt_b3[l],
            'hy_sin_freq': hy_sin_freq[l], 'hy_bias_d': hy_bias_d[l],
            'ml_conv_w': ml_conv_w[l], 'ml_conv_b': ml_conv_b[l], 'ml_norm_w': ml_norm_w[l],
            'da_lambda': da_lambda[l], 'da_subln_w': da_subln_w[l],
            'moe_wg': moe_wg[l], 'moe_bg': moe_bg[l], 'moe_we': moe_we[l], 'moe_be': moe_be[l],
            'moe_w1': moe_w1[l], 'moe_w3': moe_w3[l], 'moe_w2': moe_w2[l],
        }
        x, xc = trunk_layer(x, xc, c_act, cc_act, p, cos, sin, l, l == DEPTH - 1)
    return rmsnorm(x, final_norm_w)
```

```python
import math
import os
import numpy as np
import concourse.bass as bass
import concourse.mybir as mybir
from concourse.bass_utils import run_bass_kernel_spmd

F32 = mybir.dt.float32
BF16 = mybir.dt.bfloat16
AF = mybir.ActivationFunctionType
ALU = mybir.AluOpType
AX = mybir.AxisListType

D = 1024
L = 4096
CTX = 256
T = L + CTX
NT = T // 128
DEPTH = 2
EPS = 1e-6
N_IN = 3344
ML_OFF = 768
DA_OFF = 1808
NFM = 2304
NTM = 1040
FM_COLS = list(range(0, 768)) + list(range(768, 1280)) + list(range(1808, 2832))
TM_COLS = list(range(1280, 1792)) + list(range(2832, 3344)) + list(range(1792, 1808))


class Em:
    NDMA = 32

    def __init__(self, nc):
        self.nc = nc
        self.eng = {'pe': nc.tensor, 'act': nc.scalar, 'dve': nc.vector, 'pool': nc.gpsimd, 'sp': nc.sync}
        self.sem = {k: nc.alloc_semaphore('s_' + k) for k in ('pe', 'act', 'dve', 'pool')}
        self.cnt = {k: 0 for k in self.sem}
        self.dsem = [nc.alloc_semaphore('s_dma%d' % i) for i in range(self.NDMA)]
        self.dcnt = [0] * self.NDMA
        self.dnext = 0
        self.waited = {e: {} for e in self.eng}
        self.lastw = {}
        self.readers = {}
        self.ninst = 0

    def _semh(self, key):
        return self.sem[key] if isinstance(key, str) else self.dsem[key[1]]

    def _wait(self, e, ev):
        key, val = ev
        w = self.waited[e]
        if w.get(key, 0) >= val:
            return
        self.eng[e].wait_ge(self._semh(key), val)
        w[key] = val

    def _deps(self, e, reads, writes):
        best = {}
        for k in reads:
            ev = self.lastw.get(k)
            if ev is not None and best.get(ev[0], 0) < ev[1]:
                best[ev[0]] = ev[1]
        for k in writes:
            ev = self.lastw.get(k)
            if ev is not None and best.get(ev[0], 0) < ev[1]:
                best[ev[0]] = ev[1]
            for ev in self.readers.get(k, ()):
                if best.get(ev[0], 0) < ev[1]:
                    best[ev[0]] = ev[1]
        for key, val in best.items():
            if key == e and e == 'pe':
                continue
            self._wait(e, (key, val))

    def _record(self, ev, reads, writes):
        for k in reads:
            lst = self.readers.setdefault(k, [])
            lst[:] = [x for x in lst if x[0] != ev[0]]
            lst.append(ev)
        for k in writes:
            self.lastw[k] = ev
            self.readers[k] = []

    def op(self, e, reads, writes, fn):
        self._deps(e, reads, writes)
        ins = fn(self.eng[e])
        self.cnt[e] += 1
        ins.then_inc(self.sem[e], 1)
        self._record((e, self.cnt[e]), reads, writes)
        self.ninst += 1

    def dma(self, q, reads, writes, out, in_, **kw):
        i = self.dnext
        self.dnext = (i + 1) % self.NDMA
        if self.dcnt[i] > 0:
            self._wait(q, (('d', i), 16 * self.dcnt[i]))
        self._deps(q, reads, writes)
        ins = self.eng[q].dma_start(out=out, in_=in_, **kw)
        self.dcnt[i] += 1
        ins.then_inc(self.dsem[i], 16)
        self._record((('d', i), 16 * self.dcnt[i]), reads, writes)
        self.ninst += 1

    def barrier(self):
        for e in self.eng:
            for k in self.sem:
                if self.cnt[k] > 0 and k != e:
                    self._wait(e, (k, self.cnt[k]))
            for i in range(self.NDMA):
                if self.dcnt[i] > 0:
                    self._wait(e, (('d', i), 16 * self.dcnt[i]))
        self.lastw = {}
        self.readers = {}


class SB:
    _arena = {}

    def __init__(self, nc, base=0, limit=None):
        self.nc = nc
        if id(nc) not in SB._arena:
            nwords = (nc.sbuf_bytes_remaining - 256) // 4
            SB._arena[id(nc)] = (nc.alloc_sbuf_tensor("arena", [128, nwords], F32), nwords * 4)
        self.arena, cap = SB._arena[id(nc)]
        self.off = base
        self.limit = cap if limit is None else limit

    def t(self, shape, dtype, name=None):
        per = 1
        for s in shape[1:]:
            per *= s
        esz = 2 if dtype == BF16 else 4
        nbytes = (per * esz + 63) // 64 * 64
        assert self.off % 4 == 0
        w0 = self.off // 4
        ap = self.arena[0:shape[0], w0:w0 + nbytes // 4]
        if dtype != F32:
            ap = ap.bitcast(dtype)
        ap = ap[:, 0:per]
        if len(shape) == 3:
            ap = ap.rearrange("p (a b) -> p a b", b=shape[2])
        elif len(shape) == 4:
            ap = ap.rearrange("p (a b c) -> p a b c", b=shape[2], c=shape[3])
        self.off += nbytes
        assert self.off <= self.limit, ("SBUF overflow", name, self.off, self.limit)
        return ap


class K:
    def __init__(self, nc, debug=()):
        self.nc = nc
        self.em = Em(nc)
        self.debug = set(debug)
        self.dram = {}
        self.ps = [nc.alloc_psum_tensor("psb%d" % i, [128, 512], F32) for i in range(8)]

    def din(self, name, shape, dtype=F32):
        ap = self.nc.dram_tensor(name, list(shape), dtype, kind="ExternalInput").ap()
        self.dram[name] = ap
        return ap

    def dscratch(self, name, shape, dtype=F32):
        kind = "ExternalOutput" if name in self.debug else "Internal"
        ap = self.nc.dram_tensor(name, list(shape), dtype, kind=kind).ap()
        self.dram[name] = ap
        return ap

    def dout(self, name, shape, dtype=F32):
        ap = self.nc.dram_tensor(name, list(shape), dtype, kind="ExternalOutput").ap()
        self.dram[name] = ap
        return ap

    def dma(self, q, r, w, out, in_, **kw):
        self.em.dma(q, r, w, out, in_, **kw)

    def mm(self, r, w, out, lhsT, rhs, start=True, stop=True):
        self.em.op('pe', r, w, lambda e: e.matmul(out, lhsT=lhsT, rhs=rhs, start=start, stop=stop))

    def tr(self, r, w, out, in_, ident):
        self.em.op('pe', r, w, lambda e: e.transpose(out, in_, ident))

    def act(self, r, w, out, in_, func, eng='act', **kw):
        self.em.op(eng, r, w, lambda e: e.activation(out=out, in_=in_, func=func, **kw))

    def ts(self, r, w, out, in0, s1, s2=None, op0=ALU.mult, op1=None, eng='dve', **kw):
        if op1 is None:
            self.em.op(eng, r, w, lambda e: e.tensor_scalar(out=out, in0=in0, scalar1=s1, scalar2=None, op0=op0, **kw))
        else:
            self.em.op(eng, r, w, lambda e: e.tensor_scalar(out=out, in0=in0, scalar1=s1, scalar2=s2, op0=op0, op1=op1, **kw))

    def tt(self, r, w, out, in0, in1, op, eng='dve'):
        self.em.op(eng, r, w, lambda e: e.tensor_tensor(out=out, in0=in0, in1=in1, op=op))

    def stt(self, r, w, out, in0, scalar, in1, op0, op1, eng='dve'):
        self.em.op(eng, r, w, lambda e: e.scalar_tensor_tensor(out=out, in0=in0, scalar=scalar, in1=in1, op0=op0, op1=op1))

    def cp(self, r, w, out, in_, eng='dve'):
        if eng == 'act':
            self.em.op(eng, r, w, lambda e: e.copy(out=out, in_=in_))
        else:
            self.em.op(eng, r, w, lambda e: e.tensor_copy(out=out, in_=in_))

    def red(self, r, w, out, in_, op, eng='dve', axis=AX.X):
        self.em.op(eng, r, w, lambda e: e.tensor_reduce(out=out, in_=in_, axis=axis, op=op))

    def recip(self, r, w, out, in_):
        self.em.op('dve', r, w, lambda e: e.reciprocal(out=out, in_=in_))

    def memset(self, r, w, out, val, eng='dve'):
        self.em.op(eng, r, w, lambda e: e.memset(out, val))


def rope_tables_T():
    half = 32
    inv = (10000.0 ** (-np.arange(0, half, 2, dtype=np.float32) / half)).astype(np.float32)
    t = np.arange(L)
    row = (t // 64).astype(np.float32)
    col = (t % 64).astype(np.float32)
    ang = np.concatenate([row[:, None] * inv, row[:, None] * inv, col[:, None] * inv, col[:, None] * inv], axis=1)
    ang = ang.astype(np.float32)
    cosT = np.cos(ang).T.astype(np.float32)
    sinT = np.sin(ang).T.astype(np.float32)
    return np.ascontiguousarray(np.concatenate([cosT, cosT], 0)), np.ascontiguousarray(np.concatenate([sinT, sinT], 0))


def rope_perm():
    R = np.zeros((128, 128), np.float32)
    for base in range(0, 128, 32):
        for i in range(16):
            R[base + 16 + i, base + i] = -1.0
            R[base + i, base + 16 + i] = 1.0
    return R


def make_consts():
    c = {}
    c['ident'] = np.eye(128, dtype=np.float32)
    c['ropeR'] = rope_perm()
    cosT, sinT = rope_tables_T()
    c['cosT'] = cosT
    c['sinT'] = sinT
    bi = np.zeros((128, 2), np.float32)
    bi[0:64, 0] = 1.0
    bi[64:128, 1] = 1.0
    c['blockind'] = bi
    sel = np.zeros((2, 2, 128), np.float32)
    sel[0, 0, :] = 1.0
    sel[1, 1, :] = 1.0
    c['sel2'] = sel
    tri = np.zeros((2, 64, 64), np.float32)
    tri[0] = np.triu(np.ones((64, 64), np.float32))
    tri[1] = np.tril(np.ones((64, 64), np.float32))
    c['tri'] = tri
    c.update(hyena_consts())
    return c


def col_layout(v):
    return np.ascontiguousarray(v.reshape(-1, 128).T)


def prep_inputs(inp, b):
    m = {}
    m['xin'] = np.ascontiguousarray(np.concatenate([inp['x'][b], inp['ctx'][b]], axis=0))
    m['ccol'] = np.ascontiguousarray(np.stack([col_layout(inp['c'][b]), col_layout(inp['c_ctx'])], axis=-1))
    m['ada_w'] = inp['ada_w']
    m['ada_b_col'] = np.ascontiguousarray(np.stack([col_layout(inp['ada_b'][l]) for l in range(DEPTH)], 1))
    m['norm1_col'] = np.ascontiguousarray(np.stack([col_layout(inp['norm1_w'][l]) for l in range(DEPTH)], 1))
    m['norm2_col'] = np.ascontiguousarray(np.stack([col_layout(inp['norm2_w'][l]) for l in range(DEPTH)], 1))
    m['w_in_fm'] = np.ascontiguousarray(inp['w_in'][:, :, FM_COLS])
    m['w_in_tm'] = np.ascontiguousarray(inp['w_in'][:, :, TM_COLS])
    m['b_fm_col'] = np.ascontiguousarray(np.stack([col_layout(inp['b_in'][l][FM_COLS]) for l in range(DEPTH)], 1))
    cw = np.concatenate([inp['ml_conv_w'], inp['ml_conv_b'][:, None, :]], axis=1)
    m['ml_conv_col'] = np.ascontiguousarray(cw.reshape(DEPTH, 4, 8, 64).transpose(0, 3, 2, 1))
    m['ml_norm_bc'] = np.ascontiguousarray(np.broadcast_to(inp['ml_norm_w'][:, None, :], (DEPTH, 64, 256)))
    m['hy_filt_w1'] = inp['hy_filt_w1']
    m['hy_filt_w2'] = inp['hy_filt_w2']
    m['hy_filt_w3'] = inp['hy_filt_w3']
    m['hy_filt_sc'] = np.ascontiguousarray(np.stack([inp['hy_sin_freq'], inp['hy_filt_b1'], inp['hy_filt_b2']], axis=-1))
    m['hy_b3_col'] = np.ascontiguousarray(np.stack([col_layout(inp['hy_filt_b3'][l]) for l in range(DEPTH)], 0))
    hw = np.concatenate([inp['hy_conv_w'], inp['hy_conv_b'][:, None, :]], axis=1)
    m['hy_conv_col'] = np.ascontiguousarray(hw.reshape(DEPTH, 4, 6, 128).transpose(0, 3, 2, 1))
    m['hy_d_bc'] = np.ascontiguousarray(np.broadcast_to(inp['hy_bias_d'][:, None, :, :], (DEPTH, 32, 2, 256)))
    m['hy_d_col'] = np.ascontiguousarray(inp['hy_bias_d'].reshape(DEPTH, 4, 128).transpose(0, 2, 1))
    m['w_out'] = inp['w_out']
    m['moe_wr'] = np.ascontiguousarray(np.concatenate([inp['moe_wg'], inp['moe_we']], axis=-1))
    rbv = np.concatenate([inp['moe_bg'], inp['moe_be']], axis=-1)
    m['moe_rb_bc'] = np.ascontiguousarray(np.broadcast_to(rbv[:, None, :], (DEPTH, 128, 36)))
    m['moe_w1'] = inp['moe_w1']
    m['moe_w3'] = inp['moe_w3']
    m['moe_w2'] = inp['moe_w2']
    m['final_bc'] = np.ascontiguousarray(np.broadcast_to(inp['final_norm_w'][None, :], (128, D)))
    m['da_lambda'] = np.ascontiguousarray(inp['da_lambda'].reshape(DEPTH, 256))
    m['da_subln_col'] = np.ascontiguousarray(inp['da_subln_w'][:, :, None])
    m['b_tm_bc'] = np.ascontiguousarray(np.broadcast_to(inp['b_in'][:, None, TM_COLS], (DEPTH, 128, NTM)))
    return m


def phase0(k, P, sbp):
    nc = k.nc
    cst = {}
    for name, shape in (('ident', [128, 128]), ('ropeR', [128, 128]), ('blockind', [128, 2])):
        k.din(name, shape)
    k.din('sel2', [2, 2, 128])
    k.din('cosT', [128, L])
    k.din('sinT', [128, L])
    P['ident32'] = sbp.t([128, 128], F32)
    P['identbf'] = sbp.t([128, 128], BF16)
    P['ropeRbf'] = sbp.t([128, 128], BF16)
    P['blockbf'] = sbp.t([128, 2], BF16)
    P['sel2'] = sbp.t([2, 2, 128], F32)
    P['ones32'] = sbp.t([128, 128], F32)
    P['onesbf'] = sbp.t([128, 128], BF16)
    tmp = sbp.t([128, 128], F32)
    k.dma('sp', [], ['ident32'], P['ident32'], k.dram['ident'][:, :])
    k.cp(['ident32'], ['identbf'], P['identbf'], P['ident32'])
    k.dma('sp', [], ['c_tmp'], tmp, k.dram['ropeR'][:, :])
    k.cp(['c_tmp'], ['ropeRbf'], P['ropeRbf'], tmp)
    k.dma('sp', ['c_tmp'], ['c_tmp'], tmp[:, 0:2], k.dram['blockind'][:, :])
    k.cp(['c_tmp'], ['blockbf'], P['blockbf'], tmp[:, 0:2])
    k.dma('sp', [], ['sel2'], P['sel2'], k.dram['sel2'][:, :, :])
    k.memset([], ['ones32'], P['ones32'], 1.0)
    k.memset([], ['onesbf'], P['onesbf'], 1.0)

    ccol = k.din('ccol', [128, 8, 2])
    adaw = k.din('ada_w', [DEPTH, D, 6 * D])
    adab = k.din('ada_b_col', [128, DEPTH, 48])
    n1 = k.din('norm1_col', [128, DEPTH, 8])
    n2 = k.din('norm2_col', [128, DEPTH, 8])
    P['mod'] = sbp.t([128, DEPTH, 48, 2], F32)
    P['A1'] = sbp.t([128, DEPTH, 8, 2], F32)
    P['A2'] = sbp.t([128, DEPTH, 8, 2], F32)
    cact = sbp.t([128, 8, 2], F32)
    adab_sb = sbp.t([128, DEPTH, 48], F32)
    n1_sb = sbp.t([128, DEPTH, 8], F32)
    n2_sb = sbp.t([128, DEPTH, 8], F32)
    k.dma('sp', [], ['cact'], cact, ccol[:, :, :])
    k.dma('sp', [], ['adab'], adab_sb, adab[:, :, :])
    k.dma('sp', [], ['n1'], n1_sb, n1[:, :, :])
    k.dma('sp', [], ['n2'], n2_sb, n2[:, :, :])
    k.act(['cact'], ['cact'], cact, cact, AF.Silu)
    sbl = SB(nc, base=sbp.off)
    stg = [sbl.t([128, 8, 512], F32) for _ in range(2)]
    for l in range(DEPTH):
        wv = adaw[l].rearrange("(k p) n -> p k n", p=128)
        pm = k.ps[0][:, 0:96].rearrange("p (c j) -> p c j", j=2)
        for cg in range(12):
            s = stg[cg % 2]
            sk = 'adastg%d' % (cg % 2)
            k.dma('sp', [], [sk], s, wv[:, :, cg * 512:(cg + 1) * 512])
            for j in range(4):
                c = cg * 4 + j
                for kk in range(8):
                    k.mm([sk, 'cact'], ['ps0'], pm[:, c, :], s[:, kk, j * 128:(j + 1) * 128], cact[:, kk, :],
                         start=(kk == 0), stop=(kk == 7))
        k.tt(['ps0', 'adab'], ['mod'], P['mod'][:, l], pm, adab_sb[:, l, :].unsqueeze(2).to_broadcast([128, 48, 2]), ALU.add)
        for (Aname, nsb, c0) in (('A1', n1_sb, 8), ('A2', n2_sb, 32)):
            k.ts(['mod'], [Aname], P[Aname][:, l], P['mod'][:, l, c0:c0 + 8, :], 1.0, op0=ALU.add)
            k.tt([Aname, 'n1', 'n2'], [Aname], P[Aname][:, l], P[Aname][:, l],
                 nsb[:, l, :].unsqueeze(2).to_broadcast([128, 8, 2]), ALU.mult)
    k.em.barrier()


def phaseA(k, P, l, base, xres):
    nc = k.nc
    sb = SB(nc, base=base)
    wfm = k.dram['w_in_fm']
    wtm = k.dram['w_in_tm']
    UT, QKT, TM = k.dram['UT'], k.dram['QKT'], k.dram['TM']
    Wfm = sb.t([128, 8, NFM], BF16)
    Wtm = sb.t([128, 8, NTM], BF16)
    bfm = sb.t([128, 18], F32)
    btm = sb.t([128, NTM], F32)
    cosT = sb.t([128, L], F32)
    sinT = sb.t([128, L], F32)
    normacc = sb.t([2, 8], F32)
    stg = [sb.t([128, 8, 512], F32) for _ in range(2)]
    k.dma('sp', [], ['bfm'], bfm, k.dram['b_fm_col'][:, l, :])
    k.dma('sp', [], ['btm'], btm, k.dram['b_tm_bc'][l])
    k.dma('sp', [], ['cosT'], cosT, k.dram['cosT'][:, :])
    k.dma('sp', [], ['sinT'], sinT, k.dram['sinT'][:, :])
    k.memset([], ['normacc'], normacc, 0.0)
    ci = 0
    for (src, dst, ncol, key) in ((wfm, Wfm, NFM, 'Wfm'), (wtm, Wtm, NTM, 'Wtm')):
        wv = src[l].rearrange("(k p) n -> p k n", p=128)
        for c0 in range(0, ncol, 512):
            c1 = min(ncol, c0 + 512)
            s = stg[ci % 2]
            sk = 'wstg%d' % (ci % 2)
            k.dma('sp', [], [sk], s[:, :, 0:c1 - c0], wv[:, :, c0:c1])
            k.cp([sk], [key], dst[:, :, c0:c1], s[:, :, 0:c1 - c0], eng=('dve' if ci % 2 == 0 else 'pool'))
            ci += 1
    xt = [sb.t([128, D], F32) for _ in range(2)]
    junk = sb.t([128, D], BF16)
    xn = [sb.t([128, D], BF16) for _ in range(2)]
    ss = [sb.t([128, 2], F32) for _ in range(2)]
    hT = [sb.t([128, 8, 512], BF16) for _ in range(2)]
    fmst = [sb.t([128, 512], F32) for _ in range(3)]
    qbf = [sb.t([128, 512], BF16) for _ in range(2)]
    t2 = [sb.t([128, 512], F32) for _ in range(2)]
    obf = [sb.t([128, 512], BF16) for _ in range(2)]
    sqbf = [sb.t([128, 512], BF16) for _ in range(2)]
    nmx = sb.t([2, 2], F32)
    tmst = [sb.t([128, NTM], F32) for _ in range(2)]
    A1, SH1 = P['A1'], P['mod']
    psi = 0
    tile_ctr = 0
    fm_ctr = 0
    rp_ctr = 0
    for g in range(9):
        t0 = g * 512
        ntok = 512 if g < 8 else 256
        j = 0 if g < 8 else 1
        hb = g % 2
        hk = 'hT%d' % hb
        for tl in range(ntok // 128):
            ti = t0 // 128 + tl
            xb = tile_ctr % 2
            tile_ctr += 1
            xk, nk, sk = 'xt%d' % xb, 'xn%d' % xb, 'ss%d' % xb
            k.dma('sp', [], [xk], xt[xb], xres[ti * 128:(ti + 1) * 128, :])
            k.memset([], [sk], ss[xb], 0.0)
            k.act([xk, sk], ['junk', sk], junk, xt[xb], AF.Square, accum_out=ss[xb][:, 0:1])
            k.ts([sk], [sk], ss[xb][:, 1:2], ss[xb][:, 0:1], 1.0 / D, EPS, op0=ALU.mult, op1=ALU.add)
            k.act([sk], [sk], ss[xb][:, 1:2], ss[xb][:, 1:2], AF.Sqrt)
            k.recip([sk], [sk], ss[xb][:, 1:2], ss[xb][:, 1:2])
            k.ts([xk, sk], [nk], xn[xb], xt[xb], ss[xb][:, 1:2], op0=ALU.mult)
            pk = 'ps%d' % psi
            pst = k.ps[psi][:].bitcast(BF16)
            psi = (psi + 1) % 8
            for kk in range(8):
                k.tr([nk, 'identbf'], [pk], pst[:, kk * 128:(kk + 1) * 128], xn[xb][:, kk * 128:(kk + 1) * 128], P['identbf'])
            for kk in range(8):
                k.act([pk, 'A1', 'mod'], [hk], hT[hb][:, kk, tl * 128:(tl + 1) * 128], pst[:, kk * 128:(kk + 1) * 128],
                      AF.Identity, scale=A1[:, l, kk, j:j + 1], bias=SH1[:, l, kk, j:j + 1])
        for jc in range(18):
            pk = 'ps%d' % psi
            pp = k.ps[psi]
            psi = (psi + 1) % 8
            for kk in range(8):
                k.mm(['Wfm', hk], [pk], pp[:, 0:ntok], Wfm[:, kk, jc * 128:(jc + 1) * 128], hT[hb][:, kk, 0:ntok],
                     start=(kk == 0), stop=(kk == 7))
            fb = fm_ctr % 3
            fm_ctr += 1
            fk = 'fmst%d' % fb
            k.act([pk, 'bfm'], [fk], fmst[fb][:, 0:ntok], pp[:, 0:ntok], AF.Identity, bias=bfm[:, jc:jc + 1], scale=1.0)
            if jc < 10:
                k.dma('pool', [fk], ['UT'], UT[jc * 128:(jc + 1) * 128, t0:t0 + ntok], fmst[fb][:, 0:ntok])
                continue
            rb = rp_ctr % 2
            rp_ctr += 1
            ok_, sqk = 'obf%d' % rb, 'sqbf%d' % rb
            if j == 0:
                qk_, tk_ = 'qbf%d' % rb, 't2%d' % rb
                k.cp([fk], [qk_], qbf[rb][:, 0:ntok], fmst[fb][:, 0:ntok], eng='pool')
                pk2 = 'ps%d' % psi
                pp2 = k.ps[psi]
                psi = (psi + 1) % 8
                k.mm(['ropeRbf', qk_], [pk2], pp2[:, 0:ntok], P['ropeRbf'], qbf[rb][:, 0:ntok])
                k.tt([pk2, 'sinT'], [tk_], t2[rb][:, 0:ntok], pp2[:, 0:ntok], sinT[:, t0:t0 + ntok], ALU.mult)
                k.tt([fk, 'cosT'], [fk], fmst[fb][:, 0:ntok], fmst[fb][:, 0:ntok], cosT[:, t0:t0 + ntok], ALU.mult, eng='pool')
                k.tt([fk, tk_], [fk], fmst[fb][:, 0:ntok], fmst[fb][:, 0:ntok], t2[rb][:, 0:ntok], ALU.add)
            k.cp([fk], [ok_], obf[rb][:, 0:ntok], fmst[fb][:, 0:ntok], eng='pool')
            k.dma('pool', [ok_], ['QKT'], QKT[(jc - 10) * 128:(jc - 9) * 128, t0:t0 + ntok], obf[rb][:, 0:ntok])
            k.act([fk], [sqk], sqbf[rb][:, 0:ntok], fmst[fb][:, 0:ntok], AF.Square)
            pk3 = 'ps%d' % psi
            pp3 = k.ps[psi]
            psi = (psi + 1) % 8
            k.mm(['blockbf', sqk], [pk3], pp3[0:2, 0:ntok], P['blockbf'], sqbf[rb][:, 0:ntok])
            k.red([pk3], ['nmx'], nmx[:, 0:1], pp3[0:2, 0:ntok], ALU.max)
            k.tt(['nmx', 'normacc'], ['normacc'], normacc[:, jc - 10:jc - 9], normacc[:, jc - 10:jc - 9], nmx[:, 0:1], ALU.max)
        for tl in range(ntok // 128):
            ti = t0 // 128 + tl
            tb = ti % 2
            tk = 'tmst%d' % tb
            for (c0, c1) in ((0, 512), (512, 1024), (1024, NTM)):
                pk = 'ps%d' % psi
                pp = k.ps[psi]
                psi = (psi + 1) % 8
                for kk in range(8):
                    k.mm(['Wtm', hk], [pk], pp[:, 0:c1 - c0], hT[hb][:, kk, tl * 128:(tl + 1) * 128], Wtm[:, kk, c0:c1],
                         start=(kk == 0), stop=(kk == 7))
                k.tt([pk, 'btm'], [tk], tmst[tb][:, c0:c1], pp[:, 0:c1 - c0], btm[:, c0:c1], ALU.add)
            k.dma('pool', [tk], ['TM'], TM[ti * 128:(ti + 1) * 128, :], tmst[tb])
    cn = sb.t([2, 4], F32)
    k.tt(['normacc'], ['cn'], cn, normacc[:, 0:4], normacc[:, 4:8], ALU.mult)
    k.act(['cn'], ['cn'], cn, cn, AF.Sqrt)
    k.ts(['cn'], ['cn'], cn, cn, -1.05 * 0.125, op0=ALU.mult)
    for m in range(2):
        pk = 'ps%d' % psi
        pp = k.ps[psi]
        psi = (psi + 1) % 8
        k.mm(['sel2', 'cn'], [pk], pp[:, 0:4], P['sel2'][:, m, :], cn)
        k.cp([pk], ['negc'], P['negc'][:, l, m, :], pp[:, 0:4])
    k.em.barrier()


def phaseB1(k, P, l, base, last):
    nc = k.nc
    sb = SB(nc, base=base)
    QKT, TM, YT = k.dram['QKT'], k.dram['TM'], k.dram['YT']
    lam_init = 0.8 - 0.6 * math.exp(-0.3 * l)
    QT = sb.t([128, 4, T], BF16)
    KT = sb.t([128, 4, T], BF16)
    V = sb.t([128, NT, 512], BF16)
    vst = [sb.t([128, 512], F32) for _ in range(2)]
    k.dma('sp', [], ['QT'], QT, QKT[0:512, :].rearrange("(c p) t -> p c t", p=128))
    k.dma('sp', [], ['KT'], KT, QKT[512:1024, :].rearrange("(c p) t -> p c t", p=128))
    for ti in range(NT):
        vb = ti % 2
        vk = 'vst%d' % vb
        k.dma('sp', [], [vk], vst[vb], TM[ti * 128:(ti + 1) * 128, 512:1024])
        k.cp([vk], ['V'], V[:, ti, :], vst[vb], eng=('dve' if ti % 2 == 0 else 'pool'))
    lt = sb.t([1, 256], F32)
    lw = sb.t([1, 8], F32)
    neglam = sb.t([128, 1], F32)
    wsc = sb.t([128, 1], F32)
    k.dma('sp', [], ['lt'], lt, k.dram['da_lambda'][l:l + 1, :])
    k.dma('sp', [], ['wsc'], wsc, k.dram['da_subln_col'][l])
    k.ts(['wsc'], ['wsc'], wsc, wsc, 1.0 - lam_init, op0=ALU.mult)
    k.tt(['lt'], ['lt'], lt[:, 0:64], lt[:, 0:64], lt[:, 64:128], ALU.mult)
    k.tt(['lt'], ['lt'], lt[:, 128:192], lt[:, 128:192], lt[:, 192:256], ALU.mult)
    k.red(['lt'], ['lw'], lw[:, 0:1], lt[:, 0:64], ALU.add)
    k.red(['lt', 'lw'], ['lw'], lw[:, 1:2], lt[:, 128:192], ALU.add)
    k.act(['lw'], ['lw'], lw[:, 0:2], lw[:, 0:2], AF.Exp)
    k.tt(['lw'], ['lw'], lw[:, 2:3], lw[:, 1:2], lw[:, 0:1], ALU.subtract)
    k.ts(['lw'], ['lw'], lw[:, 2:3], lw[:, 2:3], -lam_init, op0=ALU.add)
    k.mm(['ones32', 'lw'], ['ps7'], k.ps[7][:, 0:1], P['ones32'][0:1, :], lw[:, 2:3])
    k.cp(['ps7'], ['neglam'], neglam, k.ps[7][:, 0:1])

    pt = [sb.t([128, 512], BF16) for _ in range(4)]
    rec = [sb.t([128, 512], F32) for _ in range(2)]
    o0 = sb.t([128, 512], F32)
    o1 = sb.t([128, 512], F32)
    sq = sb.t([128, 512], BF16)
    ybf = [sb.t([128, 512], BF16) for _ in range(2)]
    pti = 0
    si = 0
    yi = 0
    chunks = [(g * 512, 512, list(range(NT))) for g in range(8)]
    if not last:
        chunks.append((L, CTX, [32, 33]))
    for h in range(4):
        for (q0, nq, blocks) in chunks:
            for bi, kb in enumerate(blocks):
                for m in range(2):
                    psk = 'ps%d' % (4 + si % 4)
                    pss = k.ps[4 + si % 4]
                    si += 1
                    lo, hi = m * 64, (m + 1) * 64
                    k.mm(['KT', 'QT'], [psk], pss[:, 0:nq], KT[lo:hi, h, kb * 128:(kb + 1) * 128], QT[lo:hi, h, q0:q0 + nq])
                    pk_ = 'pt%d' % (pti % 4)
                    ptt = pt[pti % 4]
                    pti += 1
                    k.act([psk, 'negc'], [pk_], ptt[:, 0:nq], pss[:, 0:nq], AF.Exp, scale=0.125, bias=P['negc'][:, l, m, h:h + 1])
                    st, sp_ = (bi == 0), (bi == len(blocks) - 1)
                    k.mm(['V', pk_], ['ps%d' % m], k.ps[m][:, 0:nq], V[:, kb, h * 128:(h + 1) * 128], ptt[:, 0:nq], start=st, stop=sp_)
                    k.mm(['onesbf', pk_], ['ps%d' % (2 + m)], k.ps[2 + m][:, 0:nq], P['onesbf'], ptt[:, 0:nq], start=st, stop=sp_)
            k.recip(['ps2'], ['rec0'], rec[0][:, 0:nq], k.ps[2][:, 0:nq])
            k.recip(['ps3'], ['rec1'], rec[1][:, 0:nq], k.ps[3][:, 0:nq])
            k.tt(['ps0', 'rec0'], ['o0'], o0[:, 0:nq], k.ps[0][:, 0:nq], rec[0][:, 0:nq], ALU.mult)
            k.tt(['ps1', 'rec1'], ['o1'], o1[:, 0:nq], k.ps[1][:, 0:nq], rec[1][:, 0:nq], ALU.mult)
            k.stt(['o0', 'o1', 'neglam'], ['o0'], o0[:, 0:nq], o1[:, 0:nq], neglam[:, 0:1], o0[:, 0:nq], ALU.mult, ALU.add)
            k.act(['o0'], ['sq'], sq[:, 0:nq], o0[:, 0:nq], AF.Square)
            psk = 'ps%d' % (4 + si % 4)
            pss = k.ps[4 + si % 4]
            si += 1
            k.mm(['onesbf', 'sq'], [psk], pss[:, 0:nq], P['onesbf'], sq[:, 0:nq])
            k.ts([psk], ['rec0'], rec[0][:, 0:nq], pss[:, 0:nq], 1.0 / 128.0, EPS, op0=ALU.mult, op1=ALU.add)
            k.act(['rec0'], ['rec0'], rec[0][:, 0:nq], rec[0][:, 0:nq], AF.Sqrt)
            k.recip(['rec0'], ['rec0'], rec[0][:, 0:nq], rec[0][:, 0:nq])
            k.tt(['o0', 'rec0'], ['o0'], o0[:, 0:nq], o0[:, 0:nq], rec[0][:, 0:nq], ALU.mult)
            yk = 'ybf%d' % (yi % 2)
            yb = ybf[yi % 2]
            yi += 1
            k.ts(['o0', 'wsc'], [yk], yb[:, 0:nq], o0[:, 0:nq], wsc[:, 0:1], op0=ALU.mult)
            k.dma('pool', [yk], ['YT'], YT[512 + h * 128:512 + (h + 1) * 128, q0:q0 + nq], yb[:, 0:nq])
    k.em.barrier()


NCH = T // 64


def phaseB2(k, P, l, base, last):
    nc = k.nc
    UT, TM, YT = k.dram['UT'], k.dram['TM'], k.dram['YT']
    sb0 = SB(nc, base=base)
    tri32 = sb0.t([64, 2, 64], F32)
    tribf = sb0.t([64, 2, 64], BF16)
    cw = sb0.t([64, 8, 4], F32)
    wbc = sb0.t([64, 256], F32)
    G = sb0.t([64, NCH, 16], F32)
    LF = sb0.t([64, 8, NCH], F32)
    IG = sb0.t([64, 8, NCH], F32)
    BB = sb0.t([64, 8, NCH], F32)
    BT = sb0.t([64, 8, NCH], F32)
    EB = sb0.t([64, 8, NCH], F32)
    WS = sb0.t([64, 8, NCH], F32)
    W2 = sb0.t([64, 8, NCH], F32)
    EBT = sb0.t([64, 8, NCH], F32)
    k.dma('sp', [], ['tri32'], tri32, k.dram['tri'].rearrange("a s t -> s a t"))
    k.cp(['tri32'], ['tribf'], tribf, tri32)
    k.dma('sp', [], ['cw'], cw, k.dram['ml_conv_col'][l])
    k.dma('sp', [], ['wbc'], wbc, k.dram['ml_norm_bc'][l])
    k.dma('sp', [], ['G'], G, TM[:, 1024:1040].rearrange("(n p) c -> p n c", p=64))
    for d in range(2):
        gi = G[:, :, d * 8:d * 8 + 4].rearrange("p n h -> p h n")
        gf = G[:, :, d * 8 + 4:d * 8 + 8].rearrange("p n h -> p h n")
        k.cp(['G'], ['IG'], IG[:, d * 4:d * 4 + 4, :], gi)
        k.act(['G'], ['LF'], LF[:, d * 4:d * 4 + 4, :], gf, AF.Exp, scale=-1.0)
    k.act(['LF'], ['LF'], LF, LF, AF.Ln, bias=1.0, scale=1.0)
    k.ts(['LF'], ['LF'], LF, LF, -1.0, op0=ALU.mult)
    for d in range(2):
        rhs = LF[:, d * 4:d * 4 + 4, :]
        k.mm(['tri32', 'LF'], ['ps0'], k.ps[0][0:64, 0:4 * NCH], tri32[:, d, :], rhs)
        k.cp(['ps0'], ['BB'], BB[:, d * 4:d * 4 + 4, :], k.ps[0][0:64, 0:4 * NCH].rearrange("p (h n) -> p h n", n=NCH))
        k.mm(['ones32', 'LF'], ['ps1'], k.ps[1][0:64, 0:4 * NCH], P['ones32'][0:64, 0:64], rhs)
        k.cp(['ps1'], ['BT'], BT[:, d * 4:d * 4 + 4, :], k.ps[1][0:64, 0:4 * NCH].rearrange("p (h n) -> p h n", n=NCH))
    k.act(['BB'], ['EB'], EB, BB, AF.Exp)
    k.act(['BT'], ['EBT'], EBT, BT, AF.Exp)
    k.tt(['IG', 'BB'], ['WS'], WS, IG, BB, ALU.subtract)
    k.tt(['WS', 'BT'], ['W2'], W2, WS, BT, ALU.add)
    k.act(['WS'], ['WS'], WS, WS, AF.Exp)
    k.act(['W2'], ['W2'], W2, W2, AF.Exp)
    base1 = sb0.off
    orders = [[64, 65, 66, 67] + list(range(64)), [67, 66, 65, 64] + list(range(63, -1, -1))]
    psi = [2]

    def nps():
        i = psi[0]
        psi[0] = 2 + (psi[0] - 1) % 6
        return 'ps%d' % i, k.ps[i]

    for hp in range(2):
        sb = SB(nc, base=base1)
        qT = sb.t([64, 2, T], BF16)
        kT = sb.t([64, 2, T], BF16)
        ktm = sb.t([64, NCH, 128], BF16)
        vaug = sb.t([64, NCH, 2, 65], BF16)
        hsum = sb.t([64, NCH, 128], F32)
        ra = sb.t([64, 2 * T], F32)
        raw = ra[:, 0:T]
        acc = ra[:, T:2 * T]
        k.memset([], ['hsum'], hsum, 0.0, eng='pool')
        k.memset([], ['vaug'], vaug, 1.0, eng='pool')
        for qk in range(2):
            for hl in range(2):
                hh = qk * 4 + hp * 2 + hl
                k.dma('sp', [], ['raw'], raw, UT[768 + hh * 64:768 + (hh + 1) * 64, :])
                k.ts(['raw', 'cw'], ['acc'], acc, raw, cw[:, hh, 1:2], cw[:, hh, 3:4], op0=ALU.mult, op1=ALU.add)
                for (a, b) in ((0, L), (L, T)):
                    k.stt(['raw', 'cw', 'acc'], ['acc'], acc[:, a + 1:b], raw[:, a:b - 1], cw[:, hh, 0:1], acc[:, a + 1:b], ALU.mult, ALU.add)
                    k.stt(['raw', 'cw', 'acc'], ['acc'], acc[:, a:b - 1], raw[:, a + 1:b], cw[:, hh, 2:3], acc[:, a:b - 1], ALU.mult, ALU.add)
                if qk == 0:
                    k.act(['acc'], ['qT'], qT[:, hl, :], acc, AF.Silu)
                else:
                    k.act(['acc'], ['acc'], acc, acc, AF.Silu)
                    k.ts(['acc'], ['kT'], kT[:, hl, :], acc, 0.125, op0=ALU.mult)
        for n0 in range(0, NCH, 4):
            pk, pp = nps()
            ppb = pp[:].bitcast(BF16)
            for dn in range(4):
                for hl in range(2):
                    k.tr(['kT', 'identbf'], [pk], ppb[0:64, (dn * 2 + hl) * 64:(dn * 2 + hl + 1) * 64],
                         kT[:, hl, (n0 + dn) * 64:(n0 + dn + 1) * 64], P['identbf'][0:64, 0:64])
            k.cp([pk], ['ktm'], ktm[:, n0:n0 + 4, :], ppb[0:64, 0:512].rearrange("p (n c) -> p n c", c=128))
        vst = acc[:, 0:17 * 128].rearrange("p (n c) -> p n c", c=128)
        for n0 in range(0, NCH, 17):
            k.dma('sp', ['acc'], ['acc'], vst, TM[n0 * 64:(n0 + 17) * 64, hp * 128:(hp + 1) * 128].rearrange("(n p) c -> p n c", p=64))
            k.cp(['acc'], ['vaug'], vaug[:, n0:n0 + 17, :, 0:64], vst.rearrange("p n (h e) -> p n h e", e=64))
        Cst = [[sb.t([64, 65], F32) for _ in range(2)] for _ in range(2)]
        Cbf = [[sb.t([64, 65], BF16) for _ in range(2)] for _ in range(2)]
        dg = [sb.t([64, 64], BF16) for _ in range(4)]
        meb = [sb.t([64, 64], F32) for _ in range(4)]
        pT = [sb.t([64, 64], BF16) for _ in range(4)]
        rsb = [sb.t([64, 65], F32) for _ in range(4)]
        tot = [sb.t([64, 66], F32) for _ in range(4)]
        wv = [sb.t([64, 65], BF16) for _ in range(4)]
        for d in range(2):
            for hl in range(2):
                k.memset([], ['Cst%d%d' % (d, hl)], Cst[d][hl], 0.0)
                k.memset([], ['Cbf%d%d' % (d, hl)], Cbf[d][hl], 0.0)
        for step in range(NCH):
            for d in range(2):
                n = orders[d][step]
                c0 = n * 64
                need_out = (n < 64) or (not last)
                for hl in range(2):
                    u = d * 2 + hl
                    dh = d * 4 + hp * 2 + hl
                    ck, cbk = 'Cst%d%d' % (d, hl), 'Cbf%d%d' % (d, hl)
                    if need_out:
                        pS, ppS = nps()
                        k.mm(['kT', 'qT'], [pS], ppS[0:64, 0:64], kT[:, hl, c0:c0 + 64], qT[:, hl, c0:c0 + 64])
                        k.ts(['identbf', 'EB'], ['dg%d' % u], dg[u], P['identbf'][0:64, 0:64], EB[:, dh, n:n + 1], op0=ALU.mult, eng='pool')
                        pM, ppM = nps()
                        k.mm(['tribf', 'dg%d' % u], [pM], ppM[0:64, 0:64], tribf[:, 1 - d, :], dg[u])
                        k.cp([pM], ['meb%d' % u], meb[u], ppM[0:64, 0:64], eng='act')
                        k.stt([pS, 'WS', 'meb%d' % u], ['pT%d' % u], pT[u], ppS[0:64, 0:64], WS[:, dh, n:n + 1], meb[u], ALU.mult, ALU.mult)
                        pN, ppN = nps()
                        k.mm(['pT%d' % u, 'vaug'], [pN], ppN[0:64, 0:65], pT[u], vaug[:, n, hl, :])
                        pR, ppR = nps()
                        k.mm(['qT', cbk], [pR], ppR[0:64, 0:65], qT[:, hl, c0:c0 + 64], Cbf[d][hl])
                        k.act([pR, 'EB'], ['rsb%d' % u], rsb[u], ppR[0:64, 0:65], AF.Identity, scale=EB[:, dh, n:n + 1])
                        k.tt([pN, 'rsb%d' % u], ['tot%d' % u], tot[u][:, 0:65], ppN[0:64, 0:65], rsb[u], ALU.add)
                        k.act(['tot%d' % u], ['tot%d' % u], tot[u][:, 65:66], tot[u][:, 64:65], AF.Abs)
                        k.ts(['tot%d' % u], ['tot%d' % u], tot[u][:, 65:66], tot[u][:, 65:66], 1.0, op0=ALU.max)
                        k.recip(['tot%d' % u], ['tot%d' % u], tot[u][:, 65:66], tot[u][:, 65:66])
                        hs = hsum[:, n, hl * 64:(hl + 1) * 64]
                        k.stt(['tot%d' % u, 'hsum'], ['hsum'], hs, tot[u][:, 0:64], tot[u][:, 65:66], hs, ALU.mult, ALU.add)
                    if step == NCH - 1:
                        continue
                    k.ts(['vaug', 'W2'], ['wv%d' % u], wv[u], vaug[:, n, hl, :], W2[:, dh, n:n + 1], op0=ALU.mult, eng='pool')
                    pC, ppC = nps()
                    k.mm(['ktm', 'wv%d' % u], [pC], ppC[0:64, 0:65], ktm[:, n, hl * 64:(hl + 1) * 64], wv[u])
                    k.stt([ck, 'EBT', pC], [ck], Cst[d][hl], Cst[d][hl], EBT[:, dh, n:n + 1], ppC[0:64, 0:65], ALU.mult, ALU.add)
                    k.cp([ck], [cbk], Cbf[d][hl], Cst[d][hl], eng='act')
        nout = 64 if last else NCH
        ssum = sb.t([64, NCH * 2], F32)
        ybf = sb.t([64, NCH, 128], BF16)
        ytb = [sb.t([128, 512], BF16) for _ in range(2)]
        k.act(['hsum', 'raw', 'acc'], ['acc', 'raw'], ra, hsum.rearrange("p n c -> p (n c)"), AF.Square)
        k.red(['acc', 'raw'], ['ssum'], ssum, ra.rearrange("p (g e) -> p g e", e=64), ALU.add)
        k.ts(['ssum'], ['ssum'], ssum, ssum, 1.0 / 64.0, EPS, op0=ALU.mult, op1=ALU.add)
        k.act(['ssum'], ['ssum'], ssum, ssum, AF.Sqrt)
        k.recip(['ssum'], ['ssum'], ssum, ssum)
        hv = hsum.rearrange("p n (h e) -> p (n h) e", e=64)
        k.tt(['hsum', 'ssum'], ['hsum'], hv, hv, ssum.unsqueeze(2).to_broadcast([64, NCH * 2, 64]), ALU.mult)
        k.tt(['hsum', 'wbc'], ['hsum'], hsum, hsum, wbc[:, hp * 128:(hp + 1) * 128].unsqueeze(1).to_broadcast([64, NCH, 128]), ALU.mult)
        ost = acc[:, 0:17 * 128].rearrange("p (n c) -> p n c", c=128)
        for n0 in range(0, NCH, 17):
            k.dma('sp', ['acc'], ['acc'], ost, TM[n0 * 64:(n0 + 17) * 64, 256 + hp * 128:256 + (hp + 1) * 128].rearrange("(n p) c -> p n c", p=64))
            k.act(['acc'], ['acc'], ost, ost, AF.Sigmoid)
            k.tt(['acc', 'hsum'], ['ybf'], ybf[:, n0:n0 + 17, :], hsum[:, n0:n0 + 17, :], ost, ALU.mult)
        for gi, n0 in enumerate(range(0, nout, 8)):
            nn = min(8, nout - n0)
            pk, pp = nps()
            ppb = pp[:].bitcast(BF16)
            for dn in range(nn):
                k.tr(['ybf', 'identbf'], [pk], ppb[:, dn * 64:(dn + 1) * 64], ybf[:, n0 + dn, :], P['identbf'][0:64, 0:64])
            yk = 'ytb%d' % (gi % 2)
            k.cp([pk], [yk], ytb[gi % 2][:, 0:nn * 64], ppb[:, 0:nn * 64])
            k.dma('pool', [yk], ['YT'], YT[256 + hp * 128:256 + (hp + 1) * 128, n0 * 64:(n0 + nn) * 64], ytb[gi % 2][:, 0:nn * 64])
        k.em.barrier()


HC = 32
KB = 256 // HC
NB6 = 512 // HC
NHB = 256 // HC
PI = math.pi
HSTOP = [99]


def hyena_consts():
    c = {}
    f32 = np.float32

    def zfeat(Lx, pos):
        t = np.linspace(0.0, 1.0, Lx, dtype=f32)[pos][:, None]
        w = ((2.0 * math.pi / Lx) * np.arange(Lx, dtype=f32))[pos][:, None]
        f = np.linspace(1e-4, 15.0, 16, dtype=f32)[None, :]
        z = np.concatenate([t, np.cos(f * w), -np.sin(f * w)], axis=-1).astype(f32)
        return z, t
    deltas = np.abs(np.linspace(math.log(1e-2) / 1.5, math.log(1e-2) / 0.3, 256, dtype=f32)).astype(f32)
    z, t = zfeat(L, np.arange(L))
    c['hy_z'] = np.ascontiguousarray(z.T)
    c['hy_decay'] = np.ascontiguousarray(np.exp(-t * deltas[None, :]).T.astype(f32))
    pos = np.concatenate([np.arange(CTX - 1, 0, -1), np.arange(CTX)])
    zc, tc = zfeat(CTX, pos)
    c['hy_zc'] = np.ascontiguousarray(zc.T)
    c['hy_decayc'] = np.ascontiguousarray(np.exp(-tc * deltas[None, :]).T.astype(f32))
    n1 = np.arange(32)[:, None]
    k1 = np.arange(64)[None, :]
    a = 2 * np.pi * n1 * k1 / 64.0
    c['hy_F1'] = np.concatenate([np.cos(a), -np.sin(a)], 1).astype(f32)
    n2 = np.arange(128)[:, None, None]
    kk = (np.arange(64)[None, :, None] + 64 * np.arange(128)[None, None, :])
    a = 2 * np.pi * ((n2 * kk) % 8192) / 8192.0
    c['hy_Gr'] = np.cos(a).astype(f32).reshape(128, 8192)
    c['hy_Gi'] = (-np.sin(a)).astype(f32).reshape(128, 8192)
    k2 = np.arange(128)[:, None]
    nn = np.arange(128)[None, :]
    a = 2 * np.pi * ((k2 * nn) % 128) / 128.0
    c['hy_E1'] = np.concatenate([np.cos(a), np.sin(a)], 1).astype(f32)
    c['hy_E2'] = np.concatenate([-np.sin(a), np.cos(a)], 1).astype(f32)
    k1 = np.arange(64)[:, None, None]
    nfull = np.arange(128)[None, :, None] + 128 * np.arange(32)[None, None, :]
    a = 2 * np.pi * ((k1 * nfull) % 8192) / 8192.0
    c['hy_Mr'] = np.cos(a).astype(f32).reshape(64, 4096)
    c['hy_nMi'] = (-np.sin(a)).astype(f32).reshape(64, 4096)
    return c


def hy_load_bf(k, sb, name, shape, stg, key):
    p, n = shape
    dst = sb.t([p, n], BF16)
    src = k.dram[name]
    step = 2048
    for i, c0 in enumerate(range(0, n, step)):
        c1 = min(n, c0 + step)
        k.dma('sp', [], ['hstg'], stg[0:p, 0:c1 - c0], src[:, c0:c1])
        k.cp(['hstg'], [key], dst[:, c0:c1], stg[0:p, 0:c1 - c0], eng=('dve' if i % 2 == 0 else 'pool'))
    return dst


def fft_fwd(k, C, xbf, A, nAi, psctr, consume):
    F1, Gr, Gi = C['F1'], C['Gr'], C['Gi']
    for c0 in range(0, HC, 4):
        pk, pp = psctr()
        for dc in range(4):
            k.mm(['xbf', 'F1'], [pk], pp[:, dc * 128:(dc + 1) * 128], xbf[:, c0 + dc, :], F1)
        src = pp[:, 0:512].rearrange("p (c r q) -> p c r q", r=2, q=64)
        k.cp([pk], ['A'], A[:, :, :, c0:c0 + 4].rearrange("p q r c -> p c r q"), src, eng='act')
        k.ts(['A'], ['nAi'], nAi[:, :, c0:c0 + 4], A[:, :, 1, c0:c0 + 4], -1.0, op0=ALU.mult)
    for k0 in range(0, 64, KB):
        pk, pp = psctr()
        for dk in range(KB):
            k1 = k0 + dk
            xr = pp[:, dk * 2 * HC:dk * 2 * HC + HC]
            xi = pp[:, dk * 2 * HC + HC:(dk + 1) * 2 * HC]
            k.mm(['Gr', 'A'], [pk], xr, Gr[:, k1, :], A[:, k1, 0, :], start=True, stop=False)
            k.mm(['Gi', 'nAi'], [pk], xr, Gi[:, k1, :], nAi[:, k1, :], start=False, stop=True)
            k.mm(['Gi', 'A'], [pk], xi, Gi[:, k1, :], A[:, k1, 0, :], start=True, stop=False)
            k.mm(['Gr', 'A'], [pk], xi, Gr[:, k1, :], A[:, k1, 1, :], start=False, stop=True)
        consume(pk, pp, k0)


def phaseH(k, P, l, base, last):
    nc = k.nc
    UT, YT = k.dram['UT'], k.dram['YT']
    HK, HH, CK, UC = k.dram['HK'], k.dram['HH'], k.dram['CK'], k.dram['UC']
    psi = [0]

    def nps():
        i = psi[0]
        psi[0] = (psi[0] + 1) % 8
        return 'ps%d' % i, k.ps[i]

    sb = SB(nc, base=base)
    w1 = sb.t([33, 64], F32)
    w2 = sb.t([64, 64], F32)
    w3 = sb.t([64, 1024], F32)
    sc = sb.t([64, 8], F32)
    b3 = sb.t([128, 8], F32)
    k.dma('sp', [], ['w1'], w1, k.dram['hy_filt_w1'][l])
    k.dma('sp', [], ['w2'], w2, k.dram['hy_filt_w2'][l])
    k.dma('sp', [], ['w3'], w3, k.dram['hy_filt_w3'][l])
    k.dma('sp', [], ['sc'], sc[:, 0:3], k.dram['hy_filt_sc'][l])
    k.dma('sp', [], ['b3'], b3, k.dram['hy_b3_col'][l])
    k.tt(['sc'], ['sc'], sc[:, 3:4], sc[:, 0:1], sc[:, 1:2], ALU.mult)
    k.tt(['sc'], ['sc'], sc[:, 4:5], sc[:, 0:1], sc[:, 2:3], ALU.mult)
    zT = sb.t([33, L], F32)
    h2 = sb.t([64, L], F32)
    h1 = sb.t([64, 512], F32)
    m1 = sb.t([64, 512], F32)
    m2 = sb.t([64, 512], F32)

    def sin_layer(src_ap, wt, kdim, bcol, dst_ap, n):
        pk, pp = nps()
        k.mm(['w1', 'w2', 'zT', 'h1'], [pk], pp[0:64, 0:n], wt, src_ap)
        k.ts([pk, 'sc'], ['m0'], dst_ap, pp[0:64, 0:n], sc[:, 0:1], sc[:, bcol:bcol + 1], op0=ALU.mult, op1=ALU.add)
        k.ts(['m0'], ['m1'], m1[:, 0:n], dst_ap, PI, -2.0 * PI, op0=ALU.is_gt, op1=ALU.mult)
        k.ts(['m0'], ['m2'], m2[:, 0:n], dst_ap, -PI, 2.0 * PI, op0=ALU.is_lt, op1=ALU.mult, eng='pool')
        k.tt(['m1', 'm2'], ['m1'], m1[:, 0:n], m1[:, 0:n], m2[:, 0:n], ALU.add)
        k.tt(['m0', 'm1'], ['m0'], dst_ap, dst_ap, m1[:, 0:n], ALU.add)
        k.act(['m0'], ['m0'], dst_ap, dst_ap, AF.Sin)

    def mlp(zsrc_name, ncols, h2dst):
        k.dma('sp', ['zT'], ['zT'], zT[:, 0:ncols], k.dram[zsrc_name][:, :])
        for c0 in range(0, ncols, 512):
            n = min(512, ncols - c0)
            sin_layer(zT[:, c0:c0 + n], w1, 33, 3, h1[:, 0:n], n)
            sin_layer2(c0, n, h2dst)

    def sin_layer2(c0, n, h2dst):
        pk, pp = nps()
        k.mm(['w2', 'm0'], [pk], pp[0:64, 0:n], w2, h1[:, 0:n])
        d = h2dst[:, c0:c0 + n]
        k.ts([pk, 'sc'], ['h2'], d, pp[0:64, 0:n], sc[:, 0:1], sc[:, 4:5], op0=ALU.mult, op1=ALU.add)
        k.ts(['h2'], ['m1'], m1[:, 0:n], d, PI, -2.0 * PI, op0=ALU.is_gt, op1=ALU.mult)
        k.ts(['h2'], ['m2'], m2[:, 0:n], d, -PI, 2.0 * PI, op0=ALU.is_lt, op1=ALU.mult, eng='pool')
        k.tt(['m1', 'm2'], ['m1'], m1[:, 0:n], m1[:, 0:n], m2[:, 0:n], ALU.add)
        k.tt(['h2', 'm1'], ['h2'], d, d, m1[:, 0:n], ALU.add)
        k.act(['h2'], ['h2'], d, d, AF.Sin)

    dec = [sb.t([128, L], F32) for _ in range(2)]
    kraw = [sb.t([128, L], F32) for _ in range(2)]
    kbfo = [sb.t([128, L], BF16) for _ in range(2)]
    junk = sb.t([128, L], BF16)
    asum = sb.t([128, 4], F32)

    def gen_filters(zname, dname, ncols, ctx):
        mlp(zname, ncols, h2)
        for ch in range(2):
            k.dma('sp', ['dec%d' % ch], ['dec%d' % ch], dec[ch][:, 0:ncols], k.dram[dname][ch * 128:(ch + 1) * 128, :])
        for o in range(2):
            for ch in range(2):
                k.memset([], ['asum'], asum, 0.0)
                for d in range(2):
                    fc = o * 4 + d * 2 + ch
                    kr = kraw[d]
                    kk_ = 'kraw%d' % d
                    for c0 in range(0, ncols, 512):
                        n = min(512, ncols - c0)
                        pk, pp = nps()
                        k.mm(['w3', 'h2'], [pk], pp[:, 0:n], w3[:, fc * 128:(fc + 1) * 128], h2[:, c0:c0 + n])
                        k.act([pk, 'b3'], [kk_], kr[:, c0:c0 + n], pp[:, 0:n], AF.Identity, bias=b3[:, fc:fc + 1], scale=1.0)
                    k.tt([kk_, 'dec%d' % ch], [kk_], kr[:, 0:ncols], kr[:, 0:ncols], dec[ch][:, 0:ncols], ALU.mult)
                    if not ctx:
                        if d == 1:
                            k.memset([kk_], [kk_], kr[:, 0:1], 0.0)
                        k.act([kk_, 'asum'], ['junk', 'asum'], junk[:, 0:ncols], kr[:, 0:ncols], AF.Abs, accum_out=asum[:, d:d + 1])
                    else:
                        lo, hi = (255, 511) if d == 0 else (0, 255)
                        k.act([kk_, 'asum'], ['junk', 'asum'], junk[:, lo:hi], kr[:, lo:hi], AF.Abs, accum_out=asum[:, d:d + 1])
                k.tt(['asum'], ['asum'], asum[:, 2:3], asum[:, 0:1], asum[:, 1:2], ALU.add)
                k.recip(['asum'], ['asum'], asum[:, 2:3], asum[:, 2:3])
                if not ctx:
                    for d in range(2):
                        fc = o * 4 + d * 2 + ch
                        k.ts(['kraw%d' % d, 'asum'], ['kbfo%d' % d], kbfo[d], kraw[d], asum[:, 2:3], op0=ALU.mult,
                             eng=('dve' if d == 0 else 'pool'))
                        k.dma('pool', ['kbfo%d' % d], ['HK'], HK[fc], kbfo[d])
                else:
                    k.ts(['kraw0', 'asum'], ['kraw0'], kraw[0][:, 255:511], kraw[0][:, 255:511], asum[:, 2:3], op0=ALU.mult)
                    k.ts(['kraw1', 'asum', 'kraw0'], ['kraw0'], kraw[0][:, 0:255], kraw[1][:, 0:255], asum[:, 2:3], op0=ALU.mult)
                    k.dma('pool', ['kraw0'], ['CK'], CK[o * 2 + ch], kraw[0][:, 0:511])

    gen_filters('hy_z', 'hy_decay', L, False)
    if not last:
        gen_filters('hy_zc', 'hy_decayc', 511, True)
    k.em.barrier()

    if HSTOP[0] <= 1:
        return
    sb = SB(nc, base=base)
    stg = sb.t([128, 2048], F32)
    C = {}
    C['F1'] = hy_load_bf(k, sb, 'hy_F1', [32, 128], stg, 'F1')
    C['Gr'] = hy_load_bf(k, sb, 'hy_Gr', [128, 8192], stg, 'Gr').rearrange("p (q m) -> p q m", m=128)
    C['Gi'] = hy_load_bf(k, sb, 'hy_Gi', [128, 8192], stg, 'Gi').rearrange("p (q m) -> p q m", m=128)
    C['E1'] = hy_load_bf(k, sb, 'hy_E1', [128, 256], stg, 'E1')
    C['E2'] = hy_load_bf(k, sb, 'hy_E2', [128, 256], stg, 'E2')
    C['Mr'] = hy_load_bf(k, sb, 'hy_Mr', [64, 4096], stg, 'Mr').rearrange("p (n m) -> p n m", m=32)
    C['nMi'] = hy_load_bf(k, sb, 'hy_nMi', [64, 4096], stg, 'nMi').rearrange("p (n m) -> p n m", m=32)
    A = sb.t([128, 64, 2, HC], BF16)
    nAi = sb.t([128, 64, HC], BF16)
    base2 = sb.off
    if HSTOP[0] <= 1.5:
        k.em.barrier()
        return

    sbs = SB(nc, base=base2)
    kbf = [sbs.t([32, HC, 128], BF16) for _ in range(2)]
    Hacc = sbs.t([128, 64, 2, HC], F32)
    Hbf = sbs.t([128, 64, 2, HC], BF16)
    xtmp = sbs.t([128, KB, 2, HC], F32)
    SC = 1.0 / 8192.0
    for o in range(2):
        for b4 in range(NHB):
            ch, coff = (b4 * HC) // 128, (b4 * HC) % 128
            for d in range(2):
                fc = o * 4 + d * 2 + ch
                k.dma('sp', ['xbf'], ['xbf'], kbf[d], HK[fc][coff:coff + HC, :].rearrange("c (a b) -> a c b", b=128))

                def consume(pk, pp, k0, d=d):
                    src = pp[:, 0:512].rearrange("p (q r c) -> p q r c", r=2, c=HC)
                    dst = Hacc[:, k0:k0 + KB, :, :]
                    if d == 0:
                        k.act([pk], ['Hacc'], dst, src, AF.Copy, scale=SC)
                    else:
                        k.act([pk], ['xtmp'], xtmp, src, AF.Copy, scale=SC)
                        k.tt(['xtmp', 'Hacc'], ['Hacc'], dst[:, :, 0, :], dst[:, :, 0, :], xtmp[:, :, 0, :], ALU.add)
                        k.tt(['xtmp', 'Hacc'], ['Hacc'], dst[:, :, 1, :], dst[:, :, 1, :], xtmp[:, :, 1, :], ALU.subtract, eng='pool')
                fft_fwd(k, C, kbf[d], A, nAi, nps, consume)
            k.cp(['Hacc'], ['Hbf'], Hbf, Hacc, eng='pool')
            k.dma('pool', ['Hbf'], ['HH'], HH[o * NHB + b4], Hbf.rearrange("p q r c -> p (q r c)"))
    k.em.barrier()

    if HSTOP[0] <= 2:
        return
    sbc = SB(nc, base=base2)
    raw = sbc.t([128, T], F32)
    acc = sbc.t([128, T], F32)
    ucb = sbc.t([128, T], BF16)
    cw = sbc.t([128, 6, 4], F32)
    k.dma('sp', [], ['cw'], cw, k.dram['hy_conv_col'][l])
    for cc in range(6):
        k.dma('sp', ['raw'], ['raw'], raw, UT[cc * 128:(cc + 1) * 128, :])
        k.ts(['raw', 'cw'], ['acc'], acc, raw, cw[:, cc, 1:2], cw[:, cc, 3:4], op0=ALU.mult, op1=ALU.add)
        for (a, b) in ((0, L), (L, T)):
            k.stt(['raw', 'cw', 'acc'], ['acc'], acc[:, a + 1:b], raw[:, a:b - 1], cw[:, cc, 0:1], acc[:, a + 1:b], ALU.mult, ALU.add)
            k.stt(['raw', 'cw', 'acc'], ['acc'], acc[:, a:b - 1], raw[:, a + 1:b], cw[:, cc, 2:3], acc[:, a:b - 1], ALU.mult, ALU.add)
        k.cp(['acc'], ['ucb'], ucb, acc, eng='act')
        k.dma('pool', ['ucb'], ['UC'], UC[cc * 128:(cc + 1) * 128, :], ucb)
    k.em.barrier()

    if HSTOP[0] <= 3:
        return
    sbd = SB(nc, base=base2)
    vbf = sbd.t([32, HC, 128], BF16)
    x1bf = sbd.t([32, HC, 128], BF16)
    x2bf = sbd.t([32, HC, 128], BF16)
    z1 = sbd.t([32, HC, 128], BF16)
    z2 = sbd.t([32, HC, 128], BF16)
    dv = sbd.t([32, HC, 128], BF16)
    dbc = sbd.t([32, 2, 256], F32)
    Hs = sbd.t([128, 64, 2, HC], BF16)
    Y = sbd.t([128, 64, 2, HC], BF16)
    Zs = sbd.t([64, 2, 128, HC], BF16)
    xs = [sbd.t([128, KB, 2, HC], F32) for _ in range(2)]
    ta = [sbd.t([128, KB, HC], F32) for _ in range(2)]
    tb = [sbd.t([128, KB, HC], F32) for _ in range(2)]
    tg = [sbd.t([32, HC, NB6], F32) for _ in range(2)]
    k.dma('sp', [], ['dbc'], dbc, k.dram['hy_d_bc'][l])
    xctr = [0]
    for b4 in range(NHB):
        c0g = b4 * HC
        for (tile_, key, r0) in ((vbf, 'xbf', 0), (x1bf, 'x1bf', 256), (x2bf, 'x2bf', 512)):
            k.dma('sp', [key], [key], tile_, UC[r0 + c0g:r0 + c0g + HC, 0:L].rearrange("c (a b) -> a c b", b=128))
        for o in range(2):
            xin, xkey = (vbf, 'xbf') if o == 0 else (z1, 'z1')
            gate, gkey = (x1bf, 'x1bf') if o == 0 else (x2bf, 'x2bf')
            zout, zkey = (z1, 'z1') if o == 0 else (z2, 'z2')
            k.tt([xkey, 'dbc'], ['dv'], dv, xin, dbc[:, o, c0g:c0g + HC].unsqueeze(2).to_broadcast([32, HC, 128]), ALU.mult, eng='pool')
            k.dma('sp', ['Hs'], ['Hs'], Hs.rearrange("p q r c -> p (q r c)"), HH[o * NHB + b4])

            def consume(pk, pp, k0):
                i = xctr[0] % 2
                xctr[0] += 1
                xk, tak, tbk = 'xs%d' % i, 'ta%d' % i, 'tb%d' % i
                k.cp([pk], [xk], xs[i], pp[:, 0:512].rearrange("p (q r c) -> p q r c", r=2, c=HC), eng='act')
                Xr, Xi = xs[i][:, :, 0, :], xs[i][:, :, 1, :]
                Hr, Hi = Hs[:, k0:k0 + KB, 0, :], Hs[:, k0:k0 + KB, 1, :]
                Yr, Yi = Y[:, k0:k0 + KB, 0, :], Y[:, k0:k0 + KB, 1, :]
                k.tt([xk, 'Hs'], [tak], ta[i], Xr, Hr, ALU.mult)
                k.tt([xk, 'Hs'], [tbk], tb[i], Xi, Hi, ALU.mult, eng='pool')
                k.tt([tak, tbk], ['Y'], Yr, ta[i], tb[i], ALU.subtract)
                k.tt([xk, 'Hs'], [tak], ta[i], Xr, Hi, ALU.mult, eng='pool')
                k.tt([xk, 'Hs'], [tbk], tb[i], Xi, Hr, ALU.mult)
                k.tt([tak, tbk], ['Y'], Yi, ta[i], tb[i], ALU.add, eng='pool')
            fft_fwd_keyed(k, C, xin, xkey, A, nAi, nps, consume)
            for c0 in range(0, HC, 2):
                pk, pp = nps()
                for dc in range(2):
                    cidx = c0 + dc
                    out = pp[0:64, dc * 256:(dc + 1) * 256]
                    k.mm(['Y', 'E1'], [pk], out, Y[:, :, 0, cidx], C['E1'], start=True, stop=False)
                    k.mm(['Y', 'E2'], [pk], out, Y[:, :, 1, cidx], C['E2'], start=False, stop=True)
                src = pp[0:64, 0:512].rearrange("p (c r n) -> p c r n", r=2, n=128)
                k.cp([pk], ['Zs'], Zs[:, :, :, c0:c0 + 2].rearrange("p r n c -> p c r n"), src, eng=('act' if (c0 // 2) % 2 == 0 else 'dve'))
            for g8, n0 in enumerate(range(0, 128, NB6)):
                pk, pp = nps()
                for dn in range(NB6):
                    n2 = n0 + dn
                    out = pp[0:32, dn * HC:(dn + 1) * HC]
                    k.mm(['Mr', 'Zs'], [pk], out, C['Mr'][:, n2, :], Zs[:, 0, n2, :], start=True, stop=False)
                    k.mm(['nMi', 'Zs'], [pk], out, C['nMi'][:, n2, :], Zs[:, 1, n2, :], start=False, stop=True)
                i = g8 % 2
                src = pp[0:32, 0:NB6 * HC].rearrange("p (n c) -> p c n", c=HC)
                k.tt([pk, 'dv'], ['tg%d' % i], tg[i], src, dv[:, :, n0:n0 + NB6], ALU.add)
                k.tt(['tg%d' % i, gkey], [zkey], zout[:, :, n0:n0 + NB6], tg[i], gate[:, :, n0:n0 + NB6], ALU.mult, eng='pool')
        k.dma('pool', ['z2'], ['YT'], YT[c0g:c0g + HC, 0:L].rearrange("c (a b) -> a c b", b=128), z2)
    k.em.barrier()

    if last or HSTOP[0] <= 4:
        return
    sbx = SB(nc, base=base2)
    ub = sbx.t([128, 3, CTX], BF16)
    uf = sbx.t([128, 3, CTX], F32)
    kf = sbx.t([128, 511], F32)
    accs = [sbx.t([128, CTX], F32) for _ in range(4)]
    zc = sbx.t([128, CTX], F32)
    zb = sbx.t([128, CTX], BF16)
    dcol = sbx.t([128, 4], F32)
    k.dma('sp', [], ['dcol'], dcol, k.dram['hy_d_col'][l])
    for ch in range(2):
        for j in range(3):
            k.dma('sp', ['ub'], ['ub'], ub[:, j, :], UC[j * 256 + ch * 128:j * 256 + (ch + 1) * 128, L:T])
        k.cp(['ub'], ['uf'], uf, ub)
        for o in range(2):
            uin = uf[:, 0, :] if o == 0 else zc
            gate = uf[:, 1 + o, :]
            k.dma('sp', ['kf'], ['kf'], kf, CK[o * 2 + ch])
            for a in range(4):
                k.memset([], ['acc%d' % a], accs[a], 0.0, eng=('dve' if a < 2 else 'pool'))
            for s_ in range(CTX):
                a = s_ % 4
                k.stt(['kf', 'uf', 'zc', 'acc%d' % a], ['acc%d' % a], accs[a], kf[:, 255 - s_:511 - s_], uin[:, s_:s_ + 1], accs[a],
                      ALU.mult, ALU.add)
            k.tt(['acc0', 'acc1'], ['acc0'], accs[0], accs[0], accs[1], ALU.add)
            k.tt(['acc2', 'acc3'], ['acc2'], accs[2], accs[2], accs[3], ALU.add, eng='pool')
            k.tt(['acc0', 'acc2'], ['acc0'], accs[0], accs[0], accs[2], ALU.add)
            k.stt(['uf', 'zc', 'dcol', 'acc0'], ['acc0'], accs[0], uin, dcol[:, o * 2 + ch:o * 2 + ch + 1], accs[0], ALU.mult, ALU.add)
            k.tt(['acc0', 'uf'], ['zc'], zc, accs[0], gate, ALU.mult)
        k.cp(['zc'], ['zb'], zb, zc)
        k.dma('pool', ['zb'], ['YT'], YT[ch * 128:(ch + 1) * 128, L:T], zb)
    k.em.barrier()


def fft_fwd_keyed(k, C, xin, xkey, A, nAi, psctr, consume):
    F1, Gr, Gi = C['F1'], C['Gr'], C['Gi']
    for c0 in range(0, HC, 4):
        pk, pp = psctr()
        for dc in range(4):
            k.mm([xkey, 'F1'], [pk], pp[:, dc * 128:(dc + 1) * 128], xin[:, c0 + dc, :], F1)
        src = pp[:, 0:512].rearrange("p (c r q) -> p c r q", r=2, q=64)
        k.cp([pk], ['A'], A[:, :, :, c0:c0 + 4].rearrange("p q r c -> p c r q"), src, eng='act')
        k.ts(['A'], ['nAi'], nAi[:, :, c0:c0 + 4], A[:, :, 1, c0:c0 + 4], -1.0, op0=ALU.mult)
    for k0 in range(0, 64, KB):
        pk, pp = psctr()
        for dk in range(KB):
            k1 = k0 + dk
            xr = pp[:, dk * 2 * HC:dk * 2 * HC + HC]
            xi = pp[:, dk * 2 * HC + HC:(dk + 1) * 2 * HC]
            k.mm(['Gr', 'A'], [pk], xr, Gr[:, k1, :], A[:, k1, 0, :], start=True, stop=False)
            k.mm(['Gi', 'nAi'], [pk], xr, Gi[:, k1, :], nAi[:, k1, :], start=False, stop=True)
            k.mm(['Gi', 'A'], [pk], xi, Gi[:, k1, :], A[:, k1, 0, :], start=True, stop=False)
            k.mm(['Gr', 'A'], [pk], xi, Gr[:, k1, :], A[:, k1, 1, :], start=False, stop=True)
        consume(pk, pp, k0)


BIG = 1.0e9


def phaseC(k, P, l, base, last, xres):
    nc = k.nc
    YT, XMIX, H2T, XRES = k.dram['YT'], k.dram['XMIX'], k.dram['H2T'], k.dram['XRES']
    ntiles = 32 if last else NT
    psi = [0]

    def nps():
        i = psi[0]
        psi[0] = (psi[0] + 1) % 8
        return 'ps%d' % i, k.ps[i]

    sb0 = SB(nc, base=base)
    grow = sb0.t([128, 2, 2, D], F32)
    gates = sb0.t([128, NT, 32], F32)
    dg = sb0.t([128, 128], F32)
    for gi, c0 in enumerate((16, 40)):
        for j in range(2):
            for kk in range(8):
                k.ts(['ident32', 'mod'], ['dg'], dg, P['ident32'], P['mod'][:, l, c0 + kk, j:j + 1], op0=ALU.mult)
                pk, pp = nps()
                k.mm(['ones32', 'dg'], [pk], pp[:, 0:128], P['ones32'], dg)
                k.cp([pk], ['grow'], grow[:, gi, j, kk * 128:(kk + 1) * 128], pp[:, 0:128], eng='act')
    base1 = sb0.off
    sb = SB(nc, base=base1)
    Wout = sb.t([128, 8, D], BF16)
    Wr = sb.t([128, 8, 36], F32)
    rb = sb.t([128, 36], F32)
    stg = sb.t([128, 8, 512], F32)
    wv = k.dram['w_out'][l].rearrange("(k p) n -> p k n", p=128)
    for i, c0 in enumerate((0, 512)):
        k.dma('sp', ['stg'], ['stg'], stg, wv[:, :, c0:c0 + 512])
        k.cp(['stg'], ['Wout'], Wout[:, :, c0:c0 + 512], stg)
    k.dma('sp', [], ['Wr'], Wr, k.dram['moe_wr'][l].rearrange("(k p) n -> p k n", p=128))
    k.dma('sp', [], ['rb'], rb, k.dram['moe_rb_bc'][l])
    yT = [sb.t([128, 8, 512], BF16) for _ in range(2)]
    xt = [sb.t([128, D], F32) for _ in range(2)]
    xm = [sb.t([128, D], F32) for _ in range(2)]
    xn = sb.t([128, D], F32)
    junk = sb.t([128, D], BF16)
    h32 = sb.t([128, 8, 128], F32)
    hbf = [sb.t([128, 8, 128], BF16) for _ in range(2)]
    ss = [sb.t([128, 2], F32) for _ in range(2)]
    lg = sb.t([128, 36], F32)
    rt = sb.t([128, 16], F32)
    oh = sb.t([128, 4], F32)
    ml = sb.t([128, 32], F32)
    e1 = sb.t([128, 32], F32)
    e2 = sb.t([128, 32], F32)
    tmp32 = sb.t([128, 32], F32)
    for ti in range(ntiles):
        j = 0 if ti < 32 else 1
        g, tl = ti // 4, ti % 4
        yb = g % 2
        yk = 'yT%d' % yb
        if tl == 0:
            n = min(512, ntiles * 128 - g * 512)
            k.dma('sp', [yk], [yk], yT[yb][:, :, 0:n], YT[:, g * 512:g * 512 + n].rearrange("(c p) t -> p c t", p=128))
        b = ti % 2
        xk, mk, sk, hk = 'xt%d' % b, 'xm%d' % b, 'ss%d' % b, 'hbf%d' % b
        k.dma('sp', [xk], [xk], xt[b], xres[ti * 128:(ti + 1) * 128, :])
        for half in range(2):
            pk, pp = nps()
            for f in range(8):
                k.mm([yk, 'Wout'], [pk], pp[:, 0:512], yT[yb][:, f, tl * 128:(tl + 1) * 128], Wout[:, f, half * 512:(half + 1) * 512],
                     start=(f == 0), stop=(f == 7))
            cs = slice(half * 512, (half + 1) * 512)
            k.tt([pk, 'grow'], [mk], xm[b][:, cs], pp[:, 0:512], grow[:, 0, j, cs], ALU.mult)
            k.tt([mk, xk], [mk], xm[b][:, cs], xm[b][:, cs], xt[b][:, cs], ALU.add, eng='pool')
        k.dma('pool', [mk], ['XMIX'], XMIX[ti * 128:(ti + 1) * 128, :], xm[b])
        k.memset([], [sk], ss[b], 0.0)
        k.act([mk, sk], ['junk', sk], junk, xm[b], AF.Square, accum_out=ss[b][:, 0:1])
        k.ts([sk], [sk], ss[b][:, 1:2], ss[b][:, 0:1], 1.0 / D, EPS, op0=ALU.mult, op1=ALU.add)
        k.act([sk], [sk], ss[b][:, 1:2], ss[b][:, 1:2], AF.Sqrt)
        k.recip([sk], [sk], ss[b][:, 1:2], ss[b][:, 1:2])
        k.ts([mk, sk], ['xn'], xn, xm[b], ss[b][:, 1:2], op0=ALU.mult)
        for h2 in range(2):
            pk, pp = nps()
            for q in range(4):
                kk = h2 * 4 + q
                k.tr(['xn', 'ident32'], [pk], pp[:, q * 128:(q + 1) * 128], xn[:, kk * 128:(kk + 1) * 128], P['ident32'])
            for q in range(4):
                kk = h2 * 4 + q
                k.act([pk, 'A2', 'mod'], ['h32'], h32[:, kk, :], pp[:, q * 128:(q + 1) * 128], AF.Identity,
                      scale=P['A2'][:, l, kk, j:j + 1], bias=P['mod'][:, l, 24 + kk, j:j + 1])
        k.cp(['h32'], [hk], hbf[b], h32, eng='pool')
        k.dma('pool', [hk], ['H2T'], H2T[:, ti * 128:(ti + 1) * 128].rearrange("(c p) t -> p c t", p=128), hbf[b])
        pk, pp = nps()
        for kk in range(8):
            k.mm(['h32', 'Wr'], [pk], pp[:, 0:36], h32[:, kk, :], Wr[:, kk, :], start=(kk == 0), stop=(kk == 7))
        k.tt([pk, 'rb'], ['lg'], lg, pp[:, 0:36], rb, ALU.add)
        k.red(['lg'], ['rt'], rt[:, 0:1], lg[:, 0:4], ALU.max)
        k.ts(['lg', 'rt'], ['oh'], oh, lg[:, 0:4], rt[:, 0:1], op0=ALU.is_equal)
        k.ts(['rt'], ['rt'], rt[:, 1:2], rt[:, 0:1], -1.0, op0=ALU.mult)
        k.memset(['rt'], ['rt'], rt[:, 2:3], 0.0)
        k.act(['lg', 'rt'], ['tmp32', 'rt'], tmp32[:, 0:4], lg[:, 0:4], AF.Exp, bias=rt[:, 1:2], scale=1.0, accum_out=rt[:, 2:3])
        k.recip(['rt'], ['rt'], rt[:, 3:4], rt[:, 2:3])
        k.ts(['oh'], ['oh'], oh, oh, 1.0, BIG, op0=ALU.subtract, op1=ALU.mult)
        k.tt(['lg', 'oh'], ['ml'], ml.rearrange("p (g e) -> p g e", e=8), lg[:, 4:36].rearrange("p (g e) -> p g e", e=8),
             oh.unsqueeze(2).to_broadcast([128, 4, 8]), ALU.add)
        k.red(['ml'], ['rt'], rt[:, 4:5], ml, ALU.max)
        k.ts(['ml', 'rt'], ['e1'], e1, ml, rt[:, 4:5], op0=ALU.is_equal)
        k.ts(['e1'], ['tmp32'], tmp32, e1, -BIG, op0=ALU.mult)
        k.tt(['ml', 'tmp32'], ['ml'], ml, ml, tmp32, ALU.add)
        k.red(['ml'], ['rt'], rt[:, 5:6], ml, ALU.max)
        k.ts(['ml', 'rt'], ['e2'], e2, ml, rt[:, 5:6], op0=ALU.is_equal)
        k.tt(['rt'], ['rt'], rt[:, 6:7], rt[:, 5:6], rt[:, 4:5], ALU.subtract)
        k.act(['rt'], ['rt'], rt[:, 6:7], rt[:, 6:7], AF.Exp)
        k.ts(['rt'], ['rt'], rt[:, 7:8], rt[:, 6:7], 1.0, op0=ALU.add)
        k.recip(['rt'], ['rt'], rt[:, 7:8], rt[:, 7:8])
        k.tt(['rt'], ['rt'], rt[:, 8:9], rt[:, 6:7], rt[:, 7:8], ALU.mult)
        k.tt(['rt'], ['rt'], rt[:, 9:10], rt[:, 7:8], rt[:, 3:4], ALU.mult)
        k.tt(['rt'], ['rt'], rt[:, 10:11], rt[:, 8:9], rt[:, 3:4], ALU.mult)
        k.ts(['e1', 'rt'], ['gates'], gates[:, ti, :], e1, rt[:, 9:10], op0=ALU.mult)
        k.stt(['e2', 'rt', 'gates'], ['gates'], gates[:, ti, :], e2, rt[:, 10:11], gates[:, ti, :], ALU.mult, ALU.add)
    k.em.barrier()
    NPASS = 3
    per = (ntiles + NPASS - 1) // NPASS
    sb = SB(nc, base=base1)
    h2 = sb.t([128, 8, per * 128], BF16)
    facc = sb.t([128, per, D], F32)
    wst = [sb.t([128, 8, 512], F32) for _ in range(2)]
    w1b = [sb.t([128, 8, 512], BF16) for _ in range(2)]
    w3b = [sb.t([128, 8, 512], BF16) for _ in range(2)]
    w2b = [sb.t([128, 4, D], BF16) for _ in range(2)]
    gT = [sb.t([128, 4, 512], BF16) for _ in range(2)]
    st = [sb.t([128, 512], F32) for _ in range(2)]
    xo = [sb.t([128, D], F32) for _ in range(2)]
    fw = sb.t([128, D], F32)
    if last:
        k.dma('sp', [], ['fw'], fw, k.dram['final_bc'][:, :])
    W1, W3, W2 = k.dram['moe_w1'], k.dram['moe_w3'], k.dram['moe_w2']
    sctr = [0]

    def load_w(src_ap, dst, dkey, shape3):
        i = sctr[0] % 2
        sctr[0] += 1
        sk_ = 'wst%d' % i
        view = wst[i].rearrange("p a b -> p (a b)").rearrange("p (a b) -> p a b", b=shape3)
        k.dma('sp', [sk_], [sk_], view, src_ap)
        k.cp([sk_], [dkey], dst, view, eng='pool')

    for ps_ in range(NPASS):
        t_lo = ps_ * per
        t_hi = min(ntiles, t_lo + per)
        nt_ = t_hi - t_lo
        ntok = nt_ * 128
        k.dma('sp', ['h2'], ['h2'], h2[:, :, 0:ntok], H2T[:, t_lo * 128:t_hi * 128].rearrange("(c p) t -> p c t", p=128))
        k.memset(['facc'], ['facc'], facc, 0.0, eng='pool')
        for e in range(32):
            wb = e % 2
            load_w(W1[l, e].rearrange("(k p) n -> p k n", p=128), w1b[wb], 'w1b%d' % wb, 512)
            load_w(W3[l, e].rearrange("(k p) n -> p k n", p=128), w3b[wb], 'w3b%d' % wb, 512)
            load_w(W2[l, e].rearrange("(k p) n -> p k n", p=128), w2b[wb], 'w2b%d' % wb, D)
            for c0 in range(0, ntok, 512):
                n = min(512, ntok - c0)
                gb = (c0 // 512) % 2
                gk = 'gT%d' % gb
                for f in range(4):
                    p1k, pp1 = nps()
                    for kk in range(8):
                        k.mm(['w1b%d' % wb, 'h2'], [p1k], pp1[:, 0:n], w1b[wb][:, kk, f * 128:(f + 1) * 128], h2[:, kk, c0:c0 + n],
                             start=(kk == 0), stop=(kk == 7))
                    p3k, pp3 = nps()
                    for kk in range(8):
                        k.mm(['w3b%d' % wb, 'h2'], [p3k], pp3[:, 0:n], w3b[wb][:, kk, f * 128:(f + 1) * 128], h2[:, kk, c0:c0 + n],
                             start=(kk == 0), stop=(kk == 7))
                    sbi = f % 2
                    k.act([p1k], ['st%d' % sbi], st[sbi][:, 0:n], pp1[:, 0:n], AF.Silu)
                    k.tt(['st%d' % sbi, p3k], [gk], gT[gb][:, f, 0:n], st[sbi][:, 0:n], pp3[:, 0:n], ALU.mult)
                for tl in range(n // 128):
                    t = c0 // 128 + tl
                    for half in range(2):
                        pk, pp = nps()
                        for f in range(4):
                            k.mm([gk, 'w2b%d' % wb], [pk], pp[:, 0:512], gT[gb][:, f, tl * 128:(tl + 1) * 128], w2b[wb][:, f, half * 512:(half + 1) * 512],
                                 start=(f == 0), stop=(f == 3))
                        fa = facc[:, t, half * 512:(half + 1) * 512]
                        k.stt([pk, 'gates', 'facc'], ['facc'], fa, pp[:, 0:512], gates[:, t_lo + t, e:e + 1], fa, ALU.mult, ALU.add)
        for t in range(nt_):
            ti = t_lo + t
            j = 0 if ti < 32 else 1
            b = t % 2
            ok = 'xo%d' % b
            k.dma('sp', [ok], [ok], xo[b], XMIX[ti * 128:(ti + 1) * 128, :])
            k.tt(['facc', 'grow'], ['facc'], facc[:, t, :], facc[:, t, :], grow[:, 1, j, :], ALU.mult, eng='pool')
            k.tt(['facc', ok], [ok], xo[b], xo[b], facc[:, t, :], ALU.add)
            if not last:
                k.dma('pool', [ok], ['XRES'], XRES[ti * 128:(ti + 1) * 128, :], xo[b])
            else:
                sk = 'fss'
                fs = st[0][:, 0:2]
                k.memset(['st0'], ['st0'], fs, 0.0)
                k.act([ok, 'st0'], ['gT0', 'st0'], gT[0].rearrange("p a b -> p (a b)")[:, 0:D], xo[b], AF.Square, accum_out=fs[:, 0:1])
                k.ts(['st0'], ['st0'], fs[:, 1:2], fs[:, 0:1], 1.0 / D, EPS, op0=ALU.mult, op1=ALU.add)
                k.act(['st0'], ['st0'], fs[:, 1:2], fs[:, 1:2], AF.Sqrt)
                k.recip(['st0'], ['st0'], fs[:, 1:2], fs[:, 1:2])
                k.ts([ok, 'st0'], [ok], xo[b], xo[b], fs[:, 1:2], op0=ALU.mult)
                k.tt([ok, 'fw'], [ok], xo[b], xo[b], fw, ALU.mult)
                k.dma('pool', [ok], ['out'], k.dram['out'][ti * 128:(ti + 1) * 128, :], xo[b])
    k.em.barrier()


def build(stage='full', debug=()):
    nc = bass.Bass("TRN2", target_bir_lowering=False)
    k = K(nc, debug=debug)
    P = {}
    sbp = SB(nc)
    k.din('xin', [T, D])
    k.din('w_in_fm', [DEPTH, D, NFM])
    k.din('w_in_tm', [DEPTH, D, NTM])
    k.din('b_fm_col', [128, DEPTH, 18])
    k.din('b_tm_bc', [DEPTH, 128, NTM])
    k.dscratch('UT', [1280, T])
    k.dscratch('QKT', [1024, T], BF16)
    k.dscratch('TM', [T, NTM])
    k.dscratch('XRES', [T, D])
    k.dscratch('YT', [1024, T], BF16)
    k.din('da_lambda', [DEPTH, 256])
    k.dscratch('XMIX', [T, D])
    k.dscratch('H2T', [D, T], BF16)
    k.din('w_out', [DEPTH, D, D])
    k.din('moe_wr', [DEPTH, D, 36])
    k.din('moe_rb_bc', [DEPTH, 128, 36])
    k.din('moe_w1', [DEPTH, 32, D, 512])
    k.din('moe_w3', [DEPTH, 32, D, 512])
    k.din('moe_w2', [DEPTH, 32, 512, D])
    k.din('final_bc', [128, D])
    if stage == 'full':
        k.dout('out', [L, D])
    k.dscratch('HK', [8, 128, L], BF16)
    k.dscratch('HH', [2 * NHB, 128, 64 * 2 * HC], BF16)
    k.dscratch('CK', [4, 128, 511])
    k.dscratch('UC', [768, T], BF16)
    for nm, shp in (('hy_filt_w1', [DEPTH, 33, 64]), ('hy_filt_w2', [DEPTH, 64, 64]), ('hy_filt_w3', [DEPTH, 64, 1024]),
                    ('hy_filt_sc', [DEPTH, 64, 3]), ('hy_b3_col', [DEPTH, 128, 8]), ('hy_conv_col', [DEPTH, 128, 6, 4]),
                    ('hy_d_bc', [DEPTH, 32, 2, 256]), ('hy_d_col', [DEPTH, 128, 4]),
                    ('hy_z', [33, L]), ('hy_decay', [256, L]), ('hy_zc', [33, 511]), ('hy_decayc', [256, 511]),
                    ('hy_F1', [32, 128]), ('hy_Gr', [128, 8192]), ('hy_Gi', [128, 8192]), ('hy_E1', [128, 256]),
                    ('hy_E2', [128, 256]), ('hy_Mr', [64, 4096]), ('hy_nMi', [64, 4096])):
        k.din(nm, shp)
    k.din('tri', [2, 64, 64])
    k.din('ml_conv_col', [DEPTH, 64, 8, 4])
    k.din('ml_norm_bc', [DEPTH, 64, 256])
    k.din('da_subln_col', [DEPTH, 128, 1])
    phase0(k, P, sbp)
    P['negc'] = sbp.t([128, DEPTH, 2, 4], F32)
    base = sbp.off
    for l in range(DEPTH):
        xres = k.dram['xin'] if l == 0 else k.dram['XRES']
        phaseA(k, P, l, base, xres)
        if stage == 'A':
            break
        if stage not in ('B2', 'H'):
            phaseB1(k, P, l, base, l == DEPTH - 1)
        if stage == 'B1':
            break
        if stage != 'H':
            phaseB2(k, P, l, base, l == DEPTH - 1)
        if stage == 'B2':
            break
        phaseH(k, P, l, base, l == DEPTH - 1)
        if stage == 'H':
            break
        phaseC(k, P, l, base, (l == DEPTH - 1) and stage == 'full', xres)
        if stage == 'C':
            break
    k.em.barrier()
    return nc, k


def run(inputs, stage='full', debug=(), cores=8):
    consts = make_consts()
    nc, k = build(stage, debug)
    in_maps = []
    for b in range(cores):
        m = prep_inputs(inputs, b)
        m.update(consts)
        in_maps.append({kk: v for kk, v in m.items() if kk in k.dram})
    res = run_bass_kernel_spmd(nc, in_maps, core_ids=list(range(cores)))
    return res.results


def kernel(**inputs):
    inp = {kk: np.asarray(v) for kk, v in inputs.items()}
    res = run(inp, stage='full', cores=8)
    return np.stack([np.asarray(r['out'], dtype=np.float32) for r in res], axis=0)
```

```python
import math
import os
import numpy as np
import concourse.bass as bass
import concourse.mybir as mybir
from concourse.bass_utils import run_bass_kernel_spmd

F32 = mybir.dt.float32
BF16 = mybir.dt.bfloat16
AF = mybir.ActivationFunctionType
ALU = mybir.AluOpType
AX = mybir.AxisListType

D = 1024
L = 4096
CTX = 256
T = L + CTX
NT = T // 128
DEPTH = 2
EPS = 1e-6
N_IN = 3344
ML_OFF = 768
DA_OFF = 1808
NFM = 2304
NTM = 1040
FM_COLS = list(range(0, 768)) + list(range(768, 1280)) + list(range(1808, 2832))
TM_COLS = list(range(1280, 1792)) + list(range(2832, 3344)) + list(range(1792, 1808))


class Em:
    NDMA = 32
    SAME_ENGINE_WAITS = True

    def __init__(self, nc):
        self.nc = nc
        self.eng = {'pe': nc.tensor, 'act': nc.scalar, 'dve': nc.vector, 'pool': nc.gpsimd, 'sp': nc.sync}
        self.sem = {k: nc.alloc_semaphore('s_' + k) for k in ('pe', 'act', 'dve', 'pool')}
        self.cnt = {k: 0 for k in self.sem}
        self.dsem = [nc.alloc_semaphore('s_dma%d' % i) for i in range(self.NDMA)]
        self.dcnt = [0] * self.NDMA
        self.dnext = 0
        self.waited = {e: {} for e in self.eng}
        self.lastw = {}
        self.readers = {}
        self.ninst = 0

    def _semh(self, key):
        return self.sem[key] if isinstance(key, str) else self.dsem[key[1]]

    def _wait(self, e, ev):
        key, val = ev
        w = self.waited[e]
        if w.get(key, 0) >= val:
            return
        self.eng[e].wait_ge(self._semh(key), val)
        w[key] = val

    def _deps(self, e, reads, writes):
        best = {}
        for k in reads:
            ev = self.lastw.get(k)
            if ev is not None and best.get(ev[0], 0) < ev[1]:
                best[ev[0]] = ev[1]
        for k in writes:
            ev = self.lastw.get(k)
            if ev is not None and best.get(ev[0], 0) < ev[1]:
                best[ev[0]] = ev[1]
            for ev in self.readers.get(k, ()):
                if best.get(ev[0], 0) < ev[1]:
                    best[ev[0]] = ev[1]
        for key, val in best.items():
            if key == e and (e == 'pe' or not Em.SAME_ENGINE_WAITS):
                continue
            self._wait(e, (key, val))

    def _record(self, ev, reads, writes):
        for k in reads:
            lst = self.readers.setdefault(k, [])
            lst[:] = [x for x in lst if x[0] != ev[0]]
            lst.append(ev)
        for k in writes:
            self.lastw[k] = ev
            self.readers[k] = []

    def op(self, e, reads, writes, fn):
        self._deps(e, reads, writes)
        ins = fn(self.eng[e])
        self.cnt[e] += 1
        ins.then_inc(self.sem[e], 1)
        self._record((e, self.cnt[e]), reads, writes)
        self.ninst += 1

    def dma(self, q, reads, writes, out, in_, **kw):
        i = self.dnext
        self.dnext = (i + 1) % self.NDMA
        if self.dcnt[i] > 0:
            self._wait(q, (('d', i), 16 * self.dcnt[i]))
        self._deps(q, reads, writes)
        ins = self.eng[q].dma_start(out=out, in_=in_, **kw)
        self.dcnt[i] += 1
        ins.then_inc(self.dsem[i], 16)
        self._record((('d', i), 16 * self.dcnt[i]), reads, writes)
        self.ninst += 1

    def barrier(self):
        for e in self.eng:
            for k in self.sem:
                if self.cnt[k] > 0 and k != e:
                    self._wait(e, (k, self.cnt[k]))
            for i in range(self.NDMA):
                if self.dcnt[i] > 0:
                    self._wait(e, (('d', i), 16 * self.dcnt[i]))
        self.lastw = {}
        self.readers = {}


class SB:
    _arena = {}

    def __init__(self, nc, base=0, limit=None):
        self.nc = nc
        if id(nc) not in SB._arena:
            nwords = (nc.sbuf_bytes_remaining - 256) // 4
            SB._arena[id(nc)] = (nc.alloc_sbuf_tensor("arena", [128, nwords], F32), nwords * 4)
        self.arena, cap = SB._arena[id(nc)]
        self.off = base
        self.limit = cap if limit is None else limit

    def t(self, shape, dtype, name=None):
        per = 1
        for s in shape[1:]:
            per *= s
        esz = 2 if dtype == BF16 else 4
        nbytes = (per * esz + 63) // 64 * 64
        assert self.off % 4 == 0
        w0 = self.off // 4
        ap = self.arena[0:shape[0], w0:w0 + nbytes // 4]
        if dtype != F32:
            ap = ap.bitcast(dtype)
        ap = ap[:, 0:per]
        if len(shape) == 3:
            ap = ap.rearrange("p (a b) -> p a b", b=shape[2])
        elif len(shape) == 4:
            ap = ap.rearrange("p (a b c) -> p a b c", b=shape[2], c=shape[3])
        self.off += nbytes
        assert self.off <= self.limit, ("SBUF overflow", name, self.off, self.limit)
        return ap


class K:
    def __init__(self, nc, debug=()):
        self.nc = nc
        self.em = Em(nc)
        self.debug = set(debug)
        self.dram = {}
        self.ps = [nc.alloc_psum_tensor("psb%d" % i, [128, 512], F32) for i in range(8)]

    def din(self, name, shape, dtype=F32):
        ap = self.nc.dram_tensor(name, list(shape), dtype, kind="ExternalInput").ap()
        self.dram[name] = ap
        return ap

    def dscratch(self, name, shape, dtype=F32):
        kind = "ExternalOutput" if name in self.debug else "Internal"
        ap = self.nc.dram_tensor(name, list(shape), dtype, kind=kind).ap()
        self.dram[name] = ap
        return ap

    def dout(self, name, shape, dtype=F32):
        ap = self.nc.dram_tensor(name, list(shape), dtype, kind="ExternalOutput").ap()
        self.dram[name] = ap
        return ap

    def dma(self, q, r, w, out, in_, **kw):
        self.em.dma(q, r, w, out, in_, **kw)

    def mm(self, r, w, out, lhsT, rhs, start=True, stop=True):
        self.em.op('pe', r, w, lambda e: e.matmul(out, lhsT=lhsT, rhs=rhs, start=start, stop=stop))

    def tr(self, r, w, out, in_, ident):
        self.em.op('pe', r, w, lambda e: e.transpose(out, in_, ident))

    def act(self, r, w, out, in_, func, eng='act', **kw):
        self.em.op(eng, r, w, lambda e: e.activation(out=out, in_=in_, func=func, **kw))

    def ts(self, r, w, out, in0, s1, s2=None, op0=ALU.mult, op1=None, eng='dve', **kw):
        if op1 is None:
            self.em.op(eng, r, w, lambda e: e.tensor_scalar(out=out, in0=in0, scalar1=s1, scalar2=None, op0=op0, **kw))
        else:
            self.em.op(eng, r, w, lambda e: e.tensor_scalar(out=out, in0=in0, scalar1=s1, scalar2=s2, op0=op0, op1=op1, **kw))

    def tt(self, r, w, out, in0, in1, op, eng='dve'):
        self.em.op(eng, r, w, lambda e: e.tensor_tensor(out=out, in0=in0, in1=in1, op=op))

    def stt(self, r, w, out, in0, scalar, in1, op0, op1, eng='dve'):
        self.em.op(eng, r, w, lambda e: e.scalar_tensor_tensor(out=out, in0=in0, scalar=scalar, in1=in1, op0=op0, op1=op1))

    def cp(self, r, w, out, in_, eng='dve'):
        if eng == 'act':
            self.em.op(eng, r, w, lambda e: e.copy(out=out, in_=in_))
        else:
            self.em.op(eng, r, w, lambda e: e.tensor_copy(out=out, in_=in_))

    def red(self, r, w, out, in_, op, eng='dve', axis=AX.X):
        self.em.op(eng, r, w, lambda e: e.tensor_reduce(out=out, in_=in_, axis=axis, op=op))

    def recip(self, r, w, out, in_):
        self.em.op('dve', r, w, lambda e: e.reciprocal(out=out, in_=in_))

    def memset(self, r, w, out, val, eng='dve'):
        self.em.op(eng, r, w, lambda e: e.memset(out, val))


def rope_tables_T():
    half = 32
    inv = (10000.0 ** (-np.arange(0, half, 2, dtype=np.float32) / half)).astype(np.float32)
    t = np.arange(L)
    row = (t // 64).astype(np.float32)
    col = (t % 64).astype(np.float32)
    ang = np.concatenate([row[:, None] * inv, row[:, None] * inv, col[:, None] * inv, col[:, None] * inv], axis=1)
    ang = ang.astype(np.float32)
    cosT = np.cos(ang).T.astype(np.float32)
    sinT = np.sin(ang).T.astype(np.float32)
    return np.ascontiguousarray(np.concatenate([cosT, cosT], 0)), np.ascontiguousarray(np.concatenate([sinT, sinT], 0))


def rope_perm():
    R = np.zeros((128, 128), np.float32)
    for base in range(0, 128, 32):
        for i in range(16):
            R[base + 16 + i, base + i] = -1.0
            R[base + i, base + 16 + i] = 1.0
    return R


def make_consts():
    c = {}
    c['ident'] = np.eye(128, dtype=np.float32)
    c['ropeR'] = rope_perm()
    cosT, sinT = rope_tables_T()
    c['cosT'] = cosT
    c['sinT'] = sinT
    bi = np.zeros((128, 2), np.float32)
    bi[0:64, 0] = 1.0
    bi[64:128, 1] = 1.0
    c['blockind'] = bi
    sel = np.zeros((2, 2, 128), np.float32)
    sel[0, 0, :] = 1.0
    sel[1, 1, :] = 1.0
    c['sel2'] = sel
    tri = np.zeros((2, 64, 64), np.float32)
    tri[0] = np.triu(np.ones((64, 64), np.float32))
    tri[1] = np.tril(np.ones((64, 64), np.float32))
    c['tri'] = tri
    c.update(hyena_consts())
    return c


def col_layout(v):
    return np.ascontiguousarray(v.reshape(-1, 128).T)


def prep_inputs(inp, b):
    m = {}
    m['xin'] = np.ascontiguousarray(np.concatenate([inp['x'][b], inp['ctx'][b]], axis=0))
    m['ccol'] = np.ascontiguousarray(np.stack([col_layout(inp['c'][b]), col_layout(inp['c_ctx'])], axis=-1))
    m['ada_w'] = inp['ada_w']
    m['ada_b_col'] = np.ascontiguousarray(np.stack([col_layout(inp['ada_b'][l]) for l in range(DEPTH)], 1))
    m['norm1_col'] = np.ascontiguousarray(np.stack([col_layout(inp['norm1_w'][l]) for l in range(DEPTH)], 1))
    m['norm2_col'] = np.ascontiguousarray(np.stack([col_layout(inp['norm2_w'][l]) for l in range(DEPTH)], 1))
    m['w_in_fm'] = np.ascontiguousarray(inp['w_in'][:, :, FM_COLS])
    m['w_in_tm'] = np.ascontiguousarray(inp['w_in'][:, :, TM_COLS])
    m['b_fm_col'] = np.ascontiguousarray(np.stack([col_layout(inp['b_in'][l][FM_COLS]) for l in range(DEPTH)], 1))
    cw = np.concatenate([inp['ml_conv_w'], inp['ml_conv_b'][:, None, :]], axis=1)
    m['ml_conv_col'] = np.ascontiguousarray(cw.reshape(DEPTH, 4, 8, 64).transpose(0, 3, 2, 1))
    m['ml_norm_bc'] = np.ascontiguousarray(np.broadcast_to(inp['ml_norm_w'][:, None, :], (DEPTH, 64, 256)))
    m['hy_filt_w1'] = inp['hy_filt_w1']
    m['hy_filt_w2'] = inp['hy_filt_w2']
    m['hy_filt_w3'] = inp['hy_filt_w3']
    m['hy_filt_sc'] = np.ascontiguousarray(np.stack([inp['hy_sin_freq'], inp['hy_filt_b1'], inp['hy_filt_b2']], axis=-1))
    m['hy_b3_col'] = np.ascontiguousarray(np.stack([col_layout(inp['hy_filt_b3'][l]) for l in range(DEPTH)], 0))
    hw = np.concatenate([inp['hy_conv_w'], inp['hy_conv_b'][:, None, :]], axis=1)
    m['hy_conv_col'] = np.ascontiguousarray(hw.reshape(DEPTH, 4, 6, 128).transpose(0, 3, 2, 1))
    m['hy_d_bc'] = np.ascontiguousarray(np.broadcast_to(inp['hy_bias_d'][:, None, :, :], (DEPTH, 32, 2, 256)))
    m['hy_d_col'] = np.ascontiguousarray(inp['hy_bias_d'].reshape(DEPTH, 4, 128).transpose(0, 2, 1))
    m['w_out'] = inp['w_out']
    m['moe_wr'] = np.ascontiguousarray(np.concatenate([inp['moe_wg'], inp['moe_we']], axis=-1))
    rbv = np.concatenate([inp['moe_bg'], inp['moe_be']], axis=-1)
    m['moe_rb_bc'] = np.ascontiguousarray(np.broadcast_to(rbv[:, None, :], (DEPTH, 128, 36)))
    m['moe_w1'] = inp['moe_w1']
    m['moe_w3'] = inp['moe_w3']
    m['moe_w2'] = inp['moe_w2']
    m['final_bc'] = np.ascontiguousarray(np.broadcast_to(inp['final_norm_w'][None, :], (128, D)))
    m['da_lambda'] = np.ascontiguousarray(inp['da_lambda'].reshape(DEPTH, 256))
    m['da_subln_col'] = np.ascontiguousarray(inp['da_subln_w'][:, :, None])
    m['b_tm_bc'] = np.ascontiguousarray(np.broadcast_to(inp['b_in'][:, None, TM_COLS], (DEPTH, 128, NTM)))
    return m


def phase0(k, P, sbp):
    nc = k.nc
    cst = {}
    for name, shape in (('ident', [128, 128]), ('ropeR', [128, 128]), ('blockind', [128, 2])):
        k.din(name, shape)
    k.din('sel2', [2, 2, 128])
    k.din('cosT', [128, L])
    k.din('sinT', [128, L])
    P['ident32'] = sbp.t([128, 128], F32)
    P['identbf'] = sbp.t([128, 128], BF16)
    P['ropeRbf'] = sbp.t([128, 128], BF16)
    P['blockbf'] = sbp.t([128, 2], BF16)
    P['sel2'] = sbp.t([2, 2, 128], F32)
    P['ones32'] = sbp.t([128, 128], F32)
    P['onesbf'] = sbp.t([128, 128], BF16)
    tmp = sbp.t([128, 128], F32)
    k.dma('sp', [], ['ident32'], P['ident32'], k.dram['ident'][:, :])
    k.cp(['ident32'], ['identbf'], P['identbf'], P['ident32'])
    k.dma('sp', [], ['c_tmp'], tmp, k.dram['ropeR'][:, :])
    k.cp(['c_tmp'], ['ropeRbf'], P['ropeRbf'], tmp)
    k.dma('sp', ['c_tmp'], ['c_tmp'], tmp[:, 0:2], k.dram['blockind'][:, :])
    k.cp(['c_tmp'], ['blockbf'], P['blockbf'], tmp[:, 0:2])
    k.dma('sp', [], ['sel2'], P['sel2'], k.dram['sel2'][:, :, :])
    k.memset([], ['ones32'], P['ones32'], 1.0)
    k.memset([], ['onesbf'], P['onesbf'], 1.0)

    ccol = k.din('ccol', [128, 8, 2])
    adaw = k.din('ada_w', [DEPTH, D, 6 * D])
    adab = k.din('ada_b_col', [128, DEPTH, 48])
    n1 = k.din('norm1_col', [128, DEPTH, 8])
    n2 = k.din('norm2_col', [128, DEPTH, 8])
    P['mod'] = sbp.t([128, DEPTH, 48, 2], F32)
    P['A1'] = sbp.t([128, DEPTH, 8, 2], F32)
    P['A2'] = sbp.t([128, DEPTH, 8, 2], F32)
    cact = sbp.t([128, 8, 2], F32)
    adab_sb = sbp.t([128, DEPTH, 48], F32)
    n1_sb = sbp.t([128, DEPTH, 8], F32)
    n2_sb = sbp.t([128, DEPTH, 8], F32)
    k.dma('sp', [], ['cact'], cact, ccol[:, :, :])
    k.dma('sp', [], ['adab'], adab_sb, adab[:, :, :])
    k.dma('sp', [], ['n1'], n1_sb, n1[:, :, :])
    k.dma('sp', [], ['n2'], n2_sb, n2[:, :, :])
    k.act(['cact'], ['cact'], cact, cact, AF.Silu)
    sbl = SB(nc, base=sbp.off)
    stg = [sbl.t([128, 8, 512], F32) for _ in range(2)]
    for l in range(DEPTH):
        wv = adaw[l].rearrange("(k p) n -> p k n", p=128)
        pm = k.ps[0][:, 0:96].rearrange("p (c j) -> p c j", j=2)
        for cg in range(12):
            s = stg[cg % 2]
            sk = 'adastg%d' % (cg % 2)
            k.dma('sp', [], [sk], s, wv[:, :, cg * 512:(cg + 1) * 512])
            for j in range(4):
                c = cg * 4 + j
                for kk in range(8):
                    k.mm([sk, 'cact'], ['ps0'], pm[:, c, :], s[:, kk, j * 128:(j + 1) * 128], cact[:, kk, :],
                         start=(kk == 0), stop=(kk == 7))
        k.tt(['ps0', 'adab'], ['mod'], P['mod'][:, l], pm, adab_sb[:, l, :].unsqueeze(2).to_broadcast([128, 48, 2]), ALU.add)
        for (Aname, nsb, c0) in (('A1', n1_sb, 8), ('A2', n2_sb, 32)):
            k.ts(['mod'], [Aname], P[Aname][:, l], P['mod'][:, l, c0:c0 + 8, :], 1.0, op0=ALU.add)
            k.tt([Aname, 'n1', 'n2'], [Aname], P[Aname][:, l], P[Aname][:, l],
                 nsb[:, l, :].unsqueeze(2).to_broadcast([128, 8, 2]), ALU.mult)
    k.em.barrier()


def phaseA(k, P, l, base, xres):
    nc = k.nc
    sb = SB(nc, base=base)
    wfm = k.dram['w_in_fm']
    wtm = k.dram['w_in_tm']
    UT, QKT, TM = k.dram['UT'], k.dram['QKT'], k.dram['TM']
    Wfm = sb.t([128, 8, NFM], BF16)
    Wtm = sb.t([128, 8, NTM], BF16)
    bfm = sb.t([128, 18], F32)
    btm = sb.t([128, NTM], F32)
    cosT = sb.t([128, L], F32)
    sinT = sb.t([128, L], F32)
    normacc = sb.t([2, 8], F32)
    stg = [sb.t([128, 8, 512], F32) for _ in range(2)]
    k.dma('sp', [], ['bfm'], bfm, k.dram['b_fm_col'][:, l, :])
    k.dma('sp', [], ['btm'], btm, k.dram['b_tm_bc'][l])
    k.dma('sp', [], ['cosT'], cosT, k.dram['cosT'][:, :])
    k.dma('sp', [], ['sinT'], sinT, k.dram['sinT'][:, :])
    k.memset([], ['normacc'], normacc, 0.0)
    ci = 0
    for (src, dst, ncol, key) in ((wfm, Wfm, NFM, 'Wfm'), (wtm, Wtm, NTM, 'Wtm')):
        wv = src[l].rearrange("(k p) n -> p k n", p=128)
        for c0 in range(0, ncol, 512):
            c1 = min(ncol, c0 + 512)
            s = stg[ci % 2]
            sk = 'wstg%d' % (ci % 2)
            k.dma('sp', [], [sk], s[:, :, 0:c1 - c0], wv[:, :, c0:c1])
            k.cp([sk], [key], dst[:, :, c0:c1], s[:, :, 0:c1 - c0], eng=('dve' if ci % 2 == 0 else 'pool'))
            ci += 1
    xt = [sb.t([128, D], F32) for _ in range(2)]
    junk = sb.t([128, D], BF16)
    xn = [sb.t([128, D], BF16) for _ in range(2)]
    ss = [sb.t([128, 2], F32) for _ in range(2)]
    hT = [sb.t([128, 8, 512], BF16) for _ in range(2)]
    fmst = [sb.t([128, 512], F32) for _ in range(3)]
    qbf = [sb.t([128, 512], BF16) for _ in range(2)]
    t2 = [sb.t([128, 512], F32) for _ in range(2)]
    obf = [sb.t([128, 512], BF16) for _ in range(2)]
    sqbf = [sb.t([128, 512], BF16) for _ in range(2)]
    nmx = sb.t([2, 2], F32)
    tmst = [sb.t([128, NTM], F32) for _ in range(2)]
    A1, SH1 = P['A1'], P['mod']
    psi = 0
    tile_ctr = 0
    fm_ctr = 0
    rp_ctr = 0
    for g in range(9):
        t0 = g * 512
        ntok = 512 if g < 8 else 256
        j = 0 if g < 8 else 1
        hb = g % 2
        hk = 'hT%d' % hb
        for tl in range(ntok // 128):
            ti = t0 // 128 + tl
            xb = tile_ctr % 2
            tile_ctr += 1
            xk, nk, sk = 'xt%d' % xb, 'xn%d' % xb, 'ss%d' % xb
            k.dma('sp', [], [xk], xt[xb], xres[ti * 128:(ti + 1) * 128, :])
            k.memset([], [sk], ss[xb], 0.0)
            k.act([xk, sk], ['junk', sk], junk, xt[xb], AF.Square, accum_out=ss[xb][:, 0:1])
            k.ts([sk], [sk], ss[xb][:, 1:2], ss[xb][:, 0:1], 1.0 / D, EPS, op0=ALU.mult, op1=ALU.add)
            k.act([sk], [sk], ss[xb][:, 1:2], ss[xb][:, 1:2], AF.Sqrt)
            k.recip([sk], [sk], ss[xb][:, 1:2], ss[xb][:, 1:2])
            k.ts([xk, sk], [nk], xn[xb], xt[xb], ss[xb][:, 1:2], op0=ALU.mult)
            pk = 'ps%d' % psi
            pst = k.ps[psi][:].bitcast(BF16)
            psi = (psi + 1) % 8
            for kk in range(8):
                k.tr([nk, 'identbf'], [pk], pst[:, kk * 128:(kk + 1) * 128], xn[xb][:, kk * 128:(kk + 1) * 128], P['identbf'])
            for kk in range(8):
                k.act([pk, 'A1', 'mod'], [hk], hT[hb][:, kk, tl * 128:(tl + 1) * 128], pst[:, kk * 128:(kk + 1) * 128],
                      AF.Identity, scale=A1[:, l, kk, j:j + 1], bias=SH1[:, l, kk, j:j + 1])
        for jc in range(18):
            pk = 'ps%d' % psi
            pp = k.ps[psi]
            psi = (psi + 1) % 8
            for kk in range(8):
                k.mm(['Wfm', hk], [pk], pp[:, 0:ntok], Wfm[:, kk, jc * 128:(jc + 1) * 128], hT[hb][:, kk, 0:ntok],
                     start=(kk == 0), stop=(kk == 7))
            fb = fm_ctr % 3
            fm_ctr += 1
            fk = 'fmst%d' % fb
            k.act([pk, 'bfm'], [fk], fmst[fb][:, 0:ntok], pp[:, 0:ntok], AF.Identity, bias=bfm[:, jc:jc + 1], scale=1.0)
            if jc < 10:
                k.dma('pool', [fk], ['UT'], UT[jc * 128:(jc + 1) * 128, t0:t0 + ntok], fmst[fb][:, 0:ntok])
                continue
            rb = rp_ctr % 2
            rp_ctr += 1
            ok_, sqk = 'obf%d' % rb, 'sqbf%d' % rb
            if j == 0:
                qk_, tk_ = 'qbf%d' % rb, 't2%d' % rb
                k.cp([fk], [qk_], qbf[rb][:, 0:ntok], fmst[fb][:, 0:ntok], eng='pool')
                pk2 = 'ps%d' % psi
                pp2 = k.ps[psi]
                psi = (psi + 1) % 8
                k.mm(['ropeRbf', qk_], [pk2], pp2[:, 0:ntok], P['ropeRbf'], qbf[rb][:, 0:ntok])
                k.tt([pk2, 'sinT'], [tk_], t2[rb][:, 0:ntok], pp2[:, 0:ntok], sinT[:, t0:t0 + ntok], ALU.mult)
                k.tt([fk, 'cosT'], [fk], fmst[fb][:, 0:ntok], fmst[fb][:, 0:ntok], cosT[:, t0:t0 + ntok], ALU.mult, eng='pool')
                k.tt([fk, tk_], [fk], fmst[fb][:, 0:ntok], fmst[fb][:, 0:ntok], t2[rb][:, 0:ntok], ALU.add)
            k.cp([fk], [ok_], obf[rb][:, 0:ntok], fmst[fb][:, 0:ntok], eng='pool')
            k.dma('pool', [ok_], ['QKT'], QKT[(jc - 10) * 128:(jc - 9) * 128, t0:t0 + ntok], obf[rb][:, 0:ntok])
            k.act([fk], [sqk], sqbf[rb][:, 0:ntok], fmst[fb][:, 0:ntok], AF.Square)
            pk3 = 'ps%d' % psi
            pp3 = k.ps[psi]
            psi = (psi + 1) % 8
            k.mm(['blockbf', sqk], [pk3], pp3[0:2, 0:ntok], P['blockbf'], sqbf[rb][:, 0:ntok])
            k.red([pk3], ['nmx'], nmx[:, 0:1], pp3[0:2, 0:ntok], ALU.max)
            k.tt(['nmx', 'normacc'], ['normacc'], normacc[:, jc - 10:jc - 9], normacc[:, jc - 10:jc - 9], nmx[:, 0:1], ALU.max)
        for tl in range(ntok // 128):
            ti = t0 // 128 + tl
            tb = ti % 2
            tk = 'tmst%d' % tb
            for (c0, c1) in ((0, 512), (512, 1024), (1024, NTM)):
                pk = 'ps%d' % psi
                pp = k.ps[psi]
                psi = (psi + 1) % 8
                for kk in range(8):
                    k.mm(['Wtm', hk], [pk], pp[:, 0:c1 - c0], hT[hb][:, kk, tl * 128:(tl + 1) * 128], Wtm[:, kk, c0:c1],
                         start=(kk == 0), stop=(kk == 7))
                k.tt([pk, 'btm'], [tk], tmst[tb][:, c0:c1], pp[:, 0:c1 - c0], btm[:, c0:c1], ALU.add)
            k.dma('pool', [tk], ['TM'], TM[ti * 128:(ti + 1) * 128, :], tmst[tb])
    cn = sb.t([2, 4], F32)
    k.tt(['normacc'], ['cn'], cn, normacc[:, 0:4], normacc[:, 4:8], ALU.mult)
    k.act(['cn'], ['cn'], cn, cn, AF.Sqrt)
    k.ts(['cn'], ['cn'], cn, cn, -1.05 * 0.125, op0=ALU.mult)
    for m in range(2):
        pk = 'ps%d' % psi
        pp = k.ps[psi]
        psi = (psi + 1) % 8
        k.mm(['sel2', 'cn'], [pk], pp[:, 0:4], P['sel2'][:, m, :], cn)
        k.cp([pk], ['negc'], P['negc'][:, l, m, :], pp[:, 0:4])
    k.em.barrier()


def phaseB1(k, P, l, base, last):
    nc = k.nc
    sb = SB(nc, base=base)
    QKT, TM, YT = k.dram['QKT'], k.dram['TM'], k.dram['YT']
    lam_init = 0.8 - 0.6 * math.exp(-0.3 * l)
    QT = sb.t([128, 4, T], BF16)
    KT = sb.t([128, 4, T], BF16)
    V = sb.t([128, NT, 512], BF16)
    vst = [sb.t([128, 512], F32) for _ in range(2)]
    k.dma('sp', [], ['QT'], QT, QKT[0:512, :].rearrange("(c p) t -> p c t", p=128))
    k.dma('sp', [], ['KT'], KT, QKT[512:1024, :].rearrange("(c p) t -> p c t", p=128))
    for ti in range(NT):
        vb = ti % 2
        vk = 'vst%d' % vb
        k.dma('sp', [], [vk], vst[vb], TM[ti * 128:(ti + 1) * 128, 512:1024])
        k.cp([vk], ['V'], V[:, ti, :], vst[vb], eng=('dve' if ti % 2 == 0 else 'pool'))
    lt = sb.t([1, 256], F32)
    lw = sb.t([1, 8], F32)
    neglam = sb.t([128, 1], F32)
    wsc = sb.t([128, 1], F32)
    k.dma('sp', [], ['lt'], lt, k.dram['da_lambda'][l:l + 1, :])
    k.dma('sp', [], ['wsc'], wsc, k.dram['da_subln_col'][l])
    k.ts(['wsc'], ['wsc'], wsc, wsc, 1.0 - lam_init, op0=ALU.mult)
    k.tt(['lt'], ['lt'], lt[:, 0:64], lt[:, 0:64], lt[:, 64:128], ALU.mult)
    k.tt(['lt'], ['lt'], lt[:, 128:192], lt[:, 128:192], lt[:, 192:256], ALU.mult)
    k.red(['lt'], ['lw'], lw[:, 0:1], lt[:, 0:64], ALU.add)
    k.red(['lt', 'lw'], ['lw'], lw[:, 1:2], lt[:, 128:192], ALU.add)
    k.act(['lw'], ['lw'], lw[:, 0:2], lw[:, 0:2], AF.Exp)
    k.tt(['lw'], ['lw'], lw[:, 2:3], lw[:, 1:2], lw[:, 0:1], ALU.subtract)
    k.ts(['lw'], ['lw'], lw[:, 2:3], lw[:, 2:3], -lam_init, op0=ALU.add)
    k.mm(['ones32', 'lw'], ['ps7'], k.ps[7][:, 0:1], P['ones32'][0:1, :], lw[:, 2:3])
    k.cp(['ps7'], ['neglam'], neglam, k.ps[7][:, 0:1])

    pt = [sb.t([128, 512], BF16) for _ in range(4)]
    rec = [sb.t([128, 512], F32) for _ in range(2)]
    o0 = sb.t([128, 512], F32)
    o1 = sb.t([128, 512], F32)
    sq = sb.t([128, 512], BF16)
    ybf = [sb.t([128, 512], BF16) for _ in range(2)]
    pti = 0
    si = 0
    si_box = [0]
    yi = 0
    chunks = [(g * 512, 512, list(range(NT))) for g in range(8)]
    if not last:
        chunks.append((L, CTX, [32, 33]))
    for h in range(4):
        for (q0, nq, blocks) in chunks:
            units = [(bi, kb, m) for bi, kb in enumerate(blocks) for m in range(2)]
            LOOK = 3
            issued = {}

            def issue_s(u):
                bi, kb, m = units[u]
                nonlocal_si = si_box[0]
                si_box[0] += 1
                psk = 'ps%d' % (4 + nonlocal_si % 4)
                pss = k.ps[4 + nonlocal_si % 4]
                lo, hi = m * 64, (m + 1) * 64
                k.mm(['KT', 'QT'], [psk], pss[:, 0:nq], KT[lo:hi, h, kb * 128:(kb + 1) * 128], QT[lo:hi, h, q0:q0 + nq])
                issued[u] = (psk, pss)

            for u in range(min(LOOK, len(units))):
                issue_s(u)
            for u, (bi, kb, m) in enumerate(units):
                psk, pss = issued.pop(u)
                pk_ = 'pt%d' % (pti % 4)
                ptt = pt[pti % 4]
                pti += 1
                k.act([psk, 'negc'], [pk_], ptt[:, 0:nq], pss[:, 0:nq], AF.Exp, scale=0.125, bias=P['negc'][:, l, m, h:h + 1])
                if u + LOOK < len(units):
                    issue_s(u + LOOK)
                st, sp_ = (bi == 0), (bi == len(blocks) - 1)
                k.mm(['V', pk_], ['ps%d' % m], k.ps[m][:, 0:nq], V[:, kb, h * 128:(h + 1) * 128], ptt[:, 0:nq], start=st, stop=sp_)
                k.mm(['onesbf', pk_], ['ps%d' % (2 + m)], k.ps[2 + m][:, 0:nq], P['onesbf'], ptt[:, 0:nq], start=st, stop=sp_)
            si = si_box[0]
            k.recip(['ps2'], ['rec0'], rec[0][:, 0:nq], k.ps[2][:, 0:nq])
            k.recip(['ps3'], ['rec1'], rec[1][:, 0:nq], k.ps[3][:, 0:nq])
            k.tt(['ps0', 'rec0'], ['o0'], o0[:, 0:nq], k.ps[0][:, 0:nq], rec[0][:, 0:nq], ALU.mult)
            k.tt(['ps1', 'rec1'], ['o1'], o1[:, 0:nq], k.ps[1][:, 0:nq], rec[1][:, 0:nq], ALU.mult)
            k.stt(['o0', 'o1', 'neglam'], ['o0'], o0[:, 0:nq], o1[:, 0:nq], neglam[:, 0:1], o0[:, 0:nq], ALU.mult, ALU.add)
            k.act(['o0'], ['sq'], sq[:, 0:nq], o0[:, 0:nq], AF.Square)
            psk = 'ps%d' % (4 + si % 4)
            pss = k.ps[4 + si % 4]
            si += 1
            si_box[0] = si
            k.mm(['onesbf', 'sq'], [psk], pss[:, 0:nq], P['onesbf'], sq[:, 0:nq])
            k.ts([psk], ['rec0'], rec[0][:, 0:nq], pss[:, 0:nq], 1.0 / 128.0, EPS, op0=ALU.mult, op1=ALU.add)
            k.act(['rec0'], ['rec0'], rec[0][:, 0:nq], rec[0][:, 0:nq], AF.Sqrt)
            k.recip(['rec0'], ['rec0'], rec[0][:, 0:nq], rec[0][:, 0:nq])
            k.tt(['o0', 'rec0'], ['o0'], o0[:, 0:nq], o0[:, 0:nq], rec[0][:, 0:nq], ALU.mult)
            yk = 'ybf%d' % (yi % 2)
            yb = ybf[yi % 2]
            yi += 1
            k.ts(['o0', 'wsc'], [yk], yb[:, 0:nq], o0[:, 0:nq], wsc[:, 0:1], op0=ALU.mult)
            k.dma('pool', [yk], ['YT'], YT[512 + h * 128:512 + (h + 1) * 128, q0:q0 + nq], yb[:, 0:nq])
    k.em.barrier()


NCH = T // 64


def phaseB2(k, P, l, base, last):
    nc = k.nc
    UT, TM, YT = k.dram['UT'], k.dram['TM'], k.dram['YT']
    sb0 = SB(nc, base=base)
    tri32 = sb0.t([64, 2, 64], F32)
    tribf = sb0.t([64, 2, 64], BF16)
    cw = sb0.t([64, 8, 4], F32)
    wbc = sb0.t([64, 256], F32)
    G = sb0.t([64, NCH, 16], F32)
    LF = sb0.t([64, 8, NCH], F32)
    IG = sb0.t([64, 8, NCH], F32)
    BB = sb0.t([64, 8, NCH], F32)
    BT = sb0.t([64, 8, NCH], F32)
    EB = sb0.t([64, 8, NCH], F32)
    WS = sb0.t([64, 8, NCH], F32)
    W2 = sb0.t([64, 8, NCH], F32)
    EBT = sb0.t([64, 8, NCH], F32)
    k.dma('sp', [], ['tri32'], tri32, k.dram['tri'].rearrange("a s t -> s a t"))
    k.cp(['tri32'], ['tribf'], tribf, tri32)
    k.dma('sp', [], ['cw'], cw, k.dram['ml_conv_col'][l])
    k.dma('sp', [], ['wbc'], wbc, k.dram['ml_norm_bc'][l])
    k.dma('sp', [], ['G'], G, TM[:, 1024:1040].rearrange("(n p) c -> p n c", p=64))
    for d in range(2):
        gi = G[:, :, d * 8:d * 8 + 4].rearrange("p n h -> p h n")
        gf = G[:, :, d * 8 + 4:d * 8 + 8].rearrange("p n h -> p h n")
        k.cp(['G'], ['IG'], IG[:, d * 4:d * 4 + 4, :], gi)
        k.act(['G'], ['LF'], LF[:, d * 4:d * 4 + 4, :], gf, AF.Exp, scale=-1.0)
    k.act(['LF'], ['LF'], LF, LF, AF.Ln, bias=1.0, scale=1.0)
    k.ts(['LF'], ['LF'], LF, LF, -1.0, op0=ALU.mult)
    for d in range(2):
        rhs = LF[:, d * 4:d * 4 + 4, :]
        k.mm(['tri32', 'LF'], ['ps0'], k.ps[0][0:64, 0:4 * NCH], tri32[:, d, :], rhs)
        k.cp(['ps0'], ['BB'], BB[:, d * 4:d * 4 + 4, :], k.ps[0][0:64, 0:4 * NCH].rearrange("p (h n) -> p h n", n=NCH))
        k.mm(['ones32', 'LF'], ['ps1'], k.ps[1][0:64, 0:4 * NCH], P['ones32'][0:64, 0:64], rhs)
        k.cp(['ps1'], ['BT'], BT[:, d * 4:d * 4 + 4, :], k.ps[1][0:64, 0:4 * NCH].rearrange("p (h n) -> p h n", n=NCH))
    k.act(['BB'], ['EB'], EB, BB, AF.Exp)
    k.act(['BT'], ['EBT'], EBT, BT, AF.Exp)
    k.tt(['IG', 'BB'], ['WS'], WS, IG, BB, ALU.subtract)
    k.tt(['WS', 'BT'], ['W2'], W2, WS, BT, ALU.add)
    k.act(['WS'], ['WS'], WS, WS, AF.Exp)
    k.act(['W2'], ['W2'], W2, W2, AF.Exp)
    base1 = sb0.off
    orders = [[64, 65, 66, 67] + list(range(64)), [67, 66, 65, 64] + list(range(63, -1, -1))]
    psi = [2]

    def nps():
        i = psi[0]
        psi[0] = 2 + (psi[0] - 1) % 6
        return 'ps%d' % i, k.ps[i]

    for hp in range(2):
        sb = SB(nc, base=base1)
        qT = sb.t([64, 2, T], BF16)
        kT = sb.t([64, 2, T], BF16)
        ktm = sb.t([64, NCH, 128], BF16)
        vaug = sb.t([64, NCH, 2, 65], BF16)
        hsum = sb.t([64, NCH, 128], F32)
        ra = sb.t([64, 2 * T], F32)
        raw = ra[:, 0:T]
        acc = ra[:, T:2 * T]
        k.memset([], ['hsum'], hsum, 0.0, eng='pool')
        k.memset([], ['vaug'], vaug, 1.0, eng='pool')
        for qk in range(2):
            for hl in range(2):
                hh = qk * 4 + hp * 2 + hl
                k.dma('sp', [], ['raw'], raw, UT[768 + hh * 64:768 + (hh + 1) * 64, :])
                k.ts(['raw', 'cw'], ['acc'], acc, raw, cw[:, hh, 1:2], cw[:, hh, 3:4], op0=ALU.mult, op1=ALU.add)
                for (a, b) in ((0, L), (L, T)):
                    k.stt(['raw', 'cw', 'acc'], ['acc'], acc[:, a + 1:b], raw[:, a:b - 1], cw[:, hh, 0:1], acc[:, a + 1:b], ALU.mult, ALU.add)
                    k.stt(['raw', 'cw', 'acc'], ['acc'], acc[:, a:b - 1], raw[:, a + 1:b], cw[:, hh, 2:3], acc[:, a:b - 1], ALU.mult, ALU.add)
                if qk == 0:
                    k.act(['acc'], ['qT'], qT[:, hl, :], acc, AF.Silu)
                else:
                    k.act(['acc'], ['acc'], acc, acc, AF.Silu)
                    k.ts(['acc'], ['kT'], kT[:, hl, :], acc, 0.125, op0=ALU.mult)
        for n0 in range(0, NCH, 4):
            pk, pp = nps()
            ppb = pp[:].bitcast(BF16)
            for dn in range(4):
                for hl in range(2):
                    k.tr(['kT', 'identbf'], [pk], ppb[0:64, (dn * 2 + hl) * 64:(dn * 2 + hl + 1) * 64],
                         kT[:, hl, (n0 + dn) * 64:(n0 + dn + 1) * 64], P['identbf'][0:64, 0:64])
            k.cp([pk], ['ktm'], ktm[:, n0:n0 + 4, :], ppb[0:64, 0:512].rearrange("p (n c) -> p n c", c=128))
        vst = acc[:, 0:17 * 128].rearrange("p (n c) -> p n c", c=128)
        for n0 in range(0, NCH, 17):
            k.dma('sp', ['acc'], ['acc'], vst, TM[n0 * 64:(n0 + 17) * 64, hp * 128:(hp + 1) * 128].rearrange("(n p) c -> p n c", p=64))
            k.cp(['acc'], ['vaug'], vaug[:, n0:n0 + 17, :, 0:64], vst.rearrange("p n (h e) -> p n h e", e=64))
        Cst = [[sb.t([64, 65], F32) for _ in range(2)] for _ in range(2)]
        Cbf = [[sb.t([64, 65], BF16) for _ in range(2)] for _ in range(2)]
        dg = [sb.t([64, 64], BF16) for _ in range(4)]
        meb = [sb.t([64, 64], F32) for _ in range(4)]
        pT = [sb.t([64, 64], BF16) for _ in range(4)]
        rsb = [sb.t([64, 65], F32) for _ in range(4)]
        tot = [sb.t([64, 66], F32) for _ in range(4)]
        wv = [sb.t([64, 65], BF16) for _ in range(4)]
        for d in range(2):
            for hl in range(2):
                k.memset([], ['Cst%d%d' % (d, hl)], Cst[d][hl], 0.0)
                k.memset([], ['Cbf%d%d' % (d, hl)], Cbf[d][hl], 0.0)
        def unit(step, d, hl):
            n = orders[d][step]
            c0 = n * 64
            need_out = (n < 64) or (not last)
            u = d * 2 + hl
            dh = d * 4 + hp * 2 + hl
            ck, cbk = 'Cst%d%d' % (d, hl), 'Cbf%d%d' % (d, hl)
            upd = step != NCH - 1
            kA, kB = 'ps%d' % (2 * u), 'ps%d' % (2 * u + 1)
            bA, bB = k.ps[2 * u], k.ps[2 * u + 1]
            if upd:
                k.act(['vaug', 'W2'], ['wv%d' % u], wv[u], vaug[:, n, hl, :], AF.Identity, scale=W2[:, dh, n:n + 1])
            if need_out:
                k.act(['identbf', 'EB'], ['dg%d' % u], dg[u], P['identbf'][0:64, 0:64], AF.Identity, scale=EB[:, dh, n:n + 1])
            yield
            if upd:
                k.mm(['ktm', 'wv%d' % u], [kB], bB[0:64, 0:65], ktm[:, n, hl * 64:(hl + 1) * 64], wv[u])
            if need_out:
                k.mm(['tribf', 'dg%d' % u], [kA], bA[0:64, 0:64], tribf[:, 1 - d, :], dg[u])
            yield
            if upd:
                k.stt([ck, 'EBT', kB], [ck], Cst[d][hl], Cst[d][hl], EBT[:, dh, n:n + 1], bB[0:64, 0:65], ALU.mult, ALU.add)
            if need_out:
                k.cp([kA], ['meb%d' % u], meb[u], bA[0:64, 0:64], eng='act')
            yield
            if need_out:
                k.mm(['kT', 'qT'], [kA], bA[0:64, 0:64], kT[:, hl, c0:c0 + 64], qT[:, hl, c0:c0 + 64])
                k.mm(['qT', cbk], [kB], bB[0:64, 0:65], qT[:, hl, c0:c0 + 64], Cbf[d][hl])
            yield
            if need_out:
                k.act([kB, 'EB'], ['rsb%d' % u], rsb[u], bB[0:64, 0:65], AF.Identity, scale=EB[:, dh, n:n + 1])
                k.stt([kA, 'WS', 'meb%d' % u], ['pT%d' % u], pT[u], bA[0:64, 0:64], WS[:, dh, n:n + 1], meb[u], ALU.mult, ALU.mult)
            if upd:
                k.cp([ck], [cbk], Cbf[d][hl], Cst[d][hl], eng='act')
            yield
            if not need_out:
                return
            k.mm(['pT%d' % u, 'vaug'], [kA], bA[0:64, 0:65], pT[u], vaug[:, n, hl, :])
            yield
            k.tt([kA, 'rsb%d' % u], ['tot%d' % u], tot[u][:, 0:65], bA[0:64, 0:65], rsb[u], ALU.add)
            yield
            k.act(['tot%d' % u], ['tot%d' % u], tot[u][:, 65:66], tot[u][:, 64:65], AF.Abs)
            yield
            k.ts(['tot%d' % u], ['tot%d' % u], tot[u][:, 65:66], tot[u][:, 65:66], 1.0, op0=ALU.max)
            yield
            k.recip(['tot%d' % u], ['tot%d' % u], tot[u][:, 65:66], tot[u][:, 65:66])
            yield
            hs = hsum[:, n, hl * 64:(hl + 1) * 64]
            k.stt(['tot%d' % u, 'hsum'], ['hsum'], hs, tot[u][:, 0:64], tot[u][:, 65:66], hs, ALU.mult, ALU.add)

        for step in range(NCH):
            gens = [unit(step, d, hl) for d in range(2) for hl in range(2)]
            while gens:
                alive = []
                for g_ in gens:
                    try:
                        next(g_)
                        alive.append(g_)
                    except StopIteration:
                        pass
                gens = alive
        nout = 64 if last else NCH
        ssum = sb.t([64, NCH * 2], F32)
        ybf = sb.t([64, NCH, 128], BF16)
        ytb = [sb.t([128, 512], BF16) for _ in range(2)]
        k.act(['hsum', 'raw', 'acc'], ['acc', 'raw'], ra, hsum.rearrange("p n c -> p (n c)"), AF.Square)
        k.red(['acc', 'raw'], ['ssum'], ssum, ra.rearrange("p (g e) -> p g e", e=64), ALU.add)
        k.ts(['ssum'], ['ssum'], ssum, ssum, 1.0 / 64.0, EPS, op0=ALU.mult, op1=ALU.add)
        k.act(['ssum'], ['ssum'], ssum, ssum, AF.Sqrt)
        k.recip(['ssum'], ['ssum'], ssum, ssum)
        hv = hsum.rearrange("p n (h e) -> p (n h) e", e=64)
        k.tt(['hsum', 'ssum'], ['hsum'], hv, hv, ssum.unsqueeze(2).to_broadcast([64, NCH * 2, 64]), ALU.mult)
        k.tt(['hsum', 'wbc'], ['hsum'], hsum, hsum, wbc[:, hp * 128:(hp + 1) * 128].unsqueeze(1).to_broadcast([64, NCH, 128]), ALU.mult)
        ost = acc[:, 0:17 * 128].rearrange("p (n c) -> p n c", c=128)
        for n0 in range(0, NCH, 17):
            k.dma('sp', ['acc'], ['acc'], ost, TM[n0 * 64:(n0 + 17) * 64, 256 + hp * 128:256 + (hp + 1) * 128].rearrange("(n p) c -> p n c", p=64))
            k.act(['acc'], ['acc'], ost, ost, AF.Sigmoid)
            k.tt(['acc', 'hsum'], ['ybf'], ybf[:, n0:n0 + 17, :], hsum[:, n0:n0 + 17, :], ost, ALU.mult)
        for gi, n0 in enumerate(range(0, nout, 8)):
            nn = min(8, nout - n0)
            pk, pp = nps()
            ppb = pp[:].bitcast(BF16)
            for dn in range(nn):
                k.tr(['ybf', 'identbf'], [pk], ppb[:, dn * 64:(dn + 1) * 64], ybf[:, n0 + dn, :], P['identbf'][0:64, 0:64])
            yk = 'ytb%d' % (gi % 2)
            k.cp([pk], [yk], ytb[gi % 2][:, 0:nn * 64], ppb[:, 0:nn * 64])
            k.dma('pool', [yk], ['YT'], YT[256 + hp * 128:256 + (hp + 1) * 128, n0 * 64:(n0 + nn) * 64], ytb[gi % 2][:, 0:nn * 64])
        k.em.barrier()


HC = 32
KB = 256 // HC
NB6 = 512 // HC
NHB = 256 // HC
PI = math.pi
HSTOP = [99]


def hyena_consts():
    c = {}
    f32 = np.float32

    def zfeat(Lx, pos):
        t = np.linspace(0.0, 1.0, Lx, dtype=f32)[pos][:, None]
        w = ((2.0 * math.pi / Lx) * np.arange(Lx, dtype=f32))[pos][:, None]
        f = np.linspace(1e-4, 15.0, 16, dtype=f32)[None, :]
        z = np.concatenate([t, np.cos(f * w), -np.sin(f * w)], axis=-1).astype(f32)
        return z, t
    deltas = np.abs(np.linspace(math.log(1e-2) / 1.5, math.log(1e-2) / 0.3, 256, dtype=f32)).astype(f32)
    z, t = zfeat(L, np.arange(L))
    c['hy_z'] = np.ascontiguousarray(z.T)
    c['hy_decay'] = np.ascontiguousarray(np.exp(-t * deltas[None, :]).T.astype(f32))
    pos = np.concatenate([np.arange(CTX - 1, 0, -1), np.arange(CTX)])
    zc, tc = zfeat(CTX, pos)
    c['hy_zc'] = np.ascontiguousarray(zc.T)
    c['hy_decayc'] = np.ascontiguousarray(np.exp(-tc * deltas[None, :]).T.astype(f32))
    n1 = np.arange(32)[:, None]
    k1 = np.arange(64)[None, :]
    a = 2 * np.pi * n1 * k1 / 64.0
    c['hy_F1'] = np.concatenate([np.cos(a), -np.sin(a)], 1).astype(f32)
    n2 = np.arange(128)[:, None, None]
    kk = (np.arange(64)[None, :, None] + 64 * np.arange(128)[None, None, :])
    a = 2 * np.pi * ((n2 * kk) % 8192) / 8192.0
    c['hy_Gr'] = np.cos(a).astype(f32).reshape(128, 8192)
    c['hy_Gi'] = (-np.sin(a)).astype(f32).reshape(128, 8192)
    k2 = np.arange(128)[:, None]
    nn = np.arange(128)[None, :]
    a = 2 * np.pi * ((k2 * nn) % 128) / 128.0
    c['hy_E1'] = np.concatenate([np.cos(a), np.sin(a)], 1).astype(f32)
    c['hy_E2'] = np.concatenate([-np.sin(a), np.cos(a)], 1).astype(f32)
    k1 = np.arange(64)[:, None, None]
    nfull = np.arange(128)[None, :, None] + 128 * np.arange(32)[None, None, :]
    a = 2 * np.pi * ((k1 * nfull) % 8192) / 8192.0
    c['hy_Mr'] = np.cos(a).astype(f32).reshape(64, 4096)
    c['hy_nMi'] = (-np.sin(a)).astype(f32).reshape(64, 4096)
    return c


def hy_load_bf(k, sb, name, shape, stg, key):
    p, n = shape
    dst = sb.t([p, n], BF16)
    src = k.dram[name]
    step = 2048
    for i, c0 in enumerate(range(0, n, step)):
        c1 = min(n, c0 + step)
        k.dma('sp', [], ['hstg'], stg[0:p, 0:c1 - c0], src[:, c0:c1])
        k.cp(['hstg'], [key], dst[:, c0:c1], stg[0:p, 0:c1 - c0], eng=('dve' if i % 2 == 0 else 'pool'))
    return dst


def fft_fwd(k, C, xbf, A, nAi, psctr, consume):
    F1, Gr, Gi = C['F1'], C['Gr'], C['Gi']
    for c0 in range(0, HC, 4):
        pk, pp = psctr()
        for dc in range(4):
            k.mm(['xbf', 'F1'], [pk], pp[:, dc * 128:(dc + 1) * 128], xbf[:, c0 + dc, :], F1)
        src = pp[:, 0:512].rearrange("p (c r q) -> p c r q", r=2, q=64)
        k.cp([pk], ['A'], A[:, :, :, c0:c0 + 4].rearrange("p q r c -> p c r q"), src, eng='act')
        k.ts(['A'], ['nAi'], nAi[:, :, c0:c0 + 4], A[:, :, 1, c0:c0 + 4], -1.0, op0=ALU.mult)
    for k0 in range(0, 64, KB):
        pk, pp = psctr()
        for dk in range(KB):
            k1 = k0 + dk
            xr = pp[:, dk * 2 * HC:dk * 2 * HC + HC]
            xi = pp[:, dk * 2 * HC + HC:(dk + 1) * 2 * HC]
            k.mm(['Gr', 'A'], [pk], xr, Gr[:, k1, :], A[:, k1, 0, :], start=True, stop=False)
            k.mm(['Gi', 'nAi'], [pk], xr, Gi[:, k1, :], nAi[:, k1, :], start=False, stop=True)
            k.mm(['Gi', 'A'], [pk], xi, Gi[:, k1, :], A[:, k1, 0, :], start=True, stop=False)
            k.mm(['Gr', 'A'], [pk], xi, Gr[:, k1, :], A[:, k1, 1, :], start=False, stop=True)
        consume(pk, pp, k0)


def phaseH(k, P, l, base, last):
    nc = k.nc
    UT, YT = k.dram['UT'], k.dram['YT']
    HK, HH, CK, UC = k.dram['HK'], k.dram['HH'], k.dram['CK'], k.dram['UC']
    psi = [0]

    def nps():
        i = psi[0]
        psi[0] = (psi[0] + 1) % 8
        return 'ps%d' % i, k.ps[i]

    sb = SB(nc, base=base)
    w1 = sb.t([33, 64], F32)
    w2 = sb.t([64, 64], F32)
    w3 = sb.t([64, 1024], F32)
    sc = sb.t([64, 8], F32)
    b3 = sb.t([128, 8], F32)
    k.dma('sp', [], ['w1'], w1, k.dram['hy_filt_w1'][l])
    k.dma('sp', [], ['w2'], w2, k.dram['hy_filt_w2'][l])
    k.dma('sp', [], ['w3'], w3, k.dram['hy_filt_w3'][l])
    k.dma('sp', [], ['sc'], sc[:, 0:3], k.dram['hy_filt_sc'][l])
    k.dma('sp', [], ['b3'], b3, k.dram['hy_b3_col'][l])
    k.tt(['sc'], ['sc'], sc[:, 3:4], sc[:, 0:1], sc[:, 1:2], ALU.mult)
    k.tt(['sc'], ['sc'], sc[:, 4:5], sc[:, 0:1], sc[:, 2:3], ALU.mult)
    zT = sb.t([33, L], F32)
    h2 = sb.t([64, L], F32)
    h1 = sb.t([64, 512], F32)
    m1 = sb.t([64, 512], F32)
    m2 = sb.t([64, 512], F32)

    def sin_layer(src_ap, wt, kdim, bcol, dst_ap, n):
        pk, pp = nps()
        k.mm(['w1', 'w2', 'zT', 'h1'], [pk], pp[0:64, 0:n], wt, src_ap)
        k.ts([pk, 'sc'], ['m0'], dst_ap, pp[0:64, 0:n], sc[:, 0:1], sc[:, bcol:bcol + 1], op0=ALU.mult, op1=ALU.add)
        k.ts(['m0'], ['m1'], m1[:, 0:n], dst_ap, PI, -2.0 * PI, op0=ALU.is_gt, op1=ALU.mult)
        k.ts(['m0'], ['m2'], m2[:, 0:n], dst_ap, -PI, 2.0 * PI, op0=ALU.is_lt, op1=ALU.mult, eng='pool')
        k.tt(['m1', 'm2'], ['m1'], m1[:, 0:n], m1[:, 0:n], m2[:, 0:n], ALU.add)
        k.tt(['m0', 'm1'], ['m0'], dst_ap, dst_ap, m1[:, 0:n], ALU.add)
        k.act(['m0'], ['m0'], dst_ap, dst_ap, AF.Sin)

    def mlp(zsrc_name, ncols, h2dst):
        k.dma('sp', ['zT'], ['zT'], zT[:, 0:ncols], k.dram[zsrc_name][:, :])
        for c0 in range(0, ncols, 512):
            n = min(512, ncols - c0)
            sin_layer(zT[:, c0:c0 + n], w1, 33, 3, h1[:, 0:n], n)
            sin_layer2(c0, n, h2dst)

    def sin_layer2(c0, n, h2dst):
        pk, pp = nps()
        k.mm(['w2', 'm0'], [pk], pp[0:64, 0:n], w2, h1[:, 0:n])
        d = h2dst[:, c0:c0 + n]
        k.ts([pk, 'sc'], ['h2'], d, pp[0:64, 0:n], sc[:, 0:1], sc[:, 4:5], op0=ALU.mult, op1=ALU.add)
        k.ts(['h2'], ['m1'], m1[:, 0:n], d, PI, -2.0 * PI, op0=ALU.is_gt, op1=ALU.mult)
        k.ts(['h2'], ['m2'], m2[:, 0:n], d, -PI, 2.0 * PI, op0=ALU.is_lt, op1=ALU.mult, eng='pool')
        k.tt(['m1', 'm2'], ['m1'], m1[:, 0:n], m1[:, 0:n], m2[:, 0:n], ALU.add)
        k.tt(['h2', 'm1'], ['h2'], d, d, m1[:, 0:n], ALU.add)
        k.act(['h2'], ['h2'], d, d, AF.Sin)

    dec = [sb.t([128, L], F32) for _ in range(2)]
    kraw = [sb.t([128, L], F32) for _ in range(2)]
    kbfo = [sb.t([128, L], BF16) for _ in range(2)]
    junk = sb.t([128, L], BF16)
    asum = sb.t([128, 4], F32)

    def gen_filters(zname, dname, ncols, ctx):
        mlp(zname, ncols, h2)
        for ch in range(2):
            k.dma('sp', ['dec%d' % ch], ['dec%d' % ch], dec[ch][:, 0:ncols], k.dram[dname][ch * 128:(ch + 1) * 128, :])
        for o in range(2):
            for ch in range(2):
                k.memset([], ['asum'], asum, 0.0)
                for d in range(2):
                    fc = o * 4 + d * 2 + ch
                    kr = kraw[d]
                    kk_ = 'kraw%d' % d
                    for c0 in range(0, ncols, 512):
                        n = min(512, ncols - c0)
                        pk, pp = nps()
                        k.mm(['w3', 'h2'], [pk], pp[:, 0:n], w3[:, fc * 128:(fc + 1) * 128], h2[:, c0:c0 + n])
                        k.act([pk, 'b3'], [kk_], kr[:, c0:c0 + n], pp[:, 0:n], AF.Identity, bias=b3[:, fc:fc + 1], scale=1.0)
                    k.tt([kk_, 'dec%d' % ch], [kk_], kr[:, 0:ncols], kr[:, 0:ncols], dec[ch][:, 0:ncols], ALU.mult)
                    if not ctx:
                        if d == 1:
                            k.memset([kk_], [kk_], kr[:, 0:1], 0.0)
                        k.act([kk_, 'asum'], ['junk', 'asum'], junk[:, 0:ncols], kr[:, 0:ncols], AF.Abs, accum_out=asum[:, d:d + 1])
                    else:
                        lo, hi = (255, 511) if d == 0 else (0, 255)
                        k.act([kk_, 'asum'], ['junk', 'asum'], junk[:, lo:hi], kr[:, lo:hi], AF.Abs, accum_out=asum[:, d:d + 1])
                k.tt(['asum'], ['asum'], asum[:, 2:3], asum[:, 0:1], asum[:, 1:2], ALU.add)
                k.recip(['asum'], ['asum'], asum[:, 2:3], asum[:, 2:3])
                if not ctx:
                    for d in range(2):
                        fc = o * 4 + d * 2 + ch
                        k.ts(['kraw%d' % d, 'asum'], ['kbfo%d' % d], kbfo[d], kraw[d], asum[:, 2:3], op0=ALU.mult,
                             eng=('dve' if d == 0 else 'pool'))
                        k.dma('pool', ['kbfo%d' % d], ['HK'], HK[fc], kbfo[d])
                else:
                    k.ts(['kraw0', 'asum'], ['kraw0'], kraw[0][:, 255:511], kraw[0][:, 255:511], asum[:, 2:3], op0=ALU.mult)
                    k.ts(['kraw1', 'asum', 'kraw0'], ['kraw0'], kraw[0][:, 0:255], kraw[1][:, 0:255], asum[:, 2:3], op0=ALU.mult)
                    k.dma('pool', ['kraw0'], ['CK'], CK[o * 2 + ch], kraw[0][:, 0:511])

    gen_filters('hy_z', 'hy_decay', L, False)
    if not last:
        gen_filters('hy_zc', 'hy_decayc', 511, True)
    k.em.barrier()

    if HSTOP[0] <= 1:
        return
    sb = SB(nc, base=base)
    stg = sb.t([128, 2048], F32)
    C = {}
    C['F1'] = hy_load_bf(k, sb, 'hy_F1', [32, 128], stg, 'F1')
    C['Gr'] = hy_load_bf(k, sb, 'hy_Gr', [128, 8192], stg, 'Gr').rearrange("p (q m) -> p q m", m=128)
    C['Gi'] = hy_load_bf(k, sb, 'hy_Gi', [128, 8192], stg, 'Gi').rearrange("p (q m) -> p q m", m=128)
    C['E1'] = hy_load_bf(k, sb, 'hy_E1', [128, 256], stg, 'E1')
    C['E2'] = hy_load_bf(k, sb, 'hy_E2', [128, 256], stg, 'E2')
    C['Mr'] = hy_load_bf(k, sb, 'hy_Mr', [64, 4096], stg, 'Mr').rearrange("p (n m) -> p n m", m=32)
    C['nMi'] = hy_load_bf(k, sb, 'hy_nMi', [64, 4096], stg, 'nMi').rearrange("p (n m) -> p n m", m=32)
    A = sb.t([128, 64, 2, HC], BF16)
    nAi = sb.t([128, 64, HC], BF16)
    base2 = sb.off
    if HSTOP[0] <= 1.5:
        k.em.barrier()
        return

    sbs = SB(nc, base=base2)
    kbf = [sbs.t([32, HC, 128], BF16) for _ in range(2)]
    Hacc = sbs.t([128, 64, 2, HC], F32)
    Hbf = sbs.t([128, 64, 2, HC], BF16)
    xtmp = sbs.t([128, KB, 2, HC], F32)
    SC = 1.0 / 8192.0
    for o in range(2):
        for b4 in range(NHB):
            ch, coff = (b4 * HC) // 128, (b4 * HC) % 128
            for d in range(2):
                fc = o * 4 + d * 2 + ch
                k.dma('sp', ['xbf'], ['xbf'], kbf[d], HK[fc][coff:coff + HC, :].rearrange("c (a b) -> a c b", b=128))

                def consume(pk, pp, k0, d=d):
                    src = pp[:, 0:512].rearrange("p (q r c) -> p q r c", r=2, c=HC)
                    dst = Hacc[:, k0:k0 + KB, :, :]
                    if d == 0:
                        k.act([pk], ['Hacc'], dst, src, AF.Copy, scale=SC)
                    else:
                        k.act([pk], ['xtmp'], xtmp, src, AF.Copy, scale=SC)
                        k.tt(['xtmp', 'Hacc'], ['Hacc'], dst[:, :, 0, :], dst[:, :, 0, :], xtmp[:, :, 0, :], ALU.add)
                        k.tt(['xtmp', 'Hacc'], ['Hacc'], dst[:, :, 1, :], dst[:, :, 1, :], xtmp[:, :, 1, :], ALU.subtract, eng='pool')
                fft_fwd(k, C, kbf[d], A, nAi, nps, consume)
            k.cp(['Hacc'], ['Hbf'], Hbf, Hacc, eng='pool')
            k.dma('pool', ['Hbf'], ['HH'], HH[o * NHB + b4], Hbf.rearrange("p q r c -> p (q r c)"))
    k.em.barrier()

    if HSTOP[0] <= 2:
        return
    sbc = SB(nc, base=base2)
    raw = sbc.t([128, T], F32)
    acc = sbc.t([128, T], F32)
    ucb = sbc.t([128, T], BF16)
    cw = sbc.t([128, 6, 4], F32)
    k.dma('sp', [], ['cw'], cw, k.dram['hy_conv_col'][l])
    for cc in range(6):
        k.dma('sp', ['raw'], ['raw'], raw, UT[cc * 128:(cc + 1) * 128, :])
        k.ts(['raw', 'cw'], ['acc'], acc, raw, cw[:, cc, 1:2], cw[:, cc, 3:4], op0=ALU.mult, op1=ALU.add)
        for (a, b) in ((0, L), (L, T)):
            k.stt(['raw', 'cw', 'acc'], ['acc'], acc[:, a + 1:b], raw[:, a:b - 1], cw[:, cc, 0:1], acc[:, a + 1:b], ALU.mult, ALU.add)
            k.stt(['raw', 'cw', 'acc'], ['acc'], acc[:, a:b - 1], raw[:, a + 1:b], cw[:, cc, 2:3], acc[:, a:b - 1], ALU.mult, ALU.add)
        k.cp(['acc'], ['ucb'], ucb, acc, eng='act')
        k.dma('pool', ['ucb'], ['UC'], UC[cc * 128:(cc + 1) * 128, :], ucb)
    k.em.barrier()

    if HSTOP[0] <= 3:
        return
    sbd = SB(nc, base=base2)
    vbf = sbd.t([32, HC, 128], BF16)
    x1bf = sbd.t([32, HC, 128], BF16)
    x2bf = sbd.t([32, HC, 128], BF16)
    z1 = sbd.t([32, HC, 128], BF16)
    z2 = sbd.t([32, HC, 128], BF16)
    dv = sbd.t([32, HC, 128], BF16)
    dbc = sbd.t([32, 2, 256], F32)
    Hs = sbd.t([128, 64, 2, HC], BF16)
    Y = sbd.t([128, 64, 2, HC], BF16)
    Zs = sbd.t([64, 2, 128, HC], BF16)
    xs = [sbd.t([128, KB, 2, HC], F32) for _ in range(2)]
    ta = [sbd.t([128, KB, HC], F32) for _ in range(2)]
    tb = [sbd.t([128, KB, HC], F32) for _ in range(2)]
    tg = [sbd.t([32, HC, NB6], F32) for _ in range(2)]
    k.dma('sp', [], ['dbc'], dbc, k.dram['hy_d_bc'][l])
    xctr = [0]
    for b4 in range(NHB):
        c0g = b4 * HC
        for (tile_, key, r0) in ((vbf, 'xbf', 0), (x1bf, 'x1bf', 256), (x2bf, 'x2bf', 512)):
            k.dma('sp', [key], [key], tile_, UC[r0 + c0g:r0 + c0g + HC, 0:L].rearrange("c (a b) -> a c b", b=128))
        for o in range(2):
            xin, xkey = (vbf, 'xbf') if o == 0 else (z1, 'z1')
            gate, gkey = (x1bf, 'x1bf') if o == 0 else (x2bf, 'x2bf')
            zout, zkey = (z1, 'z1') if o == 0 else (z2, 'z2')
            k.tt([xkey, 'dbc'], ['dv'], dv, xin, dbc[:, o, c0g:c0g + HC].unsqueeze(2).to_broadcast([32, HC, 128]), ALU.mult, eng='pool')
            k.dma('sp', ['Hs'], ['Hs'], Hs.rearrange("p q r c -> p (q r c)"), HH[o * NHB + b4])

            def consume(pk, pp, k0):
                i = xctr[0] % 2
                xctr[0] += 1
                xk, tak, tbk = 'xs%d' % i, 'ta%d' % i, 'tb%d' % i
                k.cp([pk], [xk], xs[i], pp[:, 0:512].rearrange("p (q r c) -> p q r c", r=2, c=HC), eng='act')
                Xr, Xi = xs[i][:, :, 0, :], xs[i][:, :, 1, :]
                Hr, Hi = Hs[:, k0:k0 + KB, 0, :], Hs[:, k0:k0 + KB, 1, :]
                Yr, Yi = Y[:, k0:k0 + KB, 0, :], Y[:, k0:k0 + KB, 1, :]
                k.tt([xk, 'Hs'], [tak], ta[i], Xr, Hr, ALU.mult)
                k.tt([xk, 'Hs'], [tbk], tb[i], Xi, Hi, ALU.mult, eng='pool')
                k.tt([tak, tbk], ['Y'], Yr, ta[i], tb[i], ALU.subtract)
                k.tt([xk, 'Hs'], [tak], ta[i], Xr, Hi, ALU.mult, eng='pool')
                k.tt([xk, 'Hs'], [tbk], tb[i], Xi, Hr, ALU.mult)
                k.tt([tak, tbk], ['Y'], Yi, ta[i], tb[i], ALU.add, eng='pool')
            fft_fwd_keyed(k, C, xin, xkey, A, nAi, nps, consume)
            for c0 in range(0, HC, 2):
                pk, pp = nps()
                for dc in range(2):
                    cidx = c0 + dc
                    out = pp[0:64, dc * 256:(dc + 1) * 256]
                    k.mm(['Y', 'E1'], [pk], out, Y[:, :, 0, cidx], C['E1'], start=True, stop=False)
                    k.mm(['Y', 'E2'], [pk], out, Y[:, :, 1, cidx], C['E2'], start=False, stop=True)
                src = pp[0:64, 0:512].rearrange("p (c r n) -> p c r n", r=2, n=128)
                k.cp([pk], ['Zs'], Zs[:, :, :, c0:c0 + 2].rearrange("p r n c -> p c r n"), src, eng=('act' if (c0 // 2) % 2 == 0 else 'dve'))
            for g8, n0 in enumerate(range(0, 128, NB6)):
                pk, pp = nps()
                for dn in range(NB6):
                    n2 = n0 + dn
                    out = pp[0:32, dn * HC:(dn + 1) * HC]
                    k.mm(['Mr', 'Zs'], [pk], out, C['Mr'][:, n2, :], Zs[:, 0, n2, :], start=True, stop=False)
                    k.mm(['nMi', 'Zs'], [pk], out, C['nMi'][:, n2, :], Zs[:, 1, n2, :], start=False, stop=True)
                i = g8 % 2
                src = pp[0:32, 0:NB6 * HC].rearrange("p (n c) -> p c n", c=HC)
                k.tt([pk, 'dv'], ['tg%d' % i], tg[i], src, dv[:, :, n0:n0 + NB6], ALU.add)
                k.tt(['tg%d' % i, gkey], [zkey], zout[:, :, n0:n0 + NB6], tg[i], gate[:, :, n0:n0 + NB6], ALU.mult, eng='pool')
        k.dma('pool', ['z2'], ['YT'], YT[c0g:c0g + HC, 0:L].rearrange("c (a b) -> a c b", b=128), z2)
    k.em.barrier()

    if last or HSTOP[0] <= 4:
        return
    sbx = SB(nc, base=base2)
    ub = sbx.t([128, 3, CTX], BF16)
    uf = sbx.t([128, 3, CTX], F32)
    kf = sbx.t([128, 511], F32)
    accs = [sbx.t([128, CTX], F32) for _ in range(4)]
    zc = sbx.t([128, CTX], F32)
    zb = sbx.t([128, CTX], BF16)
    dcol = sbx.t([128, 4], F32)
    k.dma('sp', [], ['dcol'], dcol, k.dram['hy_d_col'][l])
    for ch in range(2):
        for j in range(3):
            k.dma('sp', ['ub'], ['ub'], ub[:, j, :], UC[j * 256 + ch * 128:j * 256 + (ch + 1) * 128, L:T])
        k.cp(['ub'], ['uf'], uf, ub)
        for o in range(2):
            uin = uf[:, 0, :] if o == 0 else zc
            gate = uf[:, 1 + o, :]
            k.dma('sp', ['kf'], ['kf'], kf, CK[o * 2 + ch])
            for a in range(4):
                k.memset([], ['acc%d' % a], accs[a], 0.0, eng=('dve' if a < 2 else 'pool'))
            for s_ in range(CTX):
                a = s_ % 4
                k.stt(['kf', 'uf', 'zc', 'acc%d' % a], ['acc%d' % a], accs[a], kf[:, 255 - s_:511 - s_], uin[:, s_:s_ + 1], accs[a],
                      ALU.mult, ALU.add)
            k.tt(['acc0', 'acc1'], ['acc0'], accs[0], accs[0], accs[1], ALU.add)
            k.tt(['acc2', 'acc3'], ['acc2'], accs[2], accs[2], accs[3], ALU.add, eng='pool')
            k.tt(['acc0', 'acc2'], ['acc0'], accs[0], accs[0], accs[2], ALU.add)
            k.stt(['uf', 'zc', 'dcol', 'acc0'], ['acc0'], accs[0], uin, dcol[:, o * 2 + ch:o * 2 + ch + 1], accs[0], ALU.mult, ALU.add)
            k.tt(['acc0', 'uf'], ['zc'], zc, accs[0], gate, ALU.mult)
        k.cp(['zc'], ['zb'], zb, zc)
        k.dma('pool', ['zb'], ['YT'], YT[ch * 128:(ch + 1) * 128, L:T], zb)
    k.em.barrier()


def fft_fwd_keyed(k, C, xin, xkey, A, nAi, psctr, consume):
    F1, Gr, Gi = C['F1'], C['Gr'], C['Gi']
    for c0 in range(0, HC, 4):
        pk, pp = psctr()
        for dc in range(4):
            k.mm([xkey, 'F1'], [pk], pp[:, dc * 128:(dc + 1) * 128], xin[:, c0 + dc, :], F1)
        src = pp[:, 0:512].rearrange("p (c r q) -> p c r q", r=2, q=64)
        k.cp([pk], ['A'], A[:, :, :, c0:c0 + 4].rearrange("p q r c -> p c r q"), src, eng='act')
        k.ts(['A'], ['nAi'], nAi[:, :, c0:c0 + 4], A[:, :, 1, c0:c0 + 4], -1.0, op0=ALU.mult)
    for k0 in range(0, 64, KB):
        pk, pp = psctr()
        for dk in range(KB):
            k1 = k0 + dk
            xr = pp[:, dk * 2 * HC:dk * 2 * HC + HC]
            xi = pp[:, dk * 2 * HC + HC:(dk + 1) * 2 * HC]
            k.mm(['Gr', 'A'], [pk], xr, Gr[:, k1, :], A[:, k1, 0, :], start=True, stop=False)
            k.mm(['Gi', 'nAi'], [pk], xr, Gi[:, k1, :], nAi[:, k1, :], start=False, stop=True)
            k.mm(['Gi', 'A'], [pk], xi, Gi[:, k1, :], A[:, k1, 0, :], start=True, stop=False)
            k.mm(['Gr', 'A'], [pk], xi, Gr[:, k1, :], A[:, k1, 1, :], start=False, stop=True)
        consume(pk, pp, k0)


BIG = 1.0e9


def phaseC(k, P, l, base, last, xres):
    nc = k.nc
    YT, XMIX, H2T, XRES = k.dram['YT'], k.dram['XMIX'], k.dram['H2T'], k.dram['XRES']
    ntiles = 32 if last else NT
    psi = [0]

    def nps():
        i = psi[0]
        psi[0] = (psi[0] + 1) % 8
        return 'ps%d' % i, k.ps[i]

    sb0 = SB(nc, base=base)
    grow = sb0.t([128, 2, 2, D], F32)
    gates = sb0.t([128, NT, 32], F32)
    dg = sb0.t([128, 128], F32)
    for gi, c0 in enumerate((16, 40)):
        for j in range(2):
            for kk in range(8):
                k.ts(['ident32', 'mod'], ['dg'], dg, P['ident32'], P['mod'][:, l, c0 + kk, j:j + 1], op0=ALU.mult)
                pk, pp = nps()
                k.mm(['ones32', 'dg'], [pk], pp[:, 0:128], P['ones32'], dg)
                k.cp([pk], ['grow'], grow[:, gi, j, kk * 128:(kk + 1) * 128], pp[:, 0:128], eng='act')
    base1 = sb0.off
    sb = SB(nc, base=base1)
    Wout = sb.t([128, 8, D], BF16)
    Wr = sb.t([128, 8, 36], F32)
    rb = sb.t([128, 36], F32)
    stg = sb.t([128, 8, 512], F32)
    wv = k.dram['w_out'][l].rearrange("(k p) n -> p k n", p=128)
    for i, c0 in enumerate((0, 512)):
        k.dma('sp', ['stg'], ['stg'], stg, wv[:, :, c0:c0 + 512])
        k.cp(['stg'], ['Wout'], Wout[:, :, c0:c0 + 512], stg)
    k.dma('sp', [], ['Wr'], Wr, k.dram['moe_wr'][l].rearrange("(k p) n -> p k n", p=128))
    k.dma('sp', [], ['rb'], rb, k.dram['moe_rb_bc'][l])
    yT = [sb.t([128, 8, 512], BF16) for _ in range(2)]
    xt = [sb.t([128, D], F32) for _ in range(2)]
    xm = [sb.t([128, D], F32) for _ in range(2)]
    xn = sb.t([128, D], F32)
    junk = sb.t([128, D], BF16)
    h32 = sb.t([128, 8, 128], F32)
    hbf = [sb.t([128, 8, 128], BF16) for _ in range(2)]
    ss = [sb.t([128, 2], F32) for _ in range(2)]
    lg = sb.t([128, 36], F32)
    rt = sb.t([128, 16], F32)
    oh = sb.t([128, 4], F32)
    ml = sb.t([128, 32], F32)
    e1 = sb.t([128, 32], F32)
    e2 = sb.t([128, 32], F32)
    tmp32 = sb.t([128, 32], F32)
    for ti in range(ntiles):
        j = 0 if ti < 32 else 1
        g, tl = ti // 4, ti % 4
        yb = g % 2
        yk = 'yT%d' % yb
        if tl == 0:
            n = min(512, ntiles * 128 - g * 512)
            k.dma('sp', [yk], [yk], yT[yb][:, :, 0:n], YT[:, g * 512:g * 512 + n].rearrange("(c p) t -> p c t", p=128))
        b = ti % 2
        xk, mk, sk, hk = 'xt%d' % b, 'xm%d' % b, 'ss%d' % b, 'hbf%d' % b
        k.dma('sp', [xk], [xk], xt[b], xres[ti * 128:(ti + 1) * 128, :])
        for half in range(2):
            pk, pp = nps()
            for f in range(8):
                k.mm([yk, 'Wout'], [pk], pp[:, 0:512], yT[yb][:, f, tl * 128:(tl + 1) * 128], Wout[:, f, half * 512:(half + 1) * 512],
                     start=(f == 0), stop=(f == 7))
            cs = slice(half * 512, (half + 1) * 512)
            k.tt([pk, 'grow'], [mk], xm[b][:, cs], pp[:, 0:512], grow[:, 0, j, cs], ALU.mult)
            k.tt([mk, xk], [mk], xm[b][:, cs], xm[b][:, cs], xt[b][:, cs], ALU.add, eng='pool')
        k.dma('pool', [mk], ['XMIX'], XMIX[ti * 128:(ti + 1) * 128, :], xm[b])
        k.memset([], [sk], ss[b], 0.0)
        k.act([mk, sk], ['junk', sk], junk, xm[b], AF.Square, accum_out=ss[b][:, 0:1])
        k.ts([sk], [sk], ss[b][:, 1:2], ss[b][:, 0:1], 1.0 / D, EPS, op0=ALU.mult, op1=ALU.add)
        k.act([sk], [sk], ss[b][:, 1:2], ss[b][:, 1:2], AF.Sqrt)
        k.recip([sk], [sk], ss[b][:, 1:2], ss[b][:, 1:2])
        k.ts([mk, sk], ['xn'], xn, xm[b], ss[b][:, 1:2], op0=ALU.mult)
        for h2 in range(2):
            pk, pp = nps()
            for q in range(4):
                kk = h2 * 4 + q
                k.tr(['xn', 'ident32'], [pk], pp[:, q * 128:(q + 1) * 128], xn[:, kk * 128:(kk + 1) * 128], P['ident32'])
            for q in range(4):
                kk = h2 * 4 + q
                k.act([pk, 'A2', 'mod'], ['h32'], h32[:, kk, :], pp[:, q * 128:(q + 1) * 128], AF.Identity,
                      scale=P['A2'][:, l, kk, j:j + 1], bias=P['mod'][:, l, 24 + kk, j:j + 1])
        k.cp(['h32'], [hk], hbf[b], h32, eng='pool')
        k.dma('pool', [hk], ['H2T'], H2T[:, ti * 128:(ti + 1) * 128].rearrange("(c p) t -> p c t", p=128), hbf[b])
        pk, pp = nps()
        for kk in range(8):
            k.mm(['h32', 'Wr'], [pk], pp[:, 0:36], h32[:, kk, :], Wr[:, kk, :], start=(kk == 0), stop=(kk == 7))
        k.tt([pk, 'rb'], ['lg'], lg, pp[:, 0:36], rb, ALU.add)
        k.red(['lg'], ['rt'], rt[:, 0:1], lg[:, 0:4], ALU.max)
        k.ts(['lg', 'rt'], ['oh'], oh, lg[:, 0:4], rt[:, 0:1], op0=ALU.is_equal)
        k.ts(['rt'], ['rt'], rt[:, 1:2], rt[:, 0:1], -1.0, op0=ALU.mult)
        k.memset(['rt'], ['rt'], rt[:, 2:3], 0.0)
        k.act(['lg', 'rt'], ['tmp32', 'rt'], tmp32[:, 0:4], lg[:, 0:4], AF.Exp, bias=rt[:, 1:2], scale=1.0, accum_out=rt[:, 2:3])
        k.recip(['rt'], ['rt'], rt[:, 3:4], rt[:, 2:3])
        k.ts(['oh'], ['oh'], oh, oh, 1.0, BIG, op0=ALU.subtract, op1=ALU.mult)
        k.tt(['lg', 'oh'], ['ml'], ml.rearrange("p (g e) -> p g e", e=8), lg[:, 4:36].rearrange("p (g e) -> p g e", e=8),
             oh.unsqueeze(2).to_broadcast([128, 4, 8]), ALU.add)
        k.red(['ml'], ['rt'], rt[:, 4:5], ml, ALU.max)
        k.ts(['ml', 'rt'], ['e1'], e1, ml, rt[:, 4:5], op0=ALU.is_equal)
        k.ts(['e1'], ['tmp32'], tmp32, e1, -BIG, op0=ALU.mult)
        k.tt(['ml', 'tmp32'], ['ml'], ml, ml, tmp32, ALU.add)
        k.red(['ml'], ['rt'], rt[:, 5:6], ml, ALU.max)
        k.ts(['ml', 'rt'], ['e2'], e2, ml, rt[:, 5:6], op0=ALU.is_equal)
        k.tt(['rt'], ['rt'], rt[:, 6:7], rt[:, 5:6], rt[:, 4:5], ALU.subtract)
        k.act(['rt'], ['rt'], rt[:, 6:7], rt[:, 6:7], AF.Exp)
        k.ts(['rt'], ['rt'], rt[:, 7:8], rt[:, 6:7], 1.0, op0=ALU.add)
        k.recip(['rt'], ['rt'], rt[:, 7:8], rt[:, 7:8])
        k.tt(['rt'], ['rt'], rt[:, 8:9], rt[:, 6:7], rt[:, 7:8], ALU.mult)
        k.tt(['rt'], ['rt'], rt[:, 9:10], rt[:, 7:8], rt[:, 3:4], ALU.mult)
        k.tt(['rt'], ['rt'], rt[:, 10:11], rt[:, 8:9], rt[:, 3:4], ALU.mult)
        k.ts(['e1', 'rt'], ['gates'], gates[:, ti, :], e1, rt[:, 9:10], op0=ALU.mult)
        k.stt(['e2', 'rt', 'gates'], ['gates'], gates[:, ti, :], e2, rt[:, 10:11], gates[:, ti, :], ALU.mult, ALU.add)
    k.em.barrier()
    NPASS = 3
    per = (ntiles + NPASS - 1) // NPASS
    sb = SB(nc, base=base1)
    h2 = sb.t([128, 8, per * 128], BF16)
    facc = sb.t([128, per, D], F32)
    wst = [sb.t([128, 8, 512], F32) for _ in range(2)]
    w1b = [sb.t([128, 8, 512], BF16) for _ in range(2)]
    w3b = [sb.t([128, 8, 512], BF16) for _ in range(2)]
    w2b = [sb.t([128, 4, D], BF16) for _ in range(2)]
    gT = [sb.t([128, 4, 512], BF16) for _ in range(2)]
    st = [sb.t([128, 512], F32) for _ in range(2)]
    xo = [sb.t([128, D], F32) for _ in range(2)]
    fw = sb.t([128, D], F32)
    if last:
        k.dma('sp', [], ['fw'], fw, k.dram['final_bc'][:, :])
    W1, W3, W2 = k.dram['moe_w1'], k.dram['moe_w3'], k.dram['moe_w2']
    sctr = [0]

    def load_w(src_ap, dst, dkey, shape3):
        i = sctr[0] % 2
        sctr[0] += 1
        sk_ = 'wst%d' % i
        view = wst[i].rearrange("p a b -> p (a b)").rearrange("p (a b) -> p a b", b=shape3)
        k.dma('sp', [sk_], [sk_], view, src_ap)
        k.cp([sk_], [dkey], dst, view, eng='pool')

    for ps_ in range(NPASS):
        t_lo = ps_ * per
        t_hi = min(ntiles, t_lo + per)
        nt_ = t_hi - t_lo
        ntok = nt_ * 128
        k.dma('sp', ['h2'], ['h2'], h2[:, :, 0:ntok], H2T[:, t_lo * 128:t_hi * 128].rearrange("(c p) t -> p c t", p=128))
        k.memset(['facc'], ['facc'], facc, 0.0, eng='pool')
        for e in range(32):
            wb = e % 2
            load_w(W1[l, e].rearrange("(k p) n -> p k n", p=128), w1b[wb], 'w1b%d' % wb, 512)
            load_w(W3[l, e].rearrange("(k p) n -> p k n", p=128), w3b[wb], 'w3b%d' % wb, 512)
            load_w(W2[l, e].rearrange("(k p) n -> p k n", p=128), w2b[wb], 'w2b%d' % wb, D)
            for c0 in range(0, ntok, 512):
                n = min(512, ntok - c0)
                gb = (c0 // 512) % 2
                gk = 'gT%d' % gb
                for f in range(4):
                    p1k, pp1 = nps()
                    for kk in range(8):
                        k.mm(['w1b%d' % wb, 'h2'], [p1k], pp1[:, 0:n], w1b[wb][:, kk, f * 128:(f + 1) * 128], h2[:, kk, c0:c0 + n],
                             start=(kk == 0), stop=(kk == 7))
                    p3k, pp3 = nps()
                    for kk in range(8):
                        k.mm(['w3b%d' % wb, 'h2'], [p3k], pp3[:, 0:n], w3b[wb][:, kk, f * 128:(f + 1) * 128], h2[:, kk, c0:c0 + n],
                             start=(kk == 0), stop=(kk == 7))
                    sbi = f % 2
                    k.act([p1k], ['st%d' % sbi], st[sbi][:, 0:n], pp1[:, 0:n], AF.Silu)
                    k.tt(['st%d' % sbi, p3k], [gk], gT[gb][:, f, 0:n], st[sbi][:, 0:n], pp3[:, 0:n], ALU.mult)
                for tl in range(n // 128):
                    t = c0 // 128 + tl
                    for half in range(2):
                        pk, pp = nps()
                        for f in range(4):
                            k.mm([gk, 'w2b%d' % wb], [pk], pp[:, 0:512], gT[gb][:, f, tl * 128:(tl + 1) * 128], w2b[wb][:, f, half * 512:(half + 1) * 512],
                                 start=(f == 0), stop=(f == 3))
                        fa = facc[:, t, half * 512:(half + 1) * 512]
                        k.stt([pk, 'gates', 'facc'], ['facc'], fa, pp[:, 0:512], gates[:, t_lo + t, e:e + 1], fa, ALU.mult, ALU.add)
        for t in range(nt_):
            ti = t_lo + t
            j = 0 if ti < 32 else 1
            b = t % 2
            ok = 'xo%d' % b
            k.dma('sp', [ok], [ok], xo[b], XMIX[ti * 128:(ti + 1) * 128, :])
            k.tt(['facc', 'grow'], ['facc'], facc[:, t, :], facc[:, t, :], grow[:, 1, j, :], ALU.mult, eng='pool')
            k.tt(['facc', ok], [ok], xo[b], xo[b], facc[:, t, :], ALU.add)
            if not last:
                k.dma('pool', [ok], ['XRES'], XRES[ti * 128:(ti + 1) * 128, :], xo[b])
            else:
                sk = 'fss'
                fs = st[0][:, 0:2]
                k.memset(['st0'], ['st0'], fs, 0.0)
                k.act([ok, 'st0'], ['gT0', 'st0'], gT[0].rearrange("p a b -> p (a b)")[:, 0:D], xo[b], AF.Square, accum_out=fs[:, 0:1])
                k.ts(['st0'], ['st0'], fs[:, 1:2], fs[:, 0:1], 1.0 / D, EPS, op0=ALU.mult, op1=ALU.add)
                k.act(['st0'], ['st0'], fs[:, 1:2], fs[:, 1:2], AF.Sqrt)
                k.recip(['st0'], ['st0'], fs[:, 1:2], fs[:, 1:2])
                k.ts([ok, 'st0'], [ok], xo[b], xo[b], fs[:, 1:2], op0=ALU.mult)
                k.tt([ok, 'fw'], [ok], xo[b], xo[b], fw, ALU.mult)
                k.dma('pool', [ok], ['out'], k.dram['out'][ti * 128:(ti + 1) * 128, :], xo[b])
    k.em.barrier()


def build(stage='full', debug=()):
    nc = bass.Bass("TRN2", target_bir_lowering=False)
    k = K(nc, debug=debug)
    P = {}
    sbp = SB(nc)
    k.din('xin', [T, D])
    k.din('w_in_fm', [DEPTH, D, NFM])
    k.din('w_in_tm', [DEPTH, D, NTM])
    k.din('b_fm_col', [128, DEPTH, 18])
    k.din('b_tm_bc', [DEPTH, 128, NTM])
    k.dscratch('UT', [1280, T])
    k.dscratch('QKT', [1024, T], BF16)
    k.dscratch('TM', [T, NTM])
    k.dscratch('XRES', [T, D])
    k.dscratch('YT', [1024, T], BF16)
    k.din('da_lambda', [DEPTH, 256])
    k.dscratch('XMIX', [T, D])
    k.dscratch('H2T', [D, T], BF16)
    k.din('w_out', [DEPTH, D, D])
    k.din('moe_wr', [DEPTH, D, 36])
    k.din('moe_rb_bc', [DEPTH, 128, 36])
    k.din('moe_w1', [DEPTH, 32, D, 512])
    k.din('moe_w3', [DEPTH, 32, D, 512])
    k.din('moe_w2', [DEPTH, 32, 512, D])
    k.din('final_bc', [128, D])
    if stage == 'full':
        k.dout('out', [L, D])
    k.dscratch('HK', [8, 128, L], BF16)
    k.dscratch('HH', [2 * NHB, 128, 64 * 2 * HC], BF16)
    k.dscratch('CK', [4, 128, 511])
    k.dscratch('UC', [768, T], BF16)
    for nm, shp in (('hy_filt_w1', [DEPTH, 33, 64]), ('hy_filt_w2', [DEPTH, 64, 64]), ('hy_filt_w3', [DEPTH, 64, 1024]),
                    ('hy_filt_sc', [DEPTH, 64, 3]), ('hy_b3_col', [DEPTH, 128, 8]), ('hy_conv_col', [DEPTH, 128, 6, 4]),
                    ('hy_d_bc', [DEPTH, 32, 2, 256]), ('hy_d_col', [DEPTH, 128, 4]),
                    ('hy_z', [33, L]), ('hy_decay', [256, L]), ('hy_zc', [33, 511]), ('hy_decayc', [256, 511]),
                    ('hy_F1', [32, 128]), ('hy_Gr', [128, 8192]), ('hy_Gi', [128, 8192]), ('hy_E1', [128, 256]),
                    ('hy_E2', [128, 256]), ('hy_Mr', [64, 4096]), ('hy_nMi', [64, 4096])):
        k.din(nm, shp)
    k.din('tri', [2, 64, 64])
    k.din('ml_conv_col', [DEPTH, 64, 8, 4])
    k.din('ml_norm_bc', [DEPTH, 64, 256])
    k.din('da_subln_col', [DEPTH, 128, 1])
    phase0(k, P, sbp)
    P['negc'] = sbp.t([128, DEPTH, 2, 4], F32)
    base = sbp.off
    for l in range(DEPTH):
        xres = k.dram['xin'] if l == 0 else k.dram['XRES']
        phaseA(k, P, l, base, xres)
        if stage == 'A':
            break
        if stage not in ('B2', 'H'):
            phaseB1(k, P, l, base, l == DEPTH - 1)
        if stage == 'B1':
            break
        if stage != 'H':
            phaseB2(k, P, l, base, l == DEPTH - 1)
        if stage == 'B2':
            break
        phaseH(k, P, l, base, l == DEPTH - 1)
        if stage == 'H':
            break
        phaseC(k, P, l, base, (l == DEPTH - 1) and stage == 'full', xres)
        if stage == 'C':
            break
    k.em.barrier()
    return nc, k


def run(inputs, stage='full', debug=(), cores=8):
    consts = make_consts()
    nc, k = build(stage, debug)
    in_maps = []
    for b in range(cores):
        m = prep_inputs(inputs, b)
        m.update(consts)
        in_maps.append({kk: v for kk, v in m.items() if kk in k.dram})
    res = run_bass_kernel_spmd(nc, in_maps, core_ids=list(range(cores)))
    return res.results


def kernel(**inputs):
    inp = {kk: np.asarray(v) for kk, v in inputs.items()}
    res = run(inp, stage='full', cores=8)
    return np.stack([np.asarray(r['out'], dtype=np.float32) for r in res], axis=0)
```

```python
import math
import os
import numpy as np
import concourse.bass as bass
import concourse.mybir as mybir
from concourse.bass_utils import run_bass_kernel_spmd

F32 = mybir.dt.float32
BF16 = mybir.dt.bfloat16
AF = mybir.ActivationFunctionType
ALU = mybir.AluOpType
AX = mybir.AxisListType

D = 1024
L = 4096
CTX = 256
T = L + CTX
NT = T // 128
DEPTH = 2
EPS = 1e-6
N_IN = 3344
ML_OFF = 768
DA_OFF = 1808
NFM = 2304
NTM = 1040
FM_COLS = list(range(0, 768)) + list(range(768, 1280)) + list(range(1808, 2832))
TM_COLS = list(range(1280, 1792)) + list(range(2832, 3344)) + list(range(1792, 1808))


class Em:
    NDMA = 32
    SAME_ENGINE_WAITS = True

    def __init__(self, nc):
        self.nc = nc
        self.eng = {'pe': nc.tensor, 'act': nc.scalar, 'dve': nc.vector, 'pool': nc.gpsimd, 'sp': nc.sync}
        self.sem = {k: nc.alloc_semaphore('s_' + k) for k in ('pe', 'act', 'dve', 'pool')}
        self.cnt = {k: 0 for k in self.sem}
        self.dsem = [nc.alloc_semaphore('s_dma%d' % i) for i in range(self.NDMA)]
        self.dcnt = [0] * self.NDMA
        self.dnext = 0
        self.waited = {e: {} for e in self.eng}
        self.lastw = {}
        self.readers = {}
        self.ninst = 0

    def _semh(self, key):
        return self.sem[key] if isinstance(key, str) else self.dsem[key[1]]

    def _wait(self, e, ev):
        key, val = ev
        w = self.waited[e]
        if w.get(key, 0) >= val:
            return
        self.eng[e].wait_ge(self._semh(key), val)
        w[key] = val

    def _deps(self, e, reads, writes):
        best = {}
        for k in reads:
            ev = self.lastw.get(k)
            if ev is not None and best.get(ev[0], 0) < ev[1]:
                best[ev[0]] = ev[1]
        for k in writes:
            ev = self.lastw.get(k)
            if ev is not None and best.get(ev[0], 0) < ev[1]:
                best[ev[0]] = ev[1]
            for ev in self.readers.get(k, ()):
                if best.get(ev[0], 0) < ev[1]:
                    best[ev[0]] = ev[1]
        for key, val in best.items():
            if key == e and (e == 'pe' or not Em.SAME_ENGINE_WAITS):
                continue
            self._wait(e, (key, val))

    def _record(self, ev, reads, writes):
        for k in reads:
            lst = self.readers.setdefault(k, [])
            lst[:] = [x for x in lst if x[0] != ev[0]]
            lst.append(ev)
        for k in writes:
            self.lastw[k] = ev
            self.readers[k] = []

    def op(self, e, reads, writes, fn):
        self._deps(e, reads, writes)
        ins = fn(self.eng[e])
        self.cnt[e] += 1
        ins.then_inc(self.sem[e], 1)
        self._record((e, self.cnt[e]), reads, writes)
        self.ninst += 1

    def dma(self, q, reads, writes, out, in_, **kw):
        i = self.dnext
        self.dnext = (i + 1) % self.NDMA
        if self.dcnt[i] > 0:
            self._wait(q, (('d', i), 16 * self.dcnt[i]))
        self._deps(q, reads, writes)
        ins = self.eng[q].dma_start(out=out, in_=in_, **kw)
        self.dcnt[i] += 1
        ins.then_inc(self.dsem[i], 16)
        self._record((('d', i), 16 * self.dcnt[i]), reads, writes)
        self.ninst += 1

    def barrier(self):
        for e in self.eng:
            for k in self.sem:
                if self.cnt[k] > 0 and k != e:
                    self._wait(e, (k, self.cnt[k]))
            for i in range(self.NDMA):
                if self.dcnt[i] > 0:
                    self._wait(e, (('d', i), 16 * self.dcnt[i]))
        self.lastw = {}
        self.readers = {}


class SB:
    _arena = {}

    def __init__(self, nc, base=0, limit=None):
        self.nc = nc
        if id(nc) not in SB._arena:
            nwords = (nc.sbuf_bytes_remaining - 256) // 4
            SB._arena[id(nc)] = (nc.alloc_sbuf_tensor("arena", [128, nwords], F32), nwords * 4)
        self.arena, cap = SB._arena[id(nc)]
        self.off = base
        self.limit = cap if limit is None else limit

    def t(self, shape, dtype, name=None):
        per = 1
        for s in shape[1:]:
            per *= s
        esz = 2 if dtype == BF16 else 4
        nbytes = (per * esz + 63) // 64 * 64
        assert self.off % 4 == 0
        w0 = self.off // 4
        ap = self.arena[0:shape[0], w0:w0 + nbytes // 4]
        if dtype != F32:
            ap = ap.bitcast(dtype)
        ap = ap[:, 0:per]
        if len(shape) == 3:
            ap = ap.rearrange("p (a b) -> p a b", b=shape[2])
        elif len(shape) == 4:
            ap = ap.rearrange("p (a b c) -> p a b c", b=shape[2], c=shape[3])
        self.off += nbytes
        assert self.off <= self.limit, ("SBUF overflow", name, self.off, self.limit)
        return ap


class K:
    def __init__(self, nc, debug=()):
        self.nc = nc
        self.em = Em(nc)
        self.debug = set(debug)
        self.dram = {}
        self.ps = [nc.alloc_psum_tensor("psb%d" % i, [128, 512], F32) for i in range(8)]

    def din(self, name, shape, dtype=F32):
        ap = self.nc.dram_tensor(name, list(shape), dtype, kind="ExternalInput").ap()
        self.dram[name] = ap
        return ap

    def dscratch(self, name, shape, dtype=F32):
        kind = "ExternalOutput" if name in self.debug else "Internal"
        ap = self.nc.dram_tensor(name, list(shape), dtype, kind=kind).ap()
        self.dram[name] = ap
        return ap

    def dout(self, name, shape, dtype=F32):
        ap = self.nc.dram_tensor(name, list(shape), dtype, kind="ExternalOutput").ap()
        self.dram[name] = ap
        return ap

    def dma(self, q, r, w, out, in_, **kw):
        self.em.dma(q, r, w, out, in_, **kw)

    def mm(self, r, w, out, lhsT, rhs, start=True, stop=True):
        self.em.op('pe', r, w, lambda e: e.matmul(out, lhsT=lhsT, rhs=rhs, start=start, stop=stop))

    def tr(self, r, w, out, in_, ident):
        self.em.op('pe', r, w, lambda e: e.transpose(out, in_, ident))

    def act(self, r, w, out, in_, func, eng='act', **kw):
        self.em.op(eng, r, w, lambda e: e.activation(out=out, in_=in_, func=func, **kw))

    def ts(self, r, w, out, in0, s1, s2=None, op0=ALU.mult, op1=None, eng='dve', **kw):
        if op1 is None:
            self.em.op(eng, r, w, lambda e: e.tensor_scalar(out=out, in0=in0, scalar1=s1, scalar2=None, op0=op0, **kw))
        else:
            self.em.op(eng, r, w, lambda e: e.tensor_scalar(out=out, in0=in0, scalar1=s1, scalar2=s2, op0=op0, op1=op1, **kw))

    def tt(self, r, w, out, in0, in1, op, eng='dve'):
        self.em.op(eng, r, w, lambda e: e.tensor_tensor(out=out, in0=in0, in1=in1, op=op))

    def stt(self, r, w, out, in0, scalar, in1, op0, op1, eng='dve'):
        self.em.op(eng, r, w, lambda e: e.scalar_tensor_tensor(out=out, in0=in0, scalar=scalar, in1=in1, op0=op0, op1=op1))

    def cp(self, r, w, out, in_, eng='dve'):
        if eng == 'act':
            self.em.op(eng, r, w, lambda e: e.copy(out=out, in_=in_))
        else:
            self.em.op(eng, r, w, lambda e: e.tensor_copy(out=out, in_=in_))

    def red(self, r, w, out, in_, op, eng='dve', axis=AX.X):
        self.em.op(eng, r, w, lambda e: e.tensor_reduce(out=out, in_=in_, axis=axis, op=op))

    def recip(self, r, w, out, in_):
        self.em.op('dve', r, w, lambda e: e.reciprocal(out=out, in_=in_))

    def memset(self, r, w, out, val, eng='dve'):
        self.em.op(eng, r, w, lambda e: e.memset(out, val))


def rope_tables_T():
    half = 32
    inv = (10000.0 ** (-np.arange(0, half, 2, dtype=np.float32) / half)).astype(np.float32)
    t = np.arange(L)
    row = (t // 64).astype(np.float32)
    col = (t % 64).astype(np.float32)
    ang = np.concatenate([row[:, None] * inv, row[:, None] * inv, col[:, None] * inv, col[:, None] * inv], axis=1)
    ang = ang.astype(np.float32)
    cosT = np.cos(ang).T.astype(np.float32)
    sinT = np.sin(ang).T.astype(np.float32)
    return np.ascontiguousarray(np.concatenate([cosT, cosT], 0)), np.ascontiguousarray(np.concatenate([sinT, sinT], 0))


def rope_perm():
    R = np.zeros((128, 128), np.float32)
    for base in range(0, 128, 32):
        for i in range(16):
            R[base + 16 + i, base + i] = -1.0
            R[base + i, base + 16 + i] = 1.0
    return R


def make_consts():
    c = {}
    c['ident'] = np.eye(128, dtype=np.float32)
    c['ropeR'] = rope_perm()
    cosT, sinT = rope_tables_T()
    c['cosT'] = cosT
    c['sinT'] = sinT
    bi = np.zeros((128, 2), np.float32)
    bi[0:64, 0] = 1.0
    bi[64:128, 1] = 1.0
    c['blockind'] = bi
    sel = np.zeros((2, 2, 128), np.float32)
    sel[0, 0, :] = 1.0
    sel[1, 1, :] = 1.0
    c['sel2'] = sel
    tri = np.zeros((2, 64, 64), np.float32)
    tri[0] = np.triu(np.ones((64, 64), np.float32))
    tri[1] = np.tril(np.ones((64, 64), np.float32))
    c['tri'] = tri
    c.update(hyena_consts())
    return c


def col_layout(v):
    return np.ascontiguousarray(v.reshape(-1, 128).T)


def prep_inputs(inp, b):
    m = {}
    m['xin'] = np.ascontiguousarray(np.concatenate([inp['x'][b], inp['ctx'][b]], axis=0))
    m['ccol'] = np.ascontiguousarray(np.stack([col_layout(inp['c'][b]), col_layout(inp['c_ctx'])], axis=-1))
    m['ada_w'] = inp['ada_w']
    m['ada_b_col'] = np.ascontiguousarray(np.stack([col_layout(inp['ada_b'][l]) for l in range(DEPTH)], 1))
    m['norm1_col'] = np.ascontiguousarray(np.stack([col_layout(inp['norm1_w'][l]) for l in range(DEPTH)], 1))
    m['norm2_col'] = np.ascontiguousarray(np.stack([col_layout(inp['norm2_w'][l]) for l in range(DEPTH)], 1))
    m['w_in_fm'] = np.ascontiguousarray(inp['w_in'][:, :, FM_COLS])
    m['w_in_tm'] = np.ascontiguousarray(inp['w_in'][:, :, TM_COLS])
    m['b_fm_col'] = np.ascontiguousarray(np.stack([col_layout(inp['b_in'][l][FM_COLS]) for l in range(DEPTH)], 1))
    cw = np.concatenate([inp['ml_conv_w'], inp['ml_conv_b'][:, None, :]], axis=1)
    m['ml_conv_col'] = np.ascontiguousarray(cw.reshape(DEPTH, 4, 8, 64).transpose(0, 3, 2, 1))
    m['ml_norm_bc'] = np.ascontiguousarray(np.broadcast_to(inp['ml_norm_w'][:, None, :], (DEPTH, 64, 256)))
    m['hy_filt_w1'] = inp['hy_filt_w1']
    m['hy_filt_w2'] = inp['hy_filt_w2']
    m['hy_filt_w3'] = inp['hy_filt_w3']
    m['hy_filt_sc'] = np.ascontiguousarray(np.stack([inp['hy_sin_freq'], inp['hy_filt_b1'], inp['hy_filt_b2']], axis=-1))
    m['hy_b3_col'] = np.ascontiguousarray(np.stack([col_layout(inp['hy_filt_b3'][l]) for l in range(DEPTH)], 0))
    hw = np.concatenate([inp['hy_conv_w'], inp['hy_conv_b'][:, None, :]], axis=1)
    m['hy_conv_col'] = np.ascontiguousarray(hw.reshape(DEPTH, 4, 6, 128).transpose(0, 3, 2, 1))
    m['hy_d_bc'] = np.ascontiguousarray(np.broadcast_to(inp['hy_bias_d'][:, None, :, :], (DEPTH, 32, 2, 256)))
    m['hy_d_col'] = np.ascontiguousarray(inp['hy_bias_d'].reshape(DEPTH, 4, 128).transpose(0, 2, 1))
    m['w_out'] = inp['w_out']
    m['moe_wr'] = np.ascontiguousarray(np.concatenate([inp['moe_wg'], inp['moe_we']], axis=-1))
    rbv = np.concatenate([inp['moe_bg'], inp['moe_be']], axis=-1)
    m['moe_rb_bc'] = np.ascontiguousarray(np.broadcast_to(rbv[:, None, :], (DEPTH, 128, 36)))
    m['moe_w1'] = inp['moe_w1']
    m['moe_w3'] = inp['moe_w3']
    m['moe_w2'] = inp['moe_w2']
    m['final_bc'] = np.ascontiguousarray(np.broadcast_to(inp['final_norm_w'][None, :], (128, D)))
    m['da_lambda'] = np.ascontiguousarray(inp['da_lambda'].reshape(DEPTH, 256))
    m['da_subln_col'] = np.ascontiguousarray(inp['da_subln_w'][:, :, None])
    m['b_tm_bc'] = np.ascontiguousarray(np.broadcast_to(inp['b_in'][:, None, TM_COLS], (DEPTH, 128, NTM)))
    return m


def phase0(k, P, sbp):
    nc = k.nc
    cst = {}
    for name, shape in (('ident', [128, 128]), ('ropeR', [128, 128]), ('blockind', [128, 2])):
        k.din(name, shape)
    k.din('sel2', [2, 2, 128])
    k.din('cosT', [128, L])
    k.din('sinT', [128, L])
    P['ident32'] = sbp.t([128, 128], F32)
    P['identbf'] = sbp.t([128, 128], BF16)
    P['ropeRbf'] = sbp.t([128, 128], BF16)
    P['blockbf'] = sbp.t([128, 2], BF16)
    P['sel2'] = sbp.t([2, 2, 128], F32)
    P['ones32'] = sbp.t([128, 128], F32)
    P['onesbf'] = sbp.t([128, 128], BF16)
    tmp = sbp.t([128, 128], F32)
    k.dma('sp', [], ['ident32'], P['ident32'], k.dram['ident'][:, :])
    k.cp(['ident32'], ['identbf'], P['identbf'], P['ident32'])
    k.dma('sp', [], ['c_tmp'], tmp, k.dram['ropeR'][:, :])
    k.cp(['c_tmp'], ['ropeRbf'], P['ropeRbf'], tmp)
    k.dma('sp', ['c_tmp'], ['c_tmp'], tmp[:, 0:2], k.dram['blockind'][:, :])
    k.cp(['c_tmp'], ['blockbf'], P['blockbf'], tmp[:, 0:2])
    k.dma('sp', [], ['sel2'], P['sel2'], k.dram['sel2'][:, :, :])
    k.memset([], ['ones32'], P['ones32'], 1.0)
    k.memset([], ['onesbf'], P['onesbf'], 1.0)

    ccol = k.din('ccol', [128, 8, 2])
    adaw = k.din('ada_w', [DEPTH, D, 6 * D])
    adab = k.din('ada_b_col', [128, DEPTH, 48])
    n1 = k.din('norm1_col', [128, DEPTH, 8])
    n2 = k.din('norm2_col', [128, DEPTH, 8])
    P['mod'] = sbp.t([128, DEPTH, 48, 2], F32)
    P['A1'] = sbp.t([128, DEPTH, 8, 2], F32)
    P['A2'] = sbp.t([128, DEPTH, 8, 2], F32)
    cact = sbp.t([128, 8, 2], F32)
    adab_sb = sbp.t([128, DEPTH, 48], F32)
    n1_sb = sbp.t([128, DEPTH, 8], F32)
    n2_sb = sbp.t([128, DEPTH, 8], F32)
    k.dma('sp', [], ['cact'], cact, ccol[:, :, :])
    k.dma('sp', [], ['adab'], adab_sb, adab[:, :, :])
    k.dma('sp', [], ['n1'], n1_sb, n1[:, :, :])
    k.dma('sp', [], ['n2'], n2_sb, n2[:, :, :])
    k.act(['cact'], ['cact'], cact, cact, AF.Silu)
    sbl = SB(nc, base=sbp.off)
    stg = [sbl.t([128, 8, 512], F32) for _ in range(2)]
    for l in range(DEPTH):
        wv = adaw[l].rearrange("(k p) n -> p k n", p=128)
        pm = k.ps[0][:, 0:96].rearrange("p (c j) -> p c j", j=2)
        for cg in range(12):
            s = stg[cg % 2]
            sk = 'adastg%d' % (cg % 2)
            k.dma('sp', [], [sk], s, wv[:, :, cg * 512:(cg + 1) * 512])
            for j in range(4):
                c = cg * 4 + j
                for kk in range(8):
                    k.mm([sk, 'cact'], ['ps0'], pm[:, c, :], s[:, kk, j * 128:(j + 1) * 128], cact[:, kk, :],
                         start=(kk == 0), stop=(kk == 7))
        k.tt(['ps0', 'adab'], ['mod'], P['mod'][:, l], pm, adab_sb[:, l, :].unsqueeze(2).to_broadcast([128, 48, 2]), ALU.add)
        for (Aname, nsb, c0) in (('A1', n1_sb, 8), ('A2', n2_sb, 32)):
            k.ts(['mod'], [Aname], P[Aname][:, l], P['mod'][:, l, c0:c0 + 8, :], 1.0, op0=ALU.add)
            k.tt([Aname, 'n1', 'n2'], [Aname], P[Aname][:, l], P[Aname][:, l],
                 nsb[:, l, :].unsqueeze(2).to_broadcast([128, 8, 2]), ALU.mult)
    k.em.barrier()


def phaseA(k, P, l, base, xres):
    nc = k.nc
    sb = SB(nc, base=base)
    wfm = k.dram['w_in_fm']
    wtm = k.dram['w_in_tm']
    UT, QKT, TM = k.dram['UT'], k.dram['QKT'], k.dram['TM']
    Wfm = sb.t([128, 8, NFM], BF16)
    Wtm = sb.t([128, 8, NTM], BF16)
    bfm = sb.t([128, 18], F32)
    btm = sb.t([128, NTM], F32)
    cosT = sb.t([128, L], F32)
    sinT = sb.t([128, L], F32)
    normacc = sb.t([2, 8], F32)
    stg = [sb.t([128, 8, 512], F32) for _ in range(2)]
    k.dma('sp', [], ['bfm'], bfm, k.dram['b_fm_col'][:, l, :])
    k.dma('sp', [], ['btm'], btm, k.dram['b_tm_bc'][l])
    k.dma('sp', [], ['cosT'], cosT, k.dram['cosT'][:, :])
    k.dma('sp', [], ['sinT'], sinT, k.dram['sinT'][:, :])
    k.memset([], ['normacc'], normacc, 0.0)
    ci = 0
    for (src, dst, ncol, key) in ((wfm, Wfm, NFM, 'Wfm'), (wtm, Wtm, NTM, 'Wtm')):
        wv = src[l].rearrange("(k p) n -> p k n", p=128)
        for c0 in range(0, ncol, 512):
            c1 = min(ncol, c0 + 512)
            s = stg[ci % 2]
            sk = 'wstg%d' % (ci % 2)
            k.dma('sp', [], [sk], s[:, :, 0:c1 - c0], wv[:, :, c0:c1])
            k.cp([sk], [key], dst[:, :, c0:c1], s[:, :, 0:c1 - c0], eng=('dve' if ci % 2 == 0 else 'pool'))
            ci += 1
    xt = [sb.t([128, D], F32) for _ in range(2)]
    junk = sb.t([128, D], BF16)
    xn = [sb.t([128, D], BF16) for _ in range(2)]
    ss = [sb.t([128, 2], F32) for _ in range(2)]
    hT = [sb.t([128, 8, 512], BF16) for _ in range(2)]
    fmst = [sb.t([128, 512], F32) for _ in range(3)]
    qbf = [sb.t([128, 512], BF16) for _ in range(2)]
    t2 = [sb.t([128, 512], F32) for _ in range(2)]
    obf = [sb.t([128, 512], BF16) for _ in range(2)]
    sqbf = [sb.t([128, 512], BF16) for _ in range(2)]
    nmx = sb.t([2, 2], F32)
    tmst = [sb.t([128, NTM], F32) for _ in range(2)]
    A1, SH1 = P['A1'], P['mod']
    psi = 0
    tile_ctr = 0
    fm_ctr = 0
    rp_ctr = 0
    for g in range(9):
        t0 = g * 512
        ntok = 512 if g < 8 else 256
        j = 0 if g < 8 else 1
        hb = g % 2
        hk = 'hT%d' % hb
        for tl in range(ntok // 128):
            ti = t0 // 128 + tl
            xb = tile_ctr % 2
            tile_ctr += 1
            xk, nk, sk = 'xt%d' % xb, 'xn%d' % xb, 'ss%d' % xb
            k.dma('sp', [], [xk], xt[xb], xres[ti * 128:(ti + 1) * 128, :])
            k.memset([], [sk], ss[xb], 0.0)
            k.act([xk, sk], ['junk', sk], junk, xt[xb], AF.Square, accum_out=ss[xb][:, 0:1])
            k.ts([sk], [sk], ss[xb][:, 1:2], ss[xb][:, 0:1], 1.0 / D, EPS, op0=ALU.mult, op1=ALU.add)
            k.act([sk], [sk], ss[xb][:, 1:2], ss[xb][:, 1:2], AF.Sqrt)
            k.recip([sk], [sk], ss[xb][:, 1:2], ss[xb][:, 1:2])
            k.ts([xk, sk], [nk], xn[xb], xt[xb], ss[xb][:, 1:2], op0=ALU.mult)
            pk = 'ps%d' % psi
            pst = k.ps[psi][:].bitcast(BF16)
            psi = (psi + 1) % 8
            for kk in range(8):
                k.tr([nk, 'identbf'], [pk], pst[:, kk * 128:(kk + 1) * 128], xn[xb][:, kk * 128:(kk + 1) * 128], P['identbf'])
            for kk in range(8):
                k.act([pk, 'A1', 'mod'], [hk], hT[hb][:, kk, tl * 128:(tl + 1) * 128], pst[:, kk * 128:(kk + 1) * 128],
                      AF.Identity, scale=A1[:, l, kk, j:j + 1], bias=SH1[:, l, kk, j:j + 1])
        for jc in range(18):
            pk = 'ps%d' % psi
            pp = k.ps[psi]
            psi = (psi + 1) % 8
            for kk in range(8):
                k.mm(['Wfm', hk], [pk], pp[:, 0:ntok], Wfm[:, kk, jc * 128:(jc + 1) * 128], hT[hb][:, kk, 0:ntok],
                     start=(kk == 0), stop=(kk == 7))
            fb = fm_ctr % 3
            fm_ctr += 1
            fk = 'fmst%d' % fb
            k.act([pk, 'bfm'], [fk], fmst[fb][:, 0:ntok], pp[:, 0:ntok], AF.Identity, bias=bfm[:, jc:jc + 1], scale=1.0)
            if jc < 10:
                k.dma('pool', [fk], ['UT'], UT[jc * 128:(jc + 1) * 128, t0:t0 + ntok], fmst[fb][:, 0:ntok])
                continue
            rb = rp_ctr % 2
            rp_ctr += 1
            ok_, sqk = 'obf%d' % rb, 'sqbf%d' % rb
            if j == 0:
                qk_, tk_ = 'qbf%d' % rb, 't2%d' % rb
                k.cp([fk], [qk_], qbf[rb][:, 0:ntok], fmst[fb][:, 0:ntok], eng='pool')
                pk2 = 'ps%d' % psi
                pp2 = k.ps[psi]
                psi = (psi + 1) % 8
                k.mm(['ropeRbf', qk_], [pk2], pp2[:, 0:ntok], P['ropeRbf'], qbf[rb][:, 0:ntok])
                k.tt([pk2, 'sinT'], [tk_], t2[rb][:, 0:ntok], pp2[:, 0:ntok], sinT[:, t0:t0 + ntok], ALU.mult)
                k.tt([fk, 'cosT'], [fk], fmst[fb][:, 0:ntok], fmst[fb][:, 0:ntok], cosT[:, t0:t0 + ntok], ALU.mult, eng='pool')
                k.tt([fk, tk_], [fk], fmst[fb][:, 0:ntok], fmst[fb][:, 0:ntok], t2[rb][:, 0:ntok], ALU.add)
            k.cp([fk], [ok_], obf[rb][:, 0:ntok], fmst[fb][:, 0:ntok], eng='pool')
            k.dma('pool', [ok_], ['QKT'], QKT[(jc - 10) * 128:(jc - 9) * 128, t0:t0 + ntok], obf[rb][:, 0:ntok])
            k.act([fk], [sqk], sqbf[rb][:, 0:ntok], fmst[fb][:, 0:ntok], AF.Square)
            pk3 = 'ps%d' % psi
            pp3 = k.ps[psi]
            psi = (psi + 1) % 8
            k.mm(['blockbf', sqk], [pk3], pp3[0:2, 0:ntok], P['blockbf'], sqbf[rb][:, 0:ntok])
            k.red([pk3], ['nmx'], nmx[:, 0:1], pp3[0:2, 0:ntok], ALU.max)
            k.tt(['nmx', 'normacc'], ['normacc'], normacc[:, jc - 10:jc - 9], normacc[:, jc - 10:jc - 9], nmx[:, 0:1], ALU.max)
        for tl in range(ntok // 128):
            ti = t0 // 128 + tl
            tb = ti % 2
            tk = 'tmst%d' % tb
            for (c0, c1) in ((0, 512), (512, 1024), (1024, NTM)):
                pk = 'ps%d' % psi
                pp = k.ps[psi]
                psi = (psi + 1) % 8
                for kk in range(8):
                    k.mm(['Wtm', hk], [pk], pp[:, 0:c1 - c0], hT[hb][:, kk, tl * 128:(tl + 1) * 128], Wtm[:, kk, c0:c1],
                         start=(kk == 0), stop=(kk == 7))
                k.tt([pk, 'btm'], [tk], tmst[tb][:, c0:c1], pp[:, 0:c1 - c0], btm[:, c0:c1], ALU.add)
            k.dma('pool', [tk], ['TM'], TM[ti * 128:(ti + 1) * 128, :], tmst[tb])
    cn = sb.t([2, 4], F32)
    k.tt(['normacc'], ['cn'], cn, normacc[:, 0:4], normacc[:, 4:8], ALU.mult)
    k.act(['cn'], ['cn'], cn, cn, AF.Sqrt)
    k.ts(['cn'], ['cn'], cn, cn, -1.05 * 0.125, op0=ALU.mult)
    for m in range(2):
        pk = 'ps%d' % psi
        pp = k.ps[psi]
        psi = (psi + 1) % 8
        k.mm(['sel2', 'cn'], [pk], pp[:, 0:4], P['sel2'][:, m, :], cn)
        k.cp([pk], ['negc'], P['negc'][:, l, m, :], pp[:, 0:4])
    k.em.barrier()


def phaseB1(k, P, l, base, last):
    nc = k.nc
    sb = SB(nc, base=base)
    QKT, TM, YT = k.dram['QKT'], k.dram['TM'], k.dram['YT']
    lam_init = 0.8 - 0.6 * math.exp(-0.3 * l)
    QT = sb.t([128, 4, T], BF16)
    KTz = [sb.t([128, 4, T], BF16) for _ in range(2)]
    V = sb.t([128, NT, 512], BF16)
    vst = [sb.t([128, 512], F32) for _ in range(2)]
    k.dma('sp', [], ['QT'], QT, QKT[0:512, :].rearrange("(c p) t -> p c t", p=128))
    kv = QKT[512:1024, :].rearrange("(c p) t -> p c t", p=128)
    for m in range(2):
        lo, hi = m * 64, (m + 1) * 64
        zl, zh = (1 - m) * 64, (2 - m) * 64
        k.dma('sp', [], ['KT'], KTz[m][lo:hi], kv[lo:hi])
        k.memset([], ['KT'], KTz[m][zl:zh], 0.0, eng=('dve' if m == 0 else 'pool'))
    for ti in range(NT):
        vb = ti % 2
        vk = 'vst%d' % vb
        k.dma('sp', [], [vk], vst[vb], TM[ti * 128:(ti + 1) * 128, 512:1024])
        k.cp([vk], ['V'], V[:, ti, :], vst[vb], eng=('dve' if ti % 2 == 0 else 'pool'))
    lt = sb.t([1, 256], F32)
    lw = sb.t([1, 8], F32)
    neglam = sb.t([128, 1], F32)
    wsc = sb.t([128, 1], F32)
    k.dma('sp', [], ['lt'], lt, k.dram['da_lambda'][l:l + 1, :])
    k.dma('sp', [], ['wsc'], wsc, k.dram['da_subln_col'][l])
    k.ts(['wsc'], ['wsc'], wsc, wsc, 1.0 - lam_init, op0=ALU.mult)
    k.tt(['lt'], ['lt'], lt[:, 0:64], lt[:, 0:64], lt[:, 64:128], ALU.mult)
    k.tt(['lt'], ['lt'], lt[:, 128:192], lt[:, 128:192], lt[:, 192:256], ALU.mult)
    k.red(['lt'], ['lw'], lw[:, 0:1], lt[:, 0:64], ALU.add)
    k.red(['lt', 'lw'], ['lw'], lw[:, 1:2], lt[:, 128:192], ALU.add)
    k.act(['lw'], ['lw'], lw[:, 0:2], lw[:, 0:2], AF.Exp)
    k.tt(['lw'], ['lw'], lw[:, 2:3], lw[:, 1:2], lw[:, 0:1], ALU.subtract)
    k.ts(['lw'], ['lw'], lw[:, 2:3], lw[:, 2:3], -lam_init, op0=ALU.add)
    k.mm(['ones32', 'lw'], ['ps7'], k.ps[7][:, 0:1], P['ones32'][0:1, :], lw[:, 2:3])
    k.cp(['ps7'], ['neglam'], neglam, k.ps[7][:, 0:1])

    pt = [sb.t([128, 512], BF16) for _ in range(4)]
    racc = [sb.t([128, 512], F32) for _ in range(2)]
    rec = [sb.t([128, 512], F32) for _ in range(2)]
    o0 = sb.t([128, 512], F32)
    o1 = sb.t([128, 512], F32)
    sq = sb.t([128, 512], BF16)
    ybf = [sb.t([128, 512], BF16) for _ in range(2)]
    pti = 0
    si = 0
    si_box = [0]
    yi = 0
    chunks = [(g * 512, 512, list(range(NT))) for g in range(8)]
    if not last:
        chunks.append((L, CTX, [32, 33]))
    for h in range(4):
        for (q0, nq, blocks) in chunks:
            units = [(bi, kb, m) for bi, kb in enumerate(blocks) for m in range(2)]
            LOOK = 3
            issued = {}

            def issue_s(u):
                bi, kb, m = units[u]
                nonlocal_si = si_box[0]
                si_box[0] += 1
                psk = 'ps%d' % (4 + nonlocal_si % 4)
                pss = k.ps[4 + nonlocal_si % 4]
                k.mm(['KT', 'QT'], [psk], pss[:, 0:nq], KTz[m][:, h, kb * 128:(kb + 1) * 128], QT[:, h, q0:q0 + nq])
                issued[u] = (psk, pss)

            for u in range(min(LOOK, len(units))):
                issue_s(u)
            for u, (bi, kb, m) in enumerate(units):
                psk, pss = issued.pop(u)
                pk_ = 'pt%d' % (pti % 4)
                ptt = pt[pti % 4]
                pti += 1
                k.act([psk, 'negc'], [pk_], ptt[:, 0:nq], pss[:, 0:nq], AF.Exp, scale=0.125, bias=P['negc'][:, l, m, h:h + 1])
                if u + LOOK < len(units):
                    issue_s(u + LOOK)
                st, sp_ = (bi == 0), (bi == len(blocks) - 1)
                k.mm(['V', pk_], ['ps%d' % m], k.ps[m][:, 0:nq], V[:, kb, h * 128:(h + 1) * 128], ptt[:, 0:nq], start=st, stop=sp_)
                reng = 'dve' if m == 0 else 'pool'
                if st:
                    k.cp([pk_], ['racc%d' % m], racc[m][:, 0:nq], ptt[:, 0:nq], eng=reng)
                else:
                    k.tt([pk_, 'racc%d' % m], ['racc%d' % m], racc[m][:, 0:nq], racc[m][:, 0:nq], ptt[:, 0:nq], ALU.add, eng=reng)
            si = si_box[0]
            for m in range(2):
                k.mm(['ones32', 'racc%d' % m], ['ps%d' % (2 + m)], k.ps[2 + m][:, 0:nq], P['ones32'], racc[m][:, 0:nq])
            k.recip(['ps2'], ['rec0'], rec[0][:, 0:nq], k.ps[2][:, 0:nq])
            k.recip(['ps3'], ['rec1'], rec[1][:, 0:nq], k.ps[3][:, 0:nq])
            k.tt(['ps0', 'rec0'], ['o0'], o0[:, 0:nq], k.ps[0][:, 0:nq], rec[0][:, 0:nq], ALU.mult)
            k.tt(['ps1', 'rec1'], ['o1'], o1[:, 0:nq], k.ps[1][:, 0:nq], rec[1][:, 0:nq], ALU.mult)
            k.stt(['o0', 'o1', 'neglam'], ['o0'], o0[:, 0:nq], o1[:, 0:nq], neglam[:, 0:1], o0[:, 0:nq], ALU.mult, ALU.add)
            k.act(['o0'], ['sq'], sq[:, 0:nq], o0[:, 0:nq], AF.Square)
            psk = 'ps%d' % (4 + si % 4)
            pss = k.ps[4 + si % 4]
            si += 1
            si_box[0] = si
            k.mm(['onesbf', 'sq'], [psk], pss[:, 0:nq], P['onesbf'], sq[:, 0:nq])
            k.ts([psk], ['rec0'], rec[0][:, 0:nq], pss[:, 0:nq], 1.0 / 128.0, EPS, op0=ALU.mult, op1=ALU.add)
            k.act(['rec0'], ['rec0'], rec[0][:, 0:nq], rec[0][:, 0:nq], AF.Sqrt)
            k.recip(['rec0'], ['rec0'], rec[0][:, 0:nq], rec[0][:, 0:nq])
            k.tt(['o0', 'rec0'], ['o0'], o0[:, 0:nq], o0[:, 0:nq], rec[0][:, 0:nq], ALU.mult)
            yk = 'ybf%d' % (yi % 2)
            yb = ybf[yi % 2]
            yi += 1
            k.ts(['o0', 'wsc'], [yk], yb[:, 0:nq], o0[:, 0:nq], wsc[:, 0:1], op0=ALU.mult)
            k.dma('pool', [yk], ['YT'], YT[512 + h * 128:512 + (h + 1) * 128, q0:q0 + nq], yb[:, 0:nq])
    k.em.barrier()


NCH = T // 64


def phaseB2(k, P, l, base, last):
    nc = k.nc
    UT, TM, YT = k.dram['UT'], k.dram['TM'], k.dram['YT']
    sb0 = SB(nc, base=base)
    tri32 = sb0.t([64, 2, 64], F32)
    tribf = sb0.t([64, 2, 64], BF16)
    cw = sb0.t([64, 8, 4], F32)
    wbc = sb0.t([64, 256], F32)
    G = sb0.t([64, NCH, 16], F32)
    LF = sb0.t([64, 8, NCH], F32)
    IG = sb0.t([64, 8, NCH], F32)
    BB = sb0.t([64, 8, NCH], F32)
    BT = sb0.t([64, 8, NCH], F32)
    EB = sb0.t([64, 8, NCH], F32)
    WS = sb0.t([64, 8, NCH], F32)
    W2 = sb0.t([64, 8, NCH], F32)
    EBT = sb0.t([64, 8, NCH], F32)
    k.dma('sp', [], ['tri32'], tri32, k.dram['tri'].rearrange("a s t -> s a t"))
    k.cp(['tri32'], ['tribf'], tribf, tri32)
    k.dma('sp', [], ['cw'], cw, k.dram['ml_conv_col'][l])
    k.dma('sp', [], ['wbc'], wbc, k.dram['ml_norm_bc'][l])
    k.dma('sp', [], ['G'], G, TM[:, 1024:1040].rearrange("(n p) c -> p n c", p=64))
    for d in range(2):
        gi = G[:, :, d * 8:d * 8 + 4].rearrange("p n h -> p h n")
        gf = G[:, :, d * 8 + 4:d * 8 + 8].rearrange("p n h -> p h n")
        k.cp(['G'], ['IG'], IG[:, d * 4:d * 4 + 4, :], gi)
        k.act(['G'], ['LF'], LF[:, d * 4:d * 4 + 4, :], gf, AF.Exp, scale=-1.0)
    k.act(['LF'], ['LF'], LF, LF, AF.Ln, bias=1.0, scale=1.0)
    k.ts(['LF'], ['LF'], LF, LF, -1.0, op0=ALU.mult)
    for d in range(2):
        rhs = LF[:, d * 4:d * 4 + 4, :]
        k.mm(['tri32', 'LF'], ['ps0'], k.ps[0][0:64, 0:4 * NCH], tri32[:, d, :], rhs)
        k.cp(['ps0'], ['BB'], BB[:, d * 4:d * 4 + 4, :], k.ps[0][0:64, 0:4 * NCH].rearrange("p (h n) -> p h n", n=NCH))
        k.mm(['ones32', 'LF'], ['ps1'], k.ps[1][0:64, 0:4 * NCH], P['ones32'][0:64, 0:64], rhs)
        k.cp(['ps1'], ['BT'], BT[:, d * 4:d * 4 + 4, :], k.ps[1][0:64, 0:4 * NCH].rearrange("p (h n) -> p h n", n=NCH))
    k.act(['BB'], ['EB'], EB, BB, AF.Exp)
    k.act(['BT'], ['EBT'], EBT, BT, AF.Exp)
    k.tt(['IG', 'BB'], ['WS'], WS, IG, BB, ALU.subtract)
    k.tt(['WS', 'BT'], ['W2'], W2, WS, BT, ALU.add)
    k.act(['WS'], ['WS'], WS, WS, AF.Exp)
    k.act(['W2'], ['W2'], W2, W2, AF.Exp)
    base1 = sb0.off
    orders = [[64, 65, 66, 67] + list(range(64)), [67, 66, 65, 64] + list(range(63, -1, -1))]
    psi = [2]

    def nps():
        i = psi[0]
        psi[0] = 2 + (psi[0] - 1) % 6
        return 'ps%d' % i, k.ps[i]

    for hp in range(2):
        sb = SB(nc, base=base1)
        qT = sb.t([64, 2, T], BF16)
        kT = sb.t([64, 2, T], BF16)
        ktm = sb.t([64, NCH, 128], BF16)
        vaug = sb.t([64, NCH, 2, 65], BF16)
        hsum = sb.t([64, NCH, 128], F32)
        ra = sb.t([64, 2 * T], F32)
        raw = ra[:, 0:T]
        acc = ra[:, T:2 * T]
        k.memset([], ['hsum'], hsum, 0.0, eng='pool')
        k.memset([], ['vaug'], vaug, 1.0, eng='pool')
        for qk in range(2):
            for hl in range(2):
                hh = qk * 4 + hp * 2 + hl
                k.dma('sp', [], ['raw'], raw, UT[768 + hh * 64:768 + (hh + 1) * 64, :])
                k.ts(['raw', 'cw'], ['acc'], acc, raw, cw[:, hh, 1:2], cw[:, hh, 3:4], op0=ALU.mult, op1=ALU.add)
                for (a, b) in ((0, L), (L, T)):
                    k.stt(['raw', 'cw', 'acc'], ['acc'], acc[:, a + 1:b], raw[:, a:b - 1], cw[:, hh, 0:1], acc[:, a + 1:b], ALU.mult, ALU.add)
                    k.stt(['raw', 'cw', 'acc'], ['acc'], acc[:, a:b - 1], raw[:, a + 1:b], cw[:, hh, 2:3], acc[:, a:b - 1], ALU.mult, ALU.add)
                if qk == 0:
                    k.act(['acc'], ['qT'], qT[:, hl, :], acc, AF.Silu)
                else:
                    k.act(['acc'], ['acc'], acc, acc, AF.Silu)
                    k.ts(['acc'], ['kT'], kT[:, hl, :], acc, 0.125, op0=ALU.mult)
        for n0 in range(0, NCH, 4):
            pk, pp = nps()
            ppb = pp[:].bitcast(BF16)
            for dn in range(4):
                for hl in range(2):
                    k.tr(['kT', 'identbf'], [pk], ppb[0:64, (dn * 2 + hl) * 64:(dn * 2 + hl + 1) * 64],
                         kT[:, hl, (n0 + dn) * 64:(n0 + dn + 1) * 64], P['identbf'][0:64, 0:64])
            k.cp([pk], ['ktm'], ktm[:, n0:n0 + 4, :], ppb[0:64, 0:512].rearrange("p (n c) -> p n c", c=128))
        vst = acc[:, 0:17 * 128].rearrange("p (n c) -> p n c", c=128)
        for n0 in range(0, NCH, 17):
            k.dma('sp', ['acc'], ['acc'], vst, TM[n0 * 64:(n0 + 17) * 64, hp * 128:(hp + 1) * 128].rearrange("(n p) c -> p n c", p=64))
            k.cp(['acc'], ['vaug'], vaug[:, n0:n0 + 17, :, 0:64], vst.rearrange("p n (h e) -> p n h e", e=64))
        Cst = [[sb.t([64, 65], F32) for _ in range(2)] for _ in range(2)]
        Cbf = [[sb.t([64, 65], BF16) for _ in range(2)] for _ in range(2)]
        dg = [sb.t([64, 64], BF16) for _ in range(4)]
        meb = [sb.t([64, 64], F32) for _ in range(4)]
        pT = [sb.t([64, 64], BF16) for _ in range(4)]
        rsb = [sb.t([64, 65], F32) for _ in range(4)]
        tot = [sb.t([64, 66], F32) for _ in range(4)]
        wv = [sb.t([64, 65], BF16) for _ in range(4)]
        for d in range(2):
            for hl in range(2):
                k.memset([], ['Cst%d%d' % (d, hl)], Cst[d][hl], 0.0)
                k.memset([], ['Cbf%d%d' % (d, hl)], Cbf[d][hl], 0.0)
        def unit(step, d, hl):
            n = orders[d][step]
            c0 = n * 64
            need_out = (n < 64) or (not last)
            u = d * 2 + hl
            dh = d * 4 + hp * 2 + hl
            ck, cbk = 'Cst%d%d' % (d, hl), 'Cbf%d%d' % (d, hl)
            upd = step != NCH - 1
            kA, kB = 'ps%d' % (2 * u), 'ps%d' % (2 * u + 1)
            bA, bB = k.ps[2 * u], k.ps[2 * u + 1]
            if upd:
                k.act(['vaug', 'W2'], ['wv%d' % u], wv[u], vaug[:, n, hl, :], AF.Identity, scale=W2[:, dh, n:n + 1])
            if need_out:
                k.act(['identbf', 'EB'], ['dg%d' % u], dg[u], P['identbf'][0:64, 0:64], AF.Identity, scale=EB[:, dh, n:n + 1])
            yield
            if upd:
                k.mm(['ktm', 'wv%d' % u], [kB], bB[0:64, 0:65], ktm[:, n, hl * 64:(hl + 1) * 64], wv[u])
            if need_out:
                k.mm(['tribf', 'dg%d' % u], [kA], bA[0:64, 0:64], tribf[:, 1 - d, :], dg[u])
            yield
            if upd:
                k.stt([ck, 'EBT', kB], [ck], Cst[d][hl], Cst[d][hl], EBT[:, dh, n:n + 1], bB[0:64, 0:65], ALU.mult, ALU.add)
            if need_out:
                k.cp([kA], ['meb%d' % u], meb[u], bA[0:64, 0:64], eng='act')
            yield
            if need_out:
                k.mm(['kT', 'qT'], [kA], bA[0:64, 0:64], kT[:, hl, c0:c0 + 64], qT[:, hl, c0:c0 + 64])
                k.mm(['qT', cbk], [kB], bB[0:64, 0:65], qT[:, hl, c0:c0 + 64], Cbf[d][hl])
            yield
            if need_out:
                k.act([kB, 'EB'], ['rsb%d' % u], rsb[u], bB[0:64, 0:65], AF.Identity, scale=EB[:, dh, n:n + 1])
                k.stt([kA, 'WS', 'meb%d' % u], ['pT%d' % u], pT[u], bA[0:64, 0:64], WS[:, dh, n:n + 1], meb[u], ALU.mult, ALU.mult)
            if upd:
                k.cp([ck], [cbk], Cbf[d][hl], Cst[d][hl], eng='act')
            yield
            if not need_out:
                return
            k.mm(['pT%d' % u, 'vaug'], [kA], bA[0:64, 0:65], pT[u], vaug[:, n, hl, :])
            yield
            k.tt([kA, 'rsb%d' % u], ['tot%d' % u], tot[u][:, 0:65], bA[0:64, 0:65], rsb[u], ALU.add)
            yield
            k.act(['tot%d' % u], ['tot%d' % u], tot[u][:, 65:66], tot[u][:, 64:65], AF.Abs)
            yield
            k.ts(['tot%d' % u], ['tot%d' % u], tot[u][:, 65:66], tot[u][:, 65:66], 1.0, op0=ALU.max)
            yield
            k.recip(['tot%d' % u], ['tot%d' % u], tot[u][:, 65:66], tot[u][:, 65:66])
            yield
            hs = hsum[:, n, hl * 64:(hl + 1) * 64]
            k.stt(['tot%d' % u, 'hsum'], ['hsum'], hs, tot[u][:, 0:64], tot[u][:, 65:66], hs, ALU.mult, ALU.add)

        for step in range(NCH):
            gens = [unit(step, d, hl) for d in range(2) for hl in range(2)]
            while gens:
                alive = []
                for g_ in gens:
                    try:
                        next(g_)
                        alive.append(g_)
                    except StopIteration:
                        pass
                gens = alive
        nout = 64 if last else NCH
        ssum = sb.t([64, NCH * 2], F32)
        ybf = sb.t([64, NCH, 128], BF16)
        ytb = [sb.t([128, 512], BF16) for _ in range(2)]
        k.act(['hsum', 'raw', 'acc'], ['acc', 'raw'], ra, hsum.rearrange("p n c -> p (n c)"), AF.Square)
        k.red(['acc', 'raw'], ['ssum'], ssum, ra.rearrange("p (g e) -> p g e", e=64), ALU.add)
        k.ts(['ssum'], ['ssum'], ssum, ssum, 1.0 / 64.0, EPS, op0=ALU.mult, op1=ALU.add)
        k.act(['ssum'], ['ssum'], ssum, ssum, AF.Sqrt)
        k.recip(['ssum'], ['ssum'], ssum, ssum)
        hv = hsum.rearrange("p n (h e) -> p (n h) e", e=64)
        k.tt(['hsum', 'ssum'], ['hsum'], hv, hv, ssum.unsqueeze(2).to_broadcast([64, NCH * 2, 64]), ALU.mult)
        k.tt(['hsum', 'wbc'], ['hsum'], hsum, hsum, wbc[:, hp * 128:(hp + 1) * 128].unsqueeze(1).to_broadcast([64, NCH, 128]), ALU.mult)
        ost = acc[:, 0:17 * 128].rearrange("p (n c) -> p n c", c=128)
        for n0 in range(0, NCH, 17):
            k.dma('sp', ['acc'], ['acc'], ost, TM[n0 * 64:(n0 + 17) * 64, 256 + hp * 128:256 + (hp + 1) * 128].rearrange("(n p) c -> p n c", p=64))
            k.act(['acc'], ['acc'], ost, ost, AF.Sigmoid)
            k.tt(['acc', 'hsum'], ['ybf'], ybf[:, n0:n0 + 17, :], hsum[:, n0:n0 + 17, :], ost, ALU.mult)
        for gi, n0 in enumerate(range(0, nout, 8)):
            nn = min(8, nout - n0)
            pk, pp = nps()
            ppb = pp[:].bitcast(BF16)
            for dn in range(nn):
                k.tr(['ybf', 'identbf'], [pk], ppb[:, dn * 64:(dn + 1) * 64], ybf[:, n0 + dn, :], P['identbf'][0:64, 0:64])
            yk = 'ytb%d' % (gi % 2)
            k.cp([pk], [yk], ytb[gi % 2][:, 0:nn * 64], ppb[:, 0:nn * 64])
            k.dma('pool', [yk], ['YT'], YT[256 + hp * 128:256 + (hp + 1) * 128, n0 * 64:(n0 + nn) * 64], ytb[gi % 2][:, 0:nn * 64])
        k.em.barrier()


HC = 32
KB = 256 // HC
NB6 = 512 // HC
NHB = 256 // HC
PI = math.pi
HSTOP = [99]


def hyena_consts():
    c = {}
    f32 = np.float32

    def zfeat(Lx, pos):
        t = np.linspace(0.0, 1.0, Lx, dtype=f32)[pos][:, None]
        w = ((2.0 * math.pi / Lx) * np.arange(Lx, dtype=f32))[pos][:, None]
        f = np.linspace(1e-4, 15.0, 16, dtype=f32)[None, :]
        z = np.concatenate([t, np.cos(f * w), -np.sin(f * w)], axis=-1).astype(f32)
        return z, t
    deltas = np.abs(np.linspace(math.log(1e-2) / 1.5, math.log(1e-2) / 0.3, 256, dtype=f32)).astype(f32)
    z, t = zfeat(L, np.arange(L))
    c['hy_z'] = np.ascontiguousarray(z.T)
    c['hy_decay'] = np.ascontiguousarray(np.exp(-t * deltas[None, :]).T.astype(f32))
    pos = np.concatenate([np.arange(CTX - 1, 0, -1), np.arange(CTX)])
    zc, tc = zfeat(CTX, pos)
    c['hy_zc'] = np.ascontiguousarray(zc.T)
    c['hy_decayc'] = np.ascontiguousarray(np.exp(-tc * deltas[None, :]).T.astype(f32))
    n1 = np.arange(32)[:, None]
    k1 = np.arange(64)[None, :]
    a = 2 * np.pi * n1 * k1 / 64.0
    c['hy_F1'] = np.concatenate([np.cos(a), -np.sin(a)], 1).astype(f32)
    n2 = np.arange(128)[:, None, None]
    kk = (np.arange(64)[None, :, None] + 64 * np.arange(128)[None, None, :])
    a = 2 * np.pi * ((n2 * kk) % 8192) / 8192.0
    c['hy_Gr'] = np.cos(a).astype(f32).reshape(128, 8192)
    c['hy_Gi'] = (-np.sin(a)).astype(f32).reshape(128, 8192)
    k2 = np.arange(128)[:, None]
    nn = np.arange(128)[None, :]
    a = 2 * np.pi * ((k2 * nn) % 128) / 128.0
    c['hy_E1'] = np.concatenate([np.cos(a), np.sin(a)], 1).astype(f32)
    c['hy_E2'] = np.concatenate([-np.sin(a), np.cos(a)], 1).astype(f32)
    k1 = np.arange(64)[:, None, None]
    nfull = np.arange(128)[None, :, None] + 128 * np.arange(32)[None, None, :]
    a = 2 * np.pi * ((k1 * nfull) % 8192) / 8192.0
    c['hy_Mr'] = np.cos(a).astype(f32).reshape(64, 4096)
    c['hy_nMi'] = (-np.sin(a)).astype(f32).reshape(64, 4096)
    return c


def hy_load_bf(k, sb, name, shape, stg, key):
    p, n = shape
    dst = sb.t([p, n], BF16)
    src = k.dram[name]
    step = 2048
    for i, c0 in enumerate(range(0, n, step)):
        c1 = min(n, c0 + step)
        k.dma('sp', [], ['hstg'], stg[0:p, 0:c1 - c0], src[:, c0:c1])
        k.cp(['hstg'], [key], dst[:, c0:c1], stg[0:p, 0:c1 - c0], eng=('dve' if i % 2 == 0 else 'pool'))
    return dst


def fft_fwd(k, C, xbf, A, nAi, psctr, consume):
    F1, Gr, Gi = C['F1'], C['Gr'], C['Gi']
    for c0 in range(0, HC, 4):
        pk, pp = psctr()
        for dc in range(4):
            k.mm(['xbf', 'F1'], [pk], pp[:, dc * 128:(dc + 1) * 128], xbf[:, c0 + dc, :], F1)
        src = pp[:, 0:512].rearrange("p (c r q) -> p c r q", r=2, q=64)
        k.cp([pk], ['A'], A[:, c0:c0 + 4, :, :], src, eng='act')
        k.ts(['A'], ['nAi'], nAi[:, c0:c0 + 4, :], A[:, c0:c0 + 4, 1, :], -1.0, op0=ALU.mult)
    for k0 in range(0, 64, KB):
        pk, pp = psctr()
        for dk in range(KB):
            k1 = k0 + dk
            xr = pp[:, dk * 2 * HC:dk * 2 * HC + HC]
            xi = pp[:, dk * 2 * HC + HC:(dk + 1) * 2 * HC]
            k.mm(['Gr', 'A'], [pk], xr, Gr[:, k1, :], A[:, :, 0, k1], start=True, stop=False)
            k.mm(['Gi', 'nAi'], [pk], xr, Gi[:, k1, :], nAi[:, :, k1], start=False, stop=True)
            k.mm(['Gi', 'A'], [pk], xi, Gi[:, k1, :], A[:, :, 0, k1], start=True, stop=False)
            k.mm(['Gr', 'A'], [pk], xi, Gr[:, k1, :], A[:, :, 1, k1], start=False, stop=True)
        consume(pk, pp, k0)


def phaseH(k, P, l, base, last):
    nc = k.nc
    UT, YT = k.dram['UT'], k.dram['YT']
    HK, HH, CK, UC = k.dram['HK'], k.dram['HH'], k.dram['CK'], k.dram['UC']
    psi = [0]

    def nps():
        i = psi[0]
        psi[0] = (psi[0] + 1) % 8
        return 'ps%d' % i, k.ps[i]

    sb = SB(nc, base=base)
    w1 = sb.t([33, 64], F32)
    w2 = sb.t([64, 64], F32)
    w3 = sb.t([64, 1024], F32)
    sc = sb.t([64, 8], F32)
    b3 = sb.t([128, 8], F32)
    k.dma('sp', [], ['w1'], w1, k.dram['hy_filt_w1'][l])
    k.dma('sp', [], ['w2'], w2, k.dram['hy_filt_w2'][l])
    k.dma('sp', [], ['w3'], w3, k.dram['hy_filt_w3'][l])
    k.dma('sp', [], ['sc'], sc[:, 0:3], k.dram['hy_filt_sc'][l])
    k.dma('sp', [], ['b3'], b3, k.dram['hy_b3_col'][l])
    k.tt(['sc'], ['sc'], sc[:, 3:4], sc[:, 0:1], sc[:, 1:2], ALU.mult)
    k.tt(['sc'], ['sc'], sc[:, 4:5], sc[:, 0:1], sc[:, 2:3], ALU.mult)
    zT = sb.t([33, L], F32)
    h2 = sb.t([64, L], F32)
    h1 = sb.t([64, 512], F32)
    m1 = sb.t([64, 512], F32)
    m2 = sb.t([64, 512], F32)

    def sin_layer(src_ap, wt, kdim, bcol, dst_ap, n):
        pk, pp = nps()
        k.mm(['w1', 'w2', 'zT', 'h1'], [pk], pp[0:64, 0:n], wt, src_ap)
        k.ts([pk, 'sc'], ['m0'], dst_ap, pp[0:64, 0:n], sc[:, 0:1], sc[:, bcol:bcol + 1], op0=ALU.mult, op1=ALU.add)
        k.ts(['m0'], ['m1'], m1[:, 0:n], dst_ap, PI, -2.0 * PI, op0=ALU.is_gt, op1=ALU.mult)
        k.ts(['m0'], ['m2'], m2[:, 0:n], dst_ap, -PI, 2.0 * PI, op0=ALU.is_lt, op1=ALU.mult, eng='pool')
        k.tt(['m1', 'm2'], ['m1'], m1[:, 0:n], m1[:, 0:n], m2[:, 0:n], ALU.add)
        k.tt(['m0', 'm1'], ['m0'], dst_ap, dst_ap, m1[:, 0:n], ALU.add)
        k.act(['m0'], ['m0'], dst_ap, dst_ap, AF.Sin)

    def mlp(zsrc_name, ncols, h2dst):
        k.dma('sp', ['zT'], ['zT'], zT[:, 0:ncols], k.dram[zsrc_name][:, :])
        for c0 in range(0, ncols, 512):
            n = min(512, ncols - c0)
            sin_layer(zT[:, c0:c0 + n], w1, 33, 3, h1[:, 0:n], n)
            sin_layer2(c0, n, h2dst)

    def sin_layer2(c0, n, h2dst):
        pk, pp = nps()
        k.mm(['w2', 'm0'], [pk], pp[0:64, 0:n], w2, h1[:, 0:n])
        d = h2dst[:, c0:c0 + n]
        k.ts([pk, 'sc'], ['h2'], d, pp[0:64, 0:n], sc[:, 0:1], sc[:, 4:5], op0=ALU.mult, op1=ALU.add)
        k.ts(['h2'], ['m1'], m1[:, 0:n], d, PI, -2.0 * PI, op0=ALU.is_gt, op1=ALU.mult)
        k.ts(['h2'], ['m2'], m2[:, 0:n], d, -PI, 2.0 * PI, op0=ALU.is_lt, op1=ALU.mult, eng='pool')
        k.tt(['m1', 'm2'], ['m1'], m1[:, 0:n], m1[:, 0:n], m2[:, 0:n], ALU.add)
        k.tt(['h2', 'm1'], ['h2'], d, d, m1[:, 0:n], ALU.add)
        k.act(['h2'], ['h2'], d, d, AF.Sin)

    dec = [sb.t([128, L], F32) for _ in range(2)]
    kraw = [sb.t([128, L], F32) for _ in range(2)]
    kbfo = [sb.t([128, L], BF16) for _ in range(2)]
    junk = sb.t([128, L], BF16)
    asum = sb.t([128, 4], F32)

    def gen_filters(zname, dname, ncols, ctx):
        mlp(zname, ncols, h2)
        for ch in range(2):
            k.dma('sp', ['dec%d' % ch], ['dec%d' % ch], dec[ch][:, 0:ncols], k.dram[dname][ch * 128:(ch + 1) * 128, :])
        for o in range(2):
            for ch in range(2):
                k.memset([], ['asum'], asum, 0.0)
                for d in range(2):
                    fc = o * 4 + d * 2 + ch
                    kr = kraw[d]
                    kk_ = 'kraw%d' % d
                    for c0 in range(0, ncols, 512):
                        n = min(512, ncols - c0)
                        pk, pp = nps()
                        k.mm(['w3', 'h2'], [pk], pp[:, 0:n], w3[:, fc * 128:(fc + 1) * 128], h2[:, c0:c0 + n])
                        k.act([pk, 'b3'], [kk_], kr[:, c0:c0 + n], pp[:, 0:n], AF.Identity, bias=b3[:, fc:fc + 1], scale=1.0)
                    k.tt([kk_, 'dec%d' % ch], [kk_], kr[:, 0:ncols], kr[:, 0:ncols], dec[ch][:, 0:ncols], ALU.mult)
                    if not ctx:
                        if d == 1:
                            k.memset([kk_], [kk_], kr[:, 0:1], 0.0)
                        k.act([kk_, 'asum'], ['junk', 'asum'], junk[:, 0:ncols], kr[:, 0:ncols], AF.Abs, accum_out=asum[:, d:d + 1])
                    else:
                        lo, hi = (255, 511) if d == 0 else (0, 255)
                        k.act([kk_, 'asum'], ['junk', 'asum'], junk[:, lo:hi], kr[:, lo:hi], AF.Abs, accum_out=asum[:, d:d + 1])
                k.tt(['asum'], ['asum'], asum[:, 2:3], asum[:, 0:1], asum[:, 1:2], ALU.add)
                k.recip(['asum'], ['asum'], asum[:, 2:3], asum[:, 2:3])
                if not ctx:
                    for d in range(2):
                        fc = o * 4 + d * 2 + ch
                        k.ts(['kraw%d' % d, 'asum'], ['kbfo%d' % d], kbfo[d], kraw[d], asum[:, 2:3], op0=ALU.mult,
                             eng=('dve' if d == 0 else 'pool'))
                        k.dma('pool', ['kbfo%d' % d], ['HK'], HK[fc], kbfo[d])
                else:
                    k.ts(['kraw0', 'asum'], ['kraw0'], kraw[0][:, 255:511], kraw[0][:, 255:511], asum[:, 2:3], op0=ALU.mult)
                    k.ts(['kraw1', 'asum', 'kraw0'], ['kraw0'], kraw[0][:, 0:255], kraw[1][:, 0:255], asum[:, 2:3], op0=ALU.mult)
                    k.dma('pool', ['kraw0'], ['CK'], CK[o * 2 + ch], kraw[0][:, 0:511])

    gen_filters('hy_z', 'hy_decay', L, False)
    if not last:
        gen_filters('hy_zc', 'hy_decayc', 511, True)
    k.em.barrier()

    if HSTOP[0] <= 1:
        return
    sb = SB(nc, base=base)
    stg = sb.t([128, 2048], F32)
    C = {}
    C['F1'] = hy_load_bf(k, sb, 'hy_F1', [32, 128], stg, 'F1')
    C['Gr'] = hy_load_bf(k, sb, 'hy_Gr', [128, 8192], stg, 'Gr').rearrange("p (q m) -> p q m", m=128)
    C['Gi'] = hy_load_bf(k, sb, 'hy_Gi', [128, 8192], stg, 'Gi').rearrange("p (q m) -> p q m", m=128)
    C['E1'] = hy_load_bf(k, sb, 'hy_E1', [128, 256], stg, 'E1')
    C['E2'] = hy_load_bf(k, sb, 'hy_E2', [128, 256], stg, 'E2')
    C['Mr'] = hy_load_bf(k, sb, 'hy_Mr', [64, 4096], stg, 'Mr').rearrange("p (n m) -> p n m", m=32)
    C['nMi'] = hy_load_bf(k, sb, 'hy_nMi', [64, 4096], stg, 'nMi').rearrange("p (n m) -> p n m", m=32)
    A = sb.t([128, HC, 2, 64], BF16)
    nAi = sb.t([128, HC, 64], BF16)
    base2 = sb.off
    if HSTOP[0] <= 1.5:
        k.em.barrier()
        return

    sbs = SB(nc, base=base2)
    kbf = [sbs.t([32, HC, 128], BF16) for _ in range(2)]
    Hacc = sbs.t([128, 64, 2, HC], F32)
    Hbf = sbs.t([128, 64, 2, HC], BF16)
    xtmp = sbs.t([128, KB, 2, HC], F32)
    SC = 1.0 / 8192.0
    for o in range(2):
        for b4 in range(NHB):
            ch, coff = (b4 * HC) // 128, (b4 * HC) % 128
            for d in range(2):
                fc = o * 4 + d * 2 + ch
                k.dma('sp', ['xbf'], ['xbf'], kbf[d], HK[fc][coff:coff + HC, :].rearrange("c (a b) -> a c b", b=128))

                def consume(pk, pp, k0, d=d):
                    src = pp[:, 0:512].rearrange("p (q r c) -> p q r c", r=2, c=HC)
                    dst = Hacc[:, k0:k0 + KB, :, :]
                    if d == 0:
                        k.act([pk], ['Hacc'], dst, src, AF.Copy, scale=SC)
                    else:
                        k.act([pk], ['xtmp'], xtmp, src, AF.Copy, scale=SC)
                        k.tt(['xtmp', 'Hacc'], ['Hacc'], dst[:, :, 0, :], dst[:, :, 0, :], xtmp[:, :, 0, :], ALU.add)
                        k.tt(['xtmp', 'Hacc'], ['Hacc'], dst[:, :, 1, :], dst[:, :, 1, :], xtmp[:, :, 1, :], ALU.subtract, eng='pool')
                fft_fwd(k, C, kbf[d], A, nAi, nps, consume)
            k.cp(['Hacc'], ['Hbf'], Hbf, Hacc, eng='pool')
            k.dma('pool', ['Hbf'], ['HH'], HH[o * NHB + b4], Hbf.rearrange("p q r c -> p (q r c)"))
    k.em.barrier()

    if HSTOP[0] <= 2:
        return
    sbc = SB(nc, base=base2)
    raw = sbc.t([128, T], F32)
    acc = sbc.t([128, T], F32)
    ucb = sbc.t([128, T], BF16)
    cw = sbc.t([128, 6, 4], F32)
    k.dma('sp', [], ['cw'], cw, k.dram['hy_conv_col'][l])
    for cc in range(6):
        k.dma('sp', ['raw'], ['raw'], raw, UT[cc * 128:(cc + 1) * 128, :])
        k.ts(['raw', 'cw'], ['acc'], acc, raw, cw[:, cc, 1:2], cw[:, cc, 3:4], op0=ALU.mult, op1=ALU.add)
        for (a, b) in ((0, L), (L, T)):
            k.stt(['raw', 'cw', 'acc'], ['acc'], acc[:, a + 1:b], raw[:, a:b - 1], cw[:, cc, 0:1], acc[:, a + 1:b], ALU.mult, ALU.add)
            k.stt(['raw', 'cw', 'acc'], ['acc'], acc[:, a:b - 1], raw[:, a + 1:b], cw[:, cc, 2:3], acc[:, a:b - 1], ALU.mult, ALU.add)
        k.cp(['acc'], ['ucb'], ucb, acc, eng='act')
        k.dma('pool', ['ucb'], ['UC'], UC[cc * 128:(cc + 1) * 128, :], ucb)
    k.em.barrier()

    if HSTOP[0] <= 3:
        return
    sbd = SB(nc, base=base2)
    vbf = sbd.t([32, HC, 128], BF16)
    x1bf = sbd.t([32, HC, 128], BF16)
    x2bf = sbd.t([32, HC, 128], BF16)
    z1 = sbd.t([32, HC, 128], BF16)
    z2 = sbd.t([32, HC, 128], BF16)
    dv = sbd.t([32, HC, 128], BF16)
    dbc = sbd.t([32, 2, 256], F32)
    Hs = sbd.t([128, 64, 2, HC], BF16)
    Y = sbd.t([128, 64, 2, HC], BF16)
    Zs = sbd.t([64, HC, 2, 128], BF16)
    xs = [sbd.t([128, KB, 2, HC], F32) for _ in range(2)]
    ta = [sbd.t([128, KB, HC], F32) for _ in range(2)]
    tb = [sbd.t([128, KB, HC], F32) for _ in range(2)]
    tg = [sbd.t([32, HC, NB6], F32) for _ in range(2)]
    k.dma('sp', [], ['dbc'], dbc, k.dram['hy_d_bc'][l])
    xctr = [0]
    for b4 in range(NHB):
        c0g = b4 * HC
        for (tile_, key, r0) in ((vbf, 'xbf', 0), (x1bf, 'x1bf', 256), (x2bf, 'x2bf', 512)):
            k.dma('sp', [key], [key], tile_, UC[r0 + c0g:r0 + c0g + HC, 0:L].rearrange("c (a b) -> a c b", b=128))
        for o in range(2):
            xin, xkey = (vbf, 'xbf') if o == 0 else (z1, 'z1')
            gate, gkey = (x1bf, 'x1bf') if o == 0 else (x2bf, 'x2bf')
            zout, zkey = (z1, 'z1') if o == 0 else (z2, 'z2')
            k.tt([xkey, 'dbc'], ['dv'], dv, xin, dbc[:, o, c0g:c0g + HC].unsqueeze(2).to_broadcast([32, HC, 128]), ALU.mult, eng='pool')
            k.dma('sp', ['Hs'], ['Hs'], Hs.rearrange("p q r c -> p (q r c)"), HH[o * NHB + b4])

            def consume(pk, pp, k0):
                i = xctr[0] % 2
                xctr[0] += 1
                xk, tak, tbk = 'xs%d' % i, 'ta%d' % i, 'tb%d' % i
                k.cp([pk], [xk], xs[i], pp[:, 0:512].rearrange("p (q r c) -> p q r c", r=2, c=HC), eng='act')
                Xr, Xi = xs[i][:, :, 0, :], xs[i][:, :, 1, :]
                Hr, Hi = Hs[:, k0:k0 + KB, 0, :], Hs[:, k0:k0 + KB, 1, :]
                Yr, Yi = Y[:, k0:k0 + KB, 0, :], Y[:, k0:k0 + KB, 1, :]
                k.tt([xk, 'Hs'], [tak], ta[i], Xr, Hr, ALU.mult)
                k.tt([xk, 'Hs'], [tbk], tb[i], Xi, Hi, ALU.mult, eng='pool')
                k.tt([tak, tbk], ['Y'], Yr, ta[i], tb[i], ALU.subtract)
                k.tt([xk, 'Hs'], [tak], ta[i], Xr, Hi, ALU.mult, eng='pool')
                k.tt([xk, 'Hs'], [tbk], tb[i], Xi, Hr, ALU.mult)
                k.tt([tak, tbk], ['Y'], Yi, ta[i], tb[i], ALU.add, eng='pool')
            fft_fwd_keyed(k, C, xin, xkey, A, nAi, nps, consume)
            for c0 in range(0, HC, 2):
                pk, pp = nps()
                for dc in range(2):
                    cidx = c0 + dc
                    out = pp[0:64, dc * 256:(dc + 1) * 256]
                    k.mm(['Y', 'E1'], [pk], out, Y[:, :, 0, cidx], C['E1'], start=True, stop=False)
                    k.mm(['Y', 'E2'], [pk], out, Y[:, :, 1, cidx], C['E2'], start=False, stop=True)
                src = pp[0:64, 0:512].rearrange("p (c r n) -> p c r n", r=2, n=128)
                k.cp([pk], ['Zs'], Zs[:, c0:c0 + 2, :, :], src, eng=('act' if (c0 // 2) % 2 == 0 else 'dve'))
            for g8, n0 in enumerate(range(0, 128, NB6)):
                pk, pp = nps()
                for dn in range(NB6):
                    n2 = n0 + dn
                    out = pp[0:32, dn * HC:(dn + 1) * HC]
                    k.mm(['Mr', 'Zs'], [pk], out, C['Mr'][:, n2, :], Zs[:, :, 0, n2], start=True, stop=False)
                    k.mm(['nMi', 'Zs'], [pk], out, C['nMi'][:, n2, :], Zs[:, :, 1, n2], start=False, stop=True)
                i = g8 % 2
                src = pp[0:32, 0:NB6 * HC].rearrange("p (n c) -> p c n", c=HC)
                k.tt([pk, 'dv'], ['tg%d' % i], tg[i], src, dv[:, :, n0:n0 + NB6], ALU.add)
                k.tt(['tg%d' % i, gkey], [zkey], zout[:, :, n0:n0 + NB6], tg[i], gate[:, :, n0:n0 + NB6], ALU.mult, eng='pool')
        k.dma('pool', ['z2'], ['YT'], YT[c0g:c0g + HC, 0:L].rearrange("c (a b) -> a c b", b=128), z2)
    k.em.barrier()

    if last or HSTOP[0] <= 4:
        return
    sbx = SB(nc, base=base2)
    ub = sbx.t([128, 3, CTX], BF16)
    uf = sbx.t([128, 3, CTX], F32)
    kf = sbx.t([128, 511], F32)
    accs = [sbx.t([128, CTX], F32) for _ in range(4)]
    zc = sbx.t([128, CTX], F32)
    zb = sbx.t([128, CTX], BF16)
    dcol = sbx.t([128, 4], F32)
    k.dma('sp', [], ['dcol'], dcol, k.dram['hy_d_col'][l])
    for ch in range(2):
        for j in range(3):
            k.dma('sp', ['ub'], ['ub'], ub[:, j, :], UC[j * 256 + ch * 128:j * 256 + (ch + 1) * 128, L:T])
        k.cp(['ub'], ['uf'], uf, ub)
        for o in range(2):
            uin = uf[:, 0, :] if o == 0 else zc
            gate = uf[:, 1 + o, :]
            k.dma('sp', ['kf'], ['kf'], kf, CK[o * 2 + ch])
            for a in range(4):
                k.memset([], ['acc%d' % a], accs[a], 0.0, eng=('dve' if a < 2 else 'pool'))
            for s_ in range(CTX):
                a = s_ % 4
                k.stt(['kf', 'uf', 'zc', 'acc%d' % a], ['acc%d' % a], accs[a], kf[:, 255 - s_:511 - s_], uin[:, s_:s_ + 1], accs[a],
                      ALU.mult, ALU.add)
            k.tt(['acc0', 'acc1'], ['acc0'], accs[0], accs[0], accs[1], ALU.add)
            k.tt(['acc2', 'acc3'], ['acc2'], accs[2], accs[2], accs[3], ALU.add, eng='pool')
            k.tt(['acc0', 'acc2'], ['acc0'], accs[0], accs[0], accs[2], ALU.add)
            k.stt(['uf', 'zc', 'dcol', 'acc0'], ['acc0'], accs[0], uin, dcol[:, o * 2 + ch:o * 2 + ch + 1], accs[0], ALU.mult, ALU.add)
            k.tt(['acc0', 'uf'], ['zc'], zc, accs[0], gate, ALU.mult)
        k.cp(['zc'], ['zb'], zb, zc)
        k.dma('pool', ['zb'], ['YT'], YT[ch * 128:(ch + 1) * 128, L:T], zb)
    k.em.barrier()


def fft_fwd_keyed(k, C, xin, xkey, A, nAi, psctr, consume):
    F1, Gr, Gi = C['F1'], C['Gr'], C['Gi']
    for c0 in range(0, HC, 4):
        pk, pp = psctr()
        for dc in range(4):
            k.mm([xkey, 'F1'], [pk], pp[:, dc * 128:(dc + 1) * 128], xin[:, c0 + dc, :], F1)
        src = pp[:, 0:512].rearrange("p (c r q) -> p c r q", r=2, q=64)
        k.cp([pk], ['A'], A[:, c0:c0 + 4, :, :], src, eng='act')
        k.ts(['A'], ['nAi'], nAi[:, c0:c0 + 4, :], A[:, c0:c0 + 4, 1, :], -1.0, op0=ALU.mult)
    for k0 in range(0, 64, KB):
        pk, pp = psctr()
        for dk in range(KB):
            k1 = k0 + dk
            xr = pp[:, dk * 2 * HC:dk * 2 * HC + HC]
            xi = pp[:, dk * 2 * HC + HC:(dk + 1) * 2 * HC]
            k.mm(['Gr', 'A'], [pk], xr, Gr[:, k1, :], A[:, :, 0, k1], start=True, stop=False)
            k.mm(['Gi', 'nAi'], [pk], xr, Gi[:, k1, :], nAi[:, :, k1], start=False, stop=True)
            k.mm(['Gi', 'A'], [pk], xi, Gi[:, k1, :], A[:, :, 0, k1], start=True, stop=False)
            k.mm(['Gr', 'A'], [pk], xi, Gr[:, k1, :], A[:, :, 1, k1], start=False, stop=True)
        consume(pk, pp, k0)


BIG = 1.0e9


def phaseC(k, P, l, base, last, xres):
    nc = k.nc
    YT, XMIX, H2T, XRES = k.dram['YT'], k.dram['XMIX'], k.dram['H2T'], k.dram['XRES']
    ntiles = 32 if last else NT
    psi = [0]

    def nps():
        i = psi[0]
        psi[0] = (psi[0] + 1) % 8
        return 'ps%d' % i, k.ps[i]

    sb0 = SB(nc, base=base)
    grow = sb0.t([128, 2, 2, D], F32)
    gates = sb0.t([128, NT, 32], F32)
    dg = sb0.t([128, 128], F32)
    for gi, c0 in enumerate((16, 40)):
        for j in range(2):
            for kk in range(8):
                k.ts(['ident32', 'mod'], ['dg'], dg, P['ident32'], P['mod'][:, l, c0 + kk, j:j + 1], op0=ALU.mult)
                pk, pp = nps()
                k.mm(['ones32', 'dg'], [pk], pp[:, 0:128], P['ones32'], dg)
                k.cp([pk], ['grow'], grow[:, gi, j, kk * 128:(kk + 1) * 128], pp[:, 0:128], eng='act')
    base1 = sb0.off
    sb = SB(nc, base=base1)
    Wout = sb.t([128, 8, D], BF16)
    Wr = sb.t([128, 8, 36], F32)
    rb = sb.t([128, 36], F32)
    stg = sb.t([128, 8, 512], F32)
    wv = k.dram['w_out'][l].rearrange("(k p) n -> p k n", p=128)
    for i, c0 in enumerate((0, 512)):
        k.dma('sp', ['stg'], ['stg'], stg, wv[:, :, c0:c0 + 512])
        k.cp(['stg'], ['Wout'], Wout[:, :, c0:c0 + 512], stg)
    k.dma('sp', [], ['Wr'], Wr, k.dram['moe_wr'][l].rearrange("(k p) n -> p k n", p=128))
    k.dma('sp', [], ['rb'], rb, k.dram['moe_rb_bc'][l])
    yT = [sb.t([128, 8, 512], BF16) for _ in range(2)]
    xt = [sb.t([128, D], F32) for _ in range(2)]
    xm = [sb.t([128, D], F32) for _ in range(2)]
    xn = sb.t([128, D], F32)
    junk = sb.t([128, D], BF16)
    h32 = sb.t([128, 8, 128], F32)
    hbf = [sb.t([128, 8, 128], BF16) for _ in range(2)]
    ss = [sb.t([128, 2], F32) for _ in range(2)]
    lg = sb.t([128, 36], F32)
    rt = sb.t([128, 16], F32)
    oh = sb.t([128, 4], F32)
    ml = sb.t([128, 32], F32)
    e1 = sb.t([128, 32], F32)
    e2 = sb.t([128, 32], F32)
    tmp32 = sb.t([128, 32], F32)
    for ti in range(ntiles):
        j = 0 if ti < 32 else 1
        g, tl = ti // 4, ti % 4
        yb = g % 2
        yk = 'yT%d' % yb
        if tl == 0:
            n = min(512, ntiles * 128 - g * 512)
            k.dma('sp', [yk], [yk], yT[yb][:, :, 0:n], YT[:, g * 512:g * 512 + n].rearrange("(c p) t -> p c t", p=128))
        b = ti % 2
        xk, mk, sk, hk = 'xt%d' % b, 'xm%d' % b, 'ss%d' % b, 'hbf%d' % b
        k.dma('sp', [xk], [xk], xt[b], xres[ti * 128:(ti + 1) * 128, :])
        for half in range(2):
            pk, pp = nps()
            for f in range(8):
                k.mm([yk, 'Wout'], [pk], pp[:, 0:512], yT[yb][:, f, tl * 128:(tl + 1) * 128], Wout[:, f, half * 512:(half + 1) * 512],
                     start=(f == 0), stop=(f == 7))
            cs = slice(half * 512, (half + 1) * 512)
            k.tt([pk, 'grow'], [mk], xm[b][:, cs], pp[:, 0:512], grow[:, 0, j, cs], ALU.mult)
            k.tt([mk, xk], [mk], xm[b][:, cs], xm[b][:, cs], xt[b][:, cs], ALU.add, eng='pool')
        k.dma('pool', [mk], ['XMIX'], XMIX[ti * 128:(ti + 1) * 128, :], xm[b])
        k.memset([], [sk], ss[b], 0.0)
        k.act([mk, sk], ['junk', sk], junk, xm[b], AF.Square, accum_out=ss[b][:, 0:1])
        k.ts([sk], [sk], ss[b][:, 1:2], ss[b][:, 0:1], 1.0 / D, EPS, op0=ALU.mult, op1=ALU.add)
        k.act([sk], [sk], ss[b][:, 1:2], ss[b][:, 1:2], AF.Sqrt)
        k.recip([sk], [sk], ss[b][:, 1:2], ss[b][:, 1:2])
        k.ts([mk, sk], ['xn'], xn, xm[b], ss[b][:, 1:2], op0=ALU.mult)
        for h2 in range(2):
            pk, pp = nps()
            for q in range(4):
                kk = h2 * 4 + q
                k.tr(['xn', 'ident32'], [pk], pp[:, q * 128:(q + 1) * 128], xn[:, kk * 128:(kk + 1) * 128], P['ident32'])
            for q in range(4):
                kk = h2 * 4 + q
                k.act([pk, 'A2', 'mod'], ['h32'], h32[:, kk, :], pp[:, q * 128:(q + 1) * 128], AF.Identity,
                      scale=P['A2'][:, l, kk, j:j + 1], bias=P['mod'][:, l, 24 + kk, j:j + 1])
        k.cp(['h32'], [hk], hbf[b], h32, eng='pool')
        k.dma('pool', [hk], ['H2T'], H2T[:, ti * 128:(ti + 1) * 128].rearrange("(c p) t -> p c t", p=128), hbf[b])
        pk, pp = nps()
        for kk in range(8):
            k.mm(['h32', 'Wr'], [pk], pp[:, 0:36], h32[:, kk, :], Wr[:, kk, :], start=(kk == 0), stop=(kk == 7))
        k.tt([pk, 'rb'], ['lg'], lg, pp[:, 0:36], rb, ALU.add)
        k.red(['lg'], ['rt'], rt[:, 0:1], lg[:, 0:4], ALU.max)
        k.ts(['lg', 'rt'], ['oh'], oh, lg[:, 0:4], rt[:, 0:1], op0=ALU.is_equal)
        k.ts(['rt'], ['rt'], rt[:, 1:2], rt[:, 0:1], -1.0, op0=ALU.mult)
        k.memset(['rt'], ['rt'], rt[:, 2:3], 0.0)
        k.act(['lg', 'rt'], ['tmp32', 'rt'], tmp32[:, 0:4], lg[:, 0:4], AF.Exp, bias=rt[:, 1:2], scale=1.0, accum_out=rt[:, 2:3])
        k.recip(['rt'], ['rt'], rt[:, 3:4], rt[:, 2:3])
        k.ts(['oh'], ['oh'], oh, oh, 1.0, BIG, op0=ALU.subtract, op1=ALU.mult)
        k.tt(['lg', 'oh'], ['ml'], ml.rearrange("p (g e) -> p g e", e=8), lg[:, 4:36].rearrange("p (g e) -> p g e", e=8),
             oh.unsqueeze(2).to_broadcast([128, 4, 8]), ALU.add)
        k.red(['ml'], ['rt'], rt[:, 4:5], ml, ALU.max)
        k.ts(['ml', 'rt'], ['e1'], e1, ml, rt[:, 4:5], op0=ALU.is_equal)
        k.ts(['e1'], ['tmp32'], tmp32, e1, -BIG, op0=ALU.mult)
        k.tt(['ml', 'tmp32'], ['ml'], ml, ml, tmp32, ALU.add)
        k.red(['ml'], ['rt'], rt[:, 5:6], ml, ALU.max)
        k.ts(['ml', 'rt'], ['e2'], e2, ml, rt[:, 5:6], op0=ALU.is_equal)
        k.tt(['rt'], ['rt'], rt[:, 6:7], rt[:, 5:6], rt[:, 4:5], ALU.subtract)
        k.act(['rt'], ['rt'], rt[:, 6:7], rt[:, 6:7], AF.Exp)
        k.ts(['rt'], ['rt'], rt[:, 7:8], rt[:, 6:7], 1.0, op0=ALU.add)
        k.recip(['rt'], ['rt'], rt[:, 7:8], rt[:, 7:8])
        k.tt(['rt'], ['rt'], rt[:, 8:9], rt[:, 6:7], rt[:, 7:8], ALU.mult)
        k.tt(['rt'], ['rt'], rt[:, 9:10], rt[:, 7:8], rt[:, 3:4], ALU.mult)
        k.tt(['rt'], ['rt'], rt[:, 10:11], rt[:, 8:9], rt[:, 3:4], ALU.mult)
        k.ts(['e1', 'rt'], ['gates'], gates[:, ti, :], e1, rt[:, 9:10], op0=ALU.mult)
        k.stt(['e2', 'rt', 'gates'], ['gates'], gates[:, ti, :], e2, rt[:, 10:11], gates[:, ti, :], ALU.mult, ALU.add)
    k.em.barrier()
    NPASS = 3
    per = (ntiles + NPASS - 1) // NPASS
    sb = SB(nc, base=base1)
    h2 = sb.t([128, 8, per * 128], BF16)
    facc = sb.t([128, per, D], F32)
    wst = [sb.t([128, 8, 512], F32) for _ in range(2)]
    w1b = [sb.t([128, 8, 512], BF16) for _ in range(2)]
    w3b = [sb.t([128, 8, 512], BF16) for _ in range(2)]
    w2b = [sb.t([128, 4, D], BF16) for _ in range(2)]
    gT = [sb.t([128, 4, 512], BF16) for _ in range(2)]
    st = [sb.t([128, 512], F32) for _ in range(2)]
    xo = [sb.t([128, D], F32) for _ in range(2)]
    fw = sb.t([128, D], F32)
    if last:
        k.dma('sp', [], ['fw'], fw, k.dram['final_bc'][:, :])
    W1, W3, W2 = k.dram['moe_w1'], k.dram['moe_w3'], k.dram['moe_w2']
    sctr = [0]

    def load_w(src_ap, dst, dkey, shape3):
        i = sctr[0] % 2
        sctr[0] += 1
        sk_ = 'wst%d' % i
        view = wst[i].rearrange("p a b -> p (a b)").rearrange("p (a b) -> p a b", b=shape3)
        k.dma('sp', [sk_], [sk_], view, src_ap)
        k.cp([sk_], [dkey], dst, view, eng='pool')

    for ps_ in range(NPASS):
        t_lo = ps_ * per
        t_hi = min(ntiles, t_lo + per)
        nt_ = t_hi - t_lo
        ntok = nt_ * 128
        k.dma('sp', ['h2'], ['h2'], h2[:, :, 0:ntok], H2T[:, t_lo * 128:t_hi * 128].rearrange("(c p) t -> p c t", p=128))
        k.memset(['facc'], ['facc'], facc, 0.0, eng='pool')
        for e in range(32):
            wb = e % 2
            load_w(W1[l, e].rearrange("(k p) n -> p k n", p=128), w1b[wb], 'w1b%d' % wb, 512)
            load_w(W3[l, e].rearrange("(k p) n -> p k n", p=128), w3b[wb], 'w3b%d' % wb, 512)
            load_w(W2[l, e].rearrange("(k p) n -> p k n", p=128), w2b[wb], 'w2b%d' % wb, D)
            for c0 in range(0, ntok, 512):
                n = min(512, ntok - c0)
                gb = (c0 // 512) % 2
                gk = 'gT%d' % gb
                for f in range(4):
                    p1k, pp1 = nps()
                    for kk in range(8):
                        k.mm(['w1b%d' % wb, 'h2'], [p1k], pp1[:, 0:n], w1b[wb][:, kk, f * 128:(f + 1) * 128], h2[:, kk, c0:c0 + n],
                             start=(kk == 0), stop=(kk == 7))
                    p3k, pp3 = nps()
                    for kk in range(8):
                        k.mm(['w3b%d' % wb, 'h2'], [p3k], pp3[:, 0:n], w3b[wb][:, kk, f * 128:(f + 1) * 128], h2[:, kk, c0:c0 + n],
                             start=(kk == 0), stop=(kk == 7))
                    sbi = f % 2
                    k.act([p1k], ['st%d' % sbi], st[sbi][:, 0:n], pp1[:, 0:n], AF.Silu)
                    k.tt(['st%d' % sbi, p3k], [gk], gT[gb][:, f, 0:n], st[sbi][:, 0:n], pp3[:, 0:n], ALU.mult)
                for tl in range(n // 128):
                    t = c0 // 128 + tl
                    for half in range(2):
                        pk, pp = nps()
                        for f in range(4):
                            k.mm([gk, 'w2b%d' % wb], [pk], pp[:, 0:512], gT[gb][:, f, tl * 128:(tl + 1) * 128], w2b[wb][:, f, half * 512:(half + 1) * 512],
                                 start=(f == 0), stop=(f == 3))
                        fa = facc[:, t, half * 512:(half + 1) * 512]
                        k.stt([pk, 'gates', 'facc'], ['facc'], fa, pp[:, 0:512], gates[:, t_lo + t, e:e + 1], fa, ALU.mult, ALU.add)
        for t in range(nt_):
            ti = t_lo + t
            j = 0 if ti < 32 else 1
            b = t % 2
            ok = 'xo%d' % b
            k.dma('sp', [ok], [ok], xo[b], XMIX[ti * 128:(ti + 1) * 128, :])
            k.tt(['facc', 'grow'], ['facc'], facc[:, t, :], facc[:, t, :], grow[:, 1, j, :], ALU.mult, eng='pool')
            k.tt(['facc', ok], [ok], xo[b], xo[b], facc[:, t, :], ALU.add)
            if not last:
                k.dma('pool', [ok], ['XRES'], XRES[ti * 128:(ti + 1) * 128, :], xo[b])
            else:
                sk = 'fss'
                fs = st[0][:, 0:2]
                k.memset(['st0'], ['st0'], fs, 0.0)
                k.act([ok, 'st0'], ['gT0', 'st0'], gT[0].rearrange("p a b -> p (a b)")[:, 0:D], xo[b], AF.Square, accum_out=fs[:, 0:1])
                k.ts(['st0'], ['st0'], fs[:, 1:2], fs[:, 0:1], 1.0 / D, EPS, op0=ALU.mult, op1=ALU.add)
                k.act(['st0'], ['st0'], fs[:, 1:2], fs[:, 1:2], AF.Sqrt)
                k.recip(['st0'], ['st0'], fs[:, 1:2], fs[:, 1:2])
                k.ts([ok, 'st0'], [ok], xo[b], xo[b], fs[:, 1:2], op0=ALU.mult)
                k.tt([ok, 'fw'], [ok], xo[b], xo[b], fw, ALU.mult)
                k.dma('pool', [ok], ['out'], k.dram['out'][ti * 128:(ti + 1) * 128, :], xo[b])
    k.em.barrier()


def build(stage='full', debug=()):
    nc = bass.Bass("TRN2", target_bir_lowering=False)
    k = K(nc, debug=debug)
    P = {}
    sbp = SB(nc)
    k.din('xin', [T, D])
    k.din('w_in_fm', [DEPTH, D, NFM])
    k.din('w_in_tm', [DEPTH, D, NTM])
    k.din('b_fm_col', [128, DEPTH, 18])
    k.din('b_tm_bc', [DEPTH, 128, NTM])
    k.dscratch('UT', [1280, T])
    k.dscratch('QKT', [1024, T], BF16)
    k.dscratch('TM', [T, NTM])
    k.dscratch('XRES', [T, D])
    k.dscratch('YT', [1024, T], BF16)
    k.din('da_lambda', [DEPTH, 256])
    k.dscratch('XMIX', [T, D])
    k.dscratch('H2T', [D, T], BF16)
    k.din('w_out', [DEPTH, D, D])
    k.din('moe_wr', [DEPTH, D, 36])
    k.din('moe_rb_bc', [DEPTH, 128, 36])
    k.din('moe_w1', [DEPTH, 32, D, 512])
    k.din('moe_w3', [DEPTH, 32, D, 512])
    k.din('moe_w2', [DEPTH, 32, 512, D])
    k.din('final_bc', [128, D])
    if stage == 'full':
        k.dout('out', [L, D])
    k.dscratch('HK', [8, 128, L], BF16)
    k.dscratch('HH', [2 * NHB, 128, 64 * 2 * HC], BF16)
    k.dscratch('CK', [4, 128, 511])
    k.dscratch('UC', [768, T], BF16)
    for nm, shp in (('hy_filt_w1', [DEPTH, 33, 64]), ('hy_filt_w2', [DEPTH, 64, 64]), ('hy_filt_w3', [DEPTH, 64, 1024]),
                    ('hy_filt_sc', [DEPTH, 64, 3]), ('hy_b3_col', [DEPTH, 128, 8]), ('hy_conv_col', [DEPTH, 128, 6, 4]),
                    ('hy_d_bc', [DEPTH, 32, 2, 256]), ('hy_d_col', [DEPTH, 128, 4]),
                    ('hy_z', [33, L]), ('hy_decay', [256, L]), ('hy_zc', [33, 511]), ('hy_decayc', [256, 511]),
                    ('hy_F1', [32, 128]), ('hy_Gr', [128, 8192]), ('hy_Gi', [128, 8192]), ('hy_E1', [128, 256]),
                    ('hy_E2', [128, 256]), ('hy_Mr', [64, 4096]), ('hy_nMi', [64, 4096])):
        k.din(nm, shp)
    k.din('tri', [2, 64, 64])
    k.din('ml_conv_col', [DEPTH, 64, 8, 4])
    k.din('ml_norm_bc', [DEPTH, 64, 256])
    k.din('da_subln_col', [DEPTH, 128, 1])
    phase0(k, P, sbp)
    P['negc'] = sbp.t([128, DEPTH, 2, 4], F32)
    base = sbp.off
    for l in range(DEPTH):
        xres = k.dram['xin'] if l == 0 else k.dram['XRES']
        phaseA(k, P, l, base, xres)
        if stage == 'A':
            break
        if stage not in ('B2', 'H'):
            phaseB1(k, P, l, base, l == DEPTH - 1)
        if stage == 'B1':
            break
        if stage != 'H':
            phaseB2(k, P, l, base, l == DEPTH - 1)
        if stage == 'B2':
            break
        phaseH(k, P, l, base, l == DEPTH - 1)
        if stage == 'H':
            break
        phaseC(k, P, l, base, (l == DEPTH - 1) and stage == 'full', xres)
        if stage == 'C':
            break
    k.em.barrier()
    return nc, k


def run(inputs, stage='full', debug=(), cores=8):
    consts = make_consts()
    nc, k = build(stage, debug)
    in_maps = []
    for b in range(cores):
        m = prep_inputs(inputs, b)
        m.update(consts)
        in_maps.append({kk: v for kk, v in m.items() if kk in k.dram})
    res = run_bass_kernel_spmd(nc, in_maps, core_ids=list(range(cores)))
    return res.results


def kernel(**inputs):
    inp = {kk: np.asarray(v) for kk, v in inputs.items()}
    res = run(inp, stage='full', cores=8)
    return np.stack([np.asarray(r['out'], dtype=np.float32) for r in res], axis=0)
```

```python
import math
import os
import numpy as np
import concourse.bass as bass
import concourse.mybir as mybir
from concourse.bass_utils import run_bass_kernel_spmd

F32 = mybir.dt.float32
BF16 = mybir.dt.bfloat16
AF = mybir.ActivationFunctionType
ALU = mybir.AluOpType
AX = mybir.AxisListType

D = 1024
L = 4096
CTX = 256
T = L + CTX
NT = T // 128
DEPTH = 2
EPS = 1e-6
N_IN = 3344
ML_OFF = 768
DA_OFF = 1808
NFM = 2304
NTM = 1040
FM_COLS = list(range(0, 768)) + list(range(768, 1280)) + list(range(1808, 2832))
TM_COLS = list(range(1280, 1792)) + list(range(2832, 3344)) + list(range(1792, 1808))


class Em:
    NDMA = 32
    SAME_ENGINE_WAITS = True

    def __init__(self, nc):
        self.nc = nc
        self.eng = {'pe': nc.tensor, 'act': nc.scalar, 'dve': nc.vector, 'pool': nc.gpsimd, 'sp': nc.sync}
        self.sem = {k: nc.alloc_semaphore('s_' + k) for k in ('pe', 'act', 'dve', 'pool')}
        self.cnt = {k: 0 for k in self.sem}
        self.dsem = [nc.alloc_semaphore('s_dma%d' % i) for i in range(self.NDMA)]
        self.dcnt = [0] * self.NDMA
        self.dnext = 0
        self.waited = {e: {} for e in self.eng}
        self.lastw = {}
        self.readers = {}
        self.ninst = 0

    def _semh(self, key):
        return self.sem[key] if isinstance(key, str) else self.dsem[key[1]]

    def _wait(self, e, ev):
        key, val = ev
        w = self.waited[e]
        if w.get(key, 0) >= val:
            return
        self.eng[e].wait_ge(self._semh(key), val)
        w[key] = val

    def _deps(self, e, reads, writes):
        best = {}
        for k in reads:
            ev = self.lastw.get(k)
            if ev is not None and best.get(ev[0], 0) < ev[1]:
                best[ev[0]] = ev[1]
        for k in writes:
            ev = self.lastw.get(k)
            if ev is not None and best.get(ev[0], 0) < ev[1]:
                best[ev[0]] = ev[1]
            for ev in self.readers.get(k, ()):
                if best.get(ev[0], 0) < ev[1]:
                    best[ev[0]] = ev[1]
        for key, val in best.items():
            if key == e and (e == 'pe' or not Em.SAME_ENGINE_WAITS):
                continue
            self._wait(e, (key, val))

    def _record(self, ev, reads, writes):
        for k in reads:
            lst = self.readers.setdefault(k, [])
            lst[:] = [x for x in lst if x[0] != ev[0]]
            lst.append(ev)
        for k in writes:
            self.lastw[k] = ev
            self.readers[k] = []

    def op(self, e, reads, writes, fn):
        self._deps(e, reads, writes)
        ins = fn(self.eng[e])
        self.cnt[e] += 1
        ins.then_inc(self.sem[e], 1)
        self._record((e, self.cnt[e]), reads, writes)
        self.ninst += 1

    def dma(self, q, reads, writes, out, in_, **kw):
        i = self.dnext
        self.dnext = (i + 1) % self.NDMA
        if self.dcnt[i] > 0:
            self._wait(q, (('d', i), 16 * self.dcnt[i]))
        self._deps(q, reads, writes)
        ins = self.eng[q].dma_start(out=out, in_=in_, **kw)
        self.dcnt[i] += 1
        ins.then_inc(self.dsem[i], 16)
        self._record((('d', i), 16 * self.dcnt[i]), reads, writes)
        self.ninst += 1

    def barrier(self):
        for e in self.eng:
            for k in self.sem:
                if self.cnt[k] > 0 and k != e:
                    self._wait(e, (k, self.cnt[k]))
            for i in range(self.NDMA):
                if self.dcnt[i] > 0:
                    self._wait(e, (('d', i), 16 * self.dcnt[i]))
        self.lastw = {}
        self.readers = {}


class SB:
    _arena = {}

    def __init__(self, nc, base=0, limit=None):
        self.nc = nc
        if id(nc) not in SB._arena:
            nwords = (nc.sbuf_bytes_remaining - 256) // 4
            SB._arena[id(nc)] = (nc.alloc_sbuf_tensor("arena", [128, nwords], F32), nwords * 4)
        self.arena, cap = SB._arena[id(nc)]
        self.off = base
        self.limit = cap if limit is None else limit

    def t(self, shape, dtype, name=None):
        per = 1
        for s in shape[1:]:
            per *= s
        esz = 2 if dtype == BF16 else 4
        nbytes = (per * esz + 63) // 64 * 64
        assert self.off % 4 == 0
        w0 = self.off // 4
        ap = self.arena[0:shape[0], w0:w0 + nbytes // 4]
        if dtype != F32:
            ap = ap.bitcast(dtype)
        ap = ap[:, 0:per]
        if len(shape) == 3:
            ap = ap.rearrange("p (a b) -> p a b", b=shape[2])
        elif len(shape) == 4:
            ap = ap.rearrange("p (a b c) -> p a b c", b=shape[2], c=shape[3])
        self.off += nbytes
        assert self.off <= self.limit, ("SBUF overflow", name, self.off, self.limit)
        return ap


class K:
    def __init__(self, nc, debug=()):
        self.nc = nc
        self.em = Em(nc)
        self.debug = set(debug)
        self.dram = {}
        self.ps = [nc.alloc_psum_tensor("psb%d" % i, [128, 512], F32) for i in range(8)]

    def din(self, name, shape, dtype=F32):
        ap = self.nc.dram_tensor(name, list(shape), dtype, kind="ExternalInput").ap()
        self.dram[name] = ap
        return ap

    def dscratch(self, name, shape, dtype=F32):
        kind = "ExternalOutput" if name in self.debug else "Internal"
        ap = self.nc.dram_tensor(name, list(shape), dtype, kind=kind).ap()
        self.dram[name] = ap
        return ap

    def dout(self, name, shape, dtype=F32):
        ap = self.nc.dram_tensor(name, list(shape), dtype, kind="ExternalOutput").ap()
        self.dram[name] = ap
        return ap

    def dma(self, q, r, w, out, in_, **kw):
        self.em.dma(q, r, w, out, in_, **kw)

    def mm(self, r, w, out, lhsT, rhs, start=True, stop=True):
        self.em.op('pe', r, w, lambda e: e.matmul(out, lhsT=lhsT, rhs=rhs, start=start, stop=stop))

    def tr(self, r, w, out, in_, ident):
        self.em.op('pe', r, w, lambda e: e.transpose(out, in_, ident))

    def act(self, r, w, out, in_, func, eng='act', **kw):
        self.em.op(eng, r, w, lambda e: e.activation(out=out, in_=in_, func=func, **kw))

    def ts(self, r, w, out, in0, s1, s2=None, op0=ALU.mult, op1=None, eng='dve', **kw):
        if op1 is None:
            self.em.op(eng, r, w, lambda e: e.tensor_scalar(out=out, in0=in0, scalar1=s1, scalar2=None, op0=op0, **kw))
        else:
            self.em.op(eng, r, w, lambda e: e.tensor_scalar(out=out, in0=in0, scalar1=s1, scalar2=s2, op0=op0, op1=op1, **kw))

    def tt(self, r, w, out, in0, in1, op, eng='dve'):
        self.em.op(eng, r, w, lambda e: e.tensor_tensor(out=out, in0=in0, in1=in1, op=op))

    def stt(self, r, w, out, in0, scalar, in1, op0, op1, eng='dve'):
        self.em.op(eng, r, w, lambda e: e.scalar_tensor_tensor(out=out, in0=in0, scalar=scalar, in1=in1, op0=op0, op1=op1))

    def cp(self, r, w, out, in_, eng='dve'):
        if eng == 'act':
            self.em.op(eng, r, w, lambda e: e.copy(out=out, in_=in_))
        else:
            self.em.op(eng, r, w, lambda e: e.tensor_copy(out=out, in_=in_))

    def red(self, r, w, out, in_, op, eng='dve', axis=AX.X):
        self.em.op(eng, r, w, lambda e: e.tensor_reduce(out=out, in_=in_, axis=axis, op=op))

    def recip(self, r, w, out, in_):
        self.em.op('dve', r, w, lambda e: e.reciprocal(out=out, in_=in_))

    def memset(self, r, w, out, val, eng='dve'):
        self.em.op(eng, r, w, lambda e: e.memset(out, val))


def rope_tables_T():
    half = 32
    inv = (10000.0 ** (-np.arange(0, half, 2, dtype=np.float32) / half)).astype(np.float32)
    t = np.arange(L)
    row = (t // 64).astype(np.float32)
    col = (t % 64).astype(np.float32)
    ang = np.concatenate([row[:, None] * inv, row[:, None] * inv, col[:, None] * inv, col[:, None] * inv], axis=1)
    ang = ang.astype(np.float32)
    cosT = np.cos(ang).T.astype(np.float32)
    sinT = np.sin(ang).T.astype(np.float32)
    return np.ascontiguousarray(np.concatenate([cosT, cosT], 0)), np.ascontiguousarray(np.concatenate([sinT, sinT], 0))


def rope_perm():
    R = np.zeros((128, 128), np.float32)
    for base in range(0, 128, 32):
        for i in range(16):
            R[base + 16 + i, base + i] = -1.0
            R[base + i, base + 16 + i] = 1.0
    return R


def make_consts():
    c = {}
    c['ident'] = np.eye(128, dtype=np.float32)
    c['ropeR'] = rope_perm()
    cosT, sinT = rope_tables_T()
    c['cosT'] = cosT
    c['sinT'] = sinT
    bi = np.zeros((128, 2), np.float32)
    bi[0:64, 0] = 1.0
    bi[64:128, 1] = 1.0
    c['blockind'] = bi
    sel = np.zeros((2, 2, 128), np.float32)
    sel[0, 0, :] = 1.0
    sel[1, 1, :] = 1.0
    c['sel2'] = sel
    tri = np.zeros((2, 64, 64), np.float32)
    tri[0] = np.triu(np.ones((64, 64), np.float32))
    tri[1] = np.tril(np.ones((64, 64), np.float32))
    c['tri'] = tri
    c.update(hyena_consts())
    c['tri128'] = np.triu(np.ones((128, 128), np.float32), 1)
    c['iota32'] = np.ascontiguousarray(np.broadcast_to(np.arange(32, dtype=np.float32)[None, :], (128, 32)))
    lt = np.tril(np.ones((32, 32), np.float32), -1)
    c['ltmask'] = np.ascontiguousarray(np.broadcast_to(lt.reshape(1, 1024), (128, 1024)))
    c['pidx'] = np.ascontiguousarray(np.arange(128, dtype=np.float32)[:, None])
    c['moe_S'] = np.ascontiguousarray(np.stack([np.broadcast_to(np.array(moe_sched(nt)[1][:32], np.float32)[None, :], (128, 32))
                                                for nt in (NT, 32)], 0))
    return c


def col_layout(v):
    return np.ascontiguousarray(v.reshape(-1, 128).T)


def prep_inputs(inp, b):
    m = {}
    m['xin'] = np.ascontiguousarray(np.concatenate([inp['x'][b], inp['ctx'][b]], axis=0))
    m['ccol'] = np.ascontiguousarray(np.stack([col_layout(inp['c'][b]), col_layout(inp['c_ctx'])], axis=-1))
    m['ada_w'] = inp['ada_w']
    m['ada_b_col'] = np.ascontiguousarray(np.stack([col_layout(inp['ada_b'][l]) for l in range(DEPTH)], 1))
    m['norm1_col'] = np.ascontiguousarray(np.stack([col_layout(inp['norm1_w'][l]) for l in range(DEPTH)], 1))
    m['norm2_col'] = np.ascontiguousarray(np.stack([col_layout(inp['norm2_w'][l]) for l in range(DEPTH)], 1))
    m['w_in_fm'] = np.ascontiguousarray(inp['w_in'][:, :, FM_COLS])
    m['w_in_tm'] = np.ascontiguousarray(inp['w_in'][:, :, TM_COLS])
    m['b_fm_col'] = np.ascontiguousarray(np.stack([col_layout(inp['b_in'][l][FM_COLS]) for l in range(DEPTH)], 1))
    cw = np.concatenate([inp['ml_conv_w'], inp['ml_conv_b'][:, None, :]], axis=1)
    m['ml_conv_col'] = np.ascontiguousarray(cw.reshape(DEPTH, 4, 8, 64).transpose(0, 3, 2, 1))
    m['ml_norm_bc'] = np.ascontiguousarray(np.broadcast_to(inp['ml_norm_w'][:, None, :], (DEPTH, 64, 256)))
    m['hy_filt_w1'] = inp['hy_filt_w1']
    m['hy_filt_w2'] = inp['hy_filt_w2']
    m['hy_filt_w3'] = inp['hy_filt_w3']
    m['hy_filt_sc'] = np.ascontiguousarray(np.stack([inp['hy_sin_freq'], inp['hy_filt_b1'], inp['hy_filt_b2']], axis=-1))
    m['hy_b3_col'] = np.ascontiguousarray(np.stack([col_layout(inp['hy_filt_b3'][l]) for l in range(DEPTH)], 0))
    hw = np.concatenate([inp['hy_conv_w'], inp['hy_conv_b'][:, None, :]], axis=1)
    m['hy_conv_col'] = np.ascontiguousarray(hw.reshape(DEPTH, 4, 6, 128).transpose(0, 3, 2, 1))
    m['hy_d_bc'] = np.ascontiguousarray(np.broadcast_to(inp['hy_bias_d'][:, None, :, :], (DEPTH, 32, 2, 256)))
    m['hy_d_col'] = np.ascontiguousarray(inp['hy_bias_d'].reshape(DEPTH, 4, 128).transpose(0, 2, 1))
    m['w_out'] = inp['w_out']
    m['moe_wr'] = np.ascontiguousarray(np.concatenate([inp['moe_wg'], inp['moe_we']], axis=-1))
    rbv = np.concatenate([inp['moe_bg'], inp['moe_be']], axis=-1)
    m['moe_rb_bc'] = np.ascontiguousarray(np.broadcast_to(rbv[:, None, :], (DEPTH, 128, 36)))
    m['moe_w1'] = inp['moe_w1']
    m['moe_w3'] = inp['moe_w3']
    m['moe_w2'] = inp['moe_w2']
    m['final_bc'] = np.ascontiguousarray(np.broadcast_to(inp['final_norm_w'][None, :], (128, D)))
    m['da_lambda'] = np.ascontiguousarray(inp['da_lambda'].reshape(DEPTH, 256))
    m['da_subln_col'] = np.ascontiguousarray(inp['da_subln_w'][:, :, None])
    m['b_tm_bc'] = np.ascontiguousarray(np.broadcast_to(inp['b_in'][:, None, TM_COLS], (DEPTH, 128, NTM)))
    return m


def phase0(k, P, sbp):
    nc = k.nc
    cst = {}
    for name, shape in (('ident', [128, 128]), ('ropeR', [128, 128]), ('blockind', [128, 2])):
        k.din(name, shape)
    k.din('sel2', [2, 2, 128])
    k.din('cosT', [128, L])
    k.din('sinT', [128, L])
    P['ident32'] = sbp.t([128, 128], F32)
    P['identbf'] = sbp.t([128, 128], BF16)
    P['ropeRbf'] = sbp.t([128, 128], BF16)
    P['blockbf'] = sbp.t([128, 2], BF16)
    P['sel2'] = sbp.t([2, 2, 128], F32)
    P['ones32'] = sbp.t([128, 128], F32)
    P['onesbf'] = sbp.t([128, 128], BF16)
    tmp = sbp.t([128, 128], F32)
    k.dma('sp', [], ['ident32'], P['ident32'], k.dram['ident'][:, :])
    k.cp(['ident32'], ['identbf'], P['identbf'], P['ident32'])
    k.dma('sp', [], ['c_tmp'], tmp, k.dram['ropeR'][:, :])
    k.cp(['c_tmp'], ['ropeRbf'], P['ropeRbf'], tmp)
    k.dma('sp', ['c_tmp'], ['c_tmp'], tmp[:, 0:2], k.dram['blockind'][:, :])
    k.cp(['c_tmp'], ['blockbf'], P['blockbf'], tmp[:, 0:2])
    k.dma('sp', [], ['sel2'], P['sel2'], k.dram['sel2'][:, :, :])
    k.memset([], ['ones32'], P['ones32'], 1.0)
    k.memset([], ['onesbf'], P['onesbf'], 1.0)

    ccol = k.din('ccol', [128, 8, 2])
    adaw = k.din('ada_w', [DEPTH, D, 6 * D])
    adab = k.din('ada_b_col', [128, DEPTH, 48])
    n1 = k.din('norm1_col', [128, DEPTH, 8])
    n2 = k.din('norm2_col', [128, DEPTH, 8])
    P['mod'] = sbp.t([128, DEPTH, 48, 2], F32)
    P['A1'] = sbp.t([128, DEPTH, 8, 2], F32)
    P['A2'] = sbp.t([128, DEPTH, 8, 2], F32)
    cact = sbp.t([128, 8, 2], F32)
    adab_sb = sbp.t([128, DEPTH, 48], F32)
    n1_sb = sbp.t([128, DEPTH, 8], F32)
    n2_sb = sbp.t([128, DEPTH, 8], F32)
    k.dma('sp', [], ['cact'], cact, ccol[:, :, :])
    k.dma('sp', [], ['adab'], adab_sb, adab[:, :, :])
    k.dma('sp', [], ['n1'], n1_sb, n1[:, :, :])
    k.dma('sp', [], ['n2'], n2_sb, n2[:, :, :])
    k.act(['cact'], ['cact'], cact, cact, AF.Silu)
    sbl = SB(nc, base=sbp.off)
    stg = [sbl.t([128, 8, 512], F32) for _ in range(2)]
    for l in range(DEPTH):
        wv = adaw[l].rearrange("(k p) n -> p k n", p=128)
        pm = k.ps[0][:, 0:96].rearrange("p (c j) -> p c j", j=2)
        for cg in range(12):
            s = stg[cg % 2]
            sk = 'adastg%d' % (cg % 2)
            k.dma('sp', [], [sk], s, wv[:, :, cg * 512:(cg + 1) * 512])
            for j in range(4):
                c = cg * 4 + j
                for kk in range(8):
                    k.mm([sk, 'cact'], ['ps0'], pm[:, c, :], s[:, kk, j * 128:(j + 1) * 128], cact[:, kk, :],
                         start=(kk == 0), stop=(kk == 7))
        k.tt(['ps0', 'adab'], ['mod'], P['mod'][:, l], pm, adab_sb[:, l, :].unsqueeze(2).to_broadcast([128, 48, 2]), ALU.add)
        for (Aname, nsb, c0) in (('A1', n1_sb, 8), ('A2', n2_sb, 32)):
            k.ts(['mod'], [Aname], P[Aname][:, l], P['mod'][:, l, c0:c0 + 8, :], 1.0, op0=ALU.add)
            k.tt([Aname, 'n1', 'n2'], [Aname], P[Aname][:, l], P[Aname][:, l],
                 nsb[:, l, :].unsqueeze(2).to_broadcast([128, 8, 2]), ALU.mult)
    k.em.barrier()


def phaseA(k, P, l, base, xres):
    nc = k.nc
    sb = SB(nc, base=base)
    wfm = k.dram['w_in_fm']
    wtm = k.dram['w_in_tm']
    UT, QKT, TM = k.dram['UT'], k.dram['QKT'], k.dram['TM']
    Wfm = sb.t([128, 8, NFM], BF16)
    Wtm = sb.t([128, 8, NTM], BF16)
    bfm = sb.t([128, 18], F32)
    btm = sb.t([128, NTM], F32)
    cosT = sb.t([128, L], F32)
    sinT = sb.t([128, L], F32)
    normacc = sb.t([2, 8], F32)
    stg = [sb.t([128, 8, 512], F32) for _ in range(2)]
    k.dma('sp', [], ['bfm'], bfm, k.dram['b_fm_col'][:, l, :])
    k.dma('sp', [], ['btm'], btm, k.dram['b_tm_bc'][l])
    k.dma('sp', [], ['cosT'], cosT, k.dram['cosT'][:, :])
    k.dma('sp', [], ['sinT'], sinT, k.dram['sinT'][:, :])
    k.memset([], ['normacc'], normacc, 0.0)
    ci = 0
    for (src, dst, ncol, key) in ((wfm, Wfm, NFM, 'Wfm'), (wtm, Wtm, NTM, 'Wtm')):
        wv = src[l].rearrange("(k p) n -> p k n", p=128)
        for c0 in range(0, ncol, 512):
            c1 = min(ncol, c0 + 512)
            s = stg[ci % 2]
            sk = 'wstg%d' % (ci % 2)
            k.dma('sp', [], [sk], s[:, :, 0:c1 - c0], wv[:, :, c0:c1])
            k.cp([sk], [key], dst[:, :, c0:c1], s[:, :, 0:c1 - c0], eng=('dve' if ci % 2 == 0 else 'pool'))
            ci += 1
    xt = [sb.t([128, D], F32) for _ in range(2)]
    junk = sb.t([128, D], BF16)
    xn = [sb.t([128, D], BF16) for _ in range(2)]
    ss = [sb.t([128, 2], F32) for _ in range(2)]
    hT = [sb.t([128, 8, 512], BF16) for _ in range(2)]
    fmst = [sb.t([128, 512], F32) for _ in range(3)]
    qbf = [sb.t([128, 512], BF16) for _ in range(2)]
    t2 = [sb.t([128, 512], F32) for _ in range(2)]
    obf = [sb.t([128, 512], BF16) for _ in range(2)]
    sqbf = [sb.t([128, 512], BF16) for _ in range(2)]
    nmx = sb.t([2, 2], F32)
    tmst = [sb.t([128, NTM], F32) for _ in range(2)]
    A1, SH1 = P['A1'], P['mod']
    psi = 0
    tile_ctr = 0
    fm_ctr = 0
    rp_ctr = 0
    for g in range(9):
        t0 = g * 512
        ntok = 512 if g < 8 else 256
        j = 0 if g < 8 else 1
        hb = g % 2
        hk = 'hT%d' % hb
        for tl in range(ntok // 128):
            ti = t0 // 128 + tl
            xb = tile_ctr % 2
            tile_ctr += 1
            xk, nk, sk = 'xt%d' % xb, 'xn%d' % xb, 'ss%d' % xb
            k.dma('sp', [], [xk], xt[xb], xres[ti * 128:(ti + 1) * 128, :])
            k.memset([], [sk], ss[xb], 0.0)
            k.act([xk, sk], ['junk', sk], junk, xt[xb], AF.Square, accum_out=ss[xb][:, 0:1])
            k.ts([sk], [sk], ss[xb][:, 1:2], ss[xb][:, 0:1], 1.0 / D, EPS, op0=ALU.mult, op1=ALU.add)
            k.act([sk], [sk], ss[xb][:, 1:2], ss[xb][:, 1:2], AF.Sqrt)
            k.recip([sk], [sk], ss[xb][:, 1:2], ss[xb][:, 1:2])
            k.ts([xk, sk], [nk], xn[xb], xt[xb], ss[xb][:, 1:2], op0=ALU.mult)
            pk = 'ps%d' % psi
            pst = k.ps[psi][:].bitcast(BF16)
            psi = (psi + 1) % 8
            for kk in range(8):
                k.tr([nk, 'identbf'], [pk], pst[:, kk * 128:(kk + 1) * 128], xn[xb][:, kk * 128:(kk + 1) * 128], P['identbf'])
            for kk in range(8):
                k.act([pk, 'A1', 'mod'], [hk], hT[hb][:, kk, tl * 128:(tl + 1) * 128], pst[:, kk * 128:(kk + 1) * 128],
                      AF.Identity, scale=A1[:, l, kk, j:j + 1], bias=SH1[:, l, kk, j:j + 1])
        for jc in range(18):
            pk = 'ps%d' % psi
            pp = k.ps[psi]
            psi = (psi + 1) % 8
            for kk in range(8):
                k.mm(['Wfm', hk], [pk], pp[:, 0:ntok], Wfm[:, kk, jc * 128:(jc + 1) * 128], hT[hb][:, kk, 0:ntok],
                     start=(kk == 0), stop=(kk == 7))
            fb = fm_ctr % 3
            fm_ctr += 1
            fk = 'fmst%d' % fb
            k.act([pk, 'bfm'], [fk], fmst[fb][:, 0:ntok], pp[:, 0:ntok], AF.Identity, bias=bfm[:, jc:jc + 1], scale=1.0)
            if jc < 10:
                k.dma('pool', [fk], ['UT'], UT[jc * 128:(jc + 1) * 128, t0:t0 + ntok], fmst[fb][:, 0:ntok])
                continue
            rb = rp_ctr % 2
            rp_ctr += 1
            ok_, sqk = 'obf%d' % rb, 'sqbf%d' % rb
            if j == 0:
                qk_, tk_ = 'qbf%d' % rb, 't2%d' % rb
                k.cp([fk], [qk_], qbf[rb][:, 0:ntok], fmst[fb][:, 0:ntok], eng='pool')
                pk2 = 'ps%d' % psi
                pp2 = k.ps[psi]
                psi = (psi + 1) % 8
                k.mm(['ropeRbf', qk_], [pk2], pp2[:, 0:ntok], P['ropeRbf'], qbf[rb][:, 0:ntok])
                k.tt([pk2, 'sinT'], [tk_], t2[rb][:, 0:ntok], pp2[:, 0:ntok], sinT[:, t0:t0 + ntok], ALU.mult)
                k.tt([fk, 'cosT'], [fk], fmst[fb][:, 0:ntok], fmst[fb][:, 0:ntok], cosT[:, t0:t0 + ntok], ALU.mult, eng='pool')
                k.tt([fk, tk_], [fk], fmst[fb][:, 0:ntok], fmst[fb][:, 0:ntok], t2[rb][:, 0:ntok], ALU.add)
            k.cp([fk], [ok_], obf[rb][:, 0:ntok], fmst[fb][:, 0:ntok], eng='pool')
            k.dma('pool', [ok_], ['QKT'], QKT[(jc - 10) * 128:(jc - 9) * 128, t0:t0 + ntok], obf[rb][:, 0:ntok])
            k.act([fk], [sqk], sqbf[rb][:, 0:ntok], fmst[fb][:, 0:ntok], AF.Square)
            pk3 = 'ps%d' % psi
            pp3 = k.ps[psi]
            psi = (psi + 1) % 8
            k.mm(['blockbf', sqk], [pk3], pp3[0:2, 0:ntok], P['blockbf'], sqbf[rb][:, 0:ntok])
            k.red([pk3], ['nmx'], nmx[:, 0:1], pp3[0:2, 0:ntok], ALU.max)
            k.tt(['nmx', 'normacc'], ['normacc'], normacc[:, jc - 10:jc - 9], normacc[:, jc - 10:jc - 9], nmx[:, 0:1], ALU.max)
        for tl in range(ntok // 128):
            ti = t0 // 128 + tl
            tb = ti % 2
            tk = 'tmst%d' % tb
            for (c0, c1) in ((0, 512), (512, 1024), (1024, NTM)):
                pk = 'ps%d' % psi
                pp = k.ps[psi]
                psi = (psi + 1) % 8
                for kk in range(8):
                    k.mm(['Wtm', hk], [pk], pp[:, 0:c1 - c0], hT[hb][:, kk, tl * 128:(tl + 1) * 128], Wtm[:, kk, c0:c1],
                         start=(kk == 0), stop=(kk == 7))
                k.tt([pk, 'btm'], [tk], tmst[tb][:, c0:c1], pp[:, 0:c1 - c0], btm[:, c0:c1], ALU.add)
            k.dma('pool', [tk], ['TM'], TM[ti * 128:(ti + 1) * 128, :], tmst[tb])
    cn = sb.t([2, 4], F32)
    k.tt(['normacc'], ['cn'], cn, normacc[:, 0:4], normacc[:, 4:8], ALU.mult)
    k.act(['cn'], ['cn'], cn, cn, AF.Sqrt)
    k.ts(['cn'], ['cn'], cn, cn, -1.05 * 0.125, op0=ALU.mult)
    for m in range(2):
        pk = 'ps%d' % psi
        pp = k.ps[psi]
        psi = (psi + 1) % 8
        k.mm(['sel2', 'cn'], [pk], pp[:, 0:4], P['sel2'][:, m, :], cn)
        k.cp([pk], ['negc'], P['negc'][:, l, m, :], pp[:, 0:4])
    k.em.barrier()


def phaseB1(k, P, l, base, last):
    nc = k.nc
    sb = SB(nc, base=base)
    QKT, TM, YT = k.dram['QKT'], k.dram['TM'], k.dram['YT']
    lam_init = 0.8 - 0.6 * math.exp(-0.3 * l)
    QT = sb.t([128, 4, T], BF16)
    KTz = [sb.t([128, 4, T], BF16) for _ in range(2)]
    V = sb.t([128, NT, 512], BF16)
    vst = [sb.t([128, 512], F32) for _ in range(2)]
    k.dma('sp', [], ['QT'], QT, QKT[0:512, :].rearrange("(c p) t -> p c t", p=128))
    kv = QKT[512:1024, :].rearrange("(c p) t -> p c t", p=128)
    for m in range(2):
        lo, hi = m * 64, (m + 1) * 64
        zl, zh = (1 - m) * 64, (2 - m) * 64
        k.dma('sp', [], ['KT'], KTz[m][lo:hi], kv[lo:hi])
        k.memset([], ['KT'], KTz[m][zl:zh], 0.0, eng=('dve' if m == 0 else 'pool'))
    for ti in range(NT):
        vb = ti % 2
        vk = 'vst%d' % vb
        k.dma('sp', [], [vk], vst[vb], TM[ti * 128:(ti + 1) * 128, 512:1024])
        k.cp([vk], ['V'], V[:, ti, :], vst[vb], eng=('dve' if ti % 2 == 0 else 'pool'))
    lt = sb.t([1, 256], F32)
    lw = sb.t([1, 8], F32)
    neglam = sb.t([128, 1], F32)
    wsc = sb.t([128, 1], F32)
    k.dma('sp', [], ['lt'], lt, k.dram['da_lambda'][l:l + 1, :])
    k.dma('sp', [], ['wsc'], wsc, k.dram['da_subln_col'][l])
    k.ts(['wsc'], ['wsc'], wsc, wsc, 1.0 - lam_init, op0=ALU.mult)
    k.tt(['lt'], ['lt'], lt[:, 0:64], lt[:, 0:64], lt[:, 64:128], ALU.mult)
    k.tt(['lt'], ['lt'], lt[:, 128:192], lt[:, 128:192], lt[:, 192:256], ALU.mult)
    k.red(['lt'], ['lw'], lw[:, 0:1], lt[:, 0:64], ALU.add)
    k.red(['lt', 'lw'], ['lw'], lw[:, 1:2], lt[:, 128:192], ALU.add)
    k.act(['lw'], ['lw'], lw[:, 0:2], lw[:, 0:2], AF.Exp)
    k.tt(['lw'], ['lw'], lw[:, 2:3], lw[:, 1:2], lw[:, 0:1], ALU.subtract)
    k.ts(['lw'], ['lw'], lw[:, 2:3], lw[:, 2:3], -lam_init, op0=ALU.add)
    k.mm(['ones32', 'lw'], ['ps7'], k.ps[7][:, 0:1], P['ones32'][0:1, :], lw[:, 2:3])
    k.cp(['ps7'], ['neglam'], neglam, k.ps[7][:, 0:1])

    pt = [sb.t([128, 512], BF16) for _ in range(4)]
    racc = [sb.t([128, 512], F32) for _ in range(2)]
    rec = [sb.t([128, 512], F32) for _ in range(2)]
    o0 = sb.t([128, 512], F32)
    o1 = sb.t([128, 512], F32)
    sq = sb.t([128, 512], BF16)
    ybf = [sb.t([128, 512], BF16) for _ in range(2)]
    pti = 0
    si = 0
    si_box = [0]
    yi = 0
    chunks = [(g * 512, 512, list(range(NT))) for g in range(8)]
    if not last:
        chunks.append((L, CTX, [32, 33]))
    for h in range(4):
        for (q0, nq, blocks) in chunks:
            units = [(bi, kb, m) for bi, kb in enumerate(blocks) for m in range(2)]
            LOOK = 3
            issued = {}

            def issue_s(u):
                bi, kb, m = units[u]
                nonlocal_si = si_box[0]
                si_box[0] += 1
                psk = 'ps%d' % (4 + nonlocal_si % 4)
                pss = k.ps[4 + nonlocal_si % 4]
                k.mm(['KT', 'QT'], [psk], pss[:, 0:nq], KTz[m][:, h, kb * 128:(kb + 1) * 128], QT[:, h, q0:q0 + nq])
                issued[u] = (psk, pss)

            for u in range(min(LOOK, len(units))):
                issue_s(u)
            for u, (bi, kb, m) in enumerate(units):
                psk, pss = issued.pop(u)
                pk_ = 'pt%d' % (pti % 4)
                ptt = pt[pti % 4]
                pti += 1
                k.act([psk, 'negc'], [pk_], ptt[:, 0:nq], pss[:, 0:nq], AF.Exp, scale=0.125, bias=P['negc'][:, l, m, h:h + 1])
                if u + LOOK < len(units):
                    issue_s(u + LOOK)
                st, sp_ = (bi == 0), (bi == len(blocks) - 1)
                k.mm(['V', pk_], ['ps%d' % m], k.ps[m][:, 0:nq], V[:, kb, h * 128:(h + 1) * 128], ptt[:, 0:nq], start=st, stop=sp_)
                reng = 'dve' if m == 0 else 'pool'
                if st:
                    k.cp([pk_], ['racc%d' % m], racc[m][:, 0:nq], ptt[:, 0:nq], eng=reng)
                else:
                    k.tt([pk_, 'racc%d' % m], ['racc%d' % m], racc[m][:, 0:nq], racc[m][:, 0:nq], ptt[:, 0:nq], ALU.add, eng=reng)
            si = si_box[0]
            for m in range(2):
                k.mm(['ones32', 'racc%d' % m], ['ps%d' % (2 + m)], k.ps[2 + m][:, 0:nq], P['ones32'], racc[m][:, 0:nq])
            k.recip(['ps2'], ['rec0'], rec[0][:, 0:nq], k.ps[2][:, 0:nq])
            k.recip(['ps3'], ['rec1'], rec[1][:, 0:nq], k.ps[3][:, 0:nq])
            k.tt(['ps0', 'rec0'], ['o0'], o0[:, 0:nq], k.ps[0][:, 0:nq], rec[0][:, 0:nq], ALU.mult)
            k.tt(['ps1', 'rec1'], ['o1'], o1[:, 0:nq], k.ps[1][:, 0:nq], rec[1][:, 0:nq], ALU.mult)
            k.stt(['o0', 'o1', 'neglam'], ['o0'], o0[:, 0:nq], o1[:, 0:nq], neglam[:, 0:1], o0[:, 0:nq], ALU.mult, ALU.add)
            k.act(['o0'], ['sq'], sq[:, 0:nq], o0[:, 0:nq], AF.Square)
            psk = 'ps%d' % (4 + si % 4)
            pss = k.ps[4 + si % 4]
            si += 1
            si_box[0] = si
            k.mm(['onesbf', 'sq'], [psk], pss[:, 0:nq], P['onesbf'], sq[:, 0:nq])
            k.ts([psk], ['rec0'], rec[0][:, 0:nq], pss[:, 0:nq], 1.0 / 128.0, EPS, op0=ALU.mult, op1=ALU.add)
            k.act(['rec0'], ['rec0'], rec[0][:, 0:nq], rec[0][:, 0:nq], AF.Sqrt)
            k.recip(['rec0'], ['rec0'], rec[0][:, 0:nq], rec[0][:, 0:nq])
            k.tt(['o0', 'rec0'], ['o0'], o0[:, 0:nq], o0[:, 0:nq], rec[0][:, 0:nq], ALU.mult)
            yk = 'ybf%d' % (yi % 2)
            yb = ybf[yi % 2]
            yi += 1
            k.ts(['o0', 'wsc'], [yk], yb[:, 0:nq], o0[:, 0:nq], wsc[:, 0:1], op0=ALU.mult)
            k.dma('pool', [yk], ['YT'], YT[512 + h * 128:512 + (h + 1) * 128, q0:q0 + nq], yb[:, 0:nq])
    k.em.barrier()


NCH = T // 64


def phaseB2(k, P, l, base, last):
    nc = k.nc
    UT, TM, YT = k.dram['UT'], k.dram['TM'], k.dram['YT']
    sb0 = SB(nc, base=base)
    tri32 = sb0.t([64, 2, 64], F32)
    tribf = sb0.t([64, 2, 64], BF16)
    cw = sb0.t([64, 8, 4], F32)
    wbc = sb0.t([64, 256], F32)
    G = sb0.t([64, NCH, 16], F32)
    LF = sb0.t([64, 8, NCH], F32)
    IG = sb0.t([64, 8, NCH], F32)
    BB = sb0.t([64, 8, NCH], F32)
    BT = sb0.t([64, 8, NCH], F32)
    EB = sb0.t([64, 8, NCH], F32)
    WS = sb0.t([64, 8, NCH], F32)
    W2 = sb0.t([64, 8, NCH], F32)
    EBT = sb0.t([64, 8, NCH], F32)
    k.dma('sp', [], ['tri32'], tri32, k.dram['tri'].rearrange("a s t -> s a t"))
    k.cp(['tri32'], ['tribf'], tribf, tri32)
    k.dma('sp', [], ['cw'], cw, k.dram['ml_conv_col'][l])
    k.dma('sp', [], ['wbc'], wbc, k.dram['ml_norm_bc'][l])
    k.dma('sp', [], ['G'], G, TM[:, 1024:1040].rearrange("(n p) c -> p n c", p=64))
    for d in range(2):
        gi = G[:, :, d * 8:d * 8 + 4].rearrange("p n h -> p h n")
        gf = G[:, :, d * 8 + 4:d * 8 + 8].rearrange("p n h -> p h n")
        k.cp(['G'], ['IG'], IG[:, d * 4:d * 4 + 4, :], gi)
        k.act(['G'], ['LF'], LF[:, d * 4:d * 4 + 4, :], gf, AF.Exp, scale=-1.0)
    k.act(['LF'], ['LF'], LF, LF, AF.Ln, bias=1.0, scale=1.0)
    k.ts(['LF'], ['LF'], LF, LF, -1.0, op0=ALU.mult)
    for d in range(2):
        rhs = LF[:, d * 4:d * 4 + 4, :]
        k.mm(['tri32', 'LF'], ['ps0'], k.ps[0][0:64, 0:4 * NCH], tri32[:, d, :], rhs)
        k.cp(['ps0'], ['BB'], BB[:, d * 4:d * 4 + 4, :], k.ps[0][0:64, 0:4 * NCH].rearrange("p (h n) -> p h n", n=NCH))
        k.mm(['ones32', 'LF'], ['ps1'], k.ps[1][0:64, 0:4 * NCH], P['ones32'][0:64, 0:64], rhs)
        k.cp(['ps1'], ['BT'], BT[:, d * 4:d * 4 + 4, :], k.ps[1][0:64, 0:4 * NCH].rearrange("p (h n) -> p h n", n=NCH))
    k.act(['BB'], ['EB'], EB, BB, AF.Exp)
    k.act(['BT'], ['EBT'], EBT, BT, AF.Exp)
    k.tt(['IG', 'BB'], ['WS'], WS, IG, BB, ALU.subtract)
    k.tt(['WS', 'BT'], ['W2'], W2, WS, BT, ALU.add)
    k.act(['WS'], ['WS'], WS, WS, AF.Exp)
    k.act(['W2'], ['W2'], W2, W2, AF.Exp)
    base1 = sb0.off
    orders = [[64, 65, 66, 67] + list(range(64)), [67, 66, 65, 64] + list(range(63, -1, -1))]
    psi = [2]

    def nps():
        i = psi[0]
        psi[0] = 2 + (psi[0] - 1) % 6
        return 'ps%d' % i, k.ps[i]

    for hp in range(2):
        sb = SB(nc, base=base1)
        qT = sb.t([64, 2, T], BF16)
        kT = sb.t([64, 2, T], BF16)
        ktm = sb.t([64, NCH, 128], BF16)
        vaug = sb.t([64, NCH, 2, 65], BF16)
        hsum = sb.t([64, NCH, 128], F32)
        ra = sb.t([64, 2 * T], F32)
        raw = ra[:, 0:T]
        acc = ra[:, T:2 * T]
        k.memset([], ['hsum'], hsum, 0.0, eng='pool')
        k.memset([], ['vaug'], vaug, 1.0, eng='pool')
        for qk in range(2):
            for hl in range(2):
                hh = qk * 4 + hp * 2 + hl
                k.dma('sp', [], ['raw'], raw, UT[768 + hh * 64:768 + (hh + 1) * 64, :])
                k.ts(['raw', 'cw'], ['acc'], acc, raw, cw[:, hh, 1:2], cw[:, hh, 3:4], op0=ALU.mult, op1=ALU.add)
                for (a, b) in ((0, L), (L, T)):
                    k.stt(['raw', 'cw', 'acc'], ['acc'], acc[:, a + 1:b], raw[:, a:b - 1], cw[:, hh, 0:1], acc[:, a + 1:b], ALU.mult, ALU.add)
                    k.stt(['raw', 'cw', 'acc'], ['acc'], acc[:, a:b - 1], raw[:, a + 1:b], cw[:, hh, 2:3], acc[:, a:b - 1], ALU.mult, ALU.add)
                if qk == 0:
                    k.act(['acc'], ['qT'], qT[:, hl, :], acc, AF.Silu)
                else:
                    k.act(['acc'], ['acc'], acc, acc, AF.Silu)
                    k.ts(['acc'], ['kT'], kT[:, hl, :], acc, 0.125, op0=ALU.mult)
        for n0 in range(0, NCH, 4):
            pk, pp = nps()
            ppb = pp[:].bitcast(BF16)
            for dn in range(4):
                for hl in range(2):
                    k.tr(['kT', 'identbf'], [pk], ppb[0:64, (dn * 2 + hl) * 64:(dn * 2 + hl + 1) * 64],
                         kT[:, hl, (n0 + dn) * 64:(n0 + dn + 1) * 64], P['identbf'][0:64, 0:64])
            k.cp([pk], ['ktm'], ktm[:, n0:n0 + 4, :], ppb[0:64, 0:512].rearrange("p (n c) -> p n c", c=128))
        vst = acc[:, 0:17 * 128].rearrange("p (n c) -> p n c", c=128)
        for n0 in range(0, NCH, 17):
            k.dma('sp', ['acc'], ['acc'], vst, TM[n0 * 64:(n0 + 17) * 64, hp * 128:(hp + 1) * 128].rearrange("(n p) c -> p n c", p=64))
            k.cp(['acc'], ['vaug'], vaug[:, n0:n0 + 17, :, 0:64], vst.rearrange("p n (h e) -> p n h e", e=64))
        Cst = [[sb.t([64, 65], F32) for _ in range(2)] for _ in range(2)]
        Cbf = [[sb.t([64, 65], BF16) for _ in range(2)] for _ in range(2)]
        dg = [sb.t([64, 64], BF16) for _ in range(4)]
        meb = [sb.t([64, 64], F32) for _ in range(4)]
        pT = [sb.t([64, 64], BF16) for _ in range(4)]
        rsb = [sb.t([64, 65], F32) for _ in range(4)]
        tot = [sb.t([64, 66], F32) for _ in range(4)]
        wv = [sb.t([64, 65], BF16) for _ in range(4)]
        for d in range(2):
            for hl in range(2):
                k.memset([], ['Cst%d%d' % (d, hl)], Cst[d][hl], 0.0)
                k.memset([], ['Cbf%d%d' % (d, hl)], Cbf[d][hl], 0.0)
        def unit(step, d, hl):
            n = orders[d][step]
            c0 = n * 64
            need_out = (n < 64) or (not last)
            u = d * 2 + hl
            dh = d * 4 + hp * 2 + hl
            ck, cbk = 'Cst%d%d' % (d, hl), 'Cbf%d%d' % (d, hl)
            upd = step != NCH - 1
            kA, kB = 'ps%d' % (2 * u), 'ps%d' % (2 * u + 1)
            bA, bB = k.ps[2 * u], k.ps[2 * u + 1]
            if upd:
                k.act(['vaug', 'W2'], ['wv%d' % u], wv[u], vaug[:, n, hl, :], AF.Identity, scale=W2[:, dh, n:n + 1])
            if need_out:
                k.act(['identbf', 'EB'], ['dg%d' % u], dg[u], P['identbf'][0:64, 0:64], AF.Identity, scale=EB[:, dh, n:n + 1])
            yield
            if upd:
                k.mm(['ktm', 'wv%d' % u], [kB], bB[0:64, 0:65], ktm[:, n, hl * 64:(hl + 1) * 64], wv[u])
            if need_out:
                k.mm(['tribf', 'dg%d' % u], [kA], bA[0:64, 0:64], tribf[:, 1 - d, :], dg[u])
            yield
            if upd:
                k.stt([ck, 'EBT', kB], [ck], Cst[d][hl], Cst[d][hl], EBT[:, dh, n:n + 1], bB[0:64, 0:65], ALU.mult, ALU.add)
            if need_out:
                k.cp([kA], ['meb%d' % u], meb[u], bA[0:64, 0:64], eng='act')
            yield
            if need_out:
                k.mm(['kT', 'qT'], [kA], bA[0:64, 0:64], kT[:, hl, c0:c0 + 64], qT[:, hl, c0:c0 + 64])
                k.mm(['qT', cbk], [kB], bB[0:64, 0:65], qT[:, hl, c0:c0 + 64], Cbf[d][hl])
            yield
            if need_out:
                k.act([kB, 'EB'], ['rsb%d' % u], rsb[u], bB[0:64, 0:65], AF.Identity, scale=EB[:, dh, n:n + 1])
                k.stt([kA, 'WS', 'meb%d' % u], ['pT%d' % u], pT[u], bA[0:64, 0:64], WS[:, dh, n:n + 1], meb[u], ALU.mult, ALU.mult)
            if upd:
                k.cp([ck], [cbk], Cbf[d][hl], Cst[d][hl], eng='act')
            yield
            if not need_out:
                return
            k.mm(['pT%d' % u, 'vaug'], [kA], bA[0:64, 0:65], pT[u], vaug[:, n, hl, :])
            yield
            k.tt([kA, 'rsb%d' % u], ['tot%d' % u], tot[u][:, 0:65], bA[0:64, 0:65], rsb[u], ALU.add)
            yield
            k.act(['tot%d' % u], ['tot%d' % u], tot[u][:, 65:66], tot[u][:, 64:65], AF.Abs)
            yield
            k.ts(['tot%d' % u], ['tot%d' % u], tot[u][:, 65:66], tot[u][:, 65:66], 1.0, op0=ALU.max)
            yield
            k.recip(['tot%d' % u], ['tot%d' % u], tot[u][:, 65:66], tot[u][:, 65:66])
            yield
            hs = hsum[:, n, hl * 64:(hl + 1) * 64]
            k.stt(['tot%d' % u, 'hsum'], ['hsum'], hs, tot[u][:, 0:64], tot[u][:, 65:66], hs, ALU.mult, ALU.add)

        for step in range(NCH):
            gens = [unit(step, d, hl) for d in range(2) for hl in range(2)]
            while gens:
                alive = []
                for g_ in gens:
                    try:
                        next(g_)
                        alive.append(g_)
                    except StopIteration:
                        pass
                gens = alive
        nout = 64 if last else NCH
        ssum = sb.t([64, NCH * 2], F32)
        ybf = sb.t([64, NCH, 128], BF16)
        ytb = [sb.t([128, 512], BF16) for _ in range(2)]
        k.act(['hsum', 'raw', 'acc'], ['acc', 'raw'], ra, hsum.rearrange("p n c -> p (n c)"), AF.Square)
        k.red(['acc', 'raw'], ['ssum'], ssum, ra.rearrange("p (g e) -> p g e", e=64), ALU.add)
        k.ts(['ssum'], ['ssum'], ssum, ssum, 1.0 / 64.0, EPS, op0=ALU.mult, op1=ALU.add)
        k.act(['ssum'], ['ssum'], ssum, ssum, AF.Sqrt)
        k.recip(['ssum'], ['ssum'], ssum, ssum)
        hv = hsum.rearrange("p n (h e) -> p (n h) e", e=64)
        k.tt(['hsum', 'ssum'], ['hsum'], hv, hv, ssum.unsqueeze(2).to_broadcast([64, NCH * 2, 64]), ALU.mult)
        k.tt(['hsum', 'wbc'], ['hsum'], hsum, hsum, wbc[:, hp * 128:(hp + 1) * 128].unsqueeze(1).to_broadcast([64, NCH, 128]), ALU.mult)
        ost = acc[:, 0:17 * 128].rearrange("p (n c) -> p n c", c=128)
        for n0 in range(0, NCH, 17):
            k.dma('sp', ['acc'], ['acc'], ost, TM[n0 * 64:(n0 + 17) * 64, 256 + hp * 128:256 + (hp + 1) * 128].rearrange("(n p) c -> p n c", p=64))
            k.act(['acc'], ['acc'], ost, ost, AF.Sigmoid)
            k.tt(['acc', 'hsum'], ['ybf'], ybf[:, n0:n0 + 17, :], hsum[:, n0:n0 + 17, :], ost, ALU.mult)
        for gi, n0 in enumerate(range(0, nout, 8)):
            nn = min(8, nout - n0)
            pk, pp = nps()
            ppb = pp[:].bitcast(BF16)
            for dn in range(nn):
                k.tr(['ybf', 'identbf'], [pk], ppb[:, dn * 64:(dn + 1) * 64], ybf[:, n0 + dn, :], P['identbf'][0:64, 0:64])
            yk = 'ytb%d' % (gi % 2)
            k.cp([pk], [yk], ytb[gi % 2][:, 0:nn * 64], ppb[:, 0:nn * 64])
            k.dma('pool', [yk], ['YT'], YT[256 + hp * 128:256 + (hp + 1) * 128, n0 * 64:(n0 + nn) * 64], ytb[gi % 2][:, 0:nn * 64])
        k.em.barrier()


HC = 32
KB = 256 // HC
NB6 = 512 // HC
NHB = 256 // HC
PI = math.pi
HSTOP = [99]


def hyena_consts():
    c = {}
    f32 = np.float32

    def zfeat(Lx, pos):
        t = np.linspace(0.0, 1.0, Lx, dtype=f32)[pos][:, None]
        w = ((2.0 * math.pi / Lx) * np.arange(Lx, dtype=f32))[pos][:, None]
        f = np.linspace(1e-4, 15.0, 16, dtype=f32)[None, :]
        z = np.concatenate([t, np.cos(f * w), -np.sin(f * w)], axis=-1).astype(f32)
        return z, t
    deltas = np.abs(np.linspace(math.log(1e-2) / 1.5, math.log(1e-2) / 0.3, 256, dtype=f32)).astype(f32)
    z, t = zfeat(L, np.arange(L))
    c['hy_z'] = np.ascontiguousarray(z.T)
    c['hy_decay'] = np.ascontiguousarray(np.exp(-t * deltas[None, :]).T.astype(f32))
    pos = np.concatenate([np.arange(CTX - 1, 0, -1), np.arange(CTX)])
    zc, tc = zfeat(CTX, pos)
    c['hy_zc'] = np.ascontiguousarray(zc.T)
    c['hy_decayc'] = np.ascontiguousarray(np.exp(-tc * deltas[None, :]).T.astype(f32))
    n1 = np.arange(32)[:, None]
    k1 = np.arange(64)[None, :]
    a = 2 * np.pi * n1 * k1 / 64.0
    c['hy_F1'] = np.concatenate([np.cos(a), -np.sin(a)], 1).astype(f32)
    n2 = np.arange(128)[:, None, None]
    kk = (np.arange(64)[None, :, None] + 64 * np.arange(128)[None, None, :])
    a = 2 * np.pi * ((n2 * kk) % 8192) / 8192.0
    c['hy_Gr'] = np.cos(a).astype(f32).reshape(128, 8192)
    c['hy_Gi'] = (-np.sin(a)).astype(f32).reshape(128, 8192)
    k2 = np.arange(128)[:, None]
    nn = np.arange(128)[None, :]
    a = 2 * np.pi * ((k2 * nn) % 128) / 128.0
    c['hy_E1'] = np.concatenate([np.cos(a), np.sin(a)], 1).astype(f32)
    c['hy_E2'] = np.concatenate([-np.sin(a), np.cos(a)], 1).astype(f32)
    k1 = np.arange(64)[:, None, None]
    nfull = np.arange(128)[None, :, None] + 128 * np.arange(32)[None, None, :]
    a = 2 * np.pi * ((k1 * nfull) % 8192) / 8192.0
    c['hy_Mr'] = np.cos(a).astype(f32).reshape(64, 4096)
    c['hy_nMi'] = (-np.sin(a)).astype(f32).reshape(64, 4096)
    return c


def hy_load_bf(k, sb, name, shape, stg, key):
    p, n = shape
    dst = sb.t([p, n], BF16)
    src = k.dram[name]
    step = 2048
    for i, c0 in enumerate(range(0, n, step)):
        c1 = min(n, c0 + step)
        k.dma('sp', [], ['hstg'], stg[0:p, 0:c1 - c0], src[:, c0:c1])
        k.cp(['hstg'], [key], dst[:, c0:c1], stg[0:p, 0:c1 - c0], eng=('dve' if i % 2 == 0 else 'pool'))
    return dst


def fft_fwd(k, C, xbf, A, nAi, psctr, consume):
    F1, Gr, Gi = C['F1'], C['Gr'], C['Gi']
    for c0 in range(0, HC, 4):
        pk, pp = psctr()
        for dc in range(4):
            k.mm(['xbf', 'F1'], [pk], pp[:, dc * 128:(dc + 1) * 128], xbf[:, c0 + dc, :], F1)
        src = pp[:, 0:512].rearrange("p (c r q) -> p c r q", r=2, q=64)
        k.cp([pk], ['A'], A[:, c0:c0 + 4, :, :], src, eng='act')
        k.ts(['A'], ['nAi'], nAi[:, c0:c0 + 4, :], A[:, c0:c0 + 4, 1, :], -1.0, op0=ALU.mult)
    for k0 in range(0, 64, KB):
        pk, pp = psctr()
        for dk in range(KB):
            k1 = k0 + dk
            xr = pp[:, dk * 2 * HC:dk * 2 * HC + HC]
            xi = pp[:, dk * 2 * HC + HC:(dk + 1) * 2 * HC]
            k.mm(['Gr', 'A'], [pk], xr, Gr[:, k1, :], A[:, :, 0, k1], start=True, stop=False)
            k.mm(['Gi', 'nAi'], [pk], xr, Gi[:, k1, :], nAi[:, :, k1], start=False, stop=True)
            k.mm(['Gi', 'A'], [pk], xi, Gi[:, k1, :], A[:, :, 0, k1], start=True, stop=False)
            k.mm(['Gr', 'A'], [pk], xi, Gr[:, k1, :], A[:, :, 1, k1], start=False, stop=True)
        consume(pk, pp, k0)


def phaseH(k, P, l, base, last):
    nc = k.nc
    UT, YT = k.dram['UT'], k.dram['YT']
    HK, HH, CK, UC = k.dram['HK'], k.dram['HH'], k.dram['CK'], k.dram['UC']
    psi = [0]

    def nps():
        i = psi[0]
        psi[0] = (psi[0] + 1) % 8
        return 'ps%d' % i, k.ps[i]

    sb = SB(nc, base=base)
    w1 = sb.t([33, 64], F32)
    w2 = sb.t([64, 64], F32)
    w3 = sb.t([64, 1024], F32)
    sc = sb.t([64, 8], F32)
    b3 = sb.t([128, 8], F32)
    k.dma('sp', [], ['w1'], w1, k.dram['hy_filt_w1'][l])
    k.dma('sp', [], ['w2'], w2, k.dram['hy_filt_w2'][l])
    k.dma('sp', [], ['w3'], w3, k.dram['hy_filt_w3'][l])
    k.dma('sp', [], ['sc'], sc[:, 0:3], k.dram['hy_filt_sc'][l])
    k.dma('sp', [], ['b3'], b3, k.dram['hy_b3_col'][l])
    k.tt(['sc'], ['sc'], sc[:, 3:4], sc[:, 0:1], sc[:, 1:2], ALU.mult)
    k.tt(['sc'], ['sc'], sc[:, 4:5], sc[:, 0:1], sc[:, 2:3], ALU.mult)
    zT = sb.t([33, L], F32)
    h2 = sb.t([64, L], F32)
    h1 = sb.t([64, 512], F32)
    m1 = sb.t([64, 512], F32)
    m2 = sb.t([64, 512], F32)

    def sin_layer(src_ap, wt, kdim, bcol, dst_ap, n):
        pk, pp = nps()
        k.mm(['w1', 'w2', 'zT', 'h1'], [pk], pp[0:64, 0:n], wt, src_ap)
        k.ts([pk, 'sc'], ['m0'], dst_ap, pp[0:64, 0:n], sc[:, 0:1], sc[:, bcol:bcol + 1], op0=ALU.mult, op1=ALU.add)
        k.ts(['m0'], ['m1'], m1[:, 0:n], dst_ap, PI, -2.0 * PI, op0=ALU.is_gt, op1=ALU.mult)
        k.ts(['m0'], ['m2'], m2[:, 0:n], dst_ap, -PI, 2.0 * PI, op0=ALU.is_lt, op1=ALU.mult, eng='pool')
        k.tt(['m1', 'm2'], ['m1'], m1[:, 0:n], m1[:, 0:n], m2[:, 0:n], ALU.add)
        k.tt(['m0', 'm1'], ['m0'], dst_ap, dst_ap, m1[:, 0:n], ALU.add)
        k.act(['m0'], ['m0'], dst_ap, dst_ap, AF.Sin)

    def mlp(zsrc_name, ncols, h2dst):
        k.dma('sp', ['zT'], ['zT'], zT[:, 0:ncols], k.dram[zsrc_name][:, :])
        for c0 in range(0, ncols, 512):
            n = min(512, ncols - c0)
            sin_layer(zT[:, c0:c0 + n], w1, 33, 3, h1[:, 0:n], n)
            sin_layer2(c0, n, h2dst)

    def sin_layer2(c0, n, h2dst):
        pk, pp = nps()
        k.mm(['w2', 'm0'], [pk], pp[0:64, 0:n], w2, h1[:, 0:n])
        d = h2dst[:, c0:c0 + n]
        k.ts([pk, 'sc'], ['h2'], d, pp[0:64, 0:n], sc[:, 0:1], sc[:, 4:5], op0=ALU.mult, op1=ALU.add)
        k.ts(['h2'], ['m1'], m1[:, 0:n], d, PI, -2.0 * PI, op0=ALU.is_gt, op1=ALU.mult)
        k.ts(['h2'], ['m2'], m2[:, 0:n], d, -PI, 2.0 * PI, op0=ALU.is_lt, op1=ALU.mult, eng='pool')
        k.tt(['m1', 'm2'], ['m1'], m1[:, 0:n], m1[:, 0:n], m2[:, 0:n], ALU.add)
        k.tt(['h2', 'm1'], ['h2'], d, d, m1[:, 0:n], ALU.add)
        k.act(['h2'], ['h2'], d, d, AF.Sin)

    dec = [sb.t([128, L], F32) for _ in range(2)]
    kraw = [sb.t([128, L], F32) for _ in range(2)]
    kbfo = [sb.t([128, L], BF16) for _ in range(2)]
    junk = sb.t([128, L], BF16)
    asum = sb.t([128, 4], F32)

    def gen_filters(zname, dname, ncols, ctx):
        mlp(zname, ncols, h2)
        for ch in range(2):
            k.dma('sp', ['dec%d' % ch], ['dec%d' % ch], dec[ch][:, 0:ncols], k.dram[dname][ch * 128:(ch + 1) * 128, :])
        for o in range(2):
            for ch in range(2):
                k.memset([], ['asum'], asum, 0.0)
                for d in range(2):
                    fc = o * 4 + d * 2 + ch
                    kr = kraw[d]
                    kk_ = 'kraw%d' % d
                    for c0 in range(0, ncols, 512):
                        n = min(512, ncols - c0)
                        pk, pp = nps()
                        k.mm(['w3', 'h2'], [pk], pp[:, 0:n], w3[:, fc * 128:(fc + 1) * 128], h2[:, c0:c0 + n])
                        k.act([pk, 'b3'], [kk_], kr[:, c0:c0 + n], pp[:, 0:n], AF.Identity, bias=b3[:, fc:fc + 1], scale=1.0)
                    k.tt([kk_, 'dec%d' % ch], [kk_], kr[:, 0:ncols], kr[:, 0:ncols], dec[ch][:, 0:ncols], ALU.mult)
                    if not ctx:
                        if d == 1:
                            k.memset([kk_], [kk_], kr[:, 0:1], 0.0)
                        k.act([kk_, 'asum'], ['junk', 'asum'], junk[:, 0:ncols], kr[:, 0:ncols], AF.Abs, accum_out=asum[:, d:d + 1])
                    else:
                        lo, hi = (255, 511) if d == 0 else (0, 255)
                        k.act([kk_, 'asum'], ['junk', 'asum'], junk[:, lo:hi], kr[:, lo:hi], AF.Abs, accum_out=asum[:, d:d + 1])
                k.tt(['asum'], ['asum'], asum[:, 2:3], asum[:, 0:1], asum[:, 1:2], ALU.add)
                k.recip(['asum'], ['asum'], asum[:, 2:3], asum[:, 2:3])
                if not ctx:
                    for d in range(2):
                        fc = o * 4 + d * 2 + ch
                        k.ts(['kraw%d' % d, 'asum'], ['kbfo%d' % d], kbfo[d], kraw[d], asum[:, 2:3], op0=ALU.mult,
                             eng=('dve' if d == 0 else 'pool'))
                        k.dma('pool', ['kbfo%d' % d], ['HK'], HK[fc], kbfo[d])
                else:
                    k.ts(['kraw0', 'asum'], ['kraw0'], kraw[0][:, 255:511], kraw[0][:, 255:511], asum[:, 2:3], op0=ALU.mult)
                    k.ts(['kraw1', 'asum', 'kraw0'], ['kraw0'], kraw[0][:, 0:255], kraw[1][:, 0:255], asum[:, 2:3], op0=ALU.mult)
                    k.dma('pool', ['kraw0'], ['CK'], CK[o * 2 + ch], kraw[0][:, 0:511])

    gen_filters('hy_z', 'hy_decay', L, False)
    if not last:
        gen_filters('hy_zc', 'hy_decayc', 511, True)
    k.em.barrier()

    if HSTOP[0] <= 1:
        return
    sb = SB(nc, base=base)
    stg = sb.t([128, 2048], F32)
    C = {}
    C['F1'] = hy_load_bf(k, sb, 'hy_F1', [32, 128], stg, 'F1')
    C['Gr'] = hy_load_bf(k, sb, 'hy_Gr', [128, 8192], stg, 'Gr').rearrange("p (q m) -> p q m", m=128)
    C['Gi'] = hy_load_bf(k, sb, 'hy_Gi', [128, 8192], stg, 'Gi').rearrange("p (q m) -> p q m", m=128)
    C['E1'] = hy_load_bf(k, sb, 'hy_E1', [128, 256], stg, 'E1')
    C['E2'] = hy_load_bf(k, sb, 'hy_E2', [128, 256], stg, 'E2')
    C['Mr'] = hy_load_bf(k, sb, 'hy_Mr', [64, 4096], stg, 'Mr').rearrange("p (n m) -> p n m", m=32)
    C['nMi'] = hy_load_bf(k, sb, 'hy_nMi', [64, 4096], stg, 'nMi').rearrange("p (n m) -> p n m", m=32)
    A = sb.t([128, HC, 2, 64], BF16)
    nAi = sb.t([128, HC, 64], BF16)
    base2 = sb.off
    if HSTOP[0] <= 1.5:
        k.em.barrier()
        return

    sbs = SB(nc, base=base2)
    kbf = [sbs.t([32, HC, 128], BF16) for _ in range(2)]
    Hacc = sbs.t([128, 64, 2, HC], F32)
    Hbf = sbs.t([128, 64, 2, HC], BF16)
    xtmp = sbs.t([128, KB, 2, HC], F32)
    SC = 1.0 / 8192.0
    for o in range(2):
        for b4 in range(NHB):
            ch, coff = (b4 * HC) // 128, (b4 * HC) % 128
            for d in range(2):
                fc = o * 4 + d * 2 + ch
                k.dma('sp', ['xbf'], ['xbf'], kbf[d], HK[fc][coff:coff + HC, :].rearrange("c (a b) -> a c b", b=128))

                def consume(pk, pp, k0, d=d):
                    src = pp[:, 0:512].rearrange("p (q r c) -> p q r c", r=2, c=HC)
                    dst = Hacc[:, k0:k0 + KB, :, :]
                    if d == 0:
                        k.act([pk], ['Hacc'], dst, src, AF.Copy, scale=SC)
                    else:
                        k.act([pk], ['xtmp'], xtmp, src, AF.Copy, scale=SC)
                        k.tt(['xtmp', 'Hacc'], ['Hacc'], dst[:, :, 0, :], dst[:, :, 0, :], xtmp[:, :, 0, :], ALU.add)
                        k.tt(['xtmp', 'Hacc'], ['Hacc'], dst[:, :, 1, :], dst[:, :, 1, :], xtmp[:, :, 1, :], ALU.subtract, eng='pool')
                fft_fwd(k, C, kbf[d], A, nAi, nps, consume)
            k.cp(['Hacc'], ['Hbf'], Hbf, Hacc, eng='pool')
            k.dma('pool', ['Hbf'], ['HH'], HH[o * NHB + b4], Hbf.rearrange("p q r c -> p (q r c)"))
    k.em.barrier()

    if HSTOP[0] <= 2:
        return
    sbc = SB(nc, base=base2)
    raw = sbc.t([128, T], F32)
    acc = sbc.t([128, T], F32)
    ucb = sbc.t([128, T], BF16)
    cw = sbc.t([128, 6, 4], F32)
    k.dma('sp', [], ['cw'], cw, k.dram['hy_conv_col'][l])
    for cc in range(6):
        k.dma('sp', ['raw'], ['raw'], raw, UT[cc * 128:(cc + 1) * 128, :])
        k.ts(['raw', 'cw'], ['acc'], acc, raw, cw[:, cc, 1:2], cw[:, cc, 3:4], op0=ALU.mult, op1=ALU.add)
        for (a, b) in ((0, L), (L, T)):
            k.stt(['raw', 'cw', 'acc'], ['acc'], acc[:, a + 1:b], raw[:, a:b - 1], cw[:, cc, 0:1], acc[:, a + 1:b], ALU.mult, ALU.add)
            k.stt(['raw', 'cw', 'acc'], ['acc'], acc[:, a:b - 1], raw[:, a + 1:b], cw[:, cc, 2:3], acc[:, a:b - 1], ALU.mult, ALU.add)
        k.cp(['acc'], ['ucb'], ucb, acc, eng='act')
        k.dma('pool', ['ucb'], ['UC'], UC[cc * 128:(cc + 1) * 128, :], ucb)
    k.em.barrier()

    if HSTOP[0] <= 3:
        return
    sbd = SB(nc, base=base2)
    vbf = sbd.t([32, HC, 128], BF16)
    x1bf = sbd.t([32, HC, 128], BF16)
    x2bf = sbd.t([32, HC, 128], BF16)
    z1 = sbd.t([32, HC, 128], BF16)
    z2 = sbd.t([32, HC, 128], BF16)
    dv = sbd.t([32, HC, 128], BF16)
    dbc = sbd.t([32, 2, 256], F32)
    Hs = sbd.t([128, 64, 2, HC], BF16)
    Y = sbd.t([128, 64, 2, HC], BF16)
    Zs = sbd.t([64, HC, 2, 128], BF16)
    xs = [sbd.t([128, KB, 2, HC], F32) for _ in range(2)]
    ta = [sbd.t([128, KB, HC], F32) for _ in range(2)]
    tb = [sbd.t([128, KB, HC], F32) for _ in range(2)]
    tg = [sbd.t([32, HC, NB6], F32) for _ in range(2)]
    k.dma('sp', [], ['dbc'], dbc, k.dram['hy_d_bc'][l])
    xctr = [0]
    for b4 in range(NHB):
        c0g = b4 * HC
        for (tile_, key, r0) in ((vbf, 'xbf', 0), (x1bf, 'x1bf', 256), (x2bf, 'x2bf', 512)):
            k.dma('sp', [key], [key], tile_, UC[r0 + c0g:r0 + c0g + HC, 0:L].rearrange("c (a b) -> a c b", b=128))
        for o in range(2):
            xin, xkey = (vbf, 'xbf') if o == 0 else (z1, 'z1')
            gate, gkey = (x1bf, 'x1bf') if o == 0 else (x2bf, 'x2bf')
            zout, zkey = (z1, 'z1') if o == 0 else (z2, 'z2')
            k.tt([xkey, 'dbc'], ['dv'], dv, xin, dbc[:, o, c0g:c0g + HC].unsqueeze(2).to_broadcast([32, HC, 128]), ALU.mult, eng='pool')
            k.dma('sp', ['Hs'], ['Hs'], Hs.rearrange("p q r c -> p (q r c)"), HH[o * NHB + b4])

            def consume(pk, pp, k0):
                i = xctr[0] % 2
                xctr[0] += 1
                xk, tak, tbk = 'xs%d' % i, 'ta%d' % i, 'tb%d' % i
                k.cp([pk], [xk], xs[i], pp[:, 0:512].rearrange("p (q r c) -> p q r c", r=2, c=HC), eng='act')
                Xr, Xi = xs[i][:, :, 0, :], xs[i][:, :, 1, :]
                Hr, Hi = Hs[:, k0:k0 + KB, 0, :], Hs[:, k0:k0 + KB, 1, :]
                Yr, Yi = Y[:, k0:k0 + KB, 0, :], Y[:, k0:k0 + KB, 1, :]
                k.tt([xk, 'Hs'], [tak], ta[i], Xr, Hr, ALU.mult)
                k.tt([xk, 'Hs'], [tbk], tb[i], Xi, Hi, ALU.mult, eng='pool')
                k.tt([tak, tbk], ['Y'], Yr, ta[i], tb[i], ALU.subtract)
                k.tt([xk, 'Hs'], [tak], ta[i], Xr, Hi, ALU.mult, eng='pool')
                k.tt([xk, 'Hs'], [tbk], tb[i], Xi, Hr, ALU.mult)
                k.tt([tak, tbk], ['Y'], Yi, ta[i], tb[i], ALU.add, eng='pool')
            fft_fwd_keyed(k, C, xin, xkey, A, nAi, nps, consume)
            for c0 in range(0, HC, 2):
                pk, pp = nps()
                for dc in range(2):
                    cidx = c0 + dc
                    out = pp[0:64, dc * 256:(dc + 1) * 256]
                    k.mm(['Y', 'E1'], [pk], out, Y[:, :, 0, cidx], C['E1'], start=True, stop=False)
                    k.mm(['Y', 'E2'], [pk], out, Y[:, :, 1, cidx], C['E2'], start=False, stop=True)
                src = pp[0:64, 0:512].rearrange("p (c r n) -> p c r n", r=2, n=128)
                k.cp([pk], ['Zs'], Zs[:, c0:c0 + 2, :, :], src, eng=('act' if (c0 // 2) % 2 == 0 else 'dve'))
            for g8, n0 in enumerate(range(0, 128, NB6)):
                pk, pp = nps()
                for dn in range(NB6):
                    n2 = n0 + dn
                    out = pp[0:32, dn * HC:(dn + 1) * HC]
                    k.mm(['Mr', 'Zs'], [pk], out, C['Mr'][:, n2, :], Zs[:, :, 0, n2], start=True, stop=False)
                    k.mm(['nMi', 'Zs'], [pk], out, C['nMi'][:, n2, :], Zs[:, :, 1, n2], start=False, stop=True)
                i = g8 % 2
                src = pp[0:32, 0:NB6 * HC].rearrange("p (n c) -> p c n", c=HC)
                k.tt([pk, 'dv'], ['tg%d' % i], tg[i], src, dv[:, :, n0:n0 + NB6], ALU.add)
                k.tt(['tg%d' % i, gkey], [zkey], zout[:, :, n0:n0 + NB6], tg[i], gate[:, :, n0:n0 + NB6], ALU.mult, eng='pool')
        k.dma('pool', ['z2'], ['YT'], YT[c0g:c0g + HC, 0:L].rearrange("c (a b) -> a c b", b=128), z2)
    k.em.barrier()

    if last or HSTOP[0] <= 4:
        return
    sbx = SB(nc, base=base2)
    ub = sbx.t([128, 3, CTX], BF16)
    uf = sbx.t([128, 3, CTX], F32)
    kf = sbx.t([128, 511], F32)
    accs = [sbx.t([128, CTX], F32) for _ in range(4)]
    zc = sbx.t([128, CTX], F32)
    zb = sbx.t([128, CTX], BF16)
    dcol = sbx.t([128, 4], F32)
    k.dma('sp', [], ['dcol'], dcol, k.dram['hy_d_col'][l])
    for ch in range(2):
        for j in range(3):
            k.dma('sp', ['ub'], ['ub'], ub[:, j, :], UC[j * 256 + ch * 128:j * 256 + (ch + 1) * 128, L:T])
        k.cp(['ub'], ['uf'], uf, ub)
        for o in range(2):
            uin = uf[:, 0, :] if o == 0 else zc
            gate = uf[:, 1 + o, :]
            k.dma('sp', ['kf'], ['kf'], kf, CK[o * 2 + ch])
            for a in range(4):
                k.memset([], ['acc%d' % a], accs[a], 0.0, eng=('dve' if a < 2 else 'pool'))
            for s_ in range(CTX):
                a = s_ % 4
                k.stt(['kf', 'uf', 'zc', 'acc%d' % a], ['acc%d' % a], accs[a], kf[:, 255 - s_:511 - s_], uin[:, s_:s_ + 1], accs[a],
                      ALU.mult, ALU.add)
            k.tt(['acc0', 'acc1'], ['acc0'], accs[0], accs[0], accs[1], ALU.add)
            k.tt(['acc2', 'acc3'], ['acc2'], accs[2], accs[2], accs[3], ALU.add, eng='pool')
            k.tt(['acc0', 'acc2'], ['acc0'], accs[0], accs[0], accs[2], ALU.add)
            k.stt(['uf', 'zc', 'dcol', 'acc0'], ['acc0'], accs[0], uin, dcol[:, o * 2 + ch:o * 2 + ch + 1], accs[0], ALU.mult, ALU.add)
            k.tt(['acc0', 'uf'], ['zc'], zc, accs[0], gate, ALU.mult)
        k.cp(['zc'], ['zb'], zb, zc)
        k.dma('pool', ['zb'], ['YT'], YT[ch * 128:(ch + 1) * 128, L:T], zb)
    k.em.barrier()


def fft_fwd_keyed(k, C, xin, xkey, A, nAi, psctr, consume):
    F1, Gr, Gi = C['F1'], C['Gr'], C['Gi']
    for c0 in range(0, HC, 4):
        pk, pp = psctr()
        for dc in range(4):
            k.mm([xkey, 'F1'], [pk], pp[:, dc * 128:(dc + 1) * 128], xin[:, c0 + dc, :], F1)
        src = pp[:, 0:512].rearrange("p (c r q) -> p c r q", r=2, q=64)
        k.cp([pk], ['A'], A[:, c0:c0 + 4, :, :], src, eng='act')
        k.ts(['A'], ['nAi'], nAi[:, c0:c0 + 4, :], A[:, c0:c0 + 4, 1, :], -1.0, op0=ALU.mult)
    for k0 in range(0, 64, KB):
        pk, pp = psctr()
        for dk in range(KB):
            k1 = k0 + dk
            xr = pp[:, dk * 2 * HC:dk * 2 * HC + HC]
            xi = pp[:, dk * 2 * HC + HC:(dk + 1) * 2 * HC]
            k.mm(['Gr', 'A'], [pk], xr, Gr[:, k1, :], A[:, :, 0, k1], start=True, stop=False)
            k.mm(['Gi', 'nAi'], [pk], xr, Gi[:, k1, :], nAi[:, :, k1], start=False, stop=True)
            k.mm(['Gi', 'A'], [pk], xi, Gi[:, k1, :], A[:, :, 0, k1], start=True, stop=False)
            k.mm(['Gr', 'A'], [pk], xi, Gr[:, k1, :], A[:, :, 1, k1], start=False, stop=True)
        consume(pk, pp, k0)


BIG = 1.0e9
I32 = mybir.dt.int32


def moe_sched(ntiles):
    Tl = ntiles * 128
    J = [max(1, -(-min(Tl, (2 * Tl) // (r + 1)) // 128)) for r in range(32)]
    S = [0]
    for j in J:
        S.append(S[-1] + j * 128)
    return J, S


NSLOT = moe_sched(NT)[1][-1]


def phaseC(k, P, l, base, last, xres):
    nc = k.nc
    em = k.em
    YT, XMIX, XRES = k.dram['YT'], k.dram['XMIX'], k.dram['XRES']
    XG, YE, H2M = k.dram['XG'], k.dram['YE'], k.dram['H2M']
    ntiles = 32 if last else NT
    J, S = moe_sched(ntiles)
    psi = [0]

    def nps():
        i = psi[0]
        psi[0] = (psi[0] + 1) % 8
        return 'ps%d' % i, k.ps[i]

    def idma(reads, writes, fn):
        i = em.dnext
        em.dnext = (i + 1) % em.NDMA
        if em.dcnt[i] > 0:
            em._wait('pool', (('d', i), 16 * em.dcnt[i]))
        em._deps('pool', reads, writes)
        ins = fn(em.eng['pool'])
        em.dcnt[i] += 1
        ins.then_inc(em.dsem[i], 16)
        em._record((('d', i), 16 * em.dcnt[i]), reads, writes)
        em.ninst += 1

    sb0 = SB(nc, base=base)
    grow = sb0.t([128, 2, 2, D], F32)
    mrow = sb0.t([128, 2, 2, D], F32)
    GW = sb0.t([128, NT, 2], F32)
    SIDX = sb0.t([128, NT, 2], I32)
    IDX = sb0.t([128, 32, 12], I32)
    tri128 = sb0.t([128, 128], F32)
    iota32 = sb0.t([128, 32], F32)
    ltmask = sb0.t([128, 1024], F32)
    pidx = sb0.t([128, 1], F32)
    Srow = sb0.t([128, 32], F32)
    dg = sb0.t([128, 128], F32)
    k.dma('sp', [], ['tri128'], tri128, k.dram['tri128'][:, :])
    k.dma('sp', [], ['iota32'], iota32, k.dram['iota32'][:, :])
    k.dma('sp', [], ['ltmask'], ltmask, k.dram['ltmask'][:, :])
    k.dma('sp', [], ['pidx'], pidx, k.dram['pidx'][:, :])
    k.dma('sp', [], ['Srow'], Srow, k.dram['moe_S'][0 if ntiles == NT else 1])
    for gi, c0 in enumerate((16, 40)):
        for j in range(2):
            for kk in range(8):
                k.ts(['ident32', 'mod'], ['dg'], dg, P['ident32'], P['mod'][:, l, c0 + kk, j:j + 1], op0=ALU.mult)
                pk, pp = nps()
                k.mm(['ones32', 'dg'], [pk], pp[:, 0:128], P['ones32'], dg)
                k.cp([pk], ['grow'], grow[:, gi, j, kk * 128:(kk + 1) * 128], pp[:, 0:128], eng='act')
    for mi in range(2):
        for j in range(2):
            for kk in range(8):
                col = P['A2'][:, l, kk, j:j + 1] if mi == 0 else P['mod'][:, l, 24 + kk, j:j + 1]
                k.ts(['ident32', 'mod', 'A2'], ['dg'], dg, P['ident32'], col, op0=ALU.mult)
                pk, pp = nps()
                k.mm(['ones32', 'dg'], [pk], pp[:, 0:128], P['ones32'], dg)
                k.cp([pk], ['mrow'], mrow[:, mi, j, kk * 128:(kk + 1) * 128], pp[:, 0:128], eng='act')
    base1 = sb0.off
    sb = SB(nc, base=base1)
    Wout = sb.t([128, 8, D], BF16)
    Wr = sb.t([128, 8, 36], F32)
    rb = sb.t([128, 36], F32)
    stg = sb.t([128, 8, 512], F32)
    wv = k.dram['w_out'][l].rearrange("(k p) n -> p k n", p=128)
    for i, c0 in enumerate((0, 512)):
        k.dma('sp', ['stg'], ['stg'], stg, wv[:, :, c0:c0 + 512])
        k.cp(['stg'], ['Wout'], Wout[:, :, c0:c0 + 512], stg)
    k.dma('sp', [], ['Wr'], Wr, k.dram['moe_wr'][l].rearrange("(k p) n -> p k n", p=128))
    k.dma('sp', [], ['rb'], rb, k.dram['moe_rb_bc'][l])
    yT = [sb.t([128, 8, 512], BF16) for _ in range(2)]
    xt = [sb.t([128, D], F32) for _ in range(2)]
    xm = [sb.t([128, D], F32) for _ in range(2)]
    xn = sb.t([128, D], F32)
    tmpm = sb.t([128, D], F32)
    hm = [sb.t([128, D], BF16) for _ in range(2)]
    junk = sb.t([128, D], BF16)
    h32 = sb.t([128, 8, 128], F32)
    ss = [sb.t([128, 2], F32) for _ in range(2)]
    lg = sb.t([128, 36], F32)
    rt = sb.t([128, 16], F32)
    oh = sb.t([128, 4], F32)
    ml = sb.t([128, 32], F32)
    e1 = sb.t([128, 32], F32)
    e2 = sb.t([128, 32], F32)
    esum = sb.t([128, 32], F32)
    tmp32 = sb.t([128, 32], F32)
    basec = sb.t([128, 32], F32)
    erank = sb.t([128, 32], F32)
    SE = sb.t([128, 32], F32)
    EID = sb.t([128, 32], F32)
    idxf = sb.t([128, 2, 32], F32)
    idx2 = sb.t([128, 32, 12], F32)
    EH = sb.t([128, 2, NT, 32], F32)
    RK = sb.t([128, NT, 32], F32)
    tA = sb.t([128, NT, 32], F32)
    sidf = sb.t([128, NT, 2], F32)
    k.memset([], ['basec'], basec, 0.0)
    k.memset([], ['EH'], EH, 0.0)
    k.memset([], ['RK'], RK, 0.0)
    for ti in range(ntiles):
        j = 0 if ti < 32 else 1
        g, tl = ti // 4, ti % 4
        yb = g % 2
        yk = 'yT%d' % yb
        if tl == 0:
            n = min(512, ntiles * 128 - g * 512)
            k.dma('sp', [yk], [yk], yT[yb][:, :, 0:n], YT[:, g * 512:g * 512 + n].rearrange("(c p) t -> p c t", p=128))
        b = ti % 2
        xk, mk, sk, hk = 'xt%d' % b, 'xm%d' % b, 'ss%d' % b, 'hm%d' % b
        k.dma('sp', [xk], [xk], xt[b], xres[ti * 128:(ti + 1) * 128, :])
        for half in range(2):
            pk, pp = nps()
            for f in range(8):
                k.mm([yk, 'Wout'], [pk], pp[:, 0:512], yT[yb][:, f, tl * 128:(tl + 1) * 128], Wout[:, f, half * 512:(half + 1) * 512],
                     start=(f == 0), stop=(f == 7))
            cs = slice(half * 512, (half + 1) * 512)
            k.tt([pk, 'grow'], [mk], xm[b][:, cs], pp[:, 0:512], grow[:, 0, j, cs], ALU.mult)
            k.tt([mk, xk], [mk], xm[b][:, cs], xm[b][:, cs], xt[b][:, cs], ALU.add, eng='pool')
        k.dma('pool', [mk], ['XMIX'], XMIX[ti * 128:(ti + 1) * 128, :], xm[b])
        k.memset([], [sk], ss[b], 0.0)
        k.act([mk, sk], ['junk', sk], junk, xm[b], AF.Square, accum_out=ss[b][:, 0:1])
        k.ts([sk], [sk], ss[b][:, 1:2], ss[b][:, 0:1], 1.0 / D, EPS, op0=ALU.mult, op1=ALU.add)
        k.act([sk], [sk], ss[b][:, 1:2], ss[b][:, 1:2], AF.Sqrt)
        k.recip([sk], [sk], ss[b][:, 1:2], ss[b][:, 1:2])
        k.ts([mk, sk], ['xn'], xn, xm[b], ss[b][:, 1:2], op0=ALU.mult)
        k.tt(['xn', 'mrow'], ['tmpm'], tmpm, xn, mrow[:, 0, j, :], ALU.mult)
        k.tt(['tmpm', 'mrow'], [hk], hm[b], tmpm, mrow[:, 1, j, :], ALU.add, eng='pool')
        k.dma('sp', [hk], ['H2M%d' % ti], H2M[ti * 128:(ti + 1) * 128, :], hm[b])
        for h2 in range(2):
            pk, pp = nps()
            for q in range(4):
                kk = h2 * 4 + q
                k.tr(['xn', 'ident32'], [pk], pp[:, q * 128:(q + 1) * 128], xn[:, kk * 128:(kk + 1) * 128], P['ident32'])
            for q in range(4):
                kk = h2 * 4 + q
                k.act([pk, 'A2', 'mod'], ['h32'], h32[:, kk, :], pp[:, q * 128:(q + 1) * 128], AF.Identity,
                      scale=P['A2'][:, l, kk, j:j + 1], bias=P['mod'][:, l, 24 + kk, j:j + 1])
        pk, pp = nps()
        for kk in range(8):
            k.mm(['h32', 'Wr'], [pk], pp[:, 0:36], h32[:, kk, :], Wr[:, kk, :], start=(kk == 0), stop=(kk == 7))
        k.tt([pk, 'rb'], ['lg'], lg, pp[:, 0:36], rb, ALU.add)
        k.red(['lg'], ['rt'], rt[:, 0:1], lg[:, 0:4], ALU.max)
        k.ts(['lg', 'rt'], ['oh'], oh, lg[:, 0:4], rt[:, 0:1], op0=ALU.is_equal)
        k.ts(['rt'], ['rt'], rt[:, 1:2], rt[:, 0:1], -1.0, op0=ALU.mult)
        k.memset(['rt'], ['rt'], rt[:, 2:3], 0.0)
        k.act(['lg', 'rt'], ['tmp32', 'rt'], tmp32[:, 0:4], lg[:, 0:4], AF.Exp, bias=rt[:, 1:2], scale=1.0, accum_out=rt[:, 2:3])
        k.recip(['rt'], ['rt'], rt[:, 3:4], rt[:, 2:3])
        k.ts(['oh'], ['oh'], oh, oh, 1.0, BIG, op0=ALU.subtract, op1=ALU.mult)
        k.tt(['lg', 'oh'], ['ml'], ml.rearrange("p (g e) -> p g e", e=8), lg[:, 4:36].rearrange("p (g e) -> p g e", e=8),
             oh.unsqueeze(2).to_broadcast([128, 4, 8]), ALU.add)
        k.red(['ml'], ['rt'], rt[:, 4:5], ml, ALU.max)
        k.ts(['ml', 'rt'], ['e1'], e1, ml, rt[:, 4:5], op0=ALU.is_equal)
        k.ts(['e1'], ['tmp32'], tmp32, e1, -BIG, op0=ALU.mult)
        k.tt(['ml', 'tmp32'], ['ml'], ml, ml, tmp32, ALU.add)
        k.red(['ml'], ['rt'], rt[:, 5:6], ml, ALU.max)
        k.ts(['ml', 'rt'], ['e2'], e2, ml, rt[:, 5:6], op0=ALU.is_equal)
        k.tt(['rt'], ['rt'], rt[:, 6:7], rt[:, 5:6], rt[:, 4:5], ALU.subtract)
        k.act(['rt'], ['rt'], rt[:, 6:7], rt[:, 6:7], AF.Exp)
        k.ts(['rt'], ['rt'], rt[:, 7:8], rt[:, 6:7], 1.0, op0=ALU.add)
        k.recip(['rt'], ['rt'], rt[:, 7:8], rt[:, 7:8])
        k.tt(['rt'], ['rt'], rt[:, 8:9], rt[:, 6:7], rt[:, 7:8], ALU.mult)
        k.tt(['rt'], ['rt'], rt[:, 9:10], rt[:, 7:8], rt[:, 3:4], ALU.mult)
        k.tt(['rt'], ['rt'], rt[:, 10:11], rt[:, 8:9], rt[:, 3:4], ALU.mult)
        k.cp(['rt'], ['GW'], GW[:, ti, :], rt[:, 9:11], eng='pool')
        k.cp(['e1'], ['EH'], EH[:, 0, ti, :], e1, eng='pool')
        k.cp(['e2'], ['EH'], EH[:, 1, ti, :], e2, eng='pool')
        k.tt(['e1', 'e2'], ['esum'], esum, e1, e2, ALU.add)
        pk, pp = nps()
        k.mm(['tri128', 'esum'], [pk], pp[:, 0:32], tri128, esum)
        k.mm(['ones32', 'esum'], [pk], pp[:, 32:64], P['ones32'], esum)
        k.tt([pk, 'basec'], ['RK'], RK[:, ti, :], pp[:, 0:32], basec, ALU.add)
        k.tt([pk, 'basec'], ['basec'], basec, basec, pp[:, 32:64], ALU.add)
    stgf = stg.rearrange("p a b -> p (a b)")
    A3 = stgf[:, 0:1024].rearrange("p (a b) -> p a b", b=32)
    B3 = stgf[:, 1024:2048].rearrange("p (a b) -> p a b", b=32)
    C3 = stgf[:, 2048:3072].rearrange("p (a b) -> p a b", b=32)
    cnt_o = basec.unsqueeze(1).to_broadcast([128, 32, 32])
    cnt_s = basec.unsqueeze(2).to_broadcast([128, 32, 32])
    k.tt(['basec', 'stg'], ['stg'], A3, cnt_o, cnt_s, ALU.is_gt)
    k.tt(['basec', 'stg'], ['stg'], B3, cnt_o, cnt_s, ALU.is_equal)
    k.tt(['stg', 'ltmask'], ['stg'], B3, B3, ltmask.rearrange("p (a b) -> p a b", b=32), ALU.mult)
    k.tt(['stg'], ['stg'], A3, A3, B3, ALU.add)
    k.red(['stg'], ['erank'], erank, A3, ALU.add)
    k.tt(['erank', 'iota32', 'stg'], ['stg'], A3, erank.unsqueeze(2).to_broadcast([128, 32, 32]),
         iota32.unsqueeze(1).to_broadcast([128, 32, 32]), ALU.is_equal)
    k.tt(['stg', 'Srow'], ['stg'], B3, A3, Srow.unsqueeze(1).to_broadcast([128, 32, 32]), ALU.mult)
    k.red(['stg'], ['SE'], SE, B3, ALU.add)
    k.tt(['stg', 'iota32'], ['stg'], C3, A3, iota32.unsqueeze(2).to_broadcast([128, 32, 32]), ALU.mult)
    k.red(['stg'], ['EID'], EID, C3.rearrange("p e r -> p r e"), ALU.add)
    k.ts(['EID'], ['idxf'], idxf[:, 0, :], EID, 1024.0, op0=ALU.mult)
    k.ts(['EID'], ['idxf'], idxf[:, 1, :], EID, 512.0, op0=ALU.mult)
    k.ts(['idxf', 'pidx'], ['idxf'], idxf, idxf, pidx[:, 0:1], op0=ALU.add)
    for q in range(12):
        off = float(l * 32 * D + q * 128) if q < 8 else float(l * 32 * 512 + (q - 8) * 128)
        k.ts(['idxf'], ['idx2'], idx2[:, :, q], idxf[:, 0 if q < 8 else 1, :], off, op0=ALU.add)
    k.cp(['idx2'], ['IDX'], IDX, idx2)
    for kc in range(2):
        k.tt(['RK', 'SE'], ['tA'], tA, RK, SE.unsqueeze(1).to_broadcast([128, NT, 32]), ALU.add)
        k.tt(['tA', 'EH'], ['tA'], tA, tA, EH[:, kc], ALU.mult)
        k.red(['tA'], ['sidf'], sidf[:, :, kc], tA, ALU.add)
    k.cp(['sidf'], ['SIDX'], SIDX, sidf)
    for ti in range(ntiles):
        b = ti % 2
        hk = 'hm%d' % b
        k.dma('sp', ['H2M%d' % ti], [hk], hm[b], H2M[ti * 128:(ti + 1) * 128, :])
        for kc in range(2):
            idma([hk, 'SIDX'], ['XGs%d_%d' % (ti, kc)], lambda e, ti=ti, kc=kc, b=b: e.indirect_dma_start(
                out=XG[:, :], out_offset=bass.IndirectOffsetOnAxis(ap=SIDX[:, ti, kc:kc + 1], axis=0),
                in_=hm[b], in_offset=None))
    k.em.barrier()
    sb = SB(nc, base=base1)
    wst = [sb.t([128, 8, 512], F32) for _ in range(2)]
    w1b = [sb.t([128, 8, 512], BF16) for _ in range(2)]
    w3b = [sb.t([128, 8, 512], BF16) for _ in range(2)]
    w2b = [sb.t([128, 4, D], BF16) for _ in range(2)]
    xg_tm = [sb.t([128, 4, D], BF16) for _ in range(2)]
    xgT = [sb.t([128, 8, 512], BF16) for _ in range(2)]
    gT = [sb.t([128, 4, 512], BF16) for _ in range(2)]
    st = [sb.t([128, 512], F32) for _ in range(2)]
    ysb = [sb.t([128, D], F32) for _ in range(2)]
    W1r = k.dram['moe_w1'].rearrange("l e d n -> (l e d) n")
    W3r = k.dram['moe_w3'].rearrange("l e d n -> (l e d) n")
    W2r = k.dram['moe_w2'].rearrange("l e f n -> (l e f) n")
    sctr = [0]
    cctr = [0]
    yctr = [0]

    def load_w(src_rows, nk, rowlen, r, q0, dst, dkey, ceng):
        i = sctr[0] % 2
        sctr[0] += 1
        keys = ['wst%d_%d' % (i, q) for q in range(8)]
        view = wst[i].rearrange("p a b -> p (a b)").rearrange("p (a b) -> p a b", b=rowlen)
        per = 8 // nk
        for kk in range(nk):
            idma(['IDX'], keys[kk * per:(kk + 1) * per], lambda e, kk=kk: e.indirect_dma_start(
                out=view[:, kk, :], out_offset=None, in_=src_rows[:, :],
                in_offset=bass.IndirectOffsetOnAxis(ap=IDX[:, r, q0 + kk:q0 + kk + 1], axis=0)))
        k.cp(keys, [dkey], dst, view, eng=ceng)

    for r in range(32):
        wb = r % 2
        load_w(W1r, 8, 512, r, 0, w1b[wb], 'w1b%d' % wb, 'act')
        load_w(W3r, 8, 512, r, 0, w3b[wb], 'w3b%d' % wb, 'dve')
        load_w(W2r, 4, D, r, 8, w2b[wb], 'w2b%d' % wb, 'act')
        nrow = J[r] * 128
        for c0 in range(0, nrow, 512):
            n = min(512, nrow - c0)
            nt_ = n // 128
            gb = cctr[0] % 2
            cctr[0] += 1
            xk, tk, gk = 'xgtm%d' % gb, 'xgT%d' % gb, 'gT%d' % gb
            r0 = S[r] + c0
            k.dma('sp', [], [xk], xg_tm[gb][:, 0:nt_, :], XG[r0:r0 + n, :].rearrange("(t p) d -> p t d", p=128))
            for t in range(nt_):
                pk, pp = nps()
                pst = pp[:].bitcast(BF16)
                for kk in range(8):
                    k.tr([xk, 'identbf'], [pk], pst[:, kk * 128:(kk + 1) * 128], xg_tm[gb][:, t, kk * 128:(kk + 1) * 128], P['identbf'])
                k.cp([pk], [tk], xgT[gb][:, :, t * 128:(t + 1) * 128], pst[:, 0:1024].rearrange("p (a b) -> p a b", b=128),
                     eng=('act' if t % 2 == 0 else 'dve'))
            for f in range(4):
                p1k, pp1 = nps()
                for kk in range(8):
                    k.mm(['w1b%d' % wb, tk], [p1k], pp1[:, 0:n], w1b[wb][:, kk, f * 128:(f + 1) * 128], xgT[gb][:, kk, 0:n],
                         start=(kk == 0), stop=(kk == 7))
                p3k, pp3 = nps()
                for kk in range(8):
                    k.mm(['w3b%d' % wb, tk], [p3k], pp3[:, 0:n], w3b[wb][:, kk, f * 128:(f + 1) * 128], xgT[gb][:, kk, 0:n],
                         start=(kk == 0), stop=(kk == 7))
                sbi = f % 2
                k.act([p1k], ['st%d' % sbi], st[sbi][:, 0:n], pp1[:, 0:n], AF.Silu)
                k.tt(['st%d' % sbi, p3k], [gk], gT[gb][:, f, 0:n], st[sbi][:, 0:n], pp3[:, 0:n], ALU.mult)
            for tl in range(nt_):
                yi = yctr[0] % 2
                yctr[0] += 1
                yk = 'ysb%d' % yi
                for half in range(2):
                    pk, pp = nps()
                    for f in range(4):
                        k.mm([gk, 'w2b%d' % wb], [pk], pp[:, 0:512], gT[gb][:, f, tl * 128:(tl + 1) * 128], w2b[wb][:, f, half * 512:(half + 1) * 512],
                             start=(f == 0), stop=(f == 3))
                    k.cp([pk], [yk], ysb[yi][:, half * 512:(half + 1) * 512], pp[:, 0:512], eng=('act' if half == 0 else 'dve'))
                k.dma('sp', [yk], ['YE%d' % yctr[0]], YE[r0 + tl * 128:r0 + (tl + 1) * 128, :], ysb[yi])
    k.em.barrier()
    sb = SB(nc, base=base1)
    ya = [sb.t([128, D], F32) for _ in range(2)]
    yb2 = [sb.t([128, D], F32) for _ in range(2)]
    xo = [sb.t([128, D], F32) for _ in range(2)]
    fw = sb.t([128, D], F32)
    fj = sb.t([128, D], BF16)
    fs = sb.t([128, 2], F32)
    if last:
        k.dma('sp', [], ['fw'], fw, k.dram['final_bc'][:, :])
    for ti in range(ntiles):
        j = 0 if ti < 32 else 1
        b = ti % 2
        ok, ak, bk = 'xo%d' % b, 'ya%d' % b, 'yb%d' % b
        k.dma('sp', [], [ok], xo[b], XMIX[ti * 128:(ti + 1) * 128, :])
        idma(['SIDX'], [ak], lambda e, ti=ti, b=b: e.indirect_dma_start(
            out=ya[b], out_offset=None, in_=YE[:, :], in_offset=bass.IndirectOffsetOnAxis(ap=SIDX[:, ti, 0:1], axis=0)))
        idma(['SIDX'], [bk], lambda e, ti=ti, b=b: e.indirect_dma_start(
            out=yb2[b], out_offset=None, in_=YE[:, :], in_offset=bass.IndirectOffsetOnAxis(ap=SIDX[:, ti, 1:2], axis=0)))
        k.ts([ak, 'GW'], [ak], ya[b], ya[b], GW[:, ti, 0:1], op0=ALU.mult)
        k.stt([bk, 'GW', ak], [ak], ya[b], yb2[b], GW[:, ti, 1:2], ya[b], ALU.mult, ALU.add)
        k.tt([ak, 'grow'], [ak], ya[b], ya[b], grow[:, 1, j, :], ALU.mult, eng='pool')
        k.tt([ak, ok], [ok], xo[b], xo[b], ya[b], ALU.add)
        if not last:
            k.dma('pool', [ok], ['XRES'], XRES[ti * 128:(ti + 1) * 128, :], xo[b])
        else:
            k.memset([], ['fs'], fs, 0.0)
            k.act([ok, 'fs'], ['fj', 'fs'], fj, xo[b], AF.Square, accum_out=fs[:, 0:1])
            k.ts(['fs'], ['fs'], fs[:, 1:2], fs[:, 0:1], 1.0 / D, EPS, op0=ALU.mult, op1=ALU.add)
            k.act(['fs'], ['fs'], fs[:, 1:2], fs[:, 1:2], AF.Sqrt)
            k.recip(['fs'], ['fs'], fs[:, 1:2], fs[:, 1:2])
            k.ts([ok, 'fs'], [ok], xo[b], xo[b], fs[:, 1:2], op0=ALU.mult)
            k.tt([ok, 'fw'], [ok], xo[b], xo[b], fw, ALU.mult)
            k.dma('pool', [ok], ['out'], k.dram['out'][ti * 128:(ti + 1) * 128, :], xo[b])
    k.em.barrier()


def build(stage='full', debug=()):
    nc = bass.Bass("TRN2", target_bir_lowering=False)
    k = K(nc, debug=debug)
    P = {}
    sbp = SB(nc)
    k.din('xin', [T, D])
    k.din('w_in_fm', [DEPTH, D, NFM])
    k.din('w_in_tm', [DEPTH, D, NTM])
    k.din('b_fm_col', [128, DEPTH, 18])
    k.din('b_tm_bc', [DEPTH, 128, NTM])
    k.dscratch('UT', [1280, T])
    k.dscratch('QKT', [1024, T], BF16)
    k.dscratch('TM', [T, NTM])
    k.dscratch('XRES', [T, D])
    k.dscratch('YT', [1024, T], BF16)
    k.din('da_lambda', [DEPTH, 256])
    k.dscratch('XMIX', [T, D])
    k.dscratch('H2M', [T, D], BF16)
    k.dscratch('XG', [NSLOT, D], BF16)
    k.dscratch('YE', [NSLOT, D])
    for nm, shp in (('tri128', [128, 128]), ('iota32', [128, 32]), ('ltmask', [128, 1024]), ('pidx', [128, 1]), ('moe_S', [2, 128, 32])):
        k.din(nm, shp)
    k.din('w_out', [DEPTH, D, D])
    k.din('moe_wr', [DEPTH, D, 36])
    k.din('moe_rb_bc', [DEPTH, 128, 36])
    k.din('moe_w1', [DEPTH, 32, D, 512])
    k.din('moe_w3', [DEPTH, 32, D, 512])
    k.din('moe_w2', [DEPTH, 32, 512, D])
    k.din('final_bc', [128, D])
    if stage == 'full':
        k.dout('out', [L, D])
    k.dscratch('HK', [8, 128, L], BF16)
    k.dscratch('HH', [2 * NHB, 128, 64 * 2 * HC], BF16)
    k.dscratch('CK', [4, 128, 511])
    k.dscratch('UC', [768, T], BF16)
    for nm, shp in (('hy_filt_w1', [DEPTH, 33, 64]), ('hy_filt_w2', [DEPTH, 64, 64]), ('hy_filt_w3', [DEPTH, 64, 1024]),
                    ('hy_filt_sc', [DEPTH, 64, 3]), ('hy_b3_col', [DEPTH, 128, 8]), ('hy_conv_col', [DEPTH, 128, 6, 4]),
                    ('hy_d_bc', [DEPTH, 32, 2, 256]), ('hy_d_col', [DEPTH, 128, 4]),
                    ('hy_z', [33, L]), ('hy_decay', [256, L]), ('hy_zc', [33, 511]), ('hy_decayc', [256, 511]),
                    ('hy_F1', [32, 128]), ('hy_Gr', [128, 8192]), ('hy_Gi', [128, 8192]), ('hy_E1', [128, 256]),
                    ('hy_E2', [128, 256]), ('hy_Mr', [64, 4096]), ('hy_nMi', [64, 4096])):
        k.din(nm, shp)
    k.din('tri', [2, 64, 64])
    k.din('ml_conv_col', [DEPTH, 64, 8, 4])
    k.din('ml_norm_bc', [DEPTH, 64, 256])
    k.din('da_subln_col', [DEPTH, 128, 1])
    phase0(k, P, sbp)
    P['negc'] = sbp.t([128, DEPTH, 2, 4], F32)
    base = sbp.off
    zt = SB(nc, base=base).t([128, 8 * D], BF16)
    k.memset([], ['zt'], zt, 0.0)
    XGv = k.dram['XG'].rearrange("(a p r) d -> a p (r d)", p=128, r=8)
    for a in range(NSLOT // 1024):
        k.dma('sp' if a % 2 == 0 else 'pool', ['zt'], ['XGz%d' % a], XGv[a], zt)
    k.em.barrier()
    for l in range(DEPTH):
        xres = k.dram['xin'] if l == 0 else k.dram['XRES']
        phaseA(k, P, l, base, xres)
        if stage == 'A':
            break
        if stage not in ('B2', 'H'):
            phaseB1(k, P, l, base, l == DEPTH - 1)
        if stage == 'B1':
            break
        if stage != 'H':
            phaseB2(k, P, l, base, l == DEPTH - 1)
        if stage == 'B2':
            break
        phaseH(k, P, l, base, l == DEPTH - 1)
        if stage == 'H':
            break
        phaseC(k, P, l, base, (l == DEPTH - 1) and stage == 'full', xres)
        if stage == 'C':
            break
    k.em.barrier()
    return nc, k


def run(inputs, stage='full', debug=(), cores=8):
    consts = make_consts()
    nc, k = build(stage, debug)
    in_maps = []
    for b in range(cores):
        m = prep_inputs(inputs, b)
        m.update(consts)
        in_maps.append({kk: v for kk, v in m.items() if kk in k.dram})
    res = run_bass_kernel_spmd(nc, in_maps, core_ids=list(range(cores)))
    return res.results


def kernel(**inputs):
    inp = {kk: np.asarray(v) for kk, v in inputs.items()}
    res = run(inp, stage='full', cores=8)
    return np.stack([np.asarray(r['out'], dtype=np.float32) for r in res], axis=0)
```

```python
import math
import os
import numpy as np
import concourse.bass as bass
import concourse.mybir as mybir
from concourse.bass_utils import run_bass_kernel_spmd

F32 = mybir.dt.float32
BF16 = mybir.dt.bfloat16
AF = mybir.ActivationFunctionType
ALU = mybir.AluOpType
AX = mybir.AxisListType

D = 1024
L = 4096
CTX = 256
T = L + CTX
NT = T // 128
DEPTH = 2
EPS = 1e-6
N_IN = 3344
ML_OFF = 768
DA_OFF = 1808
NFM = 2304
NTM = 1040
FM_COLS = list(range(0, 768)) + list(range(768, 1280)) + list(range(1808, 2832))
TM_COLS = list(range(1280, 1792)) + list(range(2832, 3344)) + list(range(1792, 1808))


class Em:
    NDMA = 32
    SAME_ENGINE_WAITS = True

    def __init__(self, nc):
        self.nc = nc
        self.eng = {'pe': nc.tensor, 'act': nc.scalar, 'dve': nc.vector, 'pool': nc.gpsimd, 'sp': nc.sync}
        self.sem = {k: nc.alloc_semaphore('s_' + k) for k in ('pe', 'act', 'dve', 'pool')}
        self.cnt = {k: 0 for k in self.sem}
        self.dsem = [nc.alloc_semaphore('s_dma%d' % i) for i in range(self.NDMA)]
        self.dcnt = [0] * self.NDMA
        self.dnext = 0
        self.waited = {e: {} for e in self.eng}
        self.lastw = {}
        self.readers = {}
        self.ninst = 0

    def _semh(self, key):
        return self.sem[key] if isinstance(key, str) else self.dsem[key[1]]

    def _wait(self, e, ev):
        key, val = ev
        w = self.waited[e]
        if w.get(key, 0) >= val:
            return
        self.eng[e].wait_ge(self._semh(key), val)
        w[key] = val

    def _deps(self, e, reads, writes):
        best = {}
        for k in reads:
            ev = self.lastw.get(k)
            if ev is not None and best.get(ev[0], 0) < ev[1]:
                best[ev[0]] = ev[1]
        for k in writes:
            ev = self.lastw.get(k)
            if ev is not None and best.get(ev[0], 0) < ev[1]:
                best[ev[0]] = ev[1]
            for ev in self.readers.get(k, ()):
                if best.get(ev[0], 0) < ev[1]:
                    best[ev[0]] = ev[1]
        for key, val in best.items():
            if key == e and (e == 'pe' or not Em.SAME_ENGINE_WAITS):
                continue
            self._wait(e, (key, val))

    def _record(self, ev, reads, writes):
        for k in reads:
            lst = self.readers.setdefault(k, [])
            lst[:] = [x for x in lst if x[0] != ev[0]]
            lst.append(ev)
        for k in writes:
            self.lastw[k] = ev
            self.readers[k] = []

    def op(self, e, reads, writes, fn):
        self._deps(e, reads, writes)
        ins = fn(self.eng[e])
        self.cnt[e] += 1
        ins.then_inc(self.sem[e], 1)
        self._record((e, self.cnt[e]), reads, writes)
        self.ninst += 1

    def dma(self, q, reads, writes, out, in_, **kw):
        i = self.dnext
        self.dnext = (i + 1) % self.NDMA
        if self.dcnt[i] > 0:
            self._wait(q, (('d', i), 16 * self.dcnt[i]))
        self._deps(q, reads, writes)
        ins = self.eng[q].dma_start(out=out, in_=in_, **kw)
        self.dcnt[i] += 1
        ins.then_inc(self.dsem[i], 16)
        self._record((('d', i), 16 * self.dcnt[i]), reads, writes)
        self.ninst += 1

    def barrier(self):
        for e in self.eng:
            for k in self.sem:
                if self.cnt[k] > 0 and k != e:
                    self._wait(e, (k, self.cnt[k]))
            for i in range(self.NDMA):
                if self.dcnt[i] > 0:
                    self._wait(e, (('d', i), 16 * self.dcnt[i]))
        self.lastw = {}
        self.readers = {}


class SB:
    _arena = {}

    def __init__(self, nc, base=0, limit=None):
        self.nc = nc
        if id(nc) not in SB._arena:
            nwords = (nc.sbuf_bytes_remaining - 256) // 4
            SB._arena[id(nc)] = (nc.alloc_sbuf_tensor("arena", [128, nwords], F32), nwords * 4)
        self.arena, cap = SB._arena[id(nc)]
        self.off = base
        self.limit = cap if limit is None else limit

    def t(self, shape, dtype, name=None):
        per = 1
        for s in shape[1:]:
            per *= s
        esz = 2 if dtype == BF16 else 4
        nbytes = (per * esz + 63) // 64 * 64
        assert self.off % 4 == 0
        w0 = self.off // 4
        ap = self.arena[0:shape[0], w0:w0 + nbytes // 4]
        if dtype != F32:
            ap = ap.bitcast(dtype)
        ap = ap[:, 0:per]
        if len(shape) == 3:
            ap = ap.rearrange("p (a b) -> p a b", b=shape[2])
        elif len(shape) == 4:
            ap = ap.rearrange("p (a b c) -> p a b c", b=shape[2], c=shape[3])
        self.off += nbytes
        assert self.off <= self.limit, ("SBUF overflow", name, self.off, self.limit)
        return ap


class K:
    def __init__(self, nc, debug=()):
        self.nc = nc
        self.em = Em(nc)
        self.debug = set(debug)
        self.dram = {}
        self.ps = [nc.alloc_psum_tensor("psb%d" % i, [128, 512], F32) for i in range(8)]

    def din(self, name, shape, dtype=F32):
        ap = self.nc.dram_tensor(name, list(shape), dtype, kind="ExternalInput").ap()
        self.dram[name] = ap
        return ap

    def dscratch(self, name, shape, dtype=F32):
        kind = "ExternalOutput" if name in self.debug else "Internal"
        ap = self.nc.dram_tensor(name, list(shape), dtype, kind=kind).ap()
        self.dram[name] = ap
        return ap

    def dout(self, name, shape, dtype=F32):
        ap = self.nc.dram_tensor(name, list(shape), dtype, kind="ExternalOutput").ap()
        self.dram[name] = ap
        return ap

    def dma(self, q, r, w, out, in_, **kw):
        self.em.dma(q, r, w, out, in_, **kw)

    def mm(self, r, w, out, lhsT, rhs, start=True, stop=True):
        self.em.op('pe', r, w, lambda e: e.matmul(out, lhsT=lhsT, rhs=rhs, start=start, stop=stop))

    def tr(self, r, w, out, in_, ident):
        self.em.op('pe', r, w, lambda e: e.transpose(out, in_, ident))

    def act(self, r, w, out, in_, func, eng='act', **kw):
        self.em.op(eng, r, w, lambda e: e.activation(out=out, in_=in_, func=func, **kw))

    def ts(self, r, w, out, in0, s1, s2=None, op0=ALU.mult, op1=None, eng='dve', **kw):
        if op1 is None:
            self.em.op(eng, r, w, lambda e: e.tensor_scalar(out=out, in0=in0, scalar1=s1, scalar2=None, op0=op0, **kw))
        else:
            self.em.op(eng, r, w, lambda e: e.tensor_scalar(out=out, in0=in0, scalar1=s1, scalar2=s2, op0=op0, op1=op1, **kw))

    def tt(self, r, w, out, in0, in1, op, eng='dve'):
        self.em.op(eng, r, w, lambda e: e.tensor_tensor(out=out, in0=in0, in1=in1, op=op))

    def stt(self, r, w, out, in0, scalar, in1, op0, op1, eng='dve'):
        self.em.op(eng, r, w, lambda e: e.scalar_tensor_tensor(out=out, in0=in0, scalar=scalar, in1=in1, op0=op0, op1=op1))

    def cp(self, r, w, out, in_, eng='dve'):
        if eng == 'act':
            self.em.op(eng, r, w, lambda e: e.copy(out=out, in_=in_))
        else:
            self.em.op(eng, r, w, lambda e: e.tensor_copy(out=out, in_=in_))

    def red(self, r, w, out, in_, op, eng='dve', axis=AX.X):
        self.em.op(eng, r, w, lambda e: e.tensor_reduce(out=out, in_=in_, axis=axis, op=op))

    def recip(self, r, w, out, in_):
        self.em.op('dve', r, w, lambda e: e.reciprocal(out=out, in_=in_))

    def memset(self, r, w, out, val, eng='dve'):
        self.em.op(eng, r, w, lambda e: e.memset(out, val))


def rope_tables_T():
    half = 32
    inv = (10000.0 ** (-np.arange(0, half, 2, dtype=np.float32) / half)).astype(np.float32)
    t = np.arange(L)
    row = (t // 64).astype(np.float32)
    col = (t % 64).astype(np.float32)
    ang = np.concatenate([row[:, None] * inv, row[:, None] * inv, col[:, None] * inv, col[:, None] * inv], axis=1)
    ang = ang.astype(np.float32)
    cosT = np.cos(ang).T.astype(np.float32)
    sinT = np.sin(ang).T.astype(np.float32)
    return np.ascontiguousarray(np.concatenate([cosT, cosT], 0)), np.ascontiguousarray(np.concatenate([sinT, sinT], 0))


def rope_perm():
    R = np.zeros((128, 128), np.float32)
    for base in range(0, 128, 32):
        for i in range(16):
            R[base + 16 + i, base + i] = -1.0
            R[base + i, base + 16 + i] = 1.0
    return R


def make_consts():
    c = {}
    c['ident'] = np.eye(128, dtype=np.float32)
    c['ropeR'] = rope_perm()
    cosT, sinT = rope_tables_T()
    c['cosT'] = cosT
    c['sinT'] = sinT
    bi = np.zeros((128, 2), np.float32)
    bi[0:64, 0] = 1.0
    bi[64:128, 1] = 1.0
    c['blockind'] = bi
    sel = np.zeros((2, 2, 128), np.float32)
    sel[0, 0, :] = 1.0
    sel[1, 1, :] = 1.0
    c['sel2'] = sel
    tri = np.zeros((2, 64, 64), np.float32)
    tri[0] = np.triu(np.ones((64, 64), np.float32))
    tri[1] = np.tril(np.ones((64, 64), np.float32))
    c['tri'] = tri
    c.update(hyena_consts())
    c['tri128'] = np.triu(np.ones((128, 128), np.float32), 1)
    c['iota32'] = np.ascontiguousarray(np.broadcast_to(np.arange(32, dtype=np.float32)[None, :], (128, 32)))
    lt = np.tril(np.ones((32, 32), np.float32), -1)
    c['ltmask'] = np.ascontiguousarray(np.broadcast_to(lt.reshape(1, 1024), (128, 1024)))
    c['pidx'] = np.ascontiguousarray(np.arange(128, dtype=np.float32)[:, None])
    c['moe_S'] = np.ascontiguousarray(np.stack([np.broadcast_to(np.array(moe_sched(nt)[1][:32], np.float32)[None, :], (128, 32))
                                                for nt in (NT, 32)], 0))
    return c


def col_layout(v):
    return np.ascontiguousarray(v.reshape(-1, 128).T)


def prep_inputs(inp, b):
    m = {}
    m['xin'] = np.ascontiguousarray(np.concatenate([inp['x'][b], inp['ctx'][b]], axis=0))
    m['ccol'] = np.ascontiguousarray(np.stack([col_layout(inp['c'][b]), col_layout(inp['c_ctx'])], axis=-1))
    m['ada_w'] = inp['ada_w']
    m['ada_b_col'] = np.ascontiguousarray(np.stack([col_layout(inp['ada_b'][l]) for l in range(DEPTH)], 1))
    m['norm1_col'] = np.ascontiguousarray(np.stack([col_layout(inp['norm1_w'][l]) for l in range(DEPTH)], 1))
    m['norm2_col'] = np.ascontiguousarray(np.stack([col_layout(inp['norm2_w'][l]) for l in range(DEPTH)], 1))
    m['w_in_fm'] = np.ascontiguousarray(inp['w_in'][:, :, FM_COLS])
    m['w_in_tm'] = np.ascontiguousarray(inp['w_in'][:, :, TM_COLS])
    m['b_fm_col'] = np.ascontiguousarray(np.stack([col_layout(inp['b_in'][l][FM_COLS]) for l in range(DEPTH)], 1))
    cw = np.concatenate([inp['ml_conv_w'], inp['ml_conv_b'][:, None, :]], axis=1)
    m['ml_conv_col'] = np.ascontiguousarray(cw.reshape(DEPTH, 4, 8, 64).transpose(0, 3, 2, 1))
    m['ml_norm_bc'] = np.ascontiguousarray(np.broadcast_to(inp['ml_norm_w'][:, None, :], (DEPTH, 64, 256)))
    m['hy_filt_w1'] = inp['hy_filt_w1']
    m['hy_filt_w2'] = inp['hy_filt_w2']
    m['hy_filt_w3'] = inp['hy_filt_w3']
    m['hy_filt_sc'] = np.ascontiguousarray(np.stack([inp['hy_sin_freq'], inp['hy_filt_b1'], inp['hy_filt_b2']], axis=-1))
    m['hy_b3_col'] = np.ascontiguousarray(np.stack([col_layout(inp['hy_filt_b3'][l]) for l in range(DEPTH)], 0))
    hw = np.concatenate([inp['hy_conv_w'], inp['hy_conv_b'][:, None, :]], axis=1)
    m['hy_conv_col'] = np.ascontiguousarray(hw.reshape(DEPTH, 4, 6, 128).transpose(0, 3, 2, 1))
    m['hy_d_bc'] = np.ascontiguousarray(np.broadcast_to(inp['hy_bias_d'][:, None, :, :], (DEPTH, 32, 2, 256)))
    m['hy_d_col'] = np.ascontiguousarray(inp['hy_bias_d'].reshape(DEPTH, 4, 128).transpose(0, 2, 1))
    m['w_out'] = inp['w_out']
    m['moe_wr'] = np.ascontiguousarray(np.concatenate([inp['moe_wg'], inp['moe_we']], axis=-1))
    rbv = np.concatenate([inp['moe_bg'], inp['moe_be']], axis=-1)
    m['moe_rb_bc'] = np.ascontiguousarray(np.broadcast_to(rbv[:, None, :], (DEPTH, 128, 36)))
    m['final_bc'] = np.ascontiguousarray(np.broadcast_to(inp['final_norm_w'][None, :], (128, D)))
    m['da_lambda'] = np.ascontiguousarray(inp['da_lambda'].reshape(DEPTH, 256))
    m['da_subln_col'] = np.ascontiguousarray(inp['da_subln_w'][:, :, None])
    m['b_tm_bc'] = np.ascontiguousarray(np.broadcast_to(inp['b_in'][:, None, TM_COLS], (DEPTH, 128, NTM)))
    return m


def phase0(k, P, sbp):
    nc = k.nc
    cst = {}
    for name, shape in (('ident', [128, 128]), ('ropeR', [128, 128]), ('blockind', [128, 2])):
        k.din(name, shape)
    k.din('sel2', [2, 2, 128])
    k.din('cosT', [128, L])
    k.din('sinT', [128, L])
    P['ident32'] = sbp.t([128, 128], F32)
    P['identbf'] = sbp.t([128, 128], BF16)
    P['ropeRbf'] = sbp.t([128, 128], BF16)
    P['blockbf'] = sbp.t([128, 2], BF16)
    P['sel2'] = sbp.t([2, 2, 128], F32)
    P['ones32'] = sbp.t([128, 128], F32)
    P['onesbf'] = sbp.t([128, 128], BF16)
    tmp = sbp.t([128, 128], F32)
    k.dma('sp', [], ['ident32'], P['ident32'], k.dram['ident'][:, :])
    k.cp(['ident32'], ['identbf'], P['identbf'], P['ident32'])
    k.dma('sp', [], ['c_tmp'], tmp, k.dram['ropeR'][:, :])
    k.cp(['c_tmp'], ['ropeRbf'], P['ropeRbf'], tmp)
    k.dma('sp', ['c_tmp'], ['c_tmp'], tmp[:, 0:2], k.dram['blockind'][:, :])
    k.cp(['c_tmp'], ['blockbf'], P['blockbf'], tmp[:, 0:2])
    k.dma('sp', [], ['sel2'], P['sel2'], k.dram['sel2'][:, :, :])
    k.memset([], ['ones32'], P['ones32'], 1.0)
    k.memset([], ['onesbf'], P['onesbf'], 1.0)

    ccol = k.din('ccol', [128, 8, 2])
    adaw = k.din('ada_w', [DEPTH, D, 6 * D])
    adab = k.din('ada_b_col', [128, DEPTH, 48])
    n1 = k.din('norm1_col', [128, DEPTH, 8])
    n2 = k.din('norm2_col', [128, DEPTH, 8])
    P['mod'] = sbp.t([128, DEPTH, 48, 2], F32)
    P['A1'] = sbp.t([128, DEPTH, 8, 2], F32)
    P['A2'] = sbp.t([128, DEPTH, 8, 2], F32)
    cact = sbp.t([128, 8, 2], F32)
    adab_sb = sbp.t([128, DEPTH, 48], F32)
    n1_sb = sbp.t([128, DEPTH, 8], F32)
    n2_sb = sbp.t([128, DEPTH, 8], F32)
    k.dma('sp', [], ['cact'], cact, ccol[:, :, :])
    k.dma('sp', [], ['adab'], adab_sb, adab[:, :, :])
    k.dma('sp', [], ['n1'], n1_sb, n1[:, :, :])
    k.dma('sp', [], ['n2'], n2_sb, n2[:, :, :])
    k.act(['cact'], ['cact'], cact, cact, AF.Silu)
    sbl = SB(nc, base=sbp.off)
    stg = [sbl.t([128, 8, 512], F32) for _ in range(2)]
    for l in range(DEPTH):
        wv = adaw[l].rearrange("(k p) n -> p k n", p=128)
        pm = k.ps[0][:, 0:96].rearrange("p (c j) -> p c j", j=2)
        for cg in range(12):
            s = stg[cg % 2]
            sk = 'adastg%d' % (cg % 2)
            k.dma('sp', [], [sk], s, wv[:, :, cg * 512:(cg + 1) * 512])
            for j in range(4):
                c = cg * 4 + j
                for kk in range(8):
                    k.mm([sk, 'cact'], ['ps0'], pm[:, c, :], s[:, kk, j * 128:(j + 1) * 128], cact[:, kk, :],
                         start=(kk == 0), stop=(kk == 7))
        k.tt(['ps0', 'adab'], ['mod'], P['mod'][:, l], pm, adab_sb[:, l, :].unsqueeze(2).to_broadcast([128, 48, 2]), ALU.add)
        for (Aname, nsb, c0) in (('A1', n1_sb, 8), ('A2', n2_sb, 32)):
            k.ts(['mod'], [Aname], P[Aname][:, l], P['mod'][:, l, c0:c0 + 8, :], 1.0, op0=ALU.add)
            k.tt([Aname, 'n1', 'n2'], [Aname], P[Aname][:, l], P[Aname][:, l],
                 nsb[:, l, :].unsqueeze(2).to_broadcast([128, 8, 2]), ALU.mult)
    k.em.barrier()


def phaseA(k, P, l, base, xres):
    nc = k.nc
    sb = SB(nc, base=base)
    wfm = k.dram['w_in_fm']
    wtm = k.dram['w_in_tm']
    UT, QKT, TM = k.dram['UT'], k.dram['QKT'], k.dram['TM']
    Wfm = sb.t([128, 8, NFM], BF16)
    Wtm = sb.t([128, 8, NTM], BF16)
    bfm = sb.t([128, 18], F32)
    btm = sb.t([128, NTM], F32)
    cosT = sb.t([128, L], F32)
    sinT = sb.t([128, L], F32)
    normacc = sb.t([2, 8], F32)
    stg = [sb.t([128, 8, 512], F32) for _ in range(2)]
    k.dma('sp', [], ['bfm'], bfm, k.dram['b_fm_col'][:, l, :])
    k.dma('sp', [], ['btm'], btm, k.dram['b_tm_bc'][l])
    k.dma('sp', [], ['cosT'], cosT, k.dram['cosT'][:, :])
    k.dma('sp', [], ['sinT'], sinT, k.dram['sinT'][:, :])
    k.memset([], ['normacc'], normacc, 0.0)
    ci = 0
    for (src, dst, ncol, key) in ((wfm, Wfm, NFM, 'Wfm'), (wtm, Wtm, NTM, 'Wtm')):
        wv = src[l].rearrange("(k p) n -> p k n", p=128)
        for c0 in range(0, ncol, 512):
            c1 = min(ncol, c0 + 512)
            s = stg[ci % 2]
            sk = 'wstg%d' % (ci % 2)
            k.dma('sp', [], [sk], s[:, :, 0:c1 - c0], wv[:, :, c0:c1])
            k.cp([sk], [key], dst[:, :, c0:c1], s[:, :, 0:c1 - c0], eng=('dve' if ci % 2 == 0 else 'pool'))
            ci += 1
    xt = [sb.t([128, D], F32) for _ in range(2)]
    junk = sb.t([128, D], BF16)
    xn = [sb.t([128, D], BF16) for _ in range(2)]
    ss = [sb.t([128, 2], F32) for _ in range(2)]
    hT = [sb.t([128, 8, 512], BF16) for _ in range(2)]
    fmst = [sb.t([128, 512], F32) for _ in range(3)]
    qbf = [sb.t([128, 512], BF16) for _ in range(2)]
    t2 = [sb.t([128, 512], F32) for _ in range(2)]
    obf = [sb.t([128, 512], BF16) for _ in range(2)]
    sqbf = [sb.t([128, 512], BF16) for _ in range(2)]
    nmx = sb.t([2, 2], F32)
    tmst = [sb.t([128, NTM], F32) for _ in range(2)]
    A1, SH1 = P['A1'], P['mod']
    psi = 0
    tile_ctr = 0
    fm_ctr = 0
    rp_ctr = 0
    for g in range(9):
        t0 = g * 512
        ntok = 512 if g < 8 else 256
        j = 0 if g < 8 else 1
        hb = g % 2
        hk = 'hT%d' % hb
        for tl in range(ntok // 128):
            ti = t0 // 128 + tl
            xb = tile_ctr % 2
            tile_ctr += 1
            xk, nk, sk = 'xt%d' % xb, 'xn%d' % xb, 'ss%d' % xb
            k.dma('sp', [], [xk], xt[xb], xres[ti * 128:(ti + 1) * 128, :])
            k.memset([], [sk], ss[xb], 0.0)
            k.act([xk, sk], ['junk', sk], junk, xt[xb], AF.Square, accum_out=ss[xb][:, 0:1])
            k.ts([sk], [sk], ss[xb][:, 1:2], ss[xb][:, 0:1], 1.0 / D, EPS, op0=ALU.mult, op1=ALU.add)
            k.act([sk], [sk], ss[xb][:, 1:2], ss[xb][:, 1:2], AF.Sqrt)
            k.recip([sk], [sk], ss[xb][:, 1:2], ss[xb][:, 1:2])
            k.ts([xk, sk], [nk], xn[xb], xt[xb], ss[xb][:, 1:2], op0=ALU.mult)
            pk = 'ps%d' % psi
            pst = k.ps[psi][:].bitcast(BF16)
            psi = (psi + 1) % 8
            for kk in range(8):
                k.tr([nk, 'identbf'], [pk], pst[:, kk * 128:(kk + 1) * 128], xn[xb][:, kk * 128:(kk + 1) * 128], P['identbf'])
            for kk in range(8):
                k.act([pk, 'A1', 'mod'], [hk], hT[hb][:, kk, tl * 128:(tl + 1) * 128], pst[:, kk * 128:(kk + 1) * 128],
                      AF.Identity, scale=A1[:, l, kk, j:j + 1], bias=SH1[:, l, kk, j:j + 1])
        for jc in range(18):
            pk = 'ps%d' % psi
            pp = k.ps[psi]
            psi = (psi + 1) % 8
            for kk in range(8):
                k.mm(['Wfm', hk], [pk], pp[:, 0:ntok], Wfm[:, kk, jc * 128:(jc + 1) * 128], hT[hb][:, kk, 0:ntok],
                     start=(kk == 0), stop=(kk == 7))
            fb = fm_ctr % 3
            fm_ctr += 1
            fk = 'fmst%d' % fb
            k.act([pk, 'bfm'], [fk], fmst[fb][:, 0:ntok], pp[:, 0:ntok], AF.Identity, bias=bfm[:, jc:jc + 1], scale=1.0)
            if jc < 10:
                k.dma('pool', [fk], ['UT'], UT[jc * 128:(jc + 1) * 128, t0:t0 + ntok], fmst[fb][:, 0:ntok])
                continue
            rb = rp_ctr % 2
            rp_ctr += 1
            ok_, sqk = 'obf%d' % rb, 'sqbf%d' % rb
            if j == 0:
                qk_, tk_ = 'qbf%d' % rb, 't2%d' % rb
                k.cp([fk], [qk_], qbf[rb][:, 0:ntok], fmst[fb][:, 0:ntok], eng='pool')
                pk2 = 'ps%d' % psi
                pp2 = k.ps[psi]
                psi = (psi + 1) % 8
                k.mm(['ropeRbf', qk_], [pk2], pp2[:, 0:ntok], P['ropeRbf'], qbf[rb][:, 0:ntok])
                k.tt([pk2, 'sinT'], [tk_], t2[rb][:, 0:ntok], pp2[:, 0:ntok], sinT[:, t0:t0 + ntok], ALU.mult)
                k.tt([fk, 'cosT'], [fk], fmst[fb][:, 0:ntok], fmst[fb][:, 0:ntok], cosT[:, t0:t0 + ntok], ALU.mult, eng='pool')
                k.tt([fk, tk_], [fk], fmst[fb][:, 0:ntok], fmst[fb][:, 0:ntok], t2[rb][:, 0:ntok], ALU.add)
            k.cp([fk], [ok_], obf[rb][:, 0:ntok], fmst[fb][:, 0:ntok], eng='pool')
            k.dma('pool', [ok_], ['QKT'], QKT[(jc - 10) * 128:(jc - 9) * 128, t0:t0 + ntok], obf[rb][:, 0:ntok])
            k.act([fk], [sqk], sqbf[rb][:, 0:ntok], fmst[fb][:, 0:ntok], AF.Square)
            pk3 = 'ps%d' % psi
            pp3 = k.ps[psi]
            psi = (psi + 1) % 8
            k.mm(['blockbf', sqk], [pk3], pp3[0:2, 0:ntok], P['blockbf'], sqbf[rb][:, 0:ntok])
            k.red([pk3], ['nmx'], nmx[:, 0:1], pp3[0:2, 0:ntok], ALU.max)
            k.tt(['nmx', 'normacc'], ['normacc'], normacc[:, jc - 10:jc - 9], normacc[:, jc - 10:jc - 9], nmx[:, 0:1], ALU.max)
        for tl in range(ntok // 128):
            ti = t0 // 128 + tl
            tb = ti % 2
            tk = 'tmst%d' % tb
            for (c0, c1) in ((0, 512), (512, 1024), (1024, NTM)):
                pk = 'ps%d' % psi
                pp = k.ps[psi]
                psi = (psi + 1) % 8
                for kk in range(8):
                    k.mm(['Wtm', hk], [pk], pp[:, 0:c1 - c0], hT[hb][:, kk, tl * 128:(tl + 1) * 128], Wtm[:, kk, c0:c1],
                         start=(kk == 0), stop=(kk == 7))
                k.tt([pk, 'btm'], [tk], tmst[tb][:, c0:c1], pp[:, 0:c1 - c0], btm[:, c0:c1], ALU.add)
            k.dma('pool', [tk], ['TM'], TM[ti * 128:(ti + 1) * 128, :], tmst[tb])
    cn = sb.t([2, 4], F32)
    k.tt(['normacc'], ['cn'], cn, normacc[:, 0:4], normacc[:, 4:8], ALU.mult)
    k.act(['cn'], ['cn'], cn, cn, AF.Sqrt)
    k.ts(['cn'], ['cn'], cn, cn, -1.05 * 0.125, op0=ALU.mult)
    for m in range(2):
        pk = 'ps%d' % psi
        pp = k.ps[psi]
        psi = (psi + 1) % 8
        k.mm(['sel2', 'cn'], [pk], pp[:, 0:4], P['sel2'][:, m, :], cn)
        k.cp([pk], ['negc'], P['negc'][:, l, m, :], pp[:, 0:4])
    k.em.barrier()


def phaseB1(k, P, l, base, last):
    nc = k.nc
    sb = SB(nc, base=base)
    QKT, TM, YT = k.dram['QKT'], k.dram['TM'], k.dram['YT']
    lam_init = 0.8 - 0.6 * math.exp(-0.3 * l)
    QT = sb.t([128, 4, T], BF16)
    KTz = [sb.t([128, 4, T], BF16) for _ in range(2)]
    V = sb.t([128, NT, 512], BF16)
    vst = [sb.t([128, 512], F32) for _ in range(2)]
    k.dma('sp', [], ['QT'], QT, QKT[0:512, :].rearrange("(c p) t -> p c t", p=128))
    kv = QKT[512:1024, :].rearrange("(c p) t -> p c t", p=128)
    for m in range(2):
        lo, hi = m * 64, (m + 1) * 64
        zl, zh = (1 - m) * 64, (2 - m) * 64
        k.dma('sp', [], ['KT'], KTz[m][lo:hi], kv[lo:hi])
        k.memset([], ['KT'], KTz[m][zl:zh], 0.0, eng=('dve' if m == 0 else 'pool'))
    for ti in range(NT):
        vb = ti % 2
        vk = 'vst%d' % vb
        k.dma('sp', [], [vk], vst[vb], TM[ti * 128:(ti + 1) * 128, 512:1024])
        k.cp([vk], ['V'], V[:, ti, :], vst[vb], eng=('dve' if ti % 2 == 0 else 'pool'))
    lt = sb.t([1, 256], F32)
    lw = sb.t([1, 8], F32)
    neglam = sb.t([128, 1], F32)
    wsc = sb.t([128, 1], F32)
    k.dma('sp', [], ['lt'], lt, k.dram['da_lambda'][l:l + 1, :])
    k.dma('sp', [], ['wsc'], wsc, k.dram['da_subln_col'][l])
    k.ts(['wsc'], ['wsc'], wsc, wsc, 1.0 - lam_init, op0=ALU.mult)
    k.tt(['lt'], ['lt'], lt[:, 0:64], lt[:, 0:64], lt[:, 64:128], ALU.mult)
    k.tt(['lt'], ['lt'], lt[:, 128:192], lt[:, 128:192], lt[:, 192:256], ALU.mult)
    k.red(['lt'], ['lw'], lw[:, 0:1], lt[:, 0:64], ALU.add)
    k.red(['lt', 'lw'], ['lw'], lw[:, 1:2], lt[:, 128:192], ALU.add)
    k.act(['lw'], ['lw'], lw[:, 0:2], lw[:, 0:2], AF.Exp)
    k.tt(['lw'], ['lw'], lw[:, 2:3], lw[:, 1:2], lw[:, 0:1], ALU.subtract)
    k.ts(['lw'], ['lw'], lw[:, 2:3], lw[:, 2:3], -lam_init, op0=ALU.add)
    k.mm(['ones32', 'lw'], ['ps7'], k.ps[7][:, 0:1], P['ones32'][0:1, :], lw[:, 2:3])
    k.cp(['ps7'], ['neglam'], neglam, k.ps[7][:, 0:1])

    pt = [sb.t([128, 512], BF16) for _ in range(4)]
    racc = [sb.t([128, 512], F32) for _ in range(2)]
    rec = [sb.t([128, 512], F32) for _ in range(2)]
    o0 = sb.t([128, 512], F32)
    o1 = sb.t([128, 512], F32)
    sq = sb.t([128, 512], BF16)
    ybf = [sb.t([128, 512], BF16) for _ in range(2)]
    pti = 0
    si = 0
    si_box = [0]
    yi = 0
    chunks = [(g * 512, 512, list(range(NT))) for g in range(8)]
    if not last:
        chunks.append((L, CTX, [32, 33]))
    for h in range(4):
        for (q0, nq, blocks) in chunks:
            units = [(bi, kb, m) for bi, kb in enumerate(blocks) for m in range(2)]
            LOOK = 3
            issued = {}

            def issue_s(u):
                bi, kb, m = units[u]
                nonlocal_si = si_box[0]
                si_box[0] += 1
                psk = 'ps%d' % (4 + nonlocal_si % 4)
                pss = k.ps[4 + nonlocal_si % 4]
                k.mm(['KT', 'QT'], [psk], pss[:, 0:nq], KTz[m][:, h, kb * 128:(kb + 1) * 128], QT[:, h, q0:q0 + nq])
                issued[u] = (psk, pss)

            for u in range(min(LOOK, len(units))):
                issue_s(u)
            for u, (bi, kb, m) in enumerate(units):
                psk, pss = issued.pop(u)
                pk_ = 'pt%d' % (pti % 4)
                ptt = pt[pti % 4]
                pti += 1
                k.act([psk, 'negc'], [pk_], ptt[:, 0:nq], pss[:, 0:nq], AF.Exp, scale=0.125, bias=P['negc'][:, l, m, h:h + 1])
                if u + LOOK < len(units):
                    issue_s(u + LOOK)
                st, sp_ = (bi == 0), (bi == len(blocks) - 1)
                k.mm(['V', pk_], ['ps%d' % m], k.ps[m][:, 0:nq], V[:, kb, h * 128:(h + 1) * 128], ptt[:, 0:nq], start=st, stop=sp_)
                reng = 'dve' if m == 0 else 'pool'
                if st:
                    k.cp([pk_], ['racc%d' % m], racc[m][:, 0:nq], ptt[:, 0:nq], eng=reng)
                else:
                    k.tt([pk_, 'racc%d' % m], ['racc%d' % m], racc[m][:, 0:nq], racc[m][:, 0:nq], ptt[:, 0:nq], ALU.add, eng=reng)
            si = si_box[0]
            for m in range(2):
                k.mm(['ones32', 'racc%d' % m], ['ps%d' % (2 + m)], k.ps[2 + m][:, 0:nq], P['ones32'], racc[m][:, 0:nq])
            k.recip(['ps2'], ['rec0'], rec[0][:, 0:nq], k.ps[2][:, 0:nq])
            k.recip(['ps3'], ['rec1'], rec[1][:, 0:nq], k.ps[3][:, 0:nq])
            k.tt(['ps0', 'rec0'], ['o0'], o0[:, 0:nq], k.ps[0][:, 0:nq], rec[0][:, 0:nq], ALU.mult)
            k.tt(['ps1', 'rec1'], ['o1'], o1[:, 0:nq], k.ps[1][:, 0:nq], rec[1][:, 0:nq], ALU.mult)
            k.stt(['o0', 'o1', 'neglam'], ['o0'], o0[:, 0:nq], o1[:, 0:nq], neglam[:, 0:1], o0[:, 0:nq], ALU.mult, ALU.add)
            k.act(['o0'], ['sq'], sq[:, 0:nq], o0[:, 0:nq], AF.Square)
            psk = 'ps%d' % (4 + si % 4)
            pss = k.ps[4 + si % 4]
            si += 1
            si_box[0] = si
            k.mm(['onesbf', 'sq'], [psk], pss[:, 0:nq], P['onesbf'], sq[:, 0:nq])
            k.ts([psk], ['rec0'], rec[0][:, 0:nq], pss[:, 0:nq], 1.0 / 128.0, EPS, op0=ALU.mult, op1=ALU.add)
            k.act(['rec0'], ['rec0'], rec[0][:, 0:nq], rec[0][:, 0:nq], AF.Sqrt)
            k.recip(['rec0'], ['rec0'], rec[0][:, 0:nq], rec[0][:, 0:nq])
            k.tt(['o0', 'rec0'], ['o0'], o0[:, 0:nq], o0[:, 0:nq], rec[0][:, 0:nq], ALU.mult)
            yk = 'ybf%d' % (yi % 2)
            yb = ybf[yi % 2]
            yi += 1
            k.ts(['o0', 'wsc'], [yk], yb[:, 0:nq], o0[:, 0:nq], wsc[:, 0:1], op0=ALU.mult)
            k.dma('pool', [yk], ['YT'], YT[512 + h * 128:512 + (h + 1) * 128, q0:q0 + nq], yb[:, 0:nq])
    k.em.barrier()


NCH = T // 64


def phaseB2(k, P, l, base, last):
    nc = k.nc
    UT, TM, YT = k.dram['UT'], k.dram['TM'], k.dram['YT']
    sb0 = SB(nc, base=base)
    tri32 = sb0.t([64, 2, 64], F32)
    tribf = sb0.t([64, 2, 64], BF16)
    cw = sb0.t([64, 8, 4], F32)
    wbc = sb0.t([64, 256], F32)
    G = sb0.t([64, NCH, 16], F32)
    LF = sb0.t([64, 8, NCH], F32)
    IG = sb0.t([64, 8, NCH], F32)
    BB = sb0.t([64, 8, NCH], F32)
    BT = sb0.t([64, 8, NCH], F32)
    EB = sb0.t([64, 8, NCH], F32)
    WS = sb0.t([64, 8, NCH], F32)
    W2 = sb0.t([64, 8, NCH], F32)
    EBT = sb0.t([64, 8, NCH], F32)
    k.dma('sp', [], ['tri32'], tri32, k.dram['tri'].rearrange("a s t -> s a t"))
    k.cp(['tri32'], ['tribf'], tribf, tri32)
    k.dma('sp', [], ['cw'], cw, k.dram['ml_conv_col'][l])
    k.dma('sp', [], ['wbc'], wbc, k.dram['ml_norm_bc'][l])
    k.dma('sp', [], ['G'], G, TM[:, 1024:1040].rearrange("(n p) c -> p n c", p=64))
    for d in range(2):
        gi = G[:, :, d * 8:d * 8 + 4].rearrange("p n h -> p h n")
        gf = G[:, :, d * 8 + 4:d * 8 + 8].rearrange("p n h -> p h n")
        k.cp(['G'], ['IG'], IG[:, d * 4:d * 4 + 4, :], gi)
        k.act(['G'], ['LF'], LF[:, d * 4:d * 4 + 4, :], gf, AF.Exp, scale=-1.0)
    k.act(['LF'], ['LF'], LF, LF, AF.Ln, bias=1.0, scale=1.0)
    k.ts(['LF'], ['LF'], LF, LF, -1.0, op0=ALU.mult)
    for d in range(2):
        rhs = LF[:, d * 4:d * 4 + 4, :]
        k.mm(['tri32', 'LF'], ['ps0'], k.ps[0][0:64, 0:4 * NCH], tri32[:, d, :], rhs)
        k.cp(['ps0'], ['BB'], BB[:, d * 4:d * 4 + 4, :], k.ps[0][0:64, 0:4 * NCH].rearrange("p (h n) -> p h n", n=NCH))
        k.mm(['ones32', 'LF'], ['ps1'], k.ps[1][0:64, 0:4 * NCH], P['ones32'][0:64, 0:64], rhs)
        k.cp(['ps1'], ['BT'], BT[:, d * 4:d * 4 + 4, :], k.ps[1][0:64, 0:4 * NCH].rearrange("p (h n) -> p h n", n=NCH))
    k.act(['BB'], ['EB'], EB, BB, AF.Exp)
    k.act(['BT'], ['EBT'], EBT, BT, AF.Exp)
    k.tt(['IG', 'BB'], ['WS'], WS, IG, BB, ALU.subtract)
    k.tt(['WS', 'BT'], ['W2'], W2, WS, BT, ALU.add)
    k.act(['WS'], ['WS'], WS, WS, AF.Exp)
    k.act(['W2'], ['W2'], W2, W2, AF.Exp)
    base1 = sb0.off
    orders = [[64, 65, 66, 67] + list(range(64)), [67, 66, 65, 64] + list(range(63, -1, -1))]
    psi = [2]

    def nps():
        i = psi[0]
        psi[0] = 2 + (psi[0] - 1) % 6
        return 'ps%d' % i, k.ps[i]

    for hp in range(2):
        sb = SB(nc, base=base1)
        qT = sb.t([64, 2, T], BF16)
        kT = sb.t([64, 2, T], BF16)
        ktm = sb.t([64, NCH, 128], BF16)
        vaug = sb.t([64, NCH, 2, 65], BF16)
        hsum = sb.t([64, NCH, 128], F32)
        ra = sb.t([64, 2 * T], F32)
        raw = ra[:, 0:T]
        acc = ra[:, T:2 * T]
        k.memset([], ['hsum'], hsum, 0.0, eng='pool')
        k.memset([], ['vaug'], vaug, 1.0, eng='pool')
        for qk in range(2):
            for hl in range(2):
                hh = qk * 4 + hp * 2 + hl
                k.dma('sp', [], ['raw'], raw, UT[768 + hh * 64:768 + (hh + 1) * 64, :])
                k.ts(['raw', 'cw'], ['acc'], acc, raw, cw[:, hh, 1:2], cw[:, hh, 3:4], op0=ALU.mult, op1=ALU.add)
                for (a, b) in ((0, L), (L, T)):
                    k.stt(['raw', 'cw', 'acc'], ['acc'], acc[:, a + 1:b], raw[:, a:b - 1], cw[:, hh, 0:1], acc[:, a + 1:b], ALU.mult, ALU.add)
                    k.stt(['raw', 'cw', 'acc'], ['acc'], acc[:, a:b - 1], raw[:, a + 1:b], cw[:, hh, 2:3], acc[:, a:b - 1], ALU.mult, ALU.add)
                if qk == 0:
                    k.act(['acc'], ['qT'], qT[:, hl, :], acc, AF.Silu)
                else:
                    k.act(['acc'], ['acc'], acc, acc, AF.Silu)
                    k.ts(['acc'], ['kT'], kT[:, hl, :], acc, 0.125, op0=ALU.mult)
        for n0 in range(0, NCH, 4):
            pk, pp = nps()
            ppb = pp[:].bitcast(BF16)
            for dn in range(4):
                for hl in range(2):
                    k.tr(['kT', 'identbf'], [pk], ppb[0:64, (dn * 2 + hl) * 64:(dn * 2 + hl + 1) * 64],
                         kT[:, hl, (n0 + dn) * 64:(n0 + dn + 1) * 64], P['identbf'][0:64, 0:64])
            k.cp([pk], ['ktm'], ktm[:, n0:n0 + 4, :], ppb[0:64, 0:512].rearrange("p (n c) -> p n c", c=128))
        vst = acc[:, 0:17 * 128].rearrange("p (n c) -> p n c", c=128)
        for n0 in range(0, NCH, 17):
            k.dma('sp', ['acc'], ['acc'], vst, TM[n0 * 64:(n0 + 17) * 64, hp * 128:(hp + 1) * 128].rearrange("(n p) c -> p n c", p=64))
            k.cp(['acc'], ['vaug'], vaug[:, n0:n0 + 17, :, 0:64], vst.rearrange("p n (h e) -> p n h e", e=64))
        Cst = [[sb.t([64, 65], F32) for _ in range(2)] for _ in range(2)]
        Cbf = [[sb.t([64, 65], BF16) for _ in range(2)] for _ in range(2)]
        dg = [sb.t([64, 64], BF16) for _ in range(4)]
        meb = [sb.t([64, 64], F32) for _ in range(4)]
        pT = [sb.t([64, 64], BF16) for _ in range(4)]
        rsb = [sb.t([64, 65], F32) for _ in range(4)]
        tot = [sb.t([64, 66], F32) for _ in range(4)]
        wv = [sb.t([64, 65], BF16) for _ in range(4)]
        for d in range(2):
            for hl in range(2):
                k.memset([], ['Cst%d%d' % (d, hl)], Cst[d][hl], 0.0)
                k.memset([], ['Cbf%d%d' % (d, hl)], Cbf[d][hl], 0.0)
        def unit(step, d, hl):
            n = orders[d][step]
            c0 = n * 64
            need_out = (n < 64) or (not last)
            u = d * 2 + hl
            dh = d * 4 + hp * 2 + hl
            ck, cbk = 'Cst%d%d' % (d, hl), 'Cbf%d%d' % (d, hl)
            upd = step != NCH - 1
            kA, kB = 'ps%d' % (2 * u), 'ps%d' % (2 * u + 1)
            bA, bB = k.ps[2 * u], k.ps[2 * u + 1]
            if upd:
                k.act(['vaug', 'W2'], ['wv%d' % u], wv[u], vaug[:, n, hl, :], AF.Identity, scale=W2[:, dh, n:n + 1])
            if need_out:
                k.act(['identbf', 'EB'], ['dg%d' % u], dg[u], P['identbf'][0:64, 0:64], AF.Identity, scale=EB[:, dh, n:n + 1])
            yield
            if upd:
                k.mm(['ktm', 'wv%d' % u], [kB], bB[0:64, 0:65], ktm[:, n, hl * 64:(hl + 1) * 64], wv[u])
            if need_out:
                k.mm(['tribf', 'dg%d' % u], [kA], bA[0:64, 0:64], tribf[:, 1 - d, :], dg[u])
            yield
            if upd:
                k.stt([ck, 'EBT', kB], [ck], Cst[d][hl], Cst[d][hl], EBT[:, dh, n:n + 1], bB[0:64, 0:65], ALU.mult, ALU.add)
            if need_out:
                k.cp([kA], ['meb%d' % u], meb[u], bA[0:64, 0:64], eng='act')
            yield
            if need_out:
                k.mm(['kT', 'qT'], [kA], bA[0:64, 0:64], kT[:, hl, c0:c0 + 64], qT[:, hl, c0:c0 + 64])
                k.mm(['qT', cbk], [kB], bB[0:64, 0:65], qT[:, hl, c0:c0 + 64], Cbf[d][hl])
            yield
            if need_out:
                k.act([kB, 'EB'], ['rsb%d' % u], rsb[u], bB[0:64, 0:65], AF.Identity, scale=EB[:, dh, n:n + 1])
                k.stt([kA, 'WS', 'meb%d' % u], ['pT%d' % u], pT[u], bA[0:64, 0:64], WS[:, dh, n:n + 1], meb[u], ALU.mult, ALU.mult)
            if upd:
                k.cp([ck], [cbk], Cbf[d][hl], Cst[d][hl], eng='act')
            yield
            if not need_out:
                return
            k.mm(['pT%d' % u, 'vaug'], [kA], bA[0:64, 0:65], pT[u], vaug[:, n, hl, :])
            yield
            k.tt([kA, 'rsb%d' % u], ['tot%d' % u], tot[u][:, 0:65], bA[0:64, 0:65], rsb[u], ALU.add)
            yield
            k.act(['tot%d' % u], ['tot%d' % u], tot[u][:, 65:66], tot[u][:, 64:65], AF.Abs)
            yield
            k.ts(['tot%d' % u], ['tot%d' % u], tot[u][:, 65:66], tot[u][:, 65:66], 1.0, op0=ALU.max)
            yield
            k.recip(['tot%d' % u], ['tot%d' % u], tot[u][:, 65:66], tot[u][:, 65:66])
            yield
            hs = hsum[:, n, hl * 64:(hl + 1) * 64]
            k.stt(['tot%d' % u, 'hsum'], ['hsum'], hs, tot[u][:, 0:64], tot[u][:, 65:66], hs, ALU.mult, ALU.add)

        for step in range(NCH):
            gens = [unit(step, d, hl) for d in range(2) for hl in range(2)]
            while gens:
                alive = []
                for g_ in gens:
                    try:
                        next(g_)
                        alive.append(g_)
                    except StopIteration:
                        pass
                gens = alive
        nout = 64 if last else NCH
        ssum = sb.t([64, NCH * 2], F32)
        ybf = sb.t([64, NCH, 128], BF16)
        ytb = [sb.t([128, 512], BF16) for _ in range(2)]
        k.act(['hsum', 'raw', 'acc'], ['acc', 'raw'], ra, hsum.rearrange("p n c -> p (n c)"), AF.Square)
        k.red(['acc', 'raw'], ['ssum'], ssum, ra.rearrange("p (g e) -> p g e", e=64), ALU.add)
        k.ts(['ssum'], ['ssum'], ssum, ssum, 1.0 / 64.0, EPS, op0=ALU.mult, op1=ALU.add)
        k.act(['ssum'], ['ssum'], ssum, ssum, AF.Sqrt)
        k.recip(['ssum'], ['ssum'], ssum, ssum)
        hv = hsum.rearrange("p n (h e) -> p (n h) e", e=64)
        k.tt(['hsum', 'ssum'], ['hsum'], hv, hv, ssum.unsqueeze(2).to_broadcast([64, NCH * 2, 64]), ALU.mult)
        k.tt(['hsum', 'wbc'], ['hsum'], hsum, hsum, wbc[:, hp * 128:(hp + 1) * 128].unsqueeze(1).to_broadcast([64, NCH, 128]), ALU.mult)
        ost = acc[:, 0:17 * 128].rearrange("p (n c) -> p n c", c=128)
        for n0 in range(0, NCH, 17):
            k.dma('sp', ['acc'], ['acc'], ost, TM[n0 * 64:(n0 + 17) * 64, 256 + hp * 128:256 + (hp + 1) * 128].rearrange("(n p) c -> p n c", p=64))
            k.act(['acc'], ['acc'], ost, ost, AF.Sigmoid)
            k.tt(['acc', 'hsum'], ['ybf'], ybf[:, n0:n0 + 17, :], hsum[:, n0:n0 + 17, :], ost, ALU.mult)
        for gi, n0 in enumerate(range(0, nout, 8)):
            nn = min(8, nout - n0)
            pk, pp = nps()
            ppb = pp[:].bitcast(BF16)
            for dn in range(nn):
                k.tr(['ybf', 'identbf'], [pk], ppb[:, dn * 64:(dn + 1) * 64], ybf[:, n0 + dn, :], P['identbf'][0:64, 0:64])
            yk = 'ytb%d' % (gi % 2)
            k.cp([pk], [yk], ytb[gi % 2][:, 0:nn * 64], ppb[:, 0:nn * 64])
            k.dma('pool', [yk], ['YT'], YT[256 + hp * 128:256 + (hp + 1) * 128, n0 * 64:(n0 + nn) * 64], ytb[gi % 2][:, 0:nn * 64])
        k.em.barrier()


HC = 32
KB = 256 // HC
NB6 = 512 // HC
NHB = 256 // HC
PI = math.pi
HSTOP = [99]


def hyena_consts():
    c = {}
    f32 = np.float32

    def zfeat(Lx, pos):
        t = np.linspace(0.0, 1.0, Lx, dtype=f32)[pos][:, None]
        w = ((2.0 * math.pi / Lx) * np.arange(Lx, dtype=f32))[pos][:, None]
        f = np.linspace(1e-4, 15.0, 16, dtype=f32)[None, :]
        z = np.concatenate([t, np.cos(f * w), -np.sin(f * w)], axis=-1).astype(f32)
        return z, t
    deltas = np.abs(np.linspace(math.log(1e-2) / 1.5, math.log(1e-2) / 0.3, 256, dtype=f32)).astype(f32)
    z, t = zfeat(L, np.arange(L))
    c['hy_z'] = np.ascontiguousarray(z.T)
    c['hy_decay'] = np.ascontiguousarray(np.exp(-t * deltas[None, :]).T.astype(f32))
    pos = np.concatenate([np.arange(CTX - 1, 0, -1), np.arange(CTX)])
    zc, tc = zfeat(CTX, pos)
    c['hy_zc'] = np.ascontiguousarray(zc.T)
    c['hy_decayc'] = np.ascontiguousarray(np.exp(-tc * deltas[None, :]).T.astype(f32))
    n1 = np.arange(32)[:, None]
    k1 = np.arange(64)[None, :]
    a = 2 * np.pi * n1 * k1 / 64.0
    c['hy_F1'] = np.concatenate([np.cos(a), -np.sin(a)], 1).astype(f32)
    n2 = np.arange(128)[:, None, None]
    kk = (np.arange(64)[None, :, None] + 64 * np.arange(128)[None, None, :])
    a = 2 * np.pi * ((n2 * kk) % 8192) / 8192.0
    c['hy_Gr'] = np.cos(a).astype(f32).reshape(128, 8192)
    c['hy_Gi'] = (-np.sin(a)).astype(f32).reshape(128, 8192)
    k2 = np.arange(128)[:, None]
    nn = np.arange(128)[None, :]
    a = 2 * np.pi * ((k2 * nn) % 128) / 128.0
    c['hy_E1'] = np.concatenate([np.cos(a), np.sin(a)], 1).astype(f32)
    c['hy_E2'] = np.concatenate([-np.sin(a), np.cos(a)], 1).astype(f32)
    k1 = np.arange(64)[:, None, None]
    nfull = np.arange(128)[None, :, None] + 128 * np.arange(32)[None, None, :]
    a = 2 * np.pi * ((k1 * nfull) % 8192) / 8192.0
    c['hy_Mr'] = np.cos(a).astype(f32).reshape(64, 4096)
    c['hy_nMi'] = (-np.sin(a)).astype(f32).reshape(64, 4096)
    return c


def hy_load_bf(k, sb, name, shape, stg, key):
    p, n = shape
    dst = sb.t([p, n], BF16)
    src = k.dram[name]
    step = 2048
    for i, c0 in enumerate(range(0, n, step)):
        c1 = min(n, c0 + step)
        k.dma('sp', [], ['hstg'], stg[0:p, 0:c1 - c0], src[:, c0:c1])
        k.cp(['hstg'], [key], dst[:, c0:c1], stg[0:p, 0:c1 - c0], eng=('dve' if i % 2 == 0 else 'pool'))
    return dst


def fft_fwd(k, C, xbf, A, nAi, psctr, consume):
    F1, Gr, Gi = C['F1'], C['Gr'], C['Gi']
    for c0 in range(0, HC, 4):
        pk, pp = psctr()
        for dc in range(4):
            k.mm(['xbf', 'F1'], [pk], pp[:, dc * 128:(dc + 1) * 128], xbf[:, c0 + dc, :], F1)
        src = pp[:, 0:512].rearrange("p (c r q) -> p c r q", r=2, q=64)
        k.cp([pk], ['A'], A[:, c0:c0 + 4, :, :], src, eng='act')
        k.ts(['A'], ['nAi'], nAi[:, c0:c0 + 4, :], A[:, c0:c0 + 4, 1, :], -1.0, op0=ALU.mult)
    for k0 in range(0, 64, KB):
        pk, pp = psctr()
        for dk in range(KB):
            k1 = k0 + dk
            xr = pp[:, dk * 2 * HC:dk * 2 * HC + HC]
            xi = pp[:, dk * 2 * HC + HC:(dk + 1) * 2 * HC]
            k.mm(['Gr', 'A'], [pk], xr, Gr[:, k1, :], A[:, :, 0, k1], start=True, stop=False)
            k.mm(['Gi', 'nAi'], [pk], xr, Gi[:, k1, :], nAi[:, :, k1], start=False, stop=True)
            k.mm(['Gi', 'A'], [pk], xi, Gi[:, k1, :], A[:, :, 0, k1], start=True, stop=False)
            k.mm(['Gr', 'A'], [pk], xi, Gr[:, k1, :], A[:, :, 1, k1], start=False, stop=True)
        consume(pk, pp, k0)


def phaseH(k, P, l, base, last):
    nc = k.nc
    UT, YT = k.dram['UT'], k.dram['YT']
    HK, HH, CK, UC = k.dram['HK'], k.dram['HH'], k.dram['CK'], k.dram['UC']
    psi = [0]

    def nps():
        i = psi[0]
        psi[0] = (psi[0] + 1) % 8
        return 'ps%d' % i, k.ps[i]

    sb = SB(nc, base=base)
    w1 = sb.t([33, 64], F32)
    w2 = sb.t([64, 64], F32)
    w3 = sb.t([64, 1024], F32)
    sc = sb.t([64, 8], F32)
    b3 = sb.t([128, 8], F32)
    k.dma('sp', [], ['w1'], w1, k.dram['hy_filt_w1'][l])
    k.dma('sp', [], ['w2'], w2, k.dram['hy_filt_w2'][l])
    k.dma('sp', [], ['w3'], w3, k.dram['hy_filt_w3'][l])
    k.dma('sp', [], ['sc'], sc[:, 0:3], k.dram['hy_filt_sc'][l])
    k.dma('sp', [], ['b3'], b3, k.dram['hy_b3_col'][l])
    k.tt(['sc'], ['sc'], sc[:, 3:4], sc[:, 0:1], sc[:, 1:2], ALU.mult)
    k.tt(['sc'], ['sc'], sc[:, 4:5], sc[:, 0:1], sc[:, 2:3], ALU.mult)
    zT = sb.t([33, L], F32)
    h2 = sb.t([64, L], F32)
    h1 = sb.t([64, 512], F32)
    m1 = sb.t([64, 512], F32)
    m2 = sb.t([64, 512], F32)

    def sin_layer(src_ap, wt, kdim, bcol, dst_ap, n):
        pk, pp = nps()
        k.mm(['w1', 'w2', 'zT', 'h1'], [pk], pp[0:64, 0:n], wt, src_ap)
        k.ts([pk, 'sc'], ['m0'], dst_ap, pp[0:64, 0:n], sc[:, 0:1], sc[:, bcol:bcol + 1], op0=ALU.mult, op1=ALU.add)
        k.ts(['m0'], ['m1'], m1[:, 0:n], dst_ap, PI, -2.0 * PI, op0=ALU.is_gt, op1=ALU.mult)
        k.ts(['m0'], ['m2'], m2[:, 0:n], dst_ap, -PI, 2.0 * PI, op0=ALU.is_lt, op1=ALU.mult, eng='pool')
        k.tt(['m1', 'm2'], ['m1'], m1[:, 0:n], m1[:, 0:n], m2[:, 0:n], ALU.add)
        k.tt(['m0', 'm1'], ['m0'], dst_ap, dst_ap, m1[:, 0:n], ALU.add)
        k.act(['m0'], ['m0'], dst_ap, dst_ap, AF.Sin)

    def mlp(zsrc_name, ncols, h2dst):
        k.dma('sp', ['zT'], ['zT'], zT[:, 0:ncols], k.dram[zsrc_name][:, :])
        for c0 in range(0, ncols, 512):
            n = min(512, ncols - c0)
            sin_layer(zT[:, c0:c0 + n], w1, 33, 3, h1[:, 0:n], n)
            sin_layer2(c0, n, h2dst)

    def sin_layer2(c0, n, h2dst):
        pk, pp = nps()
        k.mm(['w2', 'm0'], [pk], pp[0:64, 0:n], w2, h1[:, 0:n])
        d = h2dst[:, c0:c0 + n]
        k.ts([pk, 'sc'], ['h2'], d, pp[0:64, 0:n], sc[:, 0:1], sc[:, 4:5], op0=ALU.mult, op1=ALU.add)
        k.ts(['h2'], ['m1'], m1[:, 0:n], d, PI, -2.0 * PI, op0=ALU.is_gt, op1=ALU.mult)
        k.ts(['h2'], ['m2'], m2[:, 0:n], d, -PI, 2.0 * PI, op0=ALU.is_lt, op1=ALU.mult, eng='pool')
        k.tt(['m1', 'm2'], ['m1'], m1[:, 0:n], m1[:, 0:n], m2[:, 0:n], ALU.add)
        k.tt(['h2', 'm1'], ['h2'], d, d, m1[:, 0:n], ALU.add)
        k.act(['h2'], ['h2'], d, d, AF.Sin)

    dec = [sb.t([128, L], F32) for _ in range(2)]
    kraw = [sb.t([128, L], F32) for _ in range(2)]
    kbfo = [sb.t([128, L], BF16) for _ in range(2)]
    junk = sb.t([128, L], BF16)
    asum = sb.t([128, 4], F32)

    def gen_filters(zname, dname, ncols, ctx):
        mlp(zname, ncols, h2)
        for ch in range(2):
            k.dma('sp', ['dec%d' % ch], ['dec%d' % ch], dec[ch][:, 0:ncols], k.dram[dname][ch * 128:(ch + 1) * 128, :])
        for o in range(2):
            for ch in range(2):
                k.memset([], ['asum'], asum, 0.0)
                for d in range(2):
                    fc = o * 4 + d * 2 + ch
                    kr = kraw[d]
                    kk_ = 'kraw%d' % d
                    for c0 in range(0, ncols, 512):
                        n = min(512, ncols - c0)
                        pk, pp = nps()
                        k.mm(['w3', 'h2'], [pk], pp[:, 0:n], w3[:, fc * 128:(fc + 1) * 128], h2[:, c0:c0 + n])
                        k.act([pk, 'b3'], [kk_], kr[:, c0:c0 + n], pp[:, 0:n], AF.Identity, bias=b3[:, fc:fc + 1], scale=1.0)
                    k.tt([kk_, 'dec%d' % ch], [kk_], kr[:, 0:ncols], kr[:, 0:ncols], dec[ch][:, 0:ncols], ALU.mult)
                    if not ctx:
                        if d == 1:
                            k.memset([kk_], [kk_], kr[:, 0:1], 0.0)
                        k.act([kk_, 'asum'], ['junk', 'asum'], junk[:, 0:ncols], kr[:, 0:ncols], AF.Abs, accum_out=asum[:, d:d + 1])
                    else:
                        lo, hi = (255, 511) if d == 0 else (0, 255)
                        k.act([kk_, 'asum'], ['junk', 'asum'], junk[:, lo:hi], kr[:, lo:hi], AF.Abs, accum_out=asum[:, d:d + 1])
                k.tt(['asum'], ['asum'], asum[:, 2:3], asum[:, 0:1], asum[:, 1:2], ALU.add)
                k.recip(['asum'], ['asum'], asum[:, 2:3], asum[:, 2:3])
                if not ctx:
                    for d in range(2):
                        fc = o * 4 + d * 2 + ch
                        k.ts(['kraw%d' % d, 'asum'], ['kbfo%d' % d], kbfo[d], kraw[d], asum[:, 2:3], op0=ALU.mult,
                             eng=('dve' if d == 0 else 'pool'))
                        k.dma('pool', ['kbfo%d' % d], ['HK'], HK[fc], kbfo[d])
                else:
                    k.ts(['kraw0', 'asum'], ['kraw0'], kraw[0][:, 255:511], kraw[0][:, 255:511], asum[:, 2:3], op0=ALU.mult)
                    k.ts(['kraw1', 'asum', 'kraw0'], ['kraw0'], kraw[0][:, 0:255], kraw[1][:, 0:255], asum[:, 2:3], op0=ALU.mult)
                    k.dma('pool', ['kraw0'], ['CK'], CK[o * 2 + ch], kraw[0][:, 0:511])

    gen_filters('hy_z', 'hy_decay', L, False)
    if not last:
        gen_filters('hy_zc', 'hy_decayc', 511, True)
    k.em.barrier()

    if HSTOP[0] <= 1:
        return
    sb = SB(nc, base=base)
    stg = sb.t([128, 2048], F32)
    C = {}
    C['F1'] = hy_load_bf(k, sb, 'hy_F1', [32, 128], stg, 'F1')
    C['Gr'] = hy_load_bf(k, sb, 'hy_Gr', [128, 8192], stg, 'Gr').rearrange("p (q m) -> p q m", m=128)
    C['Gi'] = hy_load_bf(k, sb, 'hy_Gi', [128, 8192], stg, 'Gi').rearrange("p (q m) -> p q m", m=128)
    C['E1'] = hy_load_bf(k, sb, 'hy_E1', [128, 256], stg, 'E1')
    C['E2'] = hy_load_bf(k, sb, 'hy_E2', [128, 256], stg, 'E2')
    C['Mr'] = hy_load_bf(k, sb, 'hy_Mr', [64, 4096], stg, 'Mr').rearrange("p (n m) -> p n m", m=32)
    C['nMi'] = hy_load_bf(k, sb, 'hy_nMi', [64, 4096], stg, 'nMi').rearrange("p (n m) -> p n m", m=32)
    A = sb.t([128, HC, 2, 64], BF16)
    nAi = sb.t([128, HC, 64], BF16)
    base2 = sb.off
    if HSTOP[0] <= 1.5:
        k.em.barrier()
        return

    sbs = SB(nc, base=base2)
    kbf = [sbs.t([32, HC, 128], BF16) for _ in range(2)]
    Hacc = sbs.t([128, 64, 2, HC], F32)
    Hbf = sbs.t([128, 64, 2, HC], BF16)
    xtmp = sbs.t([128, KB, 2, HC], F32)
    SC = 1.0 / 8192.0
    for o in range(2):
        for b4 in range(NHB):
            ch, coff = (b4 * HC) // 128, (b4 * HC) % 128
            for d in range(2):
                fc = o * 4 + d * 2 + ch
                k.dma('sp', ['xbf'], ['xbf'], kbf[d], HK[fc][coff:coff + HC, :].rearrange("c (a b) -> a c b", b=128))

                def consume(pk, pp, k0, d=d):
                    src = pp[:, 0:512].rearrange("p (q r c) -> p q r c", r=2, c=HC)
                    dst = Hacc[:, k0:k0 + KB, :, :]
                    if d == 0:
                        k.act([pk], ['Hacc'], dst, src, AF.Copy, scale=SC)
                    else:
                        k.act([pk], ['xtmp'], xtmp, src, AF.Copy, scale=SC)
                        k.tt(['xtmp', 'Hacc'], ['Hacc'], dst[:, :, 0, :], dst[:, :, 0, :], xtmp[:, :, 0, :], ALU.add)
                        k.tt(['xtmp', 'Hacc'], ['Hacc'], dst[:, :, 1, :], dst[:, :, 1, :], xtmp[:, :, 1, :], ALU.subtract, eng='pool')
                fft_fwd(k, C, kbf[d], A, nAi, nps, consume)
            k.cp(['Hacc'], ['Hbf'], Hbf, Hacc, eng='pool')
            k.dma('pool', ['Hbf'], ['HH'], HH[o * NHB + b4], Hbf.rearrange("p q r c -> p (q r c)"))
    k.em.barrier()

    if HSTOP[0] <= 2:
        return
    sbc = SB(nc, base=base2)
    raw = sbc.t([128, T], F32)
    acc = sbc.t([128, T], F32)
    ucb = sbc.t([128, T], BF16)
    cw = sbc.t([128, 6, 4], F32)
    k.dma('sp', [], ['cw'], cw, k.dram['hy_conv_col'][l])
    for cc in range(6):
        k.dma('sp', ['raw'], ['raw'], raw, UT[cc * 128:(cc + 1) * 128, :])
        k.ts(['raw', 'cw'], ['acc'], acc, raw, cw[:, cc, 1:2], cw[:, cc, 3:4], op0=ALU.mult, op1=ALU.add)
        for (a, b) in ((0, L), (L, T)):
            k.stt(['raw', 'cw', 'acc'], ['acc'], acc[:, a + 1:b], raw[:, a:b - 1], cw[:, cc, 0:1], acc[:, a + 1:b], ALU.mult, ALU.add)
            k.stt(['raw', 'cw', 'acc'], ['acc'], acc[:, a:b - 1], raw[:, a + 1:b], cw[:, cc, 2:3], acc[:, a:b - 1], ALU.mult, ALU.add)
        k.cp(['acc'], ['ucb'], ucb, acc, eng='act')
        k.dma('pool', ['ucb'], ['UC'], UC[cc * 128:(cc + 1) * 128, :], ucb)
    k.em.barrier()

    if HSTOP[0] <= 3:
        return
    sbd = SB(nc, base=base2)
    vbf = sbd.t([32, HC, 128], BF16)
    x1bf = sbd.t([32, HC, 128], BF16)
    x2bf = sbd.t([32, HC, 128], BF16)
    z1 = sbd.t([32, HC, 128], BF16)
    z2 = sbd.t([32, HC, 128], BF16)
    dv = sbd.t([32, HC, 128], BF16)
    dbc = sbd.t([32, 2, 256], F32)
    Hs = sbd.t([128, 64, 2, HC], BF16)
    Y = sbd.t([128, 64, 2, HC], BF16)
    Zs = sbd.t([64, HC, 2, 128], BF16)
    xs = [sbd.t([128, KB, 2, HC], F32) for _ in range(2)]
    ta = [sbd.t([128, KB, HC], F32) for _ in range(2)]
    tb = [sbd.t([128, KB, HC], F32) for _ in range(2)]
    tg = [sbd.t([32, HC, NB6], F32) for _ in range(2)]
    k.dma('sp', [], ['dbc'], dbc, k.dram['hy_d_bc'][l])
    xctr = [0]
    for b4 in range(NHB):
        c0g = b4 * HC
        for (tile_, key, r0) in ((vbf, 'xbf', 0), (x1bf, 'x1bf', 256), (x2bf, 'x2bf', 512)):
            k.dma('sp', [key], [key], tile_, UC[r0 + c0g:r0 + c0g + HC, 0:L].rearrange("c (a b) -> a c b", b=128))
        for o in range(2):
            xin, xkey = (vbf, 'xbf') if o == 0 else (z1, 'z1')
            gate, gkey = (x1bf, 'x1bf') if o == 0 else (x2bf, 'x2bf')
            zout, zkey = (z1, 'z1') if o == 0 else (z2, 'z2')
            k.tt([xkey, 'dbc'], ['dv'], dv, xin, dbc[:, o, c0g:c0g + HC].unsqueeze(2).to_broadcast([32, HC, 128]), ALU.mult, eng='pool')
            k.dma('sp', ['Hs'], ['Hs'], Hs.rearrange("p q r c -> p (q r c)"), HH[o * NHB + b4])

            def consume(pk, pp, k0):
                i = xctr[0] % 2
                xctr[0] += 1
                xk, tak, tbk = 'xs%d' % i, 'ta%d' % i, 'tb%d' % i
                k.cp([pk], [xk], xs[i], pp[:, 0:512].rearrange("p (q r c) -> p q r c", r=2, c=HC), eng='act')
                Xr, Xi = xs[i][:, :, 0, :], xs[i][:, :, 1, :]
                Hr, Hi = Hs[:, k0:k0 + KB, 0, :], Hs[:, k0:k0 + KB, 1, :]
                Yr, Yi = Y[:, k0:k0 + KB, 0, :], Y[:, k0:k0 + KB, 1, :]
                k.tt([xk, 'Hs'], [tak], ta[i], Xr, Hr, ALU.mult)
                k.tt([xk, 'Hs'], [tbk], tb[i], Xi, Hi, ALU.mult, eng='pool')
                k.tt([tak, tbk], ['Y'], Yr, ta[i], tb[i], ALU.subtract)
                k.tt([xk, 'Hs'], [tak], ta[i], Xr, Hi, ALU.mult, eng='pool')
                k.tt([xk, 'Hs'], [tbk], tb[i], Xi, Hr, ALU.mult)
                k.tt([tak, tbk], ['Y'], Yi, ta[i], tb[i], ALU.add, eng='pool')
            fft_fwd_keyed(k, C, xin, xkey, A, nAi, nps, consume)
            for c0 in range(0, HC, 2):
                pk, pp = nps()
                for dc in range(2):
                    cidx = c0 + dc
                    out = pp[0:64, dc * 256:(dc + 1) * 256]
                    k.mm(['Y', 'E1'], [pk], out, Y[:, :, 0, cidx], C['E1'], start=True, stop=False)
                    k.mm(['Y', 'E2'], [pk], out, Y[:, :, 1, cidx], C['E2'], start=False, stop=True)
                src = pp[0:64, 0:512].rearrange("p (c r n) -> p c r n", r=2, n=128)
                k.cp([pk], ['Zs'], Zs[:, c0:c0 + 2, :, :], src, eng=('act' if (c0 // 2) % 2 == 0 else 'dve'))
            for g8, n0 in enumerate(range(0, 128, NB6)):
                pk, pp = nps()
                for dn in range(NB6):
                    n2 = n0 + dn
                    out = pp[0:32, dn * HC:(dn + 1) * HC]
                    k.mm(['Mr', 'Zs'], [pk], out, C['Mr'][:, n2, :], Zs[:, :, 0, n2], start=True, stop=False)
                    k.mm(['nMi', 'Zs'], [pk], out, C['nMi'][:, n2, :], Zs[:, :, 1, n2], start=False, stop=True)
                i = g8 % 2
                src = pp[0:32, 0:NB6 * HC].rearrange("p (n c) -> p c n", c=HC)
                k.tt([pk, 'dv'], ['tg%d' % i], tg[i], src, dv[:, :, n0:n0 + NB6], ALU.add)
                k.tt(['tg%d' % i, gkey], [zkey], zout[:, :, n0:n0 + NB6], tg[i], gate[:, :, n0:n0 + NB6], ALU.mult, eng='pool')
        k.dma('pool', ['z2'], ['YT'], YT[c0g:c0g + HC, 0:L].rearrange("c (a b) -> a c b", b=128), z2)
    k.em.barrier()

    if last or HSTOP[0] <= 4:
        return
    sbx = SB(nc, base=base2)
    ub = sbx.t([128, 3, CTX], BF16)
    uf = sbx.t([128, 3, CTX], F32)
    kf = sbx.t([128, 511], F32)
    accs = [sbx.t([128, CTX], F32) for _ in range(4)]
    zc = sbx.t([128, CTX], F32)
    zb = sbx.t([128, CTX], BF16)
    dcol = sbx.t([128, 4], F32)
    k.dma('sp', [], ['dcol'], dcol, k.dram['hy_d_col'][l])
    for ch in range(2):
        for j in range(3):
            k.dma('sp', ['ub'], ['ub'], ub[:, j, :], UC[j * 256 + ch * 128:j * 256 + (ch + 1) * 128, L:T])
        k.cp(['ub'], ['uf'], uf, ub)
        for o in range(2):
            uin = uf[:, 0, :] if o == 0 else zc
            gate = uf[:, 1 + o, :]
            k.dma('sp', ['kf'], ['kf'], kf, CK[o * 2 + ch])
            for a in range(4):
                k.memset([], ['acc%d' % a], accs[a], 0.0, eng=('dve' if a < 2 else 'pool'))
            for s_ in range(CTX):
                a = s_ % 4
                k.stt(['kf', 'uf', 'zc', 'acc%d' % a], ['acc%d' % a], accs[a], kf[:, 255 - s_:511 - s_], uin[:, s_:s_ + 1], accs[a],
                      ALU.mult, ALU.add)
            k.tt(['acc0', 'acc1'], ['acc0'], accs[0], accs[0], accs[1], ALU.add)
            k.tt(['acc2', 'acc3'], ['acc2'], accs[2], accs[2], accs[3], ALU.add, eng='pool')
            k.tt(['acc0', 'acc2'], ['acc0'], accs[0], accs[0], accs[2], ALU.add)
            k.stt(['uf', 'zc', 'dcol', 'acc0'], ['acc0'], accs[0], uin, dcol[:, o * 2 + ch:o * 2 + ch + 1], accs[0], ALU.mult, ALU.add)
            k.tt(['acc0', 'uf'], ['zc'], zc, accs[0], gate, ALU.mult)
        k.cp(['zc'], ['zb'], zb, zc)
        k.dma('pool', ['zb'], ['YT'], YT[ch * 128:(ch + 1) * 128, L:T], zb)
    k.em.barrier()


def fft_fwd_keyed(k, C, xin, xkey, A, nAi, psctr, consume):
    F1, Gr, Gi = C['F1'], C['Gr'], C['Gi']
    for c0 in range(0, HC, 4):
        pk, pp = psctr()
        for dc in range(4):
            k.mm([xkey, 'F1'], [pk], pp[:, dc * 128:(dc + 1) * 128], xin[:, c0 + dc, :], F1)
        src = pp[:, 0:512].rearrange("p (c r q) -> p c r q", r=2, q=64)
        k.cp([pk], ['A'], A[:, c0:c0 + 4, :, :], src, eng='act')
        k.ts(['A'], ['nAi'], nAi[:, c0:c0 + 4, :], A[:, c0:c0 + 4, 1, :], -1.0, op0=ALU.mult)
    for k0 in range(0, 64, KB):
        pk, pp = psctr()
        for dk in range(KB):
            k1 = k0 + dk
            xr = pp[:, dk * 2 * HC:dk * 2 * HC + HC]
            xi = pp[:, dk * 2 * HC + HC:(dk + 1) * 2 * HC]
            k.mm(['Gr', 'A'], [pk], xr, Gr[:, k1, :], A[:, :, 0, k1], start=True, stop=False)
            k.mm(['Gi', 'nAi'], [pk], xr, Gi[:, k1, :], nAi[:, :, k1], start=False, stop=True)
            k.mm(['Gi', 'A'], [pk], xi, Gi[:, k1, :], A[:, :, 0, k1], start=True, stop=False)
            k.mm(['Gr', 'A'], [pk], xi, Gr[:, k1, :], A[:, :, 1, k1], start=False, stop=True)
        consume(pk, pp, k0)


BIG = 1.0e9
I32 = mybir.dt.int32


def moe_sched(ntiles):
    Tl = ntiles * 128
    J = [max(1, -(-min(Tl, (2 * Tl) // (r + 1)) // 128)) for r in range(32)]
    S = [0]
    for j in J:
        S.append(S[-1] + j * 128)
    return J, S


NSLOT = moe_sched(NT)[1][-1]


def phaseC(k, P, l, base, last, xres):
    nc = k.nc
    em = k.em
    YT, XMIX, XRES = k.dram['YT'], k.dram['XMIX'], k.dram['XRES']
    XG, YE, H2M = k.dram['XG'], k.dram['YE'], k.dram['H2M']
    ntiles = 32 if last else NT
    J, S = moe_sched(ntiles)
    psi = [0]

    def nps():
        i = psi[0]
        psi[0] = (psi[0] + 1) % 8
        return 'ps%d' % i, k.ps[i]

    recent = []

    def idma(reads, writes, fn):
        i = em.dnext
        em.dnext = (i + 1) % em.NDMA
        if em.dcnt[i] > 0:
            em._wait('pool', (('d', i), 16 * em.dcnt[i]))
        if len(recent) >= 4:
            em._wait('pool', recent[-4])
        em._deps('pool', reads, writes)
        ins = fn(em.eng['pool'])
        em.dcnt[i] += 1
        ins.then_inc(em.dsem[i], 16)
        em._record((('d', i), 16 * em.dcnt[i]), reads, writes)
        recent.append((('d', i), 16 * em.dcnt[i]))
        em.ninst += 1

    sb0 = SB(nc, base=base)
    grow = sb0.t([128, 2, 2, D], F32)
    mrow = sb0.t([128, 2, 2, D], F32)
    GW = sb0.t([128, NT, 2], F32)
    SIDX = sb0.t([128, NT, 2], I32)
    IDX = sb0.t([128, 32], I32)
    tri128 = sb0.t([128, 128], F32)
    iota32 = sb0.t([128, 32], F32)
    ltmask = sb0.t([128, 1024], F32)
    pidx = sb0.t([128, 1], F32)
    Srow = sb0.t([128, 32], F32)
    dg = sb0.t([128, 128], F32)
    k.dma('sp', [], ['tri128'], tri128, k.dram['tri128'][:, :])
    k.dma('sp', [], ['iota32'], iota32, k.dram['iota32'][:, :])
    k.dma('sp', [], ['ltmask'], ltmask, k.dram['ltmask'][:, :])
    k.dma('sp', [], ['pidx'], pidx, k.dram['pidx'][:, :])
    k.dma('sp', [], ['Srow'], Srow, k.dram['moe_S'][0 if ntiles == NT else 1])
    for gi, c0 in enumerate((16, 40)):
        for j in range(2):
            for kk in range(8):
                k.ts(['ident32', 'mod'], ['dg'], dg, P['ident32'], P['mod'][:, l, c0 + kk, j:j + 1], op0=ALU.mult)
                pk, pp = nps()
                k.mm(['ones32', 'dg'], [pk], pp[:, 0:128], P['ones32'], dg)
                k.cp([pk], ['grow'], grow[:, gi, j, kk * 128:(kk + 1) * 128], pp[:, 0:128], eng='act')
    for mi in range(2):
        for j in range(2):
            for kk in range(8):
                col = P['A2'][:, l, kk, j:j + 1] if mi == 0 else P['mod'][:, l, 24 + kk, j:j + 1]
                k.ts(['ident32', 'mod', 'A2'], ['dg'], dg, P['ident32'], col, op0=ALU.mult)
                pk, pp = nps()
                k.mm(['ones32', 'dg'], [pk], pp[:, 0:128], P['ones32'], dg)
                k.cp([pk], ['mrow'], mrow[:, mi, j, kk * 128:(kk + 1) * 128], pp[:, 0:128], eng='act')
    base1 = sb0.off
    sb = SB(nc, base=base1)
    Wout = sb.t([128, 8, D], BF16)
    Wr = sb.t([128, 8, 36], F32)
    rb = sb.t([128, 36], F32)
    stg = sb.t([128, 8, 512], F32)
    wv = k.dram['w_out'][l].rearrange("(k p) n -> p k n", p=128)
    for i, c0 in enumerate((0, 512)):
        k.dma('sp', ['stg'], ['stg'], stg, wv[:, :, c0:c0 + 512])
        k.cp(['stg'], ['Wout'], Wout[:, :, c0:c0 + 512], stg)
    k.dma('sp', [], ['Wr'], Wr, k.dram['moe_wr'][l].rearrange("(k p) n -> p k n", p=128))
    k.dma('sp', [], ['rb'], rb, k.dram['moe_rb_bc'][l])
    yT = [sb.t([128, 8, 512], BF16) for _ in range(2)]
    xt = [sb.t([128, D], F32) for _ in range(2)]
    xm = [sb.t([128, D], F32) for _ in range(2)]
    xn = sb.t([128, D], F32)
    tmpm = sb.t([128, D], F32)
    hm = [sb.t([128, D], BF16) for _ in range(2)]
    junk = sb.t([128, D], BF16)
    h32 = sb.t([128, 8, 128], F32)
    ss = [sb.t([128, 2], F32) for _ in range(2)]
    lg = sb.t([128, 36], F32)
    rt = sb.t([128, 16], F32)
    oh = sb.t([128, 4], F32)
    ml = sb.t([128, 32], F32)
    e1 = sb.t([128, 32], F32)
    e2 = sb.t([128, 32], F32)
    esum = sb.t([128, 32], F32)
    tmp32 = sb.t([128, 32], F32)
    basec = sb.t([128, 32], F32)
    erank = sb.t([128, 32], F32)
    SE = sb.t([128, 32], F32)
    EID = sb.t([128, 32], F32)
    idxf = sb.t([128, 2, 32], F32)
    EH = sb.t([128, 2, NT, 32], F32)
    RK = sb.t([128, NT, 32], F32)
    tA = sb.t([128, NT, 32], F32)
    sidf = sb.t([128, NT, 2], F32)
    k.memset([], ['basec'], basec, 0.0)
    k.memset([], ['EH'], EH, 0.0)
    k.memset([], ['RK'], RK, 0.0)
    for ti in range(ntiles):
        j = 0 if ti < 32 else 1
        g, tl = ti // 4, ti % 4
        yb = g % 2
        yk = 'yT%d' % yb
        if tl == 0:
            n = min(512, ntiles * 128 - g * 512)
            k.dma('sp', [yk], [yk], yT[yb][:, :, 0:n], YT[:, g * 512:g * 512 + n].rearrange("(c p) t -> p c t", p=128))
        b = ti % 2
        xk, mk, sk, hk = 'xt%d' % b, 'xm%d' % b, 'ss%d' % b, 'hm%d' % b
        k.dma('sp', [xk], [xk], xt[b], xres[ti * 128:(ti + 1) * 128, :])
        for half in range(2):
            pk, pp = nps()
            for f in range(8):
                k.mm([yk, 'Wout'], [pk], pp[:, 0:512], yT[yb][:, f, tl * 128:(tl + 1) * 128], Wout[:, f, half * 512:(half + 1) * 512],
                     start=(f == 0), stop=(f == 7))
            cs = slice(half * 512, (half + 1) * 512)
            k.tt([pk, 'grow'], [mk], xm[b][:, cs], pp[:, 0:512], grow[:, 0, j, cs], ALU.mult)
            k.tt([mk, xk], [mk], xm[b][:, cs], xm[b][:, cs], xt[b][:, cs], ALU.add, eng='pool')
        k.dma('pool', [mk], ['XMIX'], XMIX[ti * 128:(ti + 1) * 128, :], xm[b])
        k.memset([], [sk], ss[b], 0.0)
        k.act([mk, sk], ['junk', sk], junk, xm[b], AF.Square, accum_out=ss[b][:, 0:1])
        k.ts([sk], [sk], ss[b][:, 1:2], ss[b][:, 0:1], 1.0 / D, EPS, op0=ALU.mult, op1=ALU.add)
        k.act([sk], [sk], ss[b][:, 1:2], ss[b][:, 1:2], AF.Sqrt)
        k.recip([sk], [sk], ss[b][:, 1:2], ss[b][:, 1:2])
        k.ts([mk, sk], ['xn'], xn, xm[b], ss[b][:, 1:2], op0=ALU.mult)
        k.tt(['xn', 'mrow'], ['tmpm'], tmpm, xn, mrow[:, 0, j, :], ALU.mult)
        k.tt(['tmpm', 'mrow'], [hk], hm[b], tmpm, mrow[:, 1, j, :], ALU.add, eng='pool')
        k.dma('sp', [hk], ['H2M%d' % ti], H2M[ti * 128:(ti + 1) * 128, :], hm[b])
        for h2 in range(2):
            pk, pp = nps()
            for q in range(4):
                kk = h2 * 4 + q
                k.tr(['xn', 'ident32'], [pk], pp[:, q * 128:(q + 1) * 128], xn[:, kk * 128:(kk + 1) * 128], P['ident32'])
            for q in range(4):
                kk = h2 * 4 + q
                k.act([pk, 'A2', 'mod'], ['h32'], h32[:, kk, :], pp[:, q * 128:(q + 1) * 128], AF.Identity,
                      scale=P['A2'][:, l, kk, j:j + 1], bias=P['mod'][:, l, 24 + kk, j:j + 1])
        pk, pp = nps()
        for kk in range(8):
            k.mm(['h32', 'Wr'], [pk], pp[:, 0:36], h32[:, kk, :], Wr[:, kk, :], start=(kk == 0), stop=(kk == 7))
        k.tt([pk, 'rb'], ['lg'], lg, pp[:, 0:36], rb, ALU.add)
        k.red(['lg'], ['rt'], rt[:, 0:1], lg[:, 0:4], ALU.max)
        k.ts(['lg', 'rt'], ['oh'], oh, lg[:, 0:4], rt[:, 0:1], op0=ALU.is_equal)
        k.ts(['rt'], ['rt'], rt[:, 1:2], rt[:, 0:1], -1.0, op0=ALU.mult)
        k.memset(['rt'], ['rt'], rt[:, 2:3], 0.0)
        k.act(['lg', 'rt'], ['tmp32', 'rt'], tmp32[:, 0:4], lg[:, 0:4], AF.Exp, bias=rt[:, 1:2], scale=1.0, accum_out=rt[:, 2:3])
        k.recip(['rt'], ['rt'], rt[:, 3:4], rt[:, 2:3])
        k.ts(['oh'], ['oh'], oh, oh, 1.0, BIG, op0=ALU.subtract, op1=ALU.mult)
        k.tt(['lg', 'oh'], ['ml'], ml.rearrange("p (g e) -> p g e", e=8), lg[:, 4:36].rearrange("p (g e) -> p g e", e=8),
             oh.unsqueeze(2).to_broadcast([128, 4, 8]), ALU.add)
        k.red(['ml'], ['rt'], rt[:, 4:5], ml, ALU.max)
        k.ts(['ml', 'rt'], ['e1'], e1, ml, rt[:, 4:5], op0=ALU.is_equal)
        k.ts(['e1'], ['tmp32'], tmp32, e1, -BIG, op0=ALU.mult)
        k.tt(['ml', 'tmp32'], ['ml'], ml, ml, tmp32, ALU.add)
        k.red(['ml'], ['rt'], rt[:, 5:6], ml, ALU.max)
        k.ts(['ml', 'rt'], ['e2'], e2, ml, rt[:, 5:6], op0=ALU.is_equal)
        k.tt(['rt'], ['rt'], rt[:, 6:7], rt[:, 5:6], rt[:, 4:5], ALU.subtract)
        k.act(['rt'], ['rt'], rt[:, 6:7], rt[:, 6:7], AF.Exp)
        k.ts(['rt'], ['rt'], rt[:, 7:8], rt[:, 6:7], 1.0, op0=ALU.add)
        k.recip(['rt'], ['rt'], rt[:, 7:8], rt[:, 7:8])
        k.tt(['rt'], ['rt'], rt[:, 8:9], rt[:, 6:7], rt[:, 7:8], ALU.mult)
        k.tt(['rt'], ['rt'], rt[:, 9:10], rt[:, 7:8], rt[:, 3:4], ALU.mult)
        k.tt(['rt'], ['rt'], rt[:, 10:11], rt[:, 8:9], rt[:, 3:4], ALU.mult)
        k.cp(['rt'], ['GW'], GW[:, ti, :], rt[:, 9:11], eng='pool')
        k.cp(['e1'], ['EH'], EH[:, 0, ti, :], e1, eng='pool')
        k.cp(['e2'], ['EH'], EH[:, 1, ti, :], e2, eng='pool')
        k.tt(['e1', 'e2'], ['esum'], esum, e1, e2, ALU.add)
        pk, pp = nps()
        k.mm(['tri128', 'esum'], [pk], pp[:, 0:32], tri128, esum)
        k.mm(['ones32', 'esum'], [pk], pp[:, 32:64], P['ones32'], esum)
        k.tt([pk, 'basec'], ['RK'], RK[:, ti, :], pp[:, 0:32], basec, ALU.add)
        k.tt([pk, 'basec'], ['basec'], basec, basec, pp[:, 32:64], ALU.add)
    stgf = stg.rearrange("p a b -> p (a b)")
    A3 = stgf[:, 0:1024].rearrange("p (a b) -> p a b", b=32)
    B3 = stgf[:, 1024:2048].rearrange("p (a b) -> p a b", b=32)
    C3 = stgf[:, 2048:3072].rearrange("p (a b) -> p a b", b=32)
    cnt_o = basec.unsqueeze(1).to_broadcast([128, 32, 32])
    cnt_s = basec.unsqueeze(2).to_broadcast([128, 32, 32])
    k.tt(['basec', 'stg'], ['stg'], A3, cnt_o, cnt_s, ALU.is_gt)
    k.tt(['basec', 'stg'], ['stg'], B3, cnt_o, cnt_s, ALU.is_equal)
    k.tt(['stg', 'ltmask'], ['stg'], B3, B3, ltmask.rearrange("p (a b) -> p a b", b=32), ALU.mult)
    k.tt(['stg'], ['stg'], A3, A3, B3, ALU.add)
    k.red(['stg'], ['erank'], erank, A3, ALU.add)
    k.tt(['erank', 'iota32', 'stg'], ['stg'], A3, erank.unsqueeze(2).to_broadcast([128, 32, 32]),
         iota32.unsqueeze(1).to_broadcast([128, 32, 32]), ALU.is_equal)
    k.tt(['stg', 'Srow'], ['stg'], B3, A3, Srow.unsqueeze(1).to_broadcast([128, 32, 32]), ALU.mult)
    k.red(['stg'], ['SE'], SE, B3, ALU.add)
    k.tt(['stg', 'iota32'], ['stg'], C3, A3, iota32.unsqueeze(2).to_broadcast([128, 32, 32]), ALU.mult)
    k.red(['stg'], ['EID'], EID, C3.rearrange("p e r -> p r e"), ALU.add)
    k.ts(['EID'], ['idxf'], idxf[:, 0, :], EID, 128.0, float(l * 32 * 128), op0=ALU.mult, op1=ALU.add)
    k.ts(['idxf', 'pidx'], ['idxf'], idxf[:, 0, :], idxf[:, 0, :], pidx[:, 0:1], op0=ALU.add)
    k.cp(['idxf'], ['IDX'], IDX, idxf[:, 0, :])
    for kc in range(2):
        k.tt(['RK', 'SE'], ['tA'], tA, RK, SE.unsqueeze(1).to_broadcast([128, NT, 32]), ALU.add)
        k.tt(['tA', 'EH'], ['tA'], tA, tA, EH[:, kc], ALU.mult)
        k.red(['tA'], ['sidf'], sidf[:, :, kc], tA, ALU.add)
    k.cp(['sidf'], ['SIDX'], SIDX, sidf)
    for ti in range(ntiles):
        b = ti % 2
        hk = 'hm%d' % b
        k.dma('sp', ['H2M%d' % ti], [hk], hm[b], H2M[ti * 128:(ti + 1) * 128, :])
        for kc in range(2):
            idma([hk, 'SIDX'], ['XGs%d_%d' % (ti, kc)], lambda e, ti=ti, kc=kc, b=b: e.indirect_dma_start(
                out=XG[:, :], out_offset=bass.IndirectOffsetOnAxis(ap=SIDX[:, ti, kc:kc + 1], axis=0),
                in_=hm[b], in_offset=None))
    k.em.barrier()
    sb = SB(nc, base=base1)
    wst = [sb.t([128, 8, 512], F32) for _ in range(2)]
    w1b = [sb.t([128, 8, 512], BF16) for _ in range(2)]
    w3b = [sb.t([128, 8, 512], BF16) for _ in range(2)]
    w2b = [sb.t([128, 4, D], BF16) for _ in range(2)]
    xg_tm = [sb.t([128, 4, D], BF16) for _ in range(2)]
    xgT = [sb.t([128, 8, 512], BF16) for _ in range(2)]
    gT = [sb.t([128, 4, 512], BF16) for _ in range(2)]
    st = [sb.t([128, 512], F32) for _ in range(2)]
    ysb = [sb.t([128, D], F32) for _ in range(2)]
    W1r, W3r, W2r = k.dram['moe_w1'], k.dram['moe_w3'], k.dram['moe_w2']
    sctr = [0]
    cctr = [0]
    yctr = [0]

    def load_w(src_rows, rowlen, r, dst, dkey, ceng):
        i = sctr[0] % 2
        sctr[0] += 1
        sk_ = 'wst%d' % i
        flat = wst[i].rearrange("p a b -> p (a b)")
        idma(['IDX'], [sk_], lambda e: e.indirect_dma_start(
            out=flat, out_offset=None, in_=src_rows[:, :], in_offset=bass.IndirectOffsetOnAxis(ap=IDX[:, r:r + 1], axis=0)))
        k.cp([sk_], [dkey], dst, flat.rearrange("p (a b) -> p a b", b=rowlen), eng=ceng)

    for r in range(32):
        wb = r % 2
        load_w(W1r, 512, r, w1b[wb], 'w1b%d' % wb, 'act')
        load_w(W3r, 512, r, w3b[wb], 'w3b%d' % wb, 'dve')
        load_w(W2r, D, r, w2b[wb], 'w2b%d' % wb, 'act')
        nrow = J[r] * 128
        for c0 in range(0, nrow, 512):
            n = min(512, nrow - c0)
            nt_ = n // 128
            gb = cctr[0] % 2
            cctr[0] += 1
            xk, tk, gk = 'xgtm%d' % gb, 'xgT%d' % gb, 'gT%d' % gb
            r0 = S[r] + c0
            k.dma('sp', [], [xk], xg_tm[gb][:, 0:nt_, :], XG[r0:r0 + n, :].rearrange("(t p) d -> p t d", p=128))
            for t in range(nt_):
                pk, pp = nps()
                pst = pp[:].bitcast(BF16)
                for kk in range(8):
                    k.tr([xk, 'identbf'], [pk], pst[:, kk * 128:(kk + 1) * 128], xg_tm[gb][:, t, kk * 128:(kk + 1) * 128], P['identbf'])
                k.cp([pk], [tk], xgT[gb][:, :, t * 128:(t + 1) * 128], pst[:, 0:1024].rearrange("p (a b) -> p a b", b=128),
                     eng=('act' if t % 2 == 0 else 'dve'))
            for f in range(4):
                p1k, pp1 = nps()
                for kk in range(8):
                    k.mm(['w1b%d' % wb, tk], [p1k], pp1[:, 0:n], w1b[wb][:, kk, f * 128:(f + 1) * 128], xgT[gb][:, kk, 0:n],
                         start=(kk == 0), stop=(kk == 7))
                p3k, pp3 = nps()
                for kk in range(8):
                    k.mm(['w3b%d' % wb, tk], [p3k], pp3[:, 0:n], w3b[wb][:, kk, f * 128:(f + 1) * 128], xgT[gb][:, kk, 0:n],
                         start=(kk == 0), stop=(kk == 7))
                sbi = f % 2
                k.act([p1k], ['st%d' % sbi], st[sbi][:, 0:n], pp1[:, 0:n], AF.Silu)
                k.tt(['st%d' % sbi, p3k], [gk], gT[gb][:, f, 0:n], st[sbi][:, 0:n], pp3[:, 0:n], ALU.mult)
            for tl in range(nt_):
                yi = yctr[0] % 2
                yctr[0] += 1
                yk = 'ysb%d' % yi
                for half in range(2):
                    pk, pp = nps()
                    for f in range(4):
                        k.mm([gk, 'w2b%d' % wb], [pk], pp[:, 0:512], gT[gb][:, f, tl * 128:(tl + 1) * 128], w2b[wb][:, f, half * 512:(half + 1) * 512],
                             start=(f == 0), stop=(f == 3))
                    k.cp([pk], [yk], ysb[yi][:, half * 512:(half + 1) * 512], pp[:, 0:512], eng=('act' if half == 0 else 'dve'))
                k.dma('sp', [yk], ['YE%d' % yctr[0]], YE[r0 + tl * 128:r0 + (tl + 1) * 128, :], ysb[yi])
    k.em.barrier()
    sb = SB(nc, base=base1)
    ya = [sb.t([128, D], F32) for _ in range(2)]
    yb2 = [sb.t([128, D], F32) for _ in range(2)]
    xo = [sb.t([128, D], F32) for _ in range(2)]
    fw = sb.t([128, D], F32)
    fj = sb.t([128, D], BF16)
    fs = sb.t([128, 2], F32)
    if last:
        k.dma('sp', [], ['fw'], fw, k.dram['final_bc'][:, :])
    for ti in range(ntiles):
        j = 0 if ti < 32 else 1
        b = ti % 2
        ok, ak, bk = 'xo%d' % b, 'ya%d' % b, 'yb%d' % b
        k.dma('sp', [], [ok], xo[b], XMIX[ti * 128:(ti + 1) * 128, :])
        idma(['SIDX'], [ak], lambda e, ti=ti, b=b: e.indirect_dma_start(
            out=ya[b], out_offset=None, in_=YE[:, :], in_offset=bass.IndirectOffsetOnAxis(ap=SIDX[:, ti, 0:1], axis=0)))
        idma(['SIDX'], [bk], lambda e, ti=ti, b=b: e.indirect_dma_start(
            out=yb2[b], out_offset=None, in_=YE[:, :], in_offset=bass.IndirectOffsetOnAxis(ap=SIDX[:, ti, 1:2], axis=0)))
        k.ts([ak, 'GW'], [ak], ya[b], ya[b], GW[:, ti, 0:1], op0=ALU.mult)
        k.stt([bk, 'GW', ak], [ak], ya[b], yb2[b], GW[:, ti, 1:2], ya[b], ALU.mult, ALU.add)
        k.tt([ak, 'grow'], [ak], ya[b], ya[b], grow[:, 1, j, :], ALU.mult, eng='pool')
        k.tt([ak, ok], [ok], xo[b], xo[b], ya[b], ALU.add)
        if not last:
            k.dma('pool', [ok], ['XRES'], XRES[ti * 128:(ti + 1) * 128, :], xo[b])
        else:
            k.memset([], ['fs'], fs, 0.0)
            k.act([ok, 'fs'], ['fj', 'fs'], fj, xo[b], AF.Square, accum_out=fs[:, 0:1])
            k.ts(['fs'], ['fs'], fs[:, 1:2], fs[:, 0:1], 1.0 / D, EPS, op0=ALU.mult, op1=ALU.add)
            k.act(['fs'], ['fs'], fs[:, 1:2], fs[:, 1:2], AF.Sqrt)
            k.recip(['fs'], ['fs'], fs[:, 1:2], fs[:, 1:2])
            k.ts([ok, 'fs'], [ok], xo[b], xo[b], fs[:, 1:2], op0=ALU.mult)
            k.tt([ok, 'fw'], [ok], xo[b], xo[b], fw, ALU.mult)
            k.dma('pool', [ok], ['out'], k.dram['out'][ti * 128:(ti + 1) * 128, :], xo[b])
    k.em.barrier()


def build(stage='full', debug=()):
    nc = bass.Bass("TRN2", target_bir_lowering=False)
    k = K(nc, debug=debug)
    P = {}
    sbp = SB(nc)
    k.din('xin', [T, D])
    k.din('w_in_fm', [DEPTH, D, NFM])
    k.din('w_in_tm', [DEPTH, D, NTM])
    k.din('b_fm_col', [128, DEPTH, 18])
    k.din('b_tm_bc', [DEPTH, 128, NTM])
    k.dscratch('UT', [1280, T])
    k.dscratch('QKT', [1024, T], BF16)
    k.dscratch('TM', [T, NTM])
    k.dscratch('XRES', [T, D])
    k.dscratch('YT', [1024, T], BF16)
    k.din('da_lambda', [DEPTH, 256])
    k.dscratch('XMIX', [T, D])
    k.dscratch('H2M', [T, D], BF16)
    k.dscratch('XG', [NSLOT, D], BF16)
    k.dscratch('YE', [NSLOT, D])
    for nm, shp in (('tri128', [128, 128]), ('iota32', [128, 32]), ('ltmask', [128, 1024]), ('pidx', [128, 1]), ('moe_S', [2, 128, 32])):
        k.din(nm, shp)
    k.din('w_out', [DEPTH, D, D])
    k.din('moe_wr', [DEPTH, D, 36])
    k.din('moe_rb_bc', [DEPTH, 128, 36])
    k.din('moe_w1', [DEPTH * 32 * 128, 4096])
    k.din('moe_w3', [DEPTH * 32 * 128, 4096])
    k.din('moe_w2', [DEPTH * 32 * 128, 4096])
    k.din('final_bc', [128, D])
    if stage == 'full':
        k.dout('out', [L, D])
    k.dscratch('HK', [8, 128, L], BF16)
    k.dscratch('HH', [2 * NHB, 128, 64 * 2 * HC], BF16)
    k.dscratch('CK', [4, 128, 511])
    k.dscratch('UC', [768, T], BF16)
    for nm, shp in (('hy_filt_w1', [DEPTH, 33, 64]), ('hy_filt_w2', [DEPTH, 64, 64]), ('hy_filt_w3', [DEPTH, 64, 1024]),
                    ('hy_filt_sc', [DEPTH, 64, 3]), ('hy_b3_col', [DEPTH, 128, 8]), ('hy_conv_col', [DEPTH, 128, 6, 4]),
                    ('hy_d_bc', [DEPTH, 32, 2, 256]), ('hy_d_col', [DEPTH, 128, 4]),
                    ('hy_z', [33, L]), ('hy_decay', [256, L]), ('hy_zc', [33, 511]), ('hy_decayc', [256, 511]),
                    ('hy_F1', [32, 128]), ('hy_Gr', [128, 8192]), ('hy_Gi', [128, 8192]), ('hy_E1', [128, 256]),
                    ('hy_E2', [128, 256]), ('hy_Mr', [64, 4096]), ('hy_nMi', [64, 4096])):
        k.din(nm, shp)
    k.din('tri', [2, 64, 64])
    k.din('ml_conv_col', [DEPTH, 64, 8, 4])
    k.din('ml_norm_bc', [DEPTH, 64, 256])
    k.din('da_subln_col', [DEPTH, 128, 1])
    phase0(k, P, sbp)
    P['negc'] = sbp.t([128, DEPTH, 2, 4], F32)
    base = sbp.off
    zt = SB(nc, base=base).t([128, 8 * D], BF16)
    k.memset([], ['zt'], zt, 0.0)
    XGv = k.dram['XG'].rearrange("(a p r) d -> a p (r d)", p=128, r=8)
    for a in range(NSLOT // 1024):
        k.dma('sp', ['zt'], ['XGz%d' % a], XGv[a], zt)
    k.em.barrier()
    for l in range(DEPTH):
        xres = k.dram['xin'] if l == 0 else k.dram['XRES']
        phaseA(k, P, l, base, xres)
        if stage == 'A':
            break
        if stage not in ('B2', 'H'):
            phaseB1(k, P, l, base, l == DEPTH - 1)
        if stage == 'B1':
            break
        if stage != 'H':
            phaseB2(k, P, l, base, l == DEPTH - 1)
        if stage == 'B2':
            break
        phaseH(k, P, l, base, l == DEPTH - 1)
        if stage == 'H':
            break
        phaseC(k, P, l, base, (l == DEPTH - 1) and stage == 'full', xres)
        if stage == 'C':
            break
    k.em.barrier()
    return nc, k


def run(inputs, stage='full', debug=(), cores=8):
    consts = make_consts()
    consts['moe_w1'] = np.ascontiguousarray(inputs['moe_w1'].reshape(DEPTH, 32, 8, 128, 512).transpose(0, 1, 3, 2, 4)).reshape(DEPTH * 32 * 128, 4096)
    consts['moe_w3'] = np.ascontiguousarray(inputs['moe_w3'].reshape(DEPTH, 32, 8, 128, 512).transpose(0, 1, 3, 2, 4)).reshape(DEPTH * 32 * 128, 4096)
    consts['moe_w2'] = np.ascontiguousarray(inputs['moe_w2'].reshape(DEPTH, 32, 4, 128, D).transpose(0, 1, 3, 2, 4)).reshape(DEPTH * 32 * 128, 4096)
    nc, k = build(stage, debug)
    in_maps = []
    for b in range(cores):
        m = prep_inputs(inputs, b)
        m.update(consts)
        in_maps.append({kk: v for kk, v in m.items() if kk in k.dram})
    res = run_bass_kernel_spmd(nc, in_maps, core_ids=list(range(cores)))
    return res.results


def kernel(**inputs):
    inp = {kk: np.asarray(v) for kk, v in inputs.items()}
    res = run(inp, stage='full', cores=8)
    return np.stack([np.asarray(r['out'], dtype=np.float32) for r in res], axis=0)
```

```python
import math
import os
import numpy as np
import concourse.bass as bass
import concourse.mybir as mybir
from concourse.bass_utils import run_bass_kernel_spmd

F32 = mybir.dt.float32
BF16 = mybir.dt.bfloat16
AF = mybir.ActivationFunctionType
ALU = mybir.AluOpType
AX = mybir.AxisListType

D = 1024
L = 4096
CTX = 256
T = L + CTX
NT = T // 128
DEPTH = 2
EPS = 1e-6
N_IN = 3344
ML_OFF = 768
DA_OFF = 1808
NFM = 2304
NTM = 1040
FM_COLS = list(range(0, 768)) + list(range(768, 1280)) + list(range(1808, 2832))
TM_COLS = list(range(1280, 1792)) + list(range(2832, 3344)) + list(range(1792, 1808))


class Em:
    NDMA = 32
    SAME_ENGINE_WAITS = True

    def __init__(self, nc):
        self.nc = nc
        self.eng = {'pe': nc.tensor, 'act': nc.scalar, 'dve': nc.vector, 'pool': nc.gpsimd, 'sp': nc.sync}
        self.sem = {k: nc.alloc_semaphore('s_' + k) for k in ('pe', 'act', 'dve', 'pool')}
        self.cnt = {k: 0 for k in self.sem}
        self.dsem = [nc.alloc_semaphore('s_dma%d' % i) for i in range(self.NDMA)]
        self.dcnt = [0] * self.NDMA
        self.dnext = 0
        self.waited = {e: {} for e in self.eng}
        self.lastw = {}
        self.readers = {}
        self.ninst = 0

    def _semh(self, key):
        return self.sem[key] if isinstance(key, str) else self.dsem[key[1]]

    def _wait(self, e, ev):
        key, val = ev
        w = self.waited[e]
        if w.get(key, 0) >= val:
            return
        self.eng[e].wait_ge(self._semh(key), val)
        w[key] = val

    def _deps(self, e, reads, writes):
        best = {}
        for k in reads:
            ev = self.lastw.get(k)
            if ev is not None and best.get(ev[0], 0) < ev[1]:
                best[ev[0]] = ev[1]
        for k in writes:
            ev = self.lastw.get(k)
            if ev is not None and best.get(ev[0], 0) < ev[1]:
                best[ev[0]] = ev[1]
            for ev in self.readers.get(k, ()):
                if best.get(ev[0], 0) < ev[1]:
                    best[ev[0]] = ev[1]
        for key, val in best.items():
            if key == e and (e == 'pe' or not Em.SAME_ENGINE_WAITS):
                continue
            self._wait(e, (key, val))

    def _record(self, ev, reads, writes):
        for k in reads:
            lst = self.readers.setdefault(k, [])
            lst[:] = [x for x in lst if x[0] != ev[0]]
            lst.append(ev)
        for k in writes:
            self.lastw[k] = ev
            self.readers[k] = []

    def op(self, e, reads, writes, fn):
        self._deps(e, reads, writes)
        ins = fn(self.eng[e])
        self.cnt[e] += 1
        ins.then_inc(self.sem[e], 1)
        self._record((e, self.cnt[e]), reads, writes)
        self.ninst += 1

    def dma(self, q, reads, writes, out, in_, **kw):
        i = self.dnext
        self.dnext = (i + 1) % self.NDMA
        if self.dcnt[i] > 0:
            self._wait(q, (('d', i), 16 * self.dcnt[i]))
        self._deps(q, reads, writes)
        ins = self.eng[q].dma_start(out=out, in_=in_, **kw)
        self.dcnt[i] += 1
        ins.then_inc(self.dsem[i], 16)
        self._record((('d', i), 16 * self.dcnt[i]), reads, writes)
        self.ninst += 1

    def barrier(self):
        for e in self.eng:
            for k in self.sem:
                if self.cnt[k] > 0 and k != e:
                    self._wait(e, (k, self.cnt[k]))
            for i in range(self.NDMA):
                if self.dcnt[i] > 0:
                    self._wait(e, (('d', i), 16 * self.dcnt[i]))
        self.lastw = {}
        self.readers = {}


class SB:
    _arena = {}

    def __init__(self, nc, base=0, limit=None):
        self.nc = nc
        if id(nc) not in SB._arena:
            nwords = (nc.sbuf_bytes_remaining - 256) // 4
            SB._arena[id(nc)] = (nc.alloc_sbuf_tensor("arena", [128, nwords], F32), nwords * 4)
        self.arena, cap = SB._arena[id(nc)]
        self.off = base
        self.limit = cap if limit is None else limit

    def t(self, shape, dtype, name=None):
        per = 1
        for s in shape[1:]:
            per *= s
        esz = 2 if dtype == BF16 else 4
        nbytes = (per * esz + 63) // 64 * 64
        assert self.off % 4 == 0
        w0 = self.off // 4
        ap = self.arena[0:shape[0], w0:w0 + nbytes // 4]
        if dtype != F32:
            ap = ap.bitcast(dtype)
        ap = ap[:, 0:per]
        if len(shape) == 3:
            ap = ap.rearrange("p (a b) -> p a b", b=shape[2])
        elif len(shape) == 4:
            ap = ap.rearrange("p (a b c) -> p a b c", b=shape[2], c=shape[3])
        self.off += nbytes
        assert self.off <= self.limit, ("SBUF overflow", name, self.off, self.limit)
        return ap


class K:
    def __init__(self, nc, debug=()):
        self.nc = nc
        self.em = Em(nc)
        self.debug = set(debug)
        self.dram = {}
        self.ps = [nc.alloc_psum_tensor("psb%d" % i, [128, 512], F32) for i in range(8)]

    def din(self, name, shape, dtype=F32):
        ap = self.nc.dram_tensor(name, list(shape), dtype, kind="ExternalInput").ap()
        self.dram[name] = ap
        return ap

    def dscratch(self, name, shape, dtype=F32):
        kind = "ExternalOutput" if name in self.debug else "Internal"
        ap = self.nc.dram_tensor(name, list(shape), dtype, kind=kind).ap()
        self.dram[name] = ap
        return ap

    def dout(self, name, shape, dtype=F32):
        ap = self.nc.dram_tensor(name, list(shape), dtype, kind="ExternalOutput").ap()
        self.dram[name] = ap
        return ap

    def dma(self, q, r, w, out, in_, **kw):
        self.em.dma(q, r, w, out, in_, **kw)

    def mm(self, r, w, out, lhsT, rhs, start=True, stop=True):
        self.em.op('pe', r, w, lambda e: e.matmul(out, lhsT=lhsT, rhs=rhs, start=start, stop=stop))

    def tr(self, r, w, out, in_, ident):
        self.em.op('pe', r, w, lambda e: e.transpose(out, in_, ident))

    def act(self, r, w, out, in_, func, eng='act', **kw):
        self.em.op(eng, r, w, lambda e: e.activation(out=out, in_=in_, func=func, **kw))

    def ts(self, r, w, out, in0, s1, s2=None, op0=ALU.mult, op1=None, eng='dve', **kw):
        if op1 is None:
            self.em.op(eng, r, w, lambda e: e.tensor_scalar(out=out, in0=in0, scalar1=s1, scalar2=None, op0=op0, **kw))
        else:
            self.em.op(eng, r, w, lambda e: e.tensor_scalar(out=out, in0=in0, scalar1=s1, scalar2=s2, op0=op0, op1=op1, **kw))

    def tt(self, r, w, out, in0, in1, op, eng='dve'):
        self.em.op(eng, r, w, lambda e: e.tensor_tensor(out=out, in0=in0, in1=in1, op=op))

    def stt(self, r, w, out, in0, scalar, in1, op0, op1, eng='dve'):
        self.em.op(eng, r, w, lambda e: e.scalar_tensor_tensor(out=out, in0=in0, scalar=scalar, in1=in1, op0=op0, op1=op1))

    def cp(self, r, w, out, in_, eng='dve'):
        if eng == 'act':
            self.em.op(eng, r, w, lambda e: e.copy(out=out, in_=in_))
        else:
            self.em.op(eng, r, w, lambda e: e.tensor_copy(out=out, in_=in_))

    def red(self, r, w, out, in_, op, eng='dve', axis=AX.X):
        self.em.op(eng, r, w, lambda e: e.tensor_reduce(out=out, in_=in_, axis=axis, op=op))

    def recip(self, r, w, out, in_):
        self.em.op('dve', r, w, lambda e: e.reciprocal(out=out, in_=in_))

    def memset(self, r, w, out, val, eng='dve'):
        self.em.op(eng, r, w, lambda e: e.memset(out, val))


def rope_tables_T():
    half = 32
    inv = (10000.0 ** (-np.arange(0, half, 2, dtype=np.float32) / half)).astype(np.float32)
    t = np.arange(L)
    row = (t // 64).astype(np.float32)
    col = (t % 64).astype(np.float32)
    ang = np.concatenate([row[:, None] * inv, row[:, None] * inv, col[:, None] * inv, col[:, None] * inv], axis=1)
    ang = ang.astype(np.float32)
    cosT = np.cos(ang).T.astype(np.float32)
    sinT = np.sin(ang).T.astype(np.float32)
    return np.ascontiguousarray(np.concatenate([cosT, cosT], 0)), np.ascontiguousarray(np.concatenate([sinT, sinT], 0))


def rope_perm():
    R = np.zeros((128, 128), np.float32)
    for base in range(0, 128, 32):
        for i in range(16):
            R[base + 16 + i, base + i] = -1.0
            R[base + i, base + 16 + i] = 1.0
    return R


def make_consts():
    c = {}
    c['ident'] = np.eye(128, dtype=np.float32)
    c['ropeR'] = rope_perm()
    cosT, sinT = rope_tables_T()
    c['cosT'] = cosT
    c['sinT'] = sinT
    bi = np.zeros((128, 2), np.float32)
    bi[0:64, 0] = 1.0
    bi[64:128, 1] = 1.0
    c['blockind'] = bi
    sel = np.zeros((2, 2, 128), np.float32)
    sel[0, 0, :] = 1.0
    sel[1, 1, :] = 1.0
    c['sel2'] = sel
    tri = np.zeros((2, 64, 64), np.float32)
    tri[0] = np.triu(np.ones((64, 64), np.float32))
    tri[1] = np.tril(np.ones((64, 64), np.float32))
    c['tri'] = tri
    c.update(hyena_consts())
    c['tri128'] = np.triu(np.ones((128, 128), np.float32), 1)
    c['iota32'] = np.ascontiguousarray(np.broadcast_to(np.arange(32, dtype=np.float32)[None, :], (128, 32)))
    lt = np.tril(np.ones((32, 32), np.float32), -1)
    c['ltmask'] = np.ascontiguousarray(np.broadcast_to(lt.reshape(1, 1024), (128, 1024)))
    c['pidx'] = np.ascontiguousarray(np.arange(128, dtype=np.float32)[:, None])
    c['moe_S'] = np.ascontiguousarray(np.stack([np.broadcast_to(np.array(moe_sched(nt)[1][:32], np.float32)[None, :], (128, 32))
                                                for nt in (NT, 32)], 0))
    return c


def col_layout(v):
    return np.ascontiguousarray(v.reshape(-1, 128).T)


def prep_inputs(inp, b):
    m = {}
    m['xin'] = np.ascontiguousarray(np.concatenate([inp['x'][b], inp['ctx'][b]], axis=0))
    m['ccol'] = np.ascontiguousarray(np.stack([col_layout(inp['c'][b]), col_layout(inp['c_ctx'])], axis=-1))
    m['ada_w'] = inp['ada_w']
    m['ada_b_col'] = np.ascontiguousarray(np.stack([col_layout(inp['ada_b'][l]) for l in range(DEPTH)], 1))
    m['norm1_col'] = np.ascontiguousarray(np.stack([col_layout(inp['norm1_w'][l]) for l in range(DEPTH)], 1))
    m['norm2_col'] = np.ascontiguousarray(np.stack([col_layout(inp['norm2_w'][l]) for l in range(DEPTH)], 1))
    m['w_in_fm'] = np.ascontiguousarray(inp['w_in'][:, :, FM_COLS])
    m['w_in_tm'] = np.ascontiguousarray(inp['w_in'][:, :, TM_COLS])
    m['b_fm_col'] = np.ascontiguousarray(np.stack([col_layout(inp['b_in'][l][FM_COLS]) for l in range(DEPTH)], 1))
    cw = np.concatenate([inp['ml_conv_w'], inp['ml_conv_b'][:, None, :]], axis=1)
    m['ml_conv_col'] = np.ascontiguousarray(cw.reshape(DEPTH, 4, 8, 64).transpose(0, 3, 2, 1))
    m['ml_norm_bc'] = np.ascontiguousarray(np.broadcast_to(inp['ml_norm_w'][:, None, :], (DEPTH, 64, 256)))
    m['hy_filt_w1'] = inp['hy_filt_w1']
    m['hy_filt_w2'] = inp['hy_filt_w2']
    m['hy_filt_w3'] = inp['hy_filt_w3']
    m['hy_filt_sc'] = np.ascontiguousarray(np.stack([inp['hy_sin_freq'], inp['hy_filt_b1'], inp['hy_filt_b2']], axis=-1))
    m['hy_b3_col'] = np.ascontiguousarray(np.stack([col_layout(inp['hy_filt_b3'][l]) for l in range(DEPTH)], 0))
    hw = np.concatenate([inp['hy_conv_w'], inp['hy_conv_b'][:, None, :]], axis=1)
    m['hy_conv_col'] = np.ascontiguousarray(hw.reshape(DEPTH, 4, 6, 128).transpose(0, 3, 2, 1))
    m['hy_d_bc'] = np.ascontiguousarray(np.broadcast_to(inp['hy_bias_d'][:, None, :, :], (DEPTH, 32, 2, 256)))
    m['hy_d_col'] = np.ascontiguousarray(inp['hy_bias_d'].reshape(DEPTH, 4, 128).transpose(0, 2, 1))
    m['w_out'] = inp['w_out']
    m['moe_wr'] = np.ascontiguousarray(np.concatenate([inp['moe_wg'], inp['moe_we']], axis=-1))
    rbv = np.concatenate([inp['moe_bg'], inp['moe_be']], axis=-1)
    m['moe_rb_bc'] = np.ascontiguousarray(np.broadcast_to(rbv[:, None, :], (DEPTH, 128, 36)))
    m['final_bc'] = np.ascontiguousarray(np.broadcast_to(inp['final_norm_w'][None, :], (128, D)))
    m['da_lambda'] = np.ascontiguousarray(inp['da_lambda'].reshape(DEPTH, 256))
    m['da_subln_col'] = np.ascontiguousarray(inp['da_subln_w'][:, :, None])
    m['b_tm_bc'] = np.ascontiguousarray(np.broadcast_to(inp['b_in'][:, None, TM_COLS], (DEPTH, 128, NTM)))
    return m


def phase0(k, P, sbp):
    nc = k.nc
    cst = {}
    for name, shape in (('ident', [128, 128]), ('ropeR', [128, 128]), ('blockind', [128, 2])):
        k.din(name, shape)
    k.din('sel2', [2, 2, 128])
    k.din('cosT', [128, L])
    k.din('sinT', [128, L])
    P['ident32'] = sbp.t([128, 128], F32)
    P['identbf'] = sbp.t([128, 128], BF16)
    P['ropeRbf'] = sbp.t([128, 128], BF16)
    P['blockbf'] = sbp.t([128, 2], BF16)
    P['sel2'] = sbp.t([2, 2, 128], F32)
    P['ones32'] = sbp.t([128, 128], F32)
    P['onesbf'] = sbp.t([128, 128], BF16)
    tmp = sbp.t([128, 128], F32)
    k.dma('sp', [], ['ident32'], P['ident32'], k.dram['ident'][:, :])
    k.cp(['ident32'], ['identbf'], P['identbf'], P['ident32'])
    k.dma('sp', [], ['c_tmp'], tmp, k.dram['ropeR'][:, :])
    k.cp(['c_tmp'], ['ropeRbf'], P['ropeRbf'], tmp)
    k.dma('sp', ['c_tmp'], ['c_tmp'], tmp[:, 0:2], k.dram['blockind'][:, :])
    k.cp(['c_tmp'], ['blockbf'], P['blockbf'], tmp[:, 0:2])
    k.dma('sp', [], ['sel2'], P['sel2'], k.dram['sel2'][:, :, :])
    k.memset([], ['ones32'], P['ones32'], 1.0)
    k.memset([], ['onesbf'], P['onesbf'], 1.0)

    ccol = k.din('ccol', [128, 8, 2])
    adaw = k.din('ada_w', [DEPTH, D, 6 * D])
    adab = k.din('ada_b_col', [128, DEPTH, 48])
    n1 = k.din('norm1_col', [128, DEPTH, 8])
    n2 = k.din('norm2_col', [128, DEPTH, 8])
    P['mod'] = sbp.t([128, DEPTH, 48, 2], F32)
    P['A1'] = sbp.t([128, DEPTH, 8, 2], F32)
    P['A2'] = sbp.t([128, DEPTH, 8, 2], F32)
    cact = sbp.t([128, 8, 2], F32)
    adab_sb = sbp.t([128, DEPTH, 48], F32)
    n1_sb = sbp.t([128, DEPTH, 8], F32)
    n2_sb = sbp.t([128, DEPTH, 8], F32)
    k.dma('sp', [], ['cact'], cact, ccol[:, :, :])
    k.dma('sp', [], ['adab'], adab_sb, adab[:, :, :])
    k.dma('sp', [], ['n1'], n1_sb, n1[:, :, :])
    k.dma('sp', [], ['n2'], n2_sb, n2[:, :, :])
    k.act(['cact'], ['cact'], cact, cact, AF.Silu)
    sbl = SB(nc, base=sbp.off)
    stg = [sbl.t([128, 8, 512], F32) for _ in range(2)]
    for l in range(DEPTH):
        wv = adaw[l].rearrange("(k p) n -> p k n", p=128)
        pm = k.ps[0][:, 0:96].rearrange("p (c j) -> p c j", j=2)
        for cg in range(12):
            s = stg[cg % 2]
            sk = 'adastg%d' % (cg % 2)
            k.dma('sp', [], [sk], s, wv[:, :, cg * 512:(cg + 1) * 512])
            for j in range(4):
                c = cg * 4 + j
                for kk in range(8):
                    k.mm([sk, 'cact'], ['ps0'], pm[:, c, :], s[:, kk, j * 128:(j + 1) * 128], cact[:, kk, :],
                         start=(kk == 0), stop=(kk == 7))
        k.tt(['ps0', 'adab'], ['mod'], P['mod'][:, l], pm, adab_sb[:, l, :].unsqueeze(2).to_broadcast([128, 48, 2]), ALU.add)
        for (Aname, nsb, c0) in (('A1', n1_sb, 8), ('A2', n2_sb, 32)):
            k.ts(['mod'], [Aname], P[Aname][:, l], P['mod'][:, l, c0:c0 + 8, :], 1.0, op0=ALU.add)
            k.tt([Aname, 'n1', 'n2'], [Aname], P[Aname][:, l], P[Aname][:, l],
                 nsb[:, l, :].unsqueeze(2).to_broadcast([128, 8, 2]), ALU.mult)
    k.em.barrier()


def phaseA(k, P, l, base, xres):
    nc = k.nc
    sb = SB(nc, base=base)
    wfm = k.dram['w_in_fm']
    wtm = k.dram['w_in_tm']
    UT, QKT, TM = k.dram['UT'], k.dram['QKT'], k.dram['TM']
    Wfm = sb.t([128, 8, NFM], BF16)
    Wtm = sb.t([128, 8, NTM], BF16)
    bfm = sb.t([128, 18], F32)
    btm = sb.t([128, NTM], F32)
    cosT = sb.t([128, L], F32)
    sinT = sb.t([128, L], F32)
    normacc = sb.t([2, 8], F32)
    stg = [sb.t([128, 8, 512], F32) for _ in range(2)]
    k.dma('sp', [], ['bfm'], bfm, k.dram['b_fm_col'][:, l, :])
    k.dma('sp', [], ['btm'], btm, k.dram['b_tm_bc'][l])
    k.dma('sp', [], ['cosT'], cosT, k.dram['cosT'][:, :])
    k.dma('sp', [], ['sinT'], sinT, k.dram['sinT'][:, :])
    k.memset([], ['normacc'], normacc, 0.0)
    ci = 0
    for (src, dst, ncol, key) in ((wfm, Wfm, NFM, 'Wfm'), (wtm, Wtm, NTM, 'Wtm')):
        wv = src[l].rearrange("(k p) n -> p k n", p=128)
        for c0 in range(0, ncol, 512):
            c1 = min(ncol, c0 + 512)
            s = stg[ci % 2]
            sk = 'wstg%d' % (ci % 2)
            k.dma('sp', [], [sk], s[:, :, 0:c1 - c0], wv[:, :, c0:c1])
            k.cp([sk], [key], dst[:, :, c0:c1], s[:, :, 0:c1 - c0], eng=('dve' if ci % 2 == 0 else 'pool'))
            ci += 1
    xt = [sb.t([128, D], F32) for _ in range(2)]
    junk = sb.t([128, D], BF16)
    xn = [sb.t([128, D], BF16) for _ in range(2)]
    ss = [sb.t([128, 2], F32) for _ in range(2)]
    hT = [sb.t([128, 8, 512], BF16) for _ in range(2)]
    fmst = [sb.t([128, 512], F32) for _ in range(3)]
    qbf = [sb.t([128, 512], BF16) for _ in range(2)]
    t2 = [sb.t([128, 512], F32) for _ in range(2)]
    obf = [sb.t([128, 512], BF16) for _ in range(2)]
    sqbf = [sb.t([128, 512], BF16) for _ in range(2)]
    nmx = sb.t([2, 2], F32)
    tmst = [sb.t([128, NTM], F32) for _ in range(2)]
    A1, SH1 = P['A1'], P['mod']
    psi = 0
    tile_ctr = 0
    fm_ctr = 0
    rp_ctr = 0
    for g in range(9):
        t0 = g * 512
        ntok = 512 if g < 8 else 256
        j = 0 if g < 8 else 1
        hb = g % 2
        hk = 'hT%d' % hb
        for tl in range(ntok // 128):
            ti = t0 // 128 + tl
            xb = tile_ctr % 2
            tile_ctr += 1
            xk, nk, sk = 'xt%d' % xb, 'xn%d' % xb, 'ss%d' % xb
            k.dma('sp', [], [xk], xt[xb], xres[ti * 128:(ti + 1) * 128, :])
            k.memset([], [sk], ss[xb], 0.0)
            k.act([xk, sk], ['junk', sk], junk, xt[xb], AF.Square, accum_out=ss[xb][:, 0:1])
            k.ts([sk], [sk], ss[xb][:, 1:2], ss[xb][:, 0:1], 1.0 / D, EPS, op0=ALU.mult, op1=ALU.add)
            k.act([sk], [sk], ss[xb][:, 1:2], ss[xb][:, 1:2], AF.Sqrt)
            k.recip([sk], [sk], ss[xb][:, 1:2], ss[xb][:, 1:2])
            k.ts([xk, sk], [nk], xn[xb], xt[xb], ss[xb][:, 1:2], op0=ALU.mult)
            pk = 'ps%d' % psi
            pst = k.ps[psi][:].bitcast(BF16)
            psi = (psi + 1) % 8
            for kk in range(8):
                k.tr([nk, 'identbf'], [pk], pst[:, kk * 128:(kk + 1) * 128], xn[xb][:, kk * 128:(kk + 1) * 128], P['identbf'])
            for kk in range(8):
                k.act([pk, 'A1', 'mod'], [hk], hT[hb][:, kk, tl * 128:(tl + 1) * 128], pst[:, kk * 128:(kk + 1) * 128],
                      AF.Identity, scale=A1[:, l, kk, j:j + 1], bias=SH1[:, l, kk, j:j + 1])
        for jc in range(18):
            pk = 'ps%d' % psi
            pp = k.ps[psi]
            psi = (psi + 1) % 8
            for kk in range(8):
                k.mm(['Wfm', hk], [pk], pp[:, 0:ntok], Wfm[:, kk, jc * 128:(jc + 1) * 128], hT[hb][:, kk, 0:ntok],
                     start=(kk == 0), stop=(kk == 7))
            fb = fm_ctr % 3
            fm_ctr += 1
            fk = 'fmst%d' % fb
            k.act([pk, 'bfm'], [fk], fmst[fb][:, 0:ntok], pp[:, 0:ntok], AF.Identity, bias=bfm[:, jc:jc + 1], scale=1.0)
            if jc < 10:
                k.dma('pool', [fk], ['UT'], UT[jc * 128:(jc + 1) * 128, t0:t0 + ntok], fmst[fb][:, 0:ntok])
                continue
            rb = rp_ctr % 2
            rp_ctr += 1
            ok_, sqk = 'obf%d' % rb, 'sqbf%d' % rb
            if j == 0:
                qk_, tk_ = 'qbf%d' % rb, 't2%d' % rb
                k.cp([fk], [qk_], qbf[rb][:, 0:ntok], fmst[fb][:, 0:ntok], eng='pool')
                pk2 = 'ps%d' % psi
                pp2 = k.ps[psi]
                psi = (psi + 1) % 8
                k.mm(['ropeRbf', qk_], [pk2], pp2[:, 0:ntok], P['ropeRbf'], qbf[rb][:, 0:ntok])
                k.tt([pk2, 'sinT'], [tk_], t2[rb][:, 0:ntok], pp2[:, 0:ntok], sinT[:, t0:t0 + ntok], ALU.mult)
                k.tt([fk, 'cosT'], [fk], fmst[fb][:, 0:ntok], fmst[fb][:, 0:ntok], cosT[:, t0:t0 + ntok], ALU.mult, eng='pool')
                k.tt([fk, tk_], [fk], fmst[fb][:, 0:ntok], fmst[fb][:, 0:ntok], t2[rb][:, 0:ntok], ALU.add)
            k.cp([fk], [ok_], obf[rb][:, 0:ntok], fmst[fb][:, 0:ntok], eng='pool')
            k.dma('pool', [ok_], ['QKT'], QKT[(jc - 10) * 128:(jc - 9) * 128, t0:t0 + ntok], obf[rb][:, 0:ntok])
            k.act([fk], [sqk], sqbf[rb][:, 0:ntok], fmst[fb][:, 0:ntok], AF.Square)
            pk3 = 'ps%d' % psi
            pp3 = k.ps[psi]
            psi = (psi + 1) % 8
            k.mm(['blockbf', sqk], [pk3], pp3[0:2, 0:ntok], P['blockbf'], sqbf[rb][:, 0:ntok])
            k.red([pk3], ['nmx'], nmx[:, 0:1], pp3[0:2, 0:ntok], ALU.max)
            k.tt(['nmx', 'normacc'], ['normacc'], normacc[:, jc - 10:jc - 9], normacc[:, jc - 10:jc - 9], nmx[:, 0:1], ALU.max)
        for tl in range(ntok // 128):
            ti = t0 // 128 + tl
            tb = ti % 2
            tk = 'tmst%d' % tb
            for (c0, c1) in ((0, 512), (512, 1024), (1024, NTM)):
                pk = 'ps%d' % psi
                pp = k.ps[psi]
                psi = (psi + 1) % 8
                for kk in range(8):
                    k.mm(['Wtm', hk], [pk], pp[:, 0:c1 - c0], hT[hb][:, kk, tl * 128:(tl + 1) * 128], Wtm[:, kk, c0:c1],
                         start=(kk == 0), stop=(kk == 7))
                k.tt([pk, 'btm'], [tk], tmst[tb][:, c0:c1], pp[:, 0:c1 - c0], btm[:, c0:c1], ALU.add)
            k.dma('pool', [tk], ['TM'], TM[ti * 128:(ti + 1) * 128, :], tmst[tb])
    cn = sb.t([2, 4], F32)
    k.tt(['normacc'], ['cn'], cn, normacc[:, 0:4], normacc[:, 4:8], ALU.mult)
    k.act(['cn'], ['cn'], cn, cn, AF.Sqrt)
    k.ts(['cn'], ['cn'], cn, cn, -1.05 * 0.125, op0=ALU.mult)
    for m in range(2):
        pk = 'ps%d' % psi
        pp = k.ps[psi]
        psi = (psi + 1) % 8
        k.mm(['sel2', 'cn'], [pk], pp[:, 0:4], P['sel2'][:, m, :], cn)
        k.cp([pk], ['negc'], P['negc'][:, l, m, :], pp[:, 0:4])
    k.em.barrier()


def phaseB1(k, P, l, base, last):
    nc = k.nc
    sb = SB(nc, base=base)
    QKT, TM, YT = k.dram['QKT'], k.dram['TM'], k.dram['YT']
    lam_init = 0.8 - 0.6 * math.exp(-0.3 * l)
    QT = sb.t([128, 4, T], BF16)
    KTz = [sb.t([128, 4, T], BF16) for _ in range(2)]
    V = sb.t([128, NT, 512], BF16)
    vst = [sb.t([128, 512], F32) for _ in range(2)]
    k.dma('sp', [], ['QT'], QT, QKT[0:512, :].rearrange("(c p) t -> p c t", p=128))
    kv = QKT[512:1024, :].rearrange("(c p) t -> p c t", p=128)
    for m in range(2):
        lo, hi = m * 64, (m + 1) * 64
        zl, zh = (1 - m) * 64, (2 - m) * 64
        k.dma('sp', [], ['KT'], KTz[m][lo:hi], kv[lo:hi])
        k.memset([], ['KT'], KTz[m][zl:zh], 0.0, eng=('dve' if m == 0 else 'pool'))
    for ti in range(NT):
        vb = ti % 2
        vk = 'vst%d' % vb
        k.dma('sp', [], [vk], vst[vb], TM[ti * 128:(ti + 1) * 128, 512:1024])
        k.cp([vk], ['V'], V[:, ti, :], vst[vb], eng=('dve' if ti % 2 == 0 else 'pool'))
    lt = sb.t([1, 256], F32)
    lw = sb.t([1, 8], F32)
    neglam = sb.t([128, 1], F32)
    wsc = sb.t([128, 1], F32)
    k.dma('sp', [], ['lt'], lt, k.dram['da_lambda'][l:l + 1, :])
    k.dma('sp', [], ['wsc'], wsc, k.dram['da_subln_col'][l])
    k.ts(['wsc'], ['wsc'], wsc, wsc, 1.0 - lam_init, op0=ALU.mult)
    k.tt(['lt'], ['lt'], lt[:, 0:64], lt[:, 0:64], lt[:, 64:128], ALU.mult)
    k.tt(['lt'], ['lt'], lt[:, 128:192], lt[:, 128:192], lt[:, 192:256], ALU.mult)
    k.red(['lt'], ['lw'], lw[:, 0:1], lt[:, 0:64], ALU.add)
    k.red(['lt', 'lw'], ['lw'], lw[:, 1:2], lt[:, 128:192], ALU.add)
    k.act(['lw'], ['lw'], lw[:, 0:2], lw[:, 0:2], AF.Exp)
    k.tt(['lw'], ['lw'], lw[:, 2:3], lw[:, 1:2], lw[:, 0:1], ALU.subtract)
    k.ts(['lw'], ['lw'], lw[:, 2:3], lw[:, 2:3], -lam_init, op0=ALU.add)
    k.mm(['ones32', 'lw'], ['ps7'], k.ps[7][:, 0:1], P['ones32'][0:1, :], lw[:, 2:3])
    k.cp(['ps7'], ['neglam'], neglam, k.ps[7][:, 0:1])

    pt = [sb.t([128, 512], BF16) for _ in range(4)]
    racc = [sb.t([128, 512], F32) for _ in range(2)]
    rec = [sb.t([128, 512], F32) for _ in range(2)]
    o0 = sb.t([128, 512], F32)
    o1 = sb.t([128, 512], F32)
    sq = sb.t([128, 512], BF16)
    ybf = [sb.t([128, 512], BF16) for _ in range(2)]
    pti = 0
    si = 0
    si_box = [0]
    yi = 0
    chunks = [(g * 512, 512, list(range(NT))) for g in range(8)]
    if not last:
        chunks.append((L, CTX, [32, 33]))
    for h in range(4):
        for (q0, nq, blocks) in chunks:
            units = [(bi, kb, m) for bi, kb in enumerate(blocks) for m in range(2)]
            LOOK = 3
            issued = {}

            def issue_s(u):
                bi, kb, m = units[u]
                nonlocal_si = si_box[0]
                si_box[0] += 1
                psk = 'ps%d' % (4 + nonlocal_si % 4)
                pss = k.ps[4 + nonlocal_si % 4]
                k.mm(['KT', 'QT'], [psk], pss[:, 0:nq], KTz[m][:, h, kb * 128:(kb + 1) * 128], QT[:, h, q0:q0 + nq])
                issued[u] = (psk, pss)

            for u in range(min(LOOK, len(units))):
                issue_s(u)
            for u, (bi, kb, m) in enumerate(units):
                psk, pss = issued.pop(u)
                pk_ = 'pt%d' % (pti % 4)
                ptt = pt[pti % 4]
                pti += 1
                k.act([psk, 'negc'], [pk_], ptt[:, 0:nq], pss[:, 0:nq], AF.Exp, scale=0.125, bias=P['negc'][:, l, m, h:h + 1])
                if u + LOOK < len(units):
                    issue_s(u + LOOK)
                st, sp_ = (bi == 0), (bi == len(blocks) - 1)
                k.mm(['V', pk_], ['ps%d' % m], k.ps[m][:, 0:nq], V[:, kb, h * 128:(h + 1) * 128], ptt[:, 0:nq], start=st, stop=sp_)
                reng = 'dve' if m == 0 else 'pool'
                if st:
                    k.cp([pk_], ['racc%d' % m], racc[m][:, 0:nq], ptt[:, 0:nq], eng=reng)
                else:
                    k.tt([pk_, 'racc%d' % m], ['racc%d' % m], racc[m][:, 0:nq], racc[m][:, 0:nq], ptt[:, 0:nq], ALU.add, eng=reng)
            si = si_box[0]
            for m in range(2):
                k.mm(['ones32', 'racc%d' % m], ['ps%d' % (2 + m)], k.ps[2 + m][:, 0:nq], P['ones32'], racc[m][:, 0:nq])
            k.recip(['ps2'], ['rec0'], rec[0][:, 0:nq], k.ps[2][:, 0:nq])
            k.recip(['ps3'], ['rec1'], rec[1][:, 0:nq], k.ps[3][:, 0:nq])
            k.tt(['ps0', 'rec0'], ['o0'], o0[:, 0:nq], k.ps[0][:, 0:nq], rec[0][:, 0:nq], ALU.mult)
            k.tt(['ps1', 'rec1'], ['o1'], o1[:, 0:nq], k.ps[1][:, 0:nq], rec[1][:, 0:nq], ALU.mult)
            k.stt(['o0', 'o1', 'neglam'], ['o0'], o0[:, 0:nq], o1[:, 0:nq], neglam[:, 0:1], o0[:, 0:nq], ALU.mult, ALU.add)
            k.act(['o0'], ['sq'], sq[:, 0:nq], o0[:, 0:nq], AF.Square)
            psk = 'ps%d' % (4 + si % 4)
            pss = k.ps[4 + si % 4]
            si += 1
            si_box[0] = si
            k.mm(['onesbf', 'sq'], [psk], pss[:, 0:nq], P['onesbf'], sq[:, 0:nq])
            k.ts([psk], ['rec0'], rec[0][:, 0:nq], pss[:, 0:nq], 1.0 / 128.0, EPS, op0=ALU.mult, op1=ALU.add)
            k.act(['rec0'], ['rec0'], rec[0][:, 0:nq], rec[0][:, 0:nq], AF.Sqrt)
            k.recip(['rec0'], ['rec0'], rec[0][:, 0:nq], rec[0][:, 0:nq])
            k.tt(['o0', 'rec0'], ['o0'], o0[:, 0:nq], o0[:, 0:nq], rec[0][:, 0:nq], ALU.mult)
            yk = 'ybf%d' % (yi % 2)
            yb = ybf[yi % 2]
            yi += 1
            k.ts(['o0', 'wsc'], [yk], yb[:, 0:nq], o0[:, 0:nq], wsc[:, 0:1], op0=ALU.mult)
            k.dma('pool', [yk], ['YT'], YT[512 + h * 128:512 + (h + 1) * 128, q0:q0 + nq], yb[:, 0:nq])
    k.em.barrier()


NCH = T // 64


def phaseB2(k, P, l, base, last):
    nc = k.nc
    UT, TM, YT = k.dram['UT'], k.dram['TM'], k.dram['YT']
    sb0 = SB(nc, base=base)
    tri32 = sb0.t([64, 2, 64], F32)
    tribf = sb0.t([64, 2, 64], BF16)
    cw = sb0.t([64, 8, 4], F32)
    wbc = sb0.t([64, 256], F32)
    G = sb0.t([64, NCH, 16], F32)
    LF = sb0.t([64, 8, NCH], F32)
    IG = sb0.t([64, 8, NCH], F32)
    BB = sb0.t([64, 8, NCH], F32)
    BT = sb0.t([64, 8, NCH], F32)
    EB = sb0.t([64, 8, NCH], F32)
    WS = sb0.t([64, 8, NCH], F32)
    W2 = sb0.t([64, 8, NCH], F32)
    EBT = sb0.t([64, 8, NCH], F32)
    k.dma('sp', [], ['tri32'], tri32, k.dram['tri'].rearrange("a s t -> s a t"))
    k.cp(['tri32'], ['tribf'], tribf, tri32)
    k.dma('sp', [], ['cw'], cw, k.dram['ml_conv_col'][l])
    k.dma('sp', [], ['wbc'], wbc, k.dram['ml_norm_bc'][l])
    k.dma('sp', [], ['G'], G, TM[:, 1024:1040].rearrange("(n p) c -> p n c", p=64))
    for d in range(2):
        gi = G[:, :, d * 8:d * 8 + 4].rearrange("p n h -> p h n")
        gf = G[:, :, d * 8 + 4:d * 8 + 8].rearrange("p n h -> p h n")
        k.cp(['G'], ['IG'], IG[:, d * 4:d * 4 + 4, :], gi)
        k.act(['G'], ['LF'], LF[:, d * 4:d * 4 + 4, :], gf, AF.Exp, scale=-1.0)
    k.act(['LF'], ['LF'], LF, LF, AF.Ln, bias=1.0, scale=1.0)
    k.ts(['LF'], ['LF'], LF, LF, -1.0, op0=ALU.mult)
    for d in range(2):
        rhs = LF[:, d * 4:d * 4 + 4, :]
        k.mm(['tri32', 'LF'], ['ps0'], k.ps[0][0:64, 0:4 * NCH], tri32[:, d, :], rhs)
        k.cp(['ps0'], ['BB'], BB[:, d * 4:d * 4 + 4, :], k.ps[0][0:64, 0:4 * NCH].rearrange("p (h n) -> p h n", n=NCH))
        k.mm(['ones32', 'LF'], ['ps1'], k.ps[1][0:64, 0:4 * NCH], P['ones32'][0:64, 0:64], rhs)
        k.cp(['ps1'], ['BT'], BT[:, d * 4:d * 4 + 4, :], k.ps[1][0:64, 0:4 * NCH].rearrange("p (h n) -> p h n", n=NCH))
    k.act(['BB'], ['EB'], EB, BB, AF.Exp)
    k.act(['BT'], ['EBT'], EBT, BT, AF.Exp)
    k.tt(['IG', 'BB'], ['WS'], WS, IG, BB, ALU.subtract)
    k.tt(['WS', 'BT'], ['W2'], W2, WS, BT, ALU.add)
    k.act(['WS'], ['WS'], WS, WS, AF.Exp)
    k.act(['W2'], ['W2'], W2, W2, AF.Exp)
    base1 = sb0.off
    orders = [[64, 65, 66, 67] + list(range(64)), [67, 66, 65, 64] + list(range(63, -1, -1))]
    psi = [2]

    def nps():
        i = psi[0]
        psi[0] = 2 + (psi[0] - 1) % 6
        return 'ps%d' % i, k.ps[i]

    for hp in range(2):
        sb = SB(nc, base=base1)
        qT = sb.t([64, 2, T], BF16)
        kT = sb.t([64, 2, T], BF16)
        ktm = sb.t([64, NCH, 128], BF16)
        vaug = sb.t([64, NCH, 2, 65], BF16)
        hsum = sb.t([64, NCH, 128], F32)
        ra = sb.t([64, 2 * T], F32)
        raw = ra[:, 0:T]
        acc = ra[:, T:2 * T]
        k.memset([], ['hsum'], hsum, 0.0, eng='pool')
        k.memset([], ['vaug'], vaug, 1.0, eng='pool')
        for qk in range(2):
            for hl in range(2):
                hh = qk * 4 + hp * 2 + hl
                k.dma('sp', [], ['raw'], raw, UT[768 + hh * 64:768 + (hh + 1) * 64, :])
                k.ts(['raw', 'cw'], ['acc'], acc, raw, cw[:, hh, 1:2], cw[:, hh, 3:4], op0=ALU.mult, op1=ALU.add)
                for (a, b) in ((0, L), (L, T)):
                    k.stt(['raw', 'cw', 'acc'], ['acc'], acc[:, a + 1:b], raw[:, a:b - 1], cw[:, hh, 0:1], acc[:, a + 1:b], ALU.mult, ALU.add)
                    k.stt(['raw', 'cw', 'acc'], ['acc'], acc[:, a:b - 1], raw[:, a + 1:b], cw[:, hh, 2:3], acc[:, a:b - 1], ALU.mult, ALU.add)
                if qk == 0:
                    k.act(['acc'], ['qT'], qT[:, hl, :], acc, AF.Silu)
                else:
                    k.act(['acc'], ['acc'], acc, acc, AF.Silu)
                    k.ts(['acc'], ['kT'], kT[:, hl, :], acc, 0.125, op0=ALU.mult)
        for n0 in range(0, NCH, 4):
            pk, pp = nps()
            ppb = pp[:].bitcast(BF16)
            for dn in range(4):
                for hl in range(2):
                    k.tr(['kT', 'identbf'], [pk], ppb[0:64, (dn * 2 + hl) * 64:(dn * 2 + hl + 1) * 64],
                         kT[:, hl, (n0 + dn) * 64:(n0 + dn + 1) * 64], P['identbf'][0:64, 0:64])
            k.cp([pk], ['ktm'], ktm[:, n0:n0 + 4, :], ppb[0:64, 0:512].rearrange("p (n c) -> p n c", c=128))
        vst = acc[:, 0:17 * 128].rearrange("p (n c) -> p n c", c=128)
        for n0 in range(0, NCH, 17):
            k.dma('sp', ['acc'], ['acc'], vst, TM[n0 * 64:(n0 + 17) * 64, hp * 128:(hp + 1) * 128].rearrange("(n p) c -> p n c", p=64))
            k.cp(['acc'], ['vaug'], vaug[:, n0:n0 + 17, :, 0:64], vst.rearrange("p n (h e) -> p n h e", e=64))
        Cst = [[sb.t([64, 65], F32) for _ in range(2)] for _ in range(2)]
        Cbf = [[sb.t([64, 65], BF16) for _ in range(2)] for _ in range(2)]
        dg = [sb.t([64, 64], BF16) for _ in range(4)]
        meb = [sb.t([64, 64], F32) for _ in range(4)]
        pT = [sb.t([64, 64], BF16) for _ in range(4)]
        rsb = [sb.t([64, 65], F32) for _ in range(4)]
        tot = [sb.t([64, 66], F32) for _ in range(4)]
        wv = [sb.t([64, 65], BF16) for _ in range(4)]
        for d in range(2):
            for hl in range(2):
                k.memset([], ['Cst%d%d' % (d, hl)], Cst[d][hl], 0.0)
                k.memset([], ['Cbf%d%d' % (d, hl)], Cbf[d][hl], 0.0)
        def unit(step, d, hl):
            n = orders[d][step]
            c0 = n * 64
            need_out = (n < 64) or (not last)
            u = d * 2 + hl
            dh = d * 4 + hp * 2 + hl
            ck, cbk = 'Cst%d%d' % (d, hl), 'Cbf%d%d' % (d, hl)
            upd = step != NCH - 1
            kA, kB = 'ps%d' % (2 * u), 'ps%d' % (2 * u + 1)
            bA, bB = k.ps[2 * u], k.ps[2 * u + 1]
            if upd:
                k.act(['vaug', 'W2'], ['wv%d' % u], wv[u], vaug[:, n, hl, :], AF.Identity, scale=W2[:, dh, n:n + 1])
            if need_out:
                k.act(['identbf', 'EB'], ['dg%d' % u], dg[u], P['identbf'][0:64, 0:64], AF.Identity, scale=EB[:, dh, n:n + 1])
            yield
            if upd:
                k.mm(['ktm', 'wv%d' % u], [kB], bB[0:64, 0:65], ktm[:, n, hl * 64:(hl + 1) * 64], wv[u])
            if need_out:
                k.mm(['tribf', 'dg%d' % u], [kA], bA[0:64, 0:64], tribf[:, 1 - d, :], dg[u])
            yield
            if upd:
                k.stt([ck, 'EBT', kB], [ck], Cst[d][hl], Cst[d][hl], EBT[:, dh, n:n + 1], bB[0:64, 0:65], ALU.mult, ALU.add)
            if need_out:
                k.cp([kA], ['meb%d' % u], meb[u], bA[0:64, 0:64], eng='act')
            yield
            if need_out:
                k.mm(['kT', 'qT'], [kA], bA[0:64, 0:64], kT[:, hl, c0:c0 + 64], qT[:, hl, c0:c0 + 64])
                k.mm(['qT', cbk], [kB], bB[0:64, 0:65], qT[:, hl, c0:c0 + 64], Cbf[d][hl])
            yield
            if need_out:
                k.act([kB, 'EB'], ['rsb%d' % u], rsb[u], bB[0:64, 0:65], AF.Identity, scale=EB[:, dh, n:n + 1])
                k.stt([kA, 'WS', 'meb%d' % u], ['pT%d' % u], pT[u], bA[0:64, 0:64], WS[:, dh, n:n + 1], meb[u], ALU.mult, ALU.mult)
            if upd:
                k.cp([ck], [cbk], Cbf[d][hl], Cst[d][hl], eng='act')
            yield
            if not need_out:
                return
            k.mm(['pT%d' % u, 'vaug'], [kA], bA[0:64, 0:65], pT[u], vaug[:, n, hl, :])
            yield
            k.tt([kA, 'rsb%d' % u], ['tot%d' % u], tot[u][:, 0:65], bA[0:64, 0:65], rsb[u], ALU.add)
            yield
            k.act(['tot%d' % u], ['tot%d' % u], tot[u][:, 65:66], tot[u][:, 64:65], AF.Abs)
            yield
            k.ts(['tot%d' % u], ['tot%d' % u], tot[u][:, 65:66], tot[u][:, 65:66], 1.0, op0=ALU.max)
            yield
            k.recip(['tot%d' % u], ['tot%d' % u], tot[u][:, 65:66], tot[u][:, 65:66])
            yield
            hs = hsum[:, n, hl * 64:(hl + 1) * 64]
            k.stt(['tot%d' % u, 'hsum'], ['hsum'], hs, tot[u][:, 0:64], tot[u][:, 65:66], hs, ALU.mult, ALU.add)

        for step in range(NCH):
            gens = [unit(step, d, hl) for d in range(2) for hl in range(2)]
            while gens:
                alive = []
                for g_ in gens:
                    try:
                        next(g_)
                        alive.append(g_)
                    except StopIteration:
                        pass
                gens = alive
        nout = 64 if last else NCH
        ssum = sb.t([64, NCH * 2], F32)
        ybf = sb.t([64, NCH, 128], BF16)
        ytb = [sb.t([128, 512], BF16) for _ in range(2)]
        k.act(['hsum', 'raw', 'acc'], ['acc', 'raw'], ra, hsum.rearrange("p n c -> p (n c)"), AF.Square)
        k.red(['acc', 'raw'], ['ssum'], ssum, ra.rearrange("p (g e) -> p g e", e=64), ALU.add)
        k.ts(['ssum'], ['ssum'], ssum, ssum, 1.0 / 64.0, EPS, op0=ALU.mult, op1=ALU.add)
        k.act(['ssum'], ['ssum'], ssum, ssum, AF.Sqrt)
        k.recip(['ssum'], ['ssum'], ssum, ssum)
        hv = hsum.rearrange("p n (h e) -> p (n h) e", e=64)
        k.tt(['hsum', 'ssum'], ['hsum'], hv, hv, ssum.unsqueeze(2).to_broadcast([64, NCH * 2, 64]), ALU.mult)
        k.tt(['hsum', 'wbc'], ['hsum'], hsum, hsum, wbc[:, hp * 128:(hp + 1) * 128].unsqueeze(1).to_broadcast([64, NCH, 128]), ALU.mult)
        ost = acc[:, 0:17 * 128].rearrange("p (n c) -> p n c", c=128)
        for n0 in range(0, NCH, 17):
            k.dma('sp', ['acc'], ['acc'], ost, TM[n0 * 64:(n0 + 17) * 64, 256 + hp * 128:256 + (hp + 1) * 128].rearrange("(n p) c -> p n c", p=64))
            k.act(['acc'], ['acc'], ost, ost, AF.Sigmoid)
            k.tt(['acc', 'hsum'], ['ybf'], ybf[:, n0:n0 + 17, :], hsum[:, n0:n0 + 17, :], ost, ALU.mult)
        for gi, n0 in enumerate(range(0, nout, 8)):
            nn = min(8, nout - n0)
            pk, pp = nps()
            ppb = pp[:].bitcast(BF16)
            for dn in range(nn):
                k.tr(['ybf', 'identbf'], [pk], ppb[:, dn * 64:(dn + 1) * 64], ybf[:, n0 + dn, :], P['identbf'][0:64, 0:64])
            yk = 'ytb%d' % (gi % 2)
            k.cp([pk], [yk], ytb[gi % 2][:, 0:nn * 64], ppb[:, 0:nn * 64])
            k.dma('pool', [yk], ['YT'], YT[256 + hp * 128:256 + (hp + 1) * 128, n0 * 64:(n0 + nn) * 64], ytb[gi % 2][:, 0:nn * 64])
        k.em.barrier()


HC = 32
KB = 256 // HC
NB6 = 512 // HC
NHB = 256 // HC
PI = math.pi
HSTOP = [99]


def hyena_consts():
    c = {}
    f32 = np.float32

    def zfeat(Lx, pos):
        t = np.linspace(0.0, 1.0, Lx, dtype=f32)[pos][:, None]
        w = ((2.0 * math.pi / Lx) * np.arange(Lx, dtype=f32))[pos][:, None]
        f = np.linspace(1e-4, 15.0, 16, dtype=f32)[None, :]
        z = np.concatenate([t, np.cos(f * w), -np.sin(f * w)], axis=-1).astype(f32)
        return z, t
    deltas = np.abs(np.linspace(math.log(1e-2) / 1.5, math.log(1e-2) / 0.3, 256, dtype=f32)).astype(f32)
    z, t = zfeat(L, np.arange(L))
    c['hy_z'] = np.ascontiguousarray(z.T)
    c['hy_decay'] = np.ascontiguousarray(np.exp(-t * deltas[None, :]).T.astype(f32))
    pos = np.concatenate([np.arange(CTX - 1, 0, -1), np.arange(CTX)])
    zc, tc = zfeat(CTX, pos)
    c['hy_zc'] = np.ascontiguousarray(zc.T)
    c['hy_decayc'] = np.ascontiguousarray(np.exp(-tc * deltas[None, :]).T.astype(f32))
    n1 = np.arange(32)[:, None]
    k1 = np.arange(64)[None, :]
    a = 2 * np.pi * n1 * k1 / 64.0
    c['hy_F1'] = np.concatenate([np.cos(a), -np.sin(a)], 1).astype(f32)
    n2 = np.arange(128)[:, None, None]
    kk = (np.arange(64)[None, :, None] + 64 * np.arange(128)[None, None, :])
    a = 2 * np.pi * ((n2 * kk) % 8192) / 8192.0
    c['hy_Gr'] = np.cos(a).astype(f32).reshape(128, 8192)
    c['hy_Gi'] = (-np.sin(a)).astype(f32).reshape(128, 8192)
    k2 = np.arange(128)[:, None]
    nn = np.arange(128)[None, :]
    a = 2 * np.pi * ((k2 * nn) % 128) / 128.0
    c['hy_E1'] = np.concatenate([np.cos(a), np.sin(a)], 1).astype(f32)
    c['hy_E2'] = np.concatenate([-np.sin(a), np.cos(a)], 1).astype(f32)
    k1 = np.arange(64)[:, None, None]
    nfull = np.arange(128)[None, :, None] + 128 * np.arange(32)[None, None, :]
    a = 2 * np.pi * ((k1 * nfull) % 8192) / 8192.0
    c['hy_Mr'] = np.cos(a).astype(f32).reshape(64, 4096)
    c['hy_nMi'] = (-np.sin(a)).astype(f32).reshape(64, 4096)
    return c


def hy_load_bf(k, sb, name, shape, stg, key):
    p, n = shape
    dst = sb.t([p, n], BF16)
    src = k.dram[name]
    step = 2048
    for i, c0 in enumerate(range(0, n, step)):
        c1 = min(n, c0 + step)
        k.dma('sp', [], ['hstg'], stg[0:p, 0:c1 - c0], src[:, c0:c1])
        k.cp(['hstg'], [key], dst[:, c0:c1], stg[0:p, 0:c1 - c0], eng=('dve' if i % 2 == 0 else 'pool'))
    return dst


def fft_fwd(k, C, xbf, A, nAi, psctr, consume):
    F1, Gr, Gi = C['F1'], C['Gr'], C['Gi']
    for c0 in range(0, HC, 4):
        pk, pp = psctr()
        for dc in range(4):
            k.mm(['xbf', 'F1'], [pk], pp[:, dc * 128:(dc + 1) * 128], xbf[:, c0 + dc, :], F1)
        src = pp[:, 0:512].rearrange("p (c r q) -> p c r q", r=2, q=64)
        k.cp([pk], ['A'], A[:, c0:c0 + 4, :, :], src, eng='act')
        k.ts(['A'], ['nAi'], nAi[:, c0:c0 + 4, :], A[:, c0:c0 + 4, 1, :], -1.0, op0=ALU.mult)
    for k0 in range(0, 64, KB):
        pk, pp = psctr()
        for dk in range(KB):
            k1 = k0 + dk
            xr = pp[:, dk * 2 * HC:dk * 2 * HC + HC]
            xi = pp[:, dk * 2 * HC + HC:(dk + 1) * 2 * HC]
            k.mm(['Gr', 'A'], [pk], xr, Gr[:, k1, :], A[:, :, 0, k1], start=True, stop=False)
            k.mm(['Gi', 'nAi'], [pk], xr, Gi[:, k1, :], nAi[:, :, k1], start=False, stop=True)
            k.mm(['Gi', 'A'], [pk], xi, Gi[:, k1, :], A[:, :, 0, k1], start=True, stop=False)
            k.mm(['Gr', 'A'], [pk], xi, Gr[:, k1, :], A[:, :, 1, k1], start=False, stop=True)
        consume(pk, pp, k0)


def phaseH(k, P, l, base, last):
    nc = k.nc
    UT, YT = k.dram['UT'], k.dram['YT']
    HK, HH, CK, UC = k.dram['HK'], k.dram['HH'], k.dram['CK'], k.dram['UC']
    psi = [0]

    def nps():
        i = psi[0]
        psi[0] = (psi[0] + 1) % 8
        return 'ps%d' % i, k.ps[i]

    sb = SB(nc, base=base)
    w1 = sb.t([33, 64], F32)
    w2 = sb.t([64, 64], F32)
    w3 = sb.t([64, 1024], F32)
    sc = sb.t([64, 8], F32)
    b3 = sb.t([128, 8], F32)
    k.dma('sp', [], ['w1'], w1, k.dram['hy_filt_w1'][l])
    k.dma('sp', [], ['w2'], w2, k.dram['hy_filt_w2'][l])
    k.dma('sp', [], ['w3'], w3, k.dram['hy_filt_w3'][l])
    k.dma('sp', [], ['sc'], sc[:, 0:3], k.dram['hy_filt_sc'][l])
    k.dma('sp', [], ['b3'], b3, k.dram['hy_b3_col'][l])
    k.tt(['sc'], ['sc'], sc[:, 3:4], sc[:, 0:1], sc[:, 1:2], ALU.mult)
    k.tt(['sc'], ['sc'], sc[:, 4:5], sc[:, 0:1], sc[:, 2:3], ALU.mult)
    zT = sb.t([33, L], F32)
    h2 = sb.t([64, L], F32)
    h1 = sb.t([64, 512], F32)
    m1 = sb.t([64, 512], F32)
    m2 = sb.t([64, 512], F32)

    def sin_layer(src_ap, wt, kdim, bcol, dst_ap, n):
        pk, pp = nps()
        k.mm(['w1', 'w2', 'zT', 'h1'], [pk], pp[0:64, 0:n], wt, src_ap)
        k.ts([pk, 'sc'], ['m0'], dst_ap, pp[0:64, 0:n], sc[:, 0:1], sc[:, bcol:bcol + 1], op0=ALU.mult, op1=ALU.add)
        k.ts(['m0'], ['m1'], m1[:, 0:n], dst_ap, PI, -2.0 * PI, op0=ALU.is_gt, op1=ALU.mult)
        k.ts(['m0'], ['m2'], m2[:, 0:n], dst_ap, -PI, 2.0 * PI, op0=ALU.is_lt, op1=ALU.mult, eng='pool')
        k.tt(['m1', 'm2'], ['m1'], m1[:, 0:n], m1[:, 0:n], m2[:, 0:n], ALU.add)
        k.tt(['m0', 'm1'], ['m0'], dst_ap, dst_ap, m1[:, 0:n], ALU.add)
        k.act(['m0'], ['m0'], dst_ap, dst_ap, AF.Sin)

    def mlp(zsrc_name, ncols, h2dst):
        k.dma('sp', ['zT'], ['zT'], zT[:, 0:ncols], k.dram[zsrc_name][:, :])
        for c0 in range(0, ncols, 512):
            n = min(512, ncols - c0)
            sin_layer(zT[:, c0:c0 + n], w1, 33, 3, h1[:, 0:n], n)
            sin_layer2(c0, n, h2dst)

    def sin_layer2(c0, n, h2dst):
        pk, pp = nps()
        k.mm(['w2', 'm0'], [pk], pp[0:64, 0:n], w2, h1[:, 0:n])
        d = h2dst[:, c0:c0 + n]
        k.ts([pk, 'sc'], ['h2'], d, pp[0:64, 0:n], sc[:, 0:1], sc[:, 4:5], op0=ALU.mult, op1=ALU.add)
        k.ts(['h2'], ['m1'], m1[:, 0:n], d, PI, -2.0 * PI, op0=ALU.is_gt, op1=ALU.mult)
        k.ts(['h2'], ['m2'], m2[:, 0:n], d, -PI, 2.0 * PI, op0=ALU.is_lt, op1=ALU.mult, eng='pool')
        k.tt(['m1', 'm2'], ['m1'], m1[:, 0:n], m1[:, 0:n], m2[:, 0:n], ALU.add)
        k.tt(['h2', 'm1'], ['h2'], d, d, m1[:, 0:n], ALU.add)
        k.act(['h2'], ['h2'], d, d, AF.Sin)

    dec = [sb.t([128, L], F32) for _ in range(2)]
    kraw = [sb.t([128, L], F32) for _ in range(2)]
    kbfo = [sb.t([128, L], BF16) for _ in range(2)]
    junk = sb.t([128, L], BF16)
    asum = sb.t([128, 4], F32)

    def gen_filters(zname, dname, ncols, ctx):
        mlp(zname, ncols, h2)
        for ch in range(2):
            k.dma('sp', ['dec%d' % ch], ['dec%d' % ch], dec[ch][:, 0:ncols], k.dram[dname][ch * 128:(ch + 1) * 128, :])
        for o in range(2):
            for ch in range(2):
                k.memset([], ['asum'], asum, 0.0)
                for d in range(2):
                    fc = o * 4 + d * 2 + ch
                    kr = kraw[d]
                    kk_ = 'kraw%d' % d
                    for c0 in range(0, ncols, 512):
                        n = min(512, ncols - c0)
                        pk, pp = nps()
                        k.mm(['w3', 'h2'], [pk], pp[:, 0:n], w3[:, fc * 128:(fc + 1) * 128], h2[:, c0:c0 + n])
                        k.act([pk, 'b3'], [kk_], kr[:, c0:c0 + n], pp[:, 0:n], AF.Identity, bias=b3[:, fc:fc + 1], scale=1.0)
                    k.tt([kk_, 'dec%d' % ch], [kk_], kr[:, 0:ncols], kr[:, 0:ncols], dec[ch][:, 0:ncols], ALU.mult)
                    if not ctx:
                        if d == 1:
                            k.memset([kk_], [kk_], kr[:, 0:1], 0.0)
                        k.act([kk_, 'asum'], ['junk', 'asum'], junk[:, 0:ncols], kr[:, 0:ncols], AF.Abs, accum_out=asum[:, d:d + 1])
                    else:
                        lo, hi = (255, 511) if d == 0 else (0, 255)
                        k.act([kk_, 'asum'], ['junk', 'asum'], junk[:, lo:hi], kr[:, lo:hi], AF.Abs, accum_out=asum[:, d:d + 1])
                k.tt(['asum'], ['asum'], asum[:, 2:3], asum[:, 0:1], asum[:, 1:2], ALU.add)
                k.recip(['asum'], ['asum'], asum[:, 2:3], asum[:, 2:3])
                if not ctx:
                    for d in range(2):
                        fc = o * 4 + d * 2 + ch
                        k.ts(['kraw%d' % d, 'asum'], ['kbfo%d' % d], kbfo[d], kraw[d], asum[:, 2:3], op0=ALU.mult,
                             eng=('dve' if d == 0 else 'pool'))
                        k.dma('pool', ['kbfo%d' % d], ['HK'], HK[fc], kbfo[d])
                else:
                    k.ts(['kraw0', 'asum'], ['kraw0'], kraw[0][:, 255:511], kraw[0][:, 255:511], asum[:, 2:3], op0=ALU.mult)
                    k.ts(['kraw1', 'asum', 'kraw0'], ['kraw0'], kraw[0][:, 0:255], kraw[1][:, 0:255], asum[:, 2:3], op0=ALU.mult)
                    k.dma('pool', ['kraw0'], ['CK'], CK[o * 2 + ch], kraw[0][:, 0:511])

    gen_filters('hy_z', 'hy_decay', L, False)
    if not last:
        gen_filters('hy_zc', 'hy_decayc', 511, True)
    k.em.barrier()

    if HSTOP[0] <= 1:
        return
    sb = SB(nc, base=base)
    stg = sb.t([128, 2048], F32)
    C = {}
    C['F1'] = hy_load_bf(k, sb, 'hy_F1', [32, 128], stg, 'F1')
    C['Gr'] = hy_load_bf(k, sb, 'hy_Gr', [128, 8192], stg, 'Gr').rearrange("p (q m) -> p q m", m=128)
    C['Gi'] = hy_load_bf(k, sb, 'hy_Gi', [128, 8192], stg, 'Gi').rearrange("p (q m) -> p q m", m=128)
    C['E1'] = hy_load_bf(k, sb, 'hy_E1', [128, 256], stg, 'E1')
    C['E2'] = hy_load_bf(k, sb, 'hy_E2', [128, 256], stg, 'E2')
    C['Mr'] = hy_load_bf(k, sb, 'hy_Mr', [64, 4096], stg, 'Mr').rearrange("p (n m) -> p n m", m=32)
    C['nMi'] = hy_load_bf(k, sb, 'hy_nMi', [64, 4096], stg, 'nMi').rearrange("p (n m) -> p n m", m=32)
    A = sb.t([128, HC, 2, 64], BF16)
    nAi = sb.t([128, HC, 64], BF16)
    base2 = sb.off
    if HSTOP[0] <= 1.5:
        k.em.barrier()
        return

    sbs = SB(nc, base=base2)
    kbf = [sbs.t([32, HC, 128], BF16) for _ in range(2)]
    Hacc = sbs.t([128, 64, 2, HC], F32)
    Hbf = sbs.t([128, 64, 2, HC], BF16)
    xtmp = sbs.t([128, KB, 2, HC], F32)
    SC = 1.0 / 8192.0
    for o in range(2):
        for b4 in range(NHB):
            ch, coff = (b4 * HC) // 128, (b4 * HC) % 128
            for d in range(2):
                fc = o * 4 + d * 2 + ch
                k.dma('sp', ['xbf'], ['xbf'], kbf[d], HK[fc][coff:coff + HC, :].rearrange("c (a b) -> a c b", b=128))

                def consume(pk, pp, k0, d=d):
                    src = pp[:, 0:512].rearrange("p (q r c) -> p q r c", r=2, c=HC)
                    dst = Hacc[:, k0:k0 + KB, :, :]
                    if d == 0:
                        k.act([pk], ['Hacc'], dst, src, AF.Copy, scale=SC)
                    else:
                        k.act([pk], ['xtmp'], xtmp, src, AF.Copy, scale=SC)
                        k.tt(['xtmp', 'Hacc'], ['Hacc'], dst[:, :, 0, :], dst[:, :, 0, :], xtmp[:, :, 0, :], ALU.add)
                        k.tt(['xtmp', 'Hacc'], ['Hacc'], dst[:, :, 1, :], dst[:, :, 1, :], xtmp[:, :, 1, :], ALU.subtract, eng='pool')
                fft_fwd(k, C, kbf[d], A, nAi, nps, consume)
            k.cp(['Hacc'], ['Hbf'], Hbf, Hacc, eng='pool')
            k.dma('pool', ['Hbf'], ['HH'], HH[o * NHB + b4], Hbf.rearrange("p q r c -> p (q r c)"))
    k.em.barrier()

    if HSTOP[0] <= 2:
        return
    sbc = SB(nc, base=base2)
    raw = sbc.t([128, T], F32)
    acc = sbc.t([128, T], F32)
    ucb = sbc.t([128, T], BF16)
    cw = sbc.t([128, 6, 4], F32)
    k.dma('sp', [], ['cw'], cw, k.dram['hy_conv_col'][l])
    for cc in range(6):
        k.dma('sp', ['raw'], ['raw'], raw, UT[cc * 128:(cc + 1) * 128, :])
        k.ts(['raw', 'cw'], ['acc'], acc, raw, cw[:, cc, 1:2], cw[:, cc, 3:4], op0=ALU.mult, op1=ALU.add)
        for (a, b) in ((0, L), (L, T)):
            k.stt(['raw', 'cw', 'acc'], ['acc'], acc[:, a + 1:b], raw[:, a:b - 1], cw[:, cc, 0:1], acc[:, a + 1:b], ALU.mult, ALU.add)
            k.stt(['raw', 'cw', 'acc'], ['acc'], acc[:, a:b - 1], raw[:, a + 1:b], cw[:, cc, 2:3], acc[:, a:b - 1], ALU.mult, ALU.add)
        k.cp(['acc'], ['ucb'], ucb, acc, eng='act')
        k.dma('pool', ['ucb'], ['UC'], UC[cc * 128:(cc + 1) * 128, :], ucb)
    k.em.barrier()

    if HSTOP[0] <= 3:
        return
    sbd = SB(nc, base=base2)
    vbf = sbd.t([32, HC, 128], BF16)
    x1bf = sbd.t([32, HC, 128], BF16)
    x2bf = sbd.t([32, HC, 128], BF16)
    z1 = sbd.t([32, HC, 128], BF16)
    z2 = sbd.t([32, HC, 128], BF16)
    dv = sbd.t([32, HC, 128], BF16)
    dbc = sbd.t([32, 2, 256], F32)
    Hs = sbd.t([128, 64, 2, HC], BF16)
    Y = sbd.t([128, 64, 2, HC], BF16)
    Zs = sbd.t([64, HC, 2, 128], BF16)
    xs = [sbd.t([128, KB, 2, HC], F32) for _ in range(2)]
    ta = [sbd.t([128, KB, HC], F32) for _ in range(2)]
    tb = [sbd.t([128, KB, HC], F32) for _ in range(2)]
    tg = [sbd.t([32, HC, NB6], F32) for _ in range(2)]
    k.dma('sp', [], ['dbc'], dbc, k.dram['hy_d_bc'][l])
    xctr = [0]
    for b4 in range(NHB):
        c0g = b4 * HC
        for (tile_, key, r0) in ((vbf, 'xbf', 0), (x1bf, 'x1bf', 256), (x2bf, 'x2bf', 512)):
            k.dma('sp', [key], [key], tile_, UC[r0 + c0g:r0 + c0g + HC, 0:L].rearrange("c (a b) -> a c b", b=128))
        for o in range(2):
            xin, xkey = (vbf, 'xbf') if o == 0 else (z1, 'z1')
            gate, gkey = (x1bf, 'x1bf') if o == 0 else (x2bf, 'x2bf')
            zout, zkey = (z1, 'z1') if o == 0 else (z2, 'z2')
            k.tt([xkey, 'dbc'], ['dv'], dv, xin, dbc[:, o, c0g:c0g + HC].unsqueeze(2).to_broadcast([32, HC, 128]), ALU.mult, eng='pool')
            k.dma('sp', ['Hs'], ['Hs'], Hs.rearrange("p q r c -> p (q r c)"), HH[o * NHB + b4])

            def consume(pk, pp, k0):
                i = xctr[0] % 2
                xctr[0] += 1
                xk, tak, tbk = 'xs%d' % i, 'ta%d' % i, 'tb%d' % i
                k.cp([pk], [xk], xs[i], pp[:, 0:512].rearrange("p (q r c) -> p q r c", r=2, c=HC), eng='act')
                Xr, Xi = xs[i][:, :, 0, :], xs[i][:, :, 1, :]
                Hr, Hi = Hs[:, k0:k0 + KB, 0, :], Hs[:, k0:k0 + KB, 1, :]
                Yr, Yi = Y[:, k0:k0 + KB, 0, :], Y[:, k0:k0 + KB, 1, :]
                k.tt([xk, 'Hs'], [tak], ta[i], Xr, Hr, ALU.mult)
                k.tt([xk, 'Hs'], [tbk], tb[i], Xi, Hi, ALU.mult, eng='pool')
                k.tt([tak, tbk], ['Y'], Yr, ta[i], tb[i], ALU.subtract)
                k.tt([xk, 'Hs'], [tak], ta[i], Xr, Hi, ALU.mult, eng='pool')
                k.tt([xk, 'Hs'], [tbk], tb[i], Xi, Hr, ALU.mult)
                k.tt([tak, tbk], ['Y'], Yi, ta[i], tb[i], ALU.add, eng='pool')
            fft_fwd_keyed(k, C, xin, xkey, A, nAi, nps, consume)
            for c0 in range(0, HC, 2):
                pk, pp = nps()
                for dc in range(2):
                    cidx = c0 + dc
                    out = pp[0:64, dc * 256:(dc + 1) * 256]
                    k.mm(['Y', 'E1'], [pk], out, Y[:, :, 0, cidx], C['E1'], start=True, stop=False)
                    k.mm(['Y', 'E2'], [pk], out, Y[:, :, 1, cidx], C['E2'], start=False, stop=True)
                src = pp[0:64, 0:512].rearrange("p (c r n) -> p c r n", r=2, n=128)
                k.cp([pk], ['Zs'], Zs[:, c0:c0 + 2, :, :], src, eng=('act' if (c0 // 2) % 2 == 0 else 'dve'))
            for g8, n0 in enumerate(range(0, 128, NB6)):
                pk, pp = nps()
                for dn in range(NB6):
                    n2 = n0 + dn
                    out = pp[0:32, dn * HC:(dn + 1) * HC]
                    k.mm(['Mr', 'Zs'], [pk], out, C['Mr'][:, n2, :], Zs[:, :, 0, n2], start=True, stop=False)
                    k.mm(['nMi', 'Zs'], [pk], out, C['nMi'][:, n2, :], Zs[:, :, 1, n2], start=False, stop=True)
                i = g8 % 2
                src = pp[0:32, 0:NB6 * HC].rearrange("p (n c) -> p c n", c=HC)
                k.tt([pk, 'dv'], ['tg%d' % i], tg[i], src, dv[:, :, n0:n0 + NB6], ALU.add)
                k.tt(['tg%d' % i, gkey], [zkey], zout[:, :, n0:n0 + NB6], tg[i], gate[:, :, n0:n0 + NB6], ALU.mult, eng='pool')
        k.dma('pool', ['z2'], ['YT'], YT[c0g:c0g + HC, 0:L].rearrange("c (a b) -> a c b", b=128), z2)
    k.em.barrier()

    if last or HSTOP[0] <= 4:
        return
    sbx = SB(nc, base=base2)
    ub = sbx.t([128, 3, CTX], BF16)
    uf = sbx.t([128, 3, CTX], F32)
    kf = sbx.t([128, 511], F32)
    accs = [sbx.t([128, CTX], F32) for _ in range(4)]
    zc = sbx.t([128, CTX], F32)
    zb = sbx.t([128, CTX], BF16)
    dcol = sbx.t([128, 4], F32)
    k.dma('sp', [], ['dcol'], dcol, k.dram['hy_d_col'][l])
    for ch in range(2):
        for j in range(3):
            k.dma('sp', ['ub'], ['ub'], ub[:, j, :], UC[j * 256 + ch * 128:j * 256 + (ch + 1) * 128, L:T])
        k.cp(['ub'], ['uf'], uf, ub)
        for o in range(2):
            uin = uf[:, 0, :] if o == 0 else zc
            gate = uf[:, 1 + o, :]
            k.dma('sp', ['kf'], ['kf'], kf, CK[o * 2 + ch])
            for a in range(4):
                k.memset([], ['acc%d' % a], accs[a], 0.0, eng=('dve' if a < 2 else 'pool'))
            for s_ in range(CTX):
                a = s_ % 4
                k.stt(['kf', 'uf', 'zc', 'acc%d' % a], ['acc%d' % a], accs[a], kf[:, 255 - s_:511 - s_], uin[:, s_:s_ + 1], accs[a],
                      ALU.mult, ALU.add)
            k.tt(['acc0', 'acc1'], ['acc0'], accs[0], accs[0], accs[1], ALU.add)
            k.tt(['acc2', 'acc3'], ['acc2'], accs[2], accs[2], accs[3], ALU.add, eng='pool')
            k.tt(['acc0', 'acc2'], ['acc0'], accs[0], accs[0], accs[2], ALU.add)
            k.stt(['uf', 'zc', 'dcol', 'acc0'], ['acc0'], accs[0], uin, dcol[:, o * 2 + ch:o * 2 + ch + 1], accs[0], ALU.mult, ALU.add)
            k.tt(['acc0', 'uf'], ['zc'], zc, accs[0], gate, ALU.mult)
        k.cp(['zc'], ['zb'], zb, zc)
        k.dma('pool', ['zb'], ['YT'], YT[ch * 128:(ch + 1) * 128, L:T], zb)
    k.em.barrier()


def fft_fwd_keyed(k, C, xin, xkey, A, nAi, psctr, consume):
    F1, Gr, Gi = C['F1'], C['Gr'], C['Gi']
    for c0 in range(0, HC, 4):
        pk, pp = psctr()
        for dc in range(4):
            k.mm([xkey, 'F1'], [pk], pp[:, dc * 128:(dc + 1) * 128], xin[:, c0 + dc, :], F1)
        src = pp[:, 0:512].rearrange("p (c r q) -> p c r q", r=2, q=64)
        k.cp([pk], ['A'], A[:, c0:c0 + 4, :, :], src, eng='act')
        k.ts(['A'], ['nAi'], nAi[:, c0:c0 + 4, :], A[:, c0:c0 + 4, 1, :], -1.0, op0=ALU.mult)
    for k0 in range(0, 64, KB):
        pk, pp = psctr()
        for dk in range(KB):
            k1 = k0 + dk
            xr = pp[:, dk * 2 * HC:dk * 2 * HC + HC]
            xi = pp[:, dk * 2 * HC + HC:(dk + 1) * 2 * HC]
            k.mm(['Gr', 'A'], [pk], xr, Gr[:, k1, :], A[:, :, 0, k1], start=True, stop=False)
            k.mm(['Gi', 'nAi'], [pk], xr, Gi[:, k1, :], nAi[:, :, k1], start=False, stop=True)
            k.mm(['Gi', 'A'], [pk], xi, Gi[:, k1, :], A[:, :, 0, k1], start=True, stop=False)
            k.mm(['Gr', 'A'], [pk], xi, Gr[:, k1, :], A[:, :, 1, k1], start=False, stop=True)
        consume(pk, pp, k0)


BIG = 1.0e9
I32 = mybir.dt.int32


def moe_sched(ntiles):
    Tl = ntiles * 128
    J = [max(1, -(-min(Tl, (2 * Tl) // (r + 1)) // 128)) for r in range(32)]
    S = [0]
    for j in J:
        S.append(S[-1] + j * 128)
    return J, S


NSLOT = moe_sched(NT)[1][-1]


def phaseC(k, P, l, base, last, xres):
    nc = k.nc
    em = k.em
    YT, XMIX, XRES = k.dram['YT'], k.dram['XMIX'], k.dram['XRES']
    XG, YE, H2M = k.dram['XG'], k.dram['YE'], k.dram['H2M']
    ntiles = 32 if last else NT
    J, S = moe_sched(ntiles)
    psi = [0]

    def nps():
        i = psi[0]
        psi[0] = (psi[0] + 1) % 8
        return 'ps%d' % i, k.ps[i]

    recent = []

    def idma(reads, writes, fn):
        i = em.dnext
        em.dnext = (i + 1) % em.NDMA
        if em.dcnt[i] > 0:
            em._wait('pool', (('d', i), 16 * em.dcnt[i]))
        if len(recent) >= 4:
            em._wait('pool', recent[-4])
        em._deps('pool', reads, writes)
        ins = fn(em.eng['pool'])
        em.dcnt[i] += 1
        ins.then_inc(em.dsem[i], 16)
        em._record((('d', i), 16 * em.dcnt[i]), reads, writes)
        recent.append((('d', i), 16 * em.dcnt[i]))
        em.ninst += 1

    sb0 = SB(nc, base=base)
    grow = sb0.t([128, 2, 2, D], F32)
    mrow = sb0.t([128, 2, 2, D], F32)
    GW = sb0.t([128, NT, 2], F32)
    SIDX = sb0.t([128, NT, 2], I32)
    IDX = sb0.t([128, 32], I32)
    tri128 = sb0.t([128, 128], F32)
    iota32 = sb0.t([128, 32], F32)
    ltmask = sb0.t([128, 1024], F32)
    pidx = sb0.t([128, 1], F32)
    Srow = sb0.t([128, 32], F32)
    dg = sb0.t([128, 128], F32)
    k.dma('sp', [], ['tri128'], tri128, k.dram['tri128'][:, :])
    k.dma('sp', [], ['iota32'], iota32, k.dram['iota32'][:, :])
    k.dma('sp', [], ['ltmask'], ltmask, k.dram['ltmask'][:, :])
    k.dma('sp', [], ['pidx'], pidx, k.dram['pidx'][:, :])
    k.dma('sp', [], ['Srow'], Srow, k.dram['moe_S'][0 if ntiles == NT else 1])
    for gi, c0 in enumerate((16, 40)):
        for j in range(2):
            for kk in range(8):
                k.ts(['ident32', 'mod'], ['dg'], dg, P['ident32'], P['mod'][:, l, c0 + kk, j:j + 1], op0=ALU.mult)
                pk, pp = nps()
                k.mm(['ones32', 'dg'], [pk], pp[:, 0:128], P['ones32'], dg)
                k.cp([pk], ['grow'], grow[:, gi, j, kk * 128:(kk + 1) * 128], pp[:, 0:128], eng='act')
    for mi in range(2):
        for j in range(2):
            for kk in range(8):
                col = P['A2'][:, l, kk, j:j + 1] if mi == 0 else P['mod'][:, l, 24 + kk, j:j + 1]
                k.ts(['ident32', 'mod', 'A2'], ['dg'], dg, P['ident32'], col, op0=ALU.mult)
                pk, pp = nps()
                k.mm(['ones32', 'dg'], [pk], pp[:, 0:128], P['ones32'], dg)
                k.cp([pk], ['mrow'], mrow[:, mi, j, kk * 128:(kk + 1) * 128], pp[:, 0:128], eng='act')
    base1 = sb0.off
    sb = SB(nc, base=base1)
    Wout = sb.t([128, 8, D], BF16)
    Wr = sb.t([128, 8, 36], F32)
    rb = sb.t([128, 36], F32)
    stg = sb.t([128, 8, 512], F32)
    wv = k.dram['w_out'][l].rearrange("(k p) n -> p k n", p=128)
    for i, c0 in enumerate((0, 512)):
        k.dma('sp', ['stg'], ['stg'], stg, wv[:, :, c0:c0 + 512])
        k.cp(['stg'], ['Wout'], Wout[:, :, c0:c0 + 512], stg)
    k.dma('sp', [], ['Wr'], Wr, k.dram['moe_wr'][l].rearrange("(k p) n -> p k n", p=128))
    k.dma('sp', [], ['rb'], rb, k.dram['moe_rb_bc'][l])
    yT = [sb.t([128, 8, 512], BF16) for _ in range(2)]
    xt = [sb.t([128, D], F32) for _ in range(2)]
    xm = [sb.t([128, D], F32) for _ in range(2)]
    xn = sb.t([128, D], F32)
    tmpm = sb.t([128, D], F32)
    hm = [sb.t([128, D], BF16) for _ in range(2)]
    junk = sb.t([128, D], BF16)
    h32 = sb.t([128, 8, 128], F32)
    ss = [sb.t([128, 2], F32) for _ in range(2)]
    lg = sb.t([128, 36], F32)
    rt = sb.t([128, 16], F32)
    oh = sb.t([128, 4], F32)
    ml = sb.t([128, 32], F32)
    e1 = sb.t([128, 32], F32)
    e2 = sb.t([128, 32], F32)
    esum = sb.t([128, 32], F32)
    tmp32 = sb.t([128, 32], F32)
    basec = sb.t([128, 32], F32)
    erank = sb.t([128, 32], F32)
    SE = sb.t([128, 32], F32)
    EID = sb.t([128, 32], F32)
    idxf = sb.t([128, 2, 32], F32)
    EH = sb.t([128, 2, NT, 32], F32)
    RK = sb.t([128, NT, 32], F32)
    tA = sb.t([128, NT, 32], F32)
    sidf = sb.t([128, NT, 2], F32)
    k.memset([], ['basec'], basec, 0.0)
    k.memset([], ['EH'], EH, 0.0)
    k.memset([], ['RK'], RK, 0.0)
    for ti in range(ntiles):
        j = 0 if ti < 32 else 1
        g, tl = ti // 4, ti % 4
        yb = g % 2
        yk = 'yT%d' % yb
        if tl == 0:
            n = min(512, ntiles * 128 - g * 512)
            k.dma('sp', [yk], [yk], yT[yb][:, :, 0:n], YT[:, g * 512:g * 512 + n].rearrange("(c p) t -> p c t", p=128))
        b = ti % 2
        xk, mk, sk, hk = 'xt%d' % b, 'xm%d' % b, 'ss%d' % b, 'hm%d' % b
        k.dma('sp', [xk], [xk], xt[b], xres[ti * 128:(ti + 1) * 128, :])
        for half in range(2):
            pk, pp = nps()
            for f in range(8):
                k.mm([yk, 'Wout'], [pk], pp[:, 0:512], yT[yb][:, f, tl * 128:(tl + 1) * 128], Wout[:, f, half * 512:(half + 1) * 512],
                     start=(f == 0), stop=(f == 7))
            cs = slice(half * 512, (half + 1) * 512)
            k.tt([pk, 'grow'], [mk], xm[b][:, cs], pp[:, 0:512], grow[:, 0, j, cs], ALU.mult)
            k.tt([mk, xk], [mk], xm[b][:, cs], xm[b][:, cs], xt[b][:, cs], ALU.add, eng='pool')
        k.dma('pool', [mk], ['XMIX'], XMIX[ti * 128:(ti + 1) * 128, :], xm[b])
        k.memset([], [sk], ss[b], 0.0)
        k.act([mk, sk], ['junk', sk], junk, xm[b], AF.Square, accum_out=ss[b][:, 0:1])
        k.ts([sk], [sk], ss[b][:, 1:2], ss[b][:, 0:1], 1.0 / D, EPS, op0=ALU.mult, op1=ALU.add)
        k.act([sk], [sk], ss[b][:, 1:2], ss[b][:, 1:2], AF.Sqrt)
        k.recip([sk], [sk], ss[b][:, 1:2], ss[b][:, 1:2])
        k.ts([mk, sk], ['xn'], xn, xm[b], ss[b][:, 1:2], op0=ALU.mult)
        k.tt(['xn', 'mrow'], ['tmpm'], tmpm, xn, mrow[:, 0, j, :], ALU.mult)
        k.tt(['tmpm', 'mrow'], [hk], hm[b], tmpm, mrow[:, 1, j, :], ALU.add, eng='pool')
        k.dma('sp', [hk], ['H2M%d' % ti], H2M[ti * 128:(ti + 1) * 128, :], hm[b])
        for h2 in range(2):
            pk, pp = nps()
            for q in range(4):
                kk = h2 * 4 + q
                k.tr(['xn', 'ident32'], [pk], pp[:, q * 128:(q + 1) * 128], xn[:, kk * 128:(kk + 1) * 128], P['ident32'])
            for q in range(4):
                kk = h2 * 4 + q
                k.act([pk, 'A2', 'mod'], ['h32'], h32[:, kk, :], pp[:, q * 128:(q + 1) * 128], AF.Identity,
                      scale=P['A2'][:, l, kk, j:j + 1], bias=P['mod'][:, l, 24 + kk, j:j + 1])
        pk, pp = nps()
        for kk in range(8):
            k.mm(['h32', 'Wr'], [pk], pp[:, 0:36], h32[:, kk, :], Wr[:, kk, :], start=(kk == 0), stop=(kk == 7))
        k.tt([pk, 'rb'], ['lg'], lg, pp[:, 0:36], rb, ALU.add)
        k.red(['lg'], ['rt'], rt[:, 0:1], lg[:, 0:4], ALU.max)
        k.ts(['lg', 'rt'], ['oh'], oh, lg[:, 0:4], rt[:, 0:1], op0=ALU.is_equal)
        k.ts(['rt'], ['rt'], rt[:, 1:2], rt[:, 0:1], -1.0, op0=ALU.mult)
        k.memset(['rt'], ['rt'], rt[:, 2:3], 0.0)
        k.act(['lg', 'rt'], ['tmp32', 'rt'], tmp32[:, 0:4], lg[:, 0:4], AF.Exp, bias=rt[:, 1:2], scale=1.0, accum_out=rt[:, 2:3])
        k.recip(['rt'], ['rt'], rt[:, 3:4], rt[:, 2:3])
        k.ts(['oh'], ['oh'], oh, oh, 1.0, BIG, op0=ALU.subtract, op1=ALU.mult)
        k.tt(['lg', 'oh'], ['ml'], ml.rearrange("p (g e) -> p g e", e=8), lg[:, 4:36].rearrange("p (g e) -> p g e", e=8),
             oh.unsqueeze(2).to_broadcast([128, 4, 8]), ALU.add)
        k.red(['ml'], ['rt'], rt[:, 4:5], ml, ALU.max)
        k.ts(['ml', 'rt'], ['e1'], e1, ml, rt[:, 4:5], op0=ALU.is_equal)
        k.ts(['e1'], ['tmp32'], tmp32, e1, -BIG, op0=ALU.mult)
        k.tt(['ml', 'tmp32'], ['ml'], ml, ml, tmp32, ALU.add)
        k.red(['ml'], ['rt'], rt[:, 5:6], ml, ALU.max)
        k.ts(['ml', 'rt'], ['e2'], e2, ml, rt[:, 5:6], op0=ALU.is_equal)
        k.tt(['rt'], ['rt'], rt[:, 6:7], rt[:, 5:6], rt[:, 4:5], ALU.subtract)
        k.act(['rt'], ['rt'], rt[:, 6:7], rt[:, 6:7], AF.Exp)
        k.ts(['rt'], ['rt'], rt[:, 7:8], rt[:, 6:7], 1.0, op0=ALU.add)
        k.recip(['rt'], ['rt'], rt[:, 7:8], rt[:, 7:8])
        k.tt(['rt'], ['rt'], rt[:, 8:9], rt[:, 6:7], rt[:, 7:8], ALU.mult)
        k.tt(['rt'], ['rt'], rt[:, 9:10], rt[:, 7:8], rt[:, 3:4], ALU.mult)
        k.tt(['rt'], ['rt'], rt[:, 10:11], rt[:, 8:9], rt[:, 3:4], ALU.mult)
        k.cp(['rt'], ['GW'], GW[:, ti, :], rt[:, 9:11], eng='pool')
        k.cp(['e1'], ['EH'], EH[:, 0, ti, :], e1, eng='pool')
        k.cp(['e2'], ['EH'], EH[:, 1, ti, :], e2, eng='pool')
        k.tt(['e1', 'e2'], ['esum'], esum, e1, e2, ALU.add)
        pk, pp = nps()
        k.mm(['tri128', 'esum'], [pk], pp[:, 0:32], tri128, esum)
        k.mm(['ones32', 'esum'], [pk], pp[:, 32:64], P['ones32'], esum)
        k.tt([pk, 'basec'], ['RK'], RK[:, ti, :], pp[:, 0:32], basec, ALU.add)
        k.tt([pk, 'basec'], ['basec'], basec, basec, pp[:, 32:64], ALU.add)
    stgf = stg.rearrange("p a b -> p (a b)")
    A3 = stgf[:, 0:1024].rearrange("p (a b) -> p a b", b=32)
    B3 = stgf[:, 1024:2048].rearrange("p (a b) -> p a b", b=32)
    C3 = stgf[:, 2048:3072].rearrange("p (a b) -> p a b", b=32)
    cnt_o = basec.unsqueeze(1).to_broadcast([128, 32, 32])
    cnt_s = basec.unsqueeze(2).to_broadcast([128, 32, 32])
    k.tt(['basec', 'stg'], ['stg'], A3, cnt_o, cnt_s, ALU.is_gt)
    k.tt(['basec', 'stg'], ['stg'], B3, cnt_o, cnt_s, ALU.is_equal)
    k.tt(['stg', 'ltmask'], ['stg'], B3, B3, ltmask.rearrange("p (a b) -> p a b", b=32), ALU.mult)
    k.tt(['stg'], ['stg'], A3, A3, B3, ALU.add)
    k.red(['stg'], ['erank'], erank, A3, ALU.add)
    k.tt(['erank', 'iota32', 'stg'], ['stg'], A3, erank.unsqueeze(2).to_broadcast([128, 32, 32]),
         iota32.unsqueeze(1).to_broadcast([128, 32, 32]), ALU.is_equal)
    k.tt(['stg', 'Srow'], ['stg'], B3, A3, Srow.unsqueeze(1).to_broadcast([128, 32, 32]), ALU.mult)
    k.red(['stg'], ['SE'], SE, B3, ALU.add)
    k.tt(['stg', 'iota32'], ['stg'], C3, A3, iota32.unsqueeze(2).to_broadcast([128, 32, 32]), ALU.mult)
    k.red(['stg'], ['EID'], EID, C3.rearrange("p e r -> p r e"), ALU.add)
    k.ts(['EID'], ['idxf'], idxf[:, 0, :], EID, 128.0, float(l * 32 * 128), op0=ALU.mult, op1=ALU.add)
    k.ts(['idxf', 'pidx'], ['idxf'], idxf[:, 0, :], idxf[:, 0, :], pidx[:, 0:1], op0=ALU.add)
    k.cp(['idxf'], ['IDX'], IDX, idxf[:, 0, :])
    for kc in range(2):
        k.tt(['RK', 'SE'], ['tA'], tA, RK, SE.unsqueeze(1).to_broadcast([128, NT, 32]), ALU.add)
        k.tt(['tA', 'EH'], ['tA'], tA, tA, EH[:, kc], ALU.mult)
        k.red(['tA'], ['sidf'], sidf[:, :, kc], tA, ALU.add)
    k.cp(['sidf'], ['SIDX'], SIDX, sidf)
    for ti in range(ntiles):
        b = ti % 2
        hk = 'hm%d' % b
        k.dma('sp', ['H2M%d' % ti], [hk], hm[b], H2M[ti * 128:(ti + 1) * 128, :])
        for kc in range(2):
            idma([hk, 'SIDX'], ['XGs%d_%d' % (ti, kc)], lambda e, ti=ti, kc=kc, b=b: e.indirect_dma_start(
                out=XG[:, :], out_offset=bass.IndirectOffsetOnAxis(ap=SIDX[:, ti, kc:kc + 1], axis=0),
                in_=hm[b], in_offset=None))
    k.em.barrier()
    sb = SB(nc, base=base1)
    wst = [sb.t([128, 8, 512], F32) for _ in range(4)]
    w1b = [sb.t([128, 8, 512], BF16) for _ in range(2)]
    w3b = [sb.t([128, 8, 512], BF16) for _ in range(2)]
    w2b = [sb.t([128, 4, D], BF16) for _ in range(2)]
    xg_tm = [sb.t([128, 4, D], BF16) for _ in range(2)]
    xgT = [sb.t([128, 8, 512], BF16) for _ in range(2)]
    gT = [sb.t([128, 4, 512], BF16) for _ in range(2)]
    st = [sb.t([128, 512], F32) for _ in range(2)]
    ysb = [sb.t([128, D], F32) for _ in range(2)]
    W1r, W3r, W2r = k.dram['moe_w1'], k.dram['moe_w3'], k.dram['moe_w2']
    sctr = [0]
    cctr = [0]
    yctr = [0]

    def load_w(src_rows, rowlen, r, dst, dkey, ceng):
        i = sctr[0] % 4
        sctr[0] += 1
        sk_ = 'wst%d' % i
        flat = wst[i].rearrange("p a b -> p (a b)")
        idma(['IDX'], [sk_], lambda e: e.indirect_dma_start(
            out=flat, out_offset=None, in_=src_rows[:, :], in_offset=bass.IndirectOffsetOnAxis(ap=IDX[:, r:r + 1], axis=0)))
        k.cp([sk_], [dkey], dst, flat.rearrange("p (a b) -> p a b", b=rowlen), eng=ceng)

    def load_rank(r):
        wb = r % 2
        load_w(W1r, 512, r, w1b[wb], 'w1b%d' % wb, 'act')
        load_w(W3r, 512, r, w3b[wb], 'w3b%d' % wb, 'dve')
        load_w(W2r, D, r, w2b[wb], 'w2b%d' % wb, 'act')

    load_rank(0)
    for r in range(32):
        wb = r % 2
        if r + 1 < 32:
            load_rank(r + 1)
        nrow = J[r] * 128
        for c0 in range(0, nrow, 512):
            n = min(512, nrow - c0)
            nt_ = n // 128
            gb = cctr[0] % 2
            cctr[0] += 1
            xk, tk, gk = 'xgtm%d' % gb, 'xgT%d' % gb, 'gT%d' % gb
            r0 = S[r] + c0
            k.dma('sp', [], [xk], xg_tm[gb][:, 0:nt_, :], XG[r0:r0 + n, :].rearrange("(t p) d -> p t d", p=128))
            for t in range(nt_):
                pk, pp = nps()
                pst = pp[:].bitcast(BF16)
                for kk in range(8):
                    k.tr([xk, 'identbf'], [pk], pst[:, kk * 128:(kk + 1) * 128], xg_tm[gb][:, t, kk * 128:(kk + 1) * 128], P['identbf'])
                k.cp([pk], [tk], xgT[gb][:, :, t * 128:(t + 1) * 128], pst[:, 0:1024].rearrange("p (a b) -> p a b", b=128),
                     eng=('act' if t % 2 == 0 else 'dve'))
            for f in range(4):
                p1k, pp1 = nps()
                for kk in range(8):
                    k.mm(['w1b%d' % wb, tk], [p1k], pp1[:, 0:n], w1b[wb][:, kk, f * 128:(f + 1) * 128], xgT[gb][:, kk, 0:n],
                         start=(kk == 0), stop=(kk == 7))
                p3k, pp3 = nps()
                for kk in range(8):
                    k.mm(['w3b%d' % wb, tk], [p3k], pp3[:, 0:n], w3b[wb][:, kk, f * 128:(f + 1) * 128], xgT[gb][:, kk, 0:n],
                         start=(kk == 0), stop=(kk == 7))
                sbi = f % 2
                k.act([p1k], ['st%d' % sbi], st[sbi][:, 0:n], pp1[:, 0:n], AF.Silu)
                k.tt(['st%d' % sbi, p3k], [gk], gT[gb][:, f, 0:n], st[sbi][:, 0:n], pp3[:, 0:n], ALU.mult)
            for tl in range(nt_):
                yi = yctr[0] % 2
                yctr[0] += 1
                yk = 'ysb%d' % yi
                for half in range(2):
                    pk, pp = nps()
                    for f in range(4):
                        k.mm([gk, 'w2b%d' % wb], [pk], pp[:, 0:512], gT[gb][:, f, tl * 128:(tl + 1) * 128], w2b[wb][:, f, half * 512:(half + 1) * 512],
                             start=(f == 0), stop=(f == 3))
                    k.cp([pk], [yk], ysb[yi][:, half * 512:(half + 1) * 512], pp[:, 0:512], eng=('act' if half == 0 else 'dve'))
                k.dma('sp', [yk], ['YE%d' % yctr[0]], YE[r0 + tl * 128:r0 + (tl + 1) * 128, :], ysb[yi])
    k.em.barrier()
    sb = SB(nc, base=base1)
    ya = [sb.t([128, D], F32) for _ in range(2)]
    yb2 = [sb.t([128, D], F32) for _ in range(2)]
    xo = [sb.t([128, D], F32) for _ in range(2)]
    fw = sb.t([128, D], F32)
    fj = sb.t([128, D], BF16)
    fs = sb.t([128, 2], F32)
    if last:
        k.dma('sp', [], ['fw'], fw, k.dram['final_bc'][:, :])
    for ti in range(ntiles):
        j = 0 if ti < 32 else 1
        b = ti % 2
        ok, ak, bk = 'xo%d' % b, 'ya%d' % b, 'yb%d' % b
        k.dma('sp', [], [ok], xo[b], XMIX[ti * 128:(ti + 1) * 128, :])
        idma(['SIDX'], [ak], lambda e, ti=ti, b=b: e.indirect_dma_start(
            out=ya[b], out_offset=None, in_=YE[:, :], in_offset=bass.IndirectOffsetOnAxis(ap=SIDX[:, ti, 0:1], axis=0)))
        idma(['SIDX'], [bk], lambda e, ti=ti, b=b: e.indirect_dma_start(
            out=yb2[b], out_offset=None, in_=YE[:, :], in_offset=bass.IndirectOffsetOnAxis(ap=SIDX[:, ti, 1:2], axis=0)))
        k.ts([ak, 'GW'], [ak], ya[b], ya[b], GW[:, ti, 0:1], op0=ALU.mult)
        k.stt([bk, 'GW', ak], [ak], ya[b], yb2[b], GW[:, ti, 1:2], ya[b], ALU.mult, ALU.add)
        k.tt([ak, 'grow'], [ak], ya[b], ya[b], grow[:, 1, j, :], ALU.mult, eng='pool')
        k.tt([ak, ok], [ok], xo[b], xo[b], ya[b], ALU.add)
        if not last:
            k.dma('pool', [ok], ['XRES'], XRES[ti * 128:(ti + 1) * 128, :], xo[b])
        else:
            k.memset([], ['fs'], fs, 0.0)
            k.act([ok, 'fs'], ['fj', 'fs'], fj, xo[b], AF.Square, accum_out=fs[:, 0:1])
            k.ts(['fs'], ['fs'], fs[:, 1:2], fs[:, 0:1], 1.0 / D, EPS, op0=ALU.mult, op1=ALU.add)
            k.act(['fs'], ['fs'], fs[:, 1:2], fs[:, 1:2], AF.Sqrt)
            k.recip(['fs'], ['fs'], fs[:, 1:2], fs[:, 1:2])
            k.ts([ok, 'fs'], [ok], xo[b], xo[b], fs[:, 1:2], op0=ALU.mult)
            k.tt([ok, 'fw'], [ok], xo[b], xo[b], fw, ALU.mult)
            k.dma('pool', [ok], ['out'], k.dram['out'][ti * 128:(ti + 1) * 128, :], xo[b])
    k.em.barrier()


def build(stage='full', debug=()):
    nc = bass.Bass("TRN2", target_bir_lowering=False)
    k = K(nc, debug=debug)
    P = {}
    sbp = SB(nc)
    k.din('xin', [T, D])
    k.din('w_in_fm', [DEPTH, D, NFM])
    k.din('w_in_tm', [DEPTH, D, NTM])
    k.din('b_fm_col', [128, DEPTH, 18])
    k.din('b_tm_bc', [DEPTH, 128, NTM])
    k.dscratch('UT', [1280, T])
    k.dscratch('QKT', [1024, T], BF16)
    k.dscratch('TM', [T, NTM])
    k.dscratch('XRES', [T, D])
    k.dscratch('YT', [1024, T], BF16)
    k.din('da_lambda', [DEPTH, 256])
    k.dscratch('XMIX', [T, D])
    k.dscratch('H2M', [T, D], BF16)
    k.dscratch('XG', [NSLOT, D], BF16)
    k.dscratch('YE', [NSLOT, D])
    for nm, shp in (('tri128', [128, 128]), ('iota32', [128, 32]), ('ltmask', [128, 1024]), ('pidx', [128, 1]), ('moe_S', [2, 128, 32])):
        k.din(nm, shp)
    k.din('w_out', [DEPTH, D, D])
    k.din('moe_wr', [DEPTH, D, 36])
    k.din('moe_rb_bc', [DEPTH, 128, 36])
    k.din('moe_w1', [DEPTH * 32 * 128, 4096])
    k.din('moe_w3', [DEPTH * 32 * 128, 4096])
    k.din('moe_w2', [DEPTH * 32 * 128, 4096])
    k.din('final_bc', [128, D])
    if stage == 'full':
        k.dout('out', [L, D])
    k.dscratch('HK', [8, 128, L], BF16)
    k.dscratch('HH', [2 * NHB, 128, 64 * 2 * HC], BF16)
    k.dscratch('CK', [4, 128, 511])
    k.dscratch('UC', [768, T], BF16)
    for nm, shp in (('hy_filt_w1', [DEPTH, 33, 64]), ('hy_filt_w2', [DEPTH, 64, 64]), ('hy_filt_w3', [DEPTH, 64, 1024]),
                    ('hy_filt_sc', [DEPTH, 64, 3]), ('hy_b3_col', [DEPTH, 128, 8]), ('hy_conv_col', [DEPTH, 128, 6, 4]),
                    ('hy_d_bc', [DEPTH, 32, 2, 256]), ('hy_d_col', [DEPTH, 128, 4]),
                    ('hy_z', [33, L]), ('hy_decay', [256, L]), ('hy_zc', [33, 511]), ('hy_decayc', [256, 511]),
                    ('hy_F1', [32, 128]), ('hy_Gr', [128, 8192]), ('hy_Gi', [128, 8192]), ('hy_E1', [128, 256]),
                    ('hy_E2', [128, 256]), ('hy_Mr', [64, 4096]), ('hy_nMi', [64, 4096])):
        k.din(nm, shp)
    k.din('tri', [2, 64, 64])
    k.din('ml_conv_col', [DEPTH, 64, 8, 4])
    k.din('ml_norm_bc', [DEPTH, 64, 256])
    k.din('da_subln_col', [DEPTH, 128, 1])
    phase0(k, P, sbp)
    P['negc'] = sbp.t([128, DEPTH, 2, 4], F32)
    base = sbp.off
    zt = SB(nc, base=base).t([128, 8 * D], BF16)
    k.memset([], ['zt'], zt, 0.0)
    XGv = k.dram['XG'].rearrange("(a p r) d -> a p (r d)", p=128, r=8)
    for a in range(NSLOT // 1024):
        k.dma('sp', ['zt'], ['XGz%d' % a], XGv[a], zt)
    k.em.barrier()
    for l in range(DEPTH):
        xres = k.dram['xin'] if l == 0 else k.dram['XRES']
        phaseA(k, P, l, base, xres)
        if stage == 'A':
            break
        if stage not in ('B2', 'H'):
            phaseB1(k, P, l, base, l == DEPTH - 1)
        if stage == 'B1':
            break
        if stage != 'H':
            phaseB2(k, P, l, base, l == DEPTH - 1)
        if stage == 'B2':
            break
        phaseH(k, P, l, base, l == DEPTH - 1)
        if stage == 'H':
            break
        phaseC(k, P, l, base, (l == DEPTH - 1) and stage == 'full', xres)
        if stage == 'C':
            break
    k.em.barrier()
    return nc, k


def run(inputs, stage='full', debug=(), cores=8):
    consts = make_consts()
    consts['moe_w1'] = np.ascontiguousarray(inputs['moe_w1'].reshape(DEPTH, 32, 8, 128, 512).transpose(0, 1, 3, 2, 4)).reshape(DEPTH * 32 * 128, 4096)
    consts['moe_w3'] = np.ascontiguousarray(inputs['moe_w3'].reshape(DEPTH, 32, 8, 128, 512).transpose(0, 1, 3, 2, 4)).reshape(DEPTH * 32 * 128, 4096)
    consts['moe_w2'] = np.ascontiguousarray(inputs['moe_w2'].reshape(DEPTH, 32, 4, 128, D).transpose(0, 1, 3, 2, 4)).reshape(DEPTH * 32 * 128, 4096)
    nc, k = build(stage, debug)
    in_maps = []
    for b in range(cores):
        m = prep_inputs(inputs, b)
        m.update(consts)
        in_maps.append({kk: v for kk, v in m.items() if kk in k.dram})
    res = run_bass_kernel_spmd(nc, in_maps, core_ids=list(range(cores)))
    return res.results


def kernel(**inputs):
    inp = {kk: np.asarray(v) for kk, v in inputs.items()}
    res = run(inp, stage='full', cores=8)
    return np.stack([np.asarray(r['out'], dtype=np.float32) for r in res], axis=0)
```
